# Optimizing a Trainium2 kernel written in Bass

```python
import jax, jax.numpy as jnp
from jax import lax
import numpy as np

D_MODEL = 1024
BATCH = 4
SEQ = 4096
DEPTH = 4

CHUNK = 64
N_EVEN = (DEPTH + 1) // 2
N_ODD = DEPTH // 2
ALPHA = (2 * DEPTH) ** 0.25
BETA = (8 * DEPTH) ** -0.25
LN_EPS = 1e-5
NEG = -1e30

POOL_WIDTH = D_MODEL // 2
POOL_WINDOWS = (2, 4, 8, 16)
POOL_GROUP = POOL_WIDTH // len(POOL_WINDOWS)
ML_HEADS = 4
ML_DK = (D_MODEL // 2) // ML_HEADS
ML_DV = (D_MODEL // 2) // ML_HEADS
ML_QK = ML_HEADS * ML_DK
ML_WIDTH = ML_HEADS * ML_DV
CONV_K = 4
EVEN_IN = POOL_WIDTH + 2 * ML_QK + 2 * ML_WIDTH + 2 * ML_HEADS
EVEN_MIX = POOL_WIDTH + ML_WIDTH
GLA_HEADS = 4
GLA_DK = (D_MODEL // 2) // GLA_HEADS
GLA_DV = D_MODEL // GLA_HEADS
GLA_QK = GLA_HEADS * GLA_DK
GLA_V = GLA_HEADS * GLA_DV
GLA_RANK = 16
GLA_TAU = 16.0
GLA_CHUNK = 16
ODD_IN = 2 * GLA_QK + 2 * GLA_V + GLA_RANK
N_EXPERTS = 32
TOP_K = 4
D_EXPERT = D_MODEL
SWIGLU_LIMIT = 7.0
SWIGLU_ALPHA = 1.702

kernel_name = "hybrid_pool_mlstm_gla_moe_deepnorm"


def layer_norm(x, g, b):
    xf = x.astype(jnp.float32)
    mu = xf.mean(-1, keepdims=True)
    var = jnp.square(xf - mu).mean(-1, keepdims=True)
    return ((xf - mu) * lax.rsqrt(var + LN_EPS) * g + b).astype(x.dtype)


def head_norm(h, g):
    mu = h.mean(-1, keepdims=True)
    var = jnp.square(h - mu).mean(-1, keepdims=True)
    hn = (h - mu) * lax.rsqrt(var + LN_EPS)
    return hn.reshape(h.shape[:2] + (-1,)) * g


def to_chunks(t, L):
    B_, S_ = t.shape[:2]
    t = t.astype(jnp.float32).reshape((B_, S_ // L, L) + t.shape[2:])
    return jnp.swapaxes(jnp.moveaxis(t, 1, 0), 2, 3)


def from_chunks(t):
    t = jnp.moveaxis(jnp.swapaxes(t, 2, 3), 0, 1)
    return t.reshape((t.shape[0], t.shape[1] * t.shape[2]) + t.shape[3:])


def pool_mixer(u, w_pool, pool_scale):
    B_, S_, _ = u.shape
    uf = u.astype(jnp.float32).reshape(B_, S_, len(POOL_WINDOWS), POOL_GROUP)
    cs = jnp.cumsum(uf, axis=1)
    pos = jnp.arange(1, S_ + 1, dtype=jnp.float32)
    means = []
    for gi, w in enumerate(POOL_WINDOWS):
        c = cs[:, :, gi]
        lag = jnp.pad(c, ((0, 0), (w, 0), (0, 0)))[:, :S_]
        cnt = jnp.minimum(pos, float(w))[:, None]
        means.append((c - lag) / cnt)
    d = (jnp.stack(means, axis=2) - uf).astype(u.dtype)
    y = jnp.einsum('bsgc,gcd->bsgd', d, w_pool).reshape(B_, S_, POOL_WIDTH)
    return y * pool_scale


def causal_dwconv(x, w, b):
    C = x.shape[-1]
    y = lax.conv_general_dilated(x, w[:, None, :], window_strides=(1,),
                                 padding=((CONV_K - 1, 0),),
                                 dimension_numbers=('NWC', 'WIO', 'NWC'),
                                 feature_group_count=C)
    return y + b


def mlstm(q, k, v, ig, fg):
    B_ = q.shape[0]
    k = k * (ML_DK ** -0.5)
    lf = jax.nn.log_sigmoid(fg.astype(jnp.float32))
    xs = (to_chunks(q, CHUNK), to_chunks(k, CHUNK), to_chunks(v, CHUNK),
          to_chunks(ig, CHUNK), to_chunks(lf, CHUNK))
    mask = jnp.tril(jnp.ones((CHUNK, CHUNK), dtype=bool))

    def step(carry, inp):
        C, n, m = carry
        qc, kc, vc, ic, lfc = inp
        b = jnp.cumsum(lfc, axis=-1)
        g = b[..., -1]
        D = jnp.where(mask, b[..., :, None] - b[..., None, :] + ic[..., None, :], NEG)
        m_inter = b + m[..., None]
        m_t = jnp.maximum(m_inter, D.max(-1))
        Wts = jnp.exp(D - m_t[..., None])
        Sts = jnp.einsum('bhtd,bhsd->bhts', qc, kc) * Wts
        sc = jnp.exp(m_inter - m_t)
        num = jnp.einsum('bhts,bhsv->bhtv', Sts, vc) + sc[..., None] * jnp.einsum('bhtd,bhdv->bhtv', qc, C)
        den = Sts.sum(-1) + sc * jnp.einsum('bhtd,bhd->bht', qc, n)
        h = num / jnp.maximum(jnp.abs(den), jnp.exp(-m_t))[..., None]
        a = g[..., None] - b + ic
        m_new = jnp.maximum(g + m, a.max(-1))
        decay = jnp.exp(g + m - m_new)
        wk = jnp.exp(a - m_new[..., None])[..., None] * kc
        C_new = decay[..., None, None] * C + jnp.einsum('bhsd,bhsv->bhdv', wk, vc)
        n_new = decay[..., None] * n + wk.sum(-2)
        return (C_new, n_new, m_new), h

    init = (jnp.zeros((B_, ML_HEADS, ML_DK, ML_DV), jnp.float32),
            jnp.zeros((B_, ML_HEADS, ML_DK), jnp.float32),
            jnp.zeros((B_, ML_HEADS), jnp.float32))
    _, hs = lax.scan(step, init, xs)
    return from_chunks(hs)


def gla(q, k, v, lg):
    B_ = q.shape[0]
    q = q * (GLA_DK ** -0.5)
    xs = (to_chunks(q, GLA_CHUNK), to_chunks(k, GLA_CHUNK),
          to_chunks(v, GLA_CHUNK), to_chunks(lg, GLA_CHUNK))
    mask = jnp.tril(jnp.ones((GLA_CHUNK, GLA_CHUNK), dtype=bool))[..., None]

    def step(S, inp):
        qc, kc, vc, gc = inp
        Bc = jnp.cumsum(gc, axis=-2)
        E = jnp.where(mask, Bc[..., :, None, :] - Bc[..., None, :, :], NEG)
        A = jnp.einsum('bhtd,bhsd,bhtsd->bhts', qc, kc, jnp.exp(E))
        o = jnp.einsum('bhts,bhsv->bhtv', A, vc) + jnp.einsum('bhtd,bhdv->bhtv', qc * jnp.exp(Bc), S)
        Bl = Bc[..., -1:, :]
        S_new = jnp.exp(Bl[..., 0, :])[..., None] * S + jnp.einsum('bhsd,bhsv->bhdv', kc * jnp.exp(Bl - Bc), vc)
        return S_new, o

    _, os_ = lax.scan(step, jnp.zeros((B_, GLA_HEADS, GLA_DK, GLA_DV), jnp.float32), xs)
    return from_chunks(os_)


def even_mixer(x, w_in, w_pool, pool_scale, conv_w, conv_b, i_bias, f_bias, ml_norm, w_out):
    B_, S_, _ = x.shape
    p = x @ w_in
    o1 = POOL_WIDTH
    o2 = o1 + 2 * ML_QK
    o3 = o2 + ML_WIDTH
    o4 = o3 + ML_WIDTH
    u, qk, vv, og, gates = jnp.split(p, [o1, o2, o3, o4], axis=-1)
    y_pool = pool_mixer(u, w_pool, pool_scale)
    qk = jax.nn.silu(causal_dwconv(qk, conv_w, conv_b))
    q, k = jnp.split(qk, 2, axis=-1)
    q = q.reshape(B_, S_, ML_HEADS, ML_DK)
    k = k.reshape(B_, S_, ML_HEADS, ML_DK)
    vv = vv.reshape(B_, S_, ML_HEADS, ML_DV)
    ig = gates[..., :ML_HEADS] + i_bias
    fg = gates[..., ML_HEADS:] + f_bias
    h = mlstm(q, k, vv, ig, fg)
    y_ml = (head_norm(h, ml_norm) * jax.nn.sigmoid(og.astype(jnp.float32))).astype(x.dtype)
    return jnp.concatenate([y_pool, y_ml], axis=-1) @ w_out


def odd_mixer(x, w_in, gla_w2, gla_b, gla_norm, w_out):
    B_, S_, _ = x.shape
    p = x @ w_in
    q, k, v, r, glr = jnp.split(p, [GLA_QK, 2 * GLA_QK, 2 * GLA_QK + GLA_V, 2 * GLA_QK + 2 * GLA_V], axis=-1)
    lg = jax.nn.log_sigmoid((glr @ gla_w2 + gla_b).astype(jnp.float32)) / GLA_TAU
    o = gla(q.reshape(B_, S_, GLA_HEADS, GLA_DK), k.reshape(B_, S_, GLA_HEADS, GLA_DK),
            v.reshape(B_, S_, GLA_HEADS, GLA_DV), lg.reshape(B_, S_, GLA_HEADS, GLA_DK))
    o = head_norm(o, gla_norm) * jax.nn.silu(r.astype(jnp.float32))
    return o.astype(x.dtype) @ w_out


def moe(x, router_w, router_b, w_gu, b_gu, w_down, b_down):
    B_, S_, D_ = x.shape
    xt = x.reshape(-1, D_)
    T = xt.shape[0]
    logits = (xt @ router_w + router_b).astype(jnp.float32)
    top_v, top_e = lax.top_k(logits, TOP_K)
    gate = jax.nn.softmax(top_v, axis=-1)
    flat_e = top_e.reshape(-1)
    order = jnp.argsort(flat_e)
    e_sorted = flat_e[order]
    tok = order // TOP_K
    sizes = jnp.bincount(flat_e, length=N_EXPERTS).astype(jnp.int32)
    xs = xt[tok]
    h = lax.ragged_dot(xs, w_gu, sizes) + b_gu[e_sorted]
    glu = jnp.minimum(h[:, 0::2], SWIGLU_LIMIT)
    lin = jnp.clip(h[:, 1::2], -SWIGLU_LIMIT, SWIGLU_LIMIT)
    act = glu * jax.nn.sigmoid(SWIGLU_ALPHA * glu) * (lin + 1.0)
    y = lax.ragged_dot(act, w_down, sizes) + b_down[e_sorted]
    y = y * gate.reshape(-1)[order][:, None].astype(y.dtype)
    out = jax.ops.segment_sum(y, tok, num_segments=T)
    return out.reshape(B_, S_, D_)


def setup_inputs(seed: int = 0) -> dict:
    key = jax.random.key(seed)
    ks = jax.random.split(key, 26)
    f32 = jnp.float32
    nrm = lambda k, shape, s: jax.random.normal(k, shape, f32) * s
    return {
        "x": nrm(ks[0], (BATCH, SEQ, D_MODEL), 1.0),
        "even_w_in": nrm(ks[1], (N_EVEN, D_MODEL, EVEN_IN), D_MODEL ** -0.5),
        "pool_w": nrm(ks[2], (N_EVEN, len(POOL_WINDOWS), POOL_GROUP, POOL_GROUP), POOL_GROUP ** -0.5),
        "pool_scale": 1.0 + nrm(ks[3], (N_EVEN, POOL_WIDTH), 0.1),
        "conv_w": nrm(ks[4], (N_EVEN, CONV_K, 2 * ML_QK), CONV_K ** -0.5),
        "conv_b": nrm(ks[5], (N_EVEN, 2 * ML_QK), 0.02),
        "i_bias": nrm(ks[6], (N_EVEN, ML_HEADS), 0.1),
        "f_bias": jnp.linspace(3.0, 6.0, ML_HEADS, dtype=f32)[None, :] + nrm(ks[7], (N_EVEN, ML_HEADS), 0.1),
        "ml_norm": 1.0 + nrm(ks[8], (N_EVEN, ML_WIDTH), 0.02),
        "even_w_out": nrm(ks[9], (N_EVEN, EVEN_MIX, D_MODEL), BETA * EVEN_MIX ** -0.5),
        "odd_w_in": nrm(ks[10], (N_ODD, D_MODEL, ODD_IN), D_MODEL ** -0.5),
        "gla_w2": nrm(ks[11], (N_ODD, GLA_RANK, GLA_QK), GLA_RANK ** -0.5),
        "gla_b": nrm(ks[12], (N_ODD, GLA_QK), 0.1),
        "gla_norm": 1.0 + nrm(ks[13], (N_ODD, GLA_V), 0.02),
        "odd_w_out": nrm(ks[14], (N_ODD, GLA_V, D_MODEL), BETA * GLA_V ** -0.5),
        "ln1_g": 1.0 + nrm(ks[15], (DEPTH, D_MODEL), 0.02),
        "ln1_b": nrm(ks[16], (DEPTH, D_MODEL), 0.02),
        "ln2_g": 1.0 + nrm(ks[17], (DEPTH, D_MODEL), 0.02),
        "ln2_b": nrm(ks[18], (DEPTH, D_MODEL), 0.02),
        "router_w": nrm(ks[19], (DEPTH, D_MODEL, N_EXPERTS), D_MODEL ** -0.5),
        "router_b": nrm(ks[20], (DEPTH, N_EXPERTS), 0.01),
        "w_gate_up": nrm(ks[21], (DEPTH, N_EXPERTS, D_MODEL, 2 * D_EXPERT), D_MODEL ** -0.5),
        "b_gate_up": nrm(ks[22], (DEPTH, N_EXPERTS, 2 * D_EXPERT), 0.01),
        "w_down": nrm(ks[23], (DEPTH, N_EXPERTS, D_EXPERT, D_MODEL), BETA * D_EXPERT ** -0.5),
        "b_down": nrm(ks[24], (DEPTH, N_EXPERTS, D_MODEL), 0.01),
    }


def reference(x, even_w_in, pool_w, pool_scale, conv_w, conv_b, i_bias, f_bias, ml_norm,
              even_w_out, odd_w_in, gla_w2, gla_b, gla_norm, odd_w_out,
              ln1_g, ln1_b, ln2_g, ln2_b, router_w, router_b,
              w_gate_up, b_gate_up, w_down, b_down):
    for layer in range(DEPTH):
        i = layer // 2
        if layer % 2 == 0:
            mix = even_mixer(x, even_w_in[i], pool_w[i], pool_scale[i], conv_w[i], conv_b[i],
                             i_bias[i], f_bias[i], ml_norm[i], even_w_out[i])
        else:
            mix = odd_mixer(x, odd_w_in[i], gla_w2[i], gla_b[i], gla_norm[i], odd_w_out[i])
        x = layer_norm(ALPHA * x + mix, ln1_g[layer], ln1_b[layer])
        ffn = moe(x, router_w[layer], router_b[layer], w_gate_up[layer], b_gate_up[layer],
                  w_down[layer], b_down[layer])
        x = layer_norm(ALPHA * x + ffn, ln2_g[layer], ln2_b[layer])
    return x
```

```python
import math
from contextlib import ExitStack

import numpy as np
import concourse.bass as bass
import concourse.mybir as mybir
from concourse.bass_utils import run_bass_kernel_spmd

F32 = mybir.dt.float32
BF16 = mybir.dt.bfloat16
AF = mybir.ActivationFunctionType
ALU = mybir.AluOpType

D_MODEL = 1024
BATCH = 4
SEQ = 4096
DEPTH = 4
ALPHA = (2 * DEPTH) ** 0.25
LN_EPS = 1e-5
N_EXPERTS = 32
SWIGLU_LIMIT = 7.0
SWIGLU_ALPHA = 1.702


class Tok:
    __slots__ = ("name", "writer", "readers", "dsem", "dcount", "disjoint")

    def __init__(self, name):
        self.name = name
        self.writer = None
        self.readers = []
        self.dsem = None
        self.dcount = 0
        self.disjoint = False


class Prog:
    def __init__(self):
        self.nc = bass.Bass("TRN2", target_bir_lowering=False)
        self.es = ExitStack()
        nc = self.nc
        self.eng = {"pe": nc.tensor, "dve": nc.vector, "act": nc.scalar, "pool": nc.gpsimd, "sp": nc.sync}
        self.sems = {}
        self.cnt = {}
        self.known = {k: {} for k in self.eng}
        for k in ("pe", "dve", "act", "pool"):
            self.sems[k] = self.es.enter_context(nc.semaphore("s_" + k))
            self.cnt[k] = 0
        self.nsem = 0
        self.ntens = 0
        self.scopes = []
        self.sem_pool = []
        self.dtot = {}
        self.live_toks = []

    def sb(self, shape, dtype=F32, name=None):
        self.ntens += 1
        t = self._es().enter_context(self.nc.sbuf_tensor("%s_%d" % (name or "sb", self.ntens), list(shape), dtype))
        return t

    def ps(self, shape, dtype=F32, name=None):
        self.ntens += 1
        t = self._es().enter_context(self.nc.psum_tensor("%s_%d" % (name or "ps", self.ntens), list(shape), dtype))
        return t

    def _es(self):
        return self.scopes[-1][0] if self.scopes else self.es

    def tok(self, name="t"):
        t = Tok(name)
        if self.scopes:
            self.scopes[-1][1].append(t)
        return t

    def _dsem(self, tok):
        if tok.dsem is None:
            if self.sem_pool:
                key, cnt = self.sem_pool.pop()
                tok.dcount = cnt
            else:
                self.nsem += 1
                key = "d%d" % self.nsem
                self.sems[key] = self.es.enter_context(self.nc.semaphore(key))
                self.dtot[key] = 0
            tok.dsem = key
        return tok.dsem

    def push(self):
        self.scopes.append((ExitStack(), []))

    def barrier(self):
        targets = {k: self.cnt[k] for k in ("pe", "dve", "act", "pool") if self.cnt[k] > 0}
        for k, v in self.dtot.items():
            if v > 0:
                targets[k] = v
        for e in self.eng:
            kn = self.known[e]
            for k, v in targets.items():
                if kn.get(k, 0) >= v:
                    continue
                self.eng[e].wait_ge(self.sems[k], v)
                kn[k] = v

    def pop(self):
        self.barrier()
        es, toks = self.scopes.pop()
        for t in toks:
            if t.dsem is not None:
                self.sem_pool.append((t.dsem, self.dtot[t.dsem]))
                t.dsem = None
        es.close()

    def _deps(self, e, reads, writes):
        deps = {}

        def add(d):
            if d is None:
                return
            k, v = d
            if deps.get(k, 0) < v:
                deps[k] = v

        for r in reads:
            add(r.writer)
        for w in writes:
            if not w.disjoint:
                add(w.writer)
            for rd in w.readers:
                add(rd)
        kn = self.known[e]
        for k, v in deps.items():
            if e == "pe" and k == "pe":
                continue
            if kn.get(k, 0) >= v:
                continue
            self.eng[e].wait_ge(self.sems[k], v)
            kn[k] = v

    def op(self, e, fn, reads=(), writes=()):
        self._deps(e, reads, writes)
        ins = fn(self.eng[e])
        ins.then_inc(self.sems[e], 1)
        self.cnt[e] += 1
        me = (e, self.cnt[e])
        for r in reads:
            r.readers.append(me)
        for w in writes:
            w.writer = me
            w.readers = []
        return ins

    def dma(self, q, out, in_, reads=(), writes=(), **kw):
        self._deps(q, reads, writes)
        owner = writes[0] if writes else reads[0]
        key = self._dsem(owner)
        ins = self.eng[q].dma_start(out=out, in_=in_, **kw)
        ins.then_inc(self.sems[key], 16)
        owner.dcount += 16
        self.dtot[key] = owner.dcount
        me = (key, owner.dcount)
        for r in reads:
            r.readers.append(me)
        for w in writes:
            w.writer = me
            w.readers = []
        return ins

    def wait_all(self, q, toks):
        deps = {}
        for t in toks:
            for d in [t.writer] + list(t.readers):
                if d is not None and deps.get(d[0], 0) < d[1]:
                    deps[d[0]] = d[1]
        for k, v in deps.items():
            self.eng[q].wait_ge(self.sems[k], v)

    def close(self):
        self.es.close()


def layer_norm_tile(P, src, dst, g_bc, b_bc, scr, T_src, T_dst, T_scr, tgb, gb_eng="pool"):
    st, mv, rstd = scr["st"], scr["mv"], scr["rstd"]
    P.op("dve", lambda e: e.bn_stats(out=st[:, 0:6], in_=src[:, 0:512]), reads=[T_src], writes=[T_scr])
    P.op("dve", lambda e: e.bn_stats(out=st[:, 6:12], in_=src[:, 512:1024]), reads=[T_src, T_scr], writes=[T_scr])
    P.op("dve", lambda e: e.bn_aggr(out=mv[:, 0:2], in_=st[:, 0:12]), reads=[T_scr], writes=[T_scr])
    P.op("act", lambda e: e.activation(out=rstd[:, 0:1], in_=mv[:, 1:2], func=AF.Sqrt, bias=scr["eps"][:, 0:1], scale=1.0),
         reads=[T_scr, tgb], writes=[T_scr])
    P.op("dve", lambda e: e.reciprocal(out=rstd[:, 0:1], in_=rstd[:, 0:1]), reads=[T_scr], writes=[T_scr])
    P.op("dve", lambda e: e.tensor_scalar(out=dst, in0=src, scalar1=mv[:, 0:1], scalar2=rstd[:, 0:1],
                                          op0=ALU.subtract, op1=ALU.mult), reads=[T_src, T_scr], writes=[T_dst])
    P.op(gb_eng, lambda e: e.tensor_tensor(out=dst, in0=dst, in1=g_bc, op=ALU.mult), reads=[T_dst, tgb], writes=[T_dst])
    P.op(gb_eng, lambda e: e.tensor_tensor(out=dst, in0=dst, in1=b_bc, op=ALU.add), reads=[T_dst, tgb], writes=[T_dst])


def build_moe(n_pass, n_exp=N_EXPERTS, two_mix=True):
    P = Prog()
    nc = P.nc
    io = moe_io(nc, "", n_pass * 1024, n_exp)
    for k in ("t_xin", "t_ma", "t_mb", "t_yout"):
        io[k] = P.tok(k)
    emit_moe(P, n_pass, io, n_exp)
    P.wait_all("sp", [io["t_yout"]])
    P.close()
    return nc


def moe_io(nc, pfx, NTOK, n_exp=N_EXPERTS, xin=None, ma=None, mb=None, yout=None, sparse=False):
    io = {}
    io["xin"] = xin if xin is not None else nc.dram_tensor(pfx + "xin", [NTOK, D_MODEL], F32, kind="ExternalInput").ap()
    io["ma"] = ma if ma is not None else nc.dram_tensor(pfx + "ma", [NTOK, D_MODEL], F32, kind="ExternalInput").ap()
    io["mb"] = mb if mb is not None else nc.dram_tensor(pfx + "mb", [NTOK, D_MODEL], F32, kind="ExternalInput").ap()
    io["lnp"] = nc.dram_tensor(pfx + "lnp", [4, D_MODEL], F32, kind="ExternalInput").ap()
    io["rw"] = nc.dram_tensor(pfx + "rw", [D_MODEL, N_EXPERTS], F32, kind="ExternalInput").ap()
    io["rb"] = nc.dram_tensor(pfx + "rb", [1, N_EXPERTS], F32, kind="ExternalInput").ap()
    io["wgu"] = nc.dram_tensor(pfx + "wgu", [n_exp, D_MODEL, 2048], F32, kind="ExternalInput").ap()
    io["bgu"] = nc.dram_tensor(pfx + "bgu", [128, n_exp * 16], F32, kind="ExternalInput").ap()
    io["wd"] = nc.dram_tensor(pfx + "wd", [n_exp, D_MODEL, D_MODEL], F32, kind="ExternalInput").ap()
    io["bd"] = nc.dram_tensor(pfx + "bd", [N_EXPERTS, D_MODEL], F32, kind="ExternalInput").ap()
    io["ident"] = nc.dram_tensor(pfx + "ident", [128, 417 if sparse else 128], F32, kind="ExternalInput").ap()
    io["yout"] = yout if yout is not None else nc.dram_tensor(pfx + "yout", [NTOK, D_MODEL], F32, kind="ExternalOutput").ap()
    return io


def emit_moe(P, n_pass, io, n_exp=N_EXPERTS, two_mix=True):
    TG = 1024
    NT = TG // 128
    nc = P.nc
    xin, ma, mb, lnp, rw, rb, wgu, bgu, wd, bd, ident_in, yout = (io[k] for k in
        ("xin", "ma", "mb", "lnp", "rw", "rb", "wgu", "bgu", "wd", "bd", "ident", "yout"))
    t_xin, t_ma, t_mb, t_yout = io["t_xin"], io["t_ma"], io["t_mb"], io["t_yout"]

    ident = P.sb([128, 128], F32, "ident"); t_const = P.tok("const")
    gb = P.sb([128, 4, D_MODEL], F32, "gb")
    rwt = P.sb([128, 8, N_EXPERTS], F32, "rwt")
    rbt = P.sb([128, N_EXPERTS], F32, "rbt")
    bgt = P.sb([128, n_exp * 16], F32, "bgt")
    bdt = P.sb([N_EXPERTS, D_MODEL], F32, "bdt")
    eps_t = P.sb([128, 1], F32, "eps")
    P.dma("sp", ident[:], ident_in[:, :], writes=[t_const])
    for i in range(4):
        P.dma("sp", gb[:, i, :], lnp[i:i + 1, :].partition_broadcast(128), writes=[t_const])
    P.dma("sp", rwt[:], rw.rearrange("(k p) n -> p k n", p=128), writes=[t_const])
    P.dma("sp", rbt[:], rb[0:1, :].partition_broadcast(128), writes=[t_const])
    P.dma("sp", bgt[:], bgu[:, :], writes=[t_const])
    P.dma("sp", bdt[:], bd[:, :], writes=[t_const])
    P.op("dve", lambda e: e.memset(eps_t[:], LN_EPS), writes=[t_const])
    bgv = bgt[:].rearrange("p (e c) -> p e c", c=16)
    P.op("dve", lambda e: e.tensor_scalar(out=bgv[:, :, 8:16], in0=bgv[:, :, 8:16], scalar1=1.0, scalar2=None, op0=ALU.add),
         reads=[t_const], writes=[t_const])

    acc = P.sb([128, NT, D_MODEL], F32, "acc");  t_acc = [P.tok("acc%d" % i) for i in range(NT)]
    x1T = P.sb([128, 8, TG], BF16, "x1T");       t_x1T = [P.tok("x1T%d" % i) for i in range(NT)]
    Gall = P.sb([128, NT, N_EXPERTS], F32, "G"); t_G = [P.tok("G%d" % i) for i in range(NT)]
    actT = P.sb([128, 8, 512], BF16, "actT");    t_actT = P.tok("actT")
    wgt = [P.sb([128, 8, 2048], BF16, "wgu%d" % i) for i in range(2)]; t_wg = [P.tok("wg%d" % i) for i in range(2)]
    wdt = [P.sb([128, 8, D_MODEL], BF16, "wd%d" % i) for i in range(2)]; t_wd = [P.tok("wd%d" % i) for i in range(2)]
    NS = 1
    xs = [P.sb([128, D_MODEL], F32, "xs%d" % i) for i in range(NS)]; t_xs = [P.tok("xs%d" % i) for i in range(NS)]
    pas = [P.sb([128, D_MODEL], F32, "pa0")] * NS; t_pa = [P.tok("pa0")] * NS
    pbs = [P.sb([128, D_MODEL], F32, "pb0")] * NS; t_pb = [P.tok("pb0")] * NS
    x1s = [P.sb([128, D_MODEL], F32, "x1s%d" % i) for i in range(NS)]; t_x1 = [P.tok("x1s%d" % i) for i in range(NS)]
    xTf = [P.sb([128, 8, 128], F32, "xTf0")] * NS; t_xTf = [P.tok("xTf0")] * NS
    lns = [dict(st=P.sb([128, 12], F32), mv=P.sb([128, 2], F32), rstd=P.sb([128, 1], F32), eps=eps_t) for i in range(NS)]
    t_lns = [P.tok("lns%d" % i) for i in range(NS)]
    rt = [dict(lg=P.sb([128, 32], F32), t8=P.sb([128, 8], F32), nm=P.sb([128, 1], F32), ex=P.sb([128, 32], F32),
               mk=P.sb([128, 32], F32), sm=P.sb([128, 1], F32), GT=P.sb([32, 128], F32)) for i in range(NS)]
    t_rt = [P.tok("rt%d" % i) for i in range(NS)]
    NG = 2
    glt = [P.sb([128, 512], F32, "gl%d" % i) for i in range(NG)]; t_gl = [P.tok("gl%d" % i) for i in range(NG)]
    sgt = [P.sb([128, 512], F32, "sg%d" % i) for i in range(NG)]; t_sg = [P.tok("sg%d" % i) for i in range(NG)]
    lit = [P.sb([128, 512], F32, "li0")] * NG; t_li = [P.tok("li0")] * NG
    pg = [P.ps([128, 512], F32, "pg%d" % i) for i in range(3)]; t_pg = [P.tok("pg%d" % i) for i in range(3)]
    pdn = [P.ps([128, 512], F32, "pd%d" % i) for i in range(3)]; t_pd = [P.tok("pd%d" % i) for i in range(3)]
    pm = [P.ps([128, 512], F32, "pm%d" % i) for i in range(2)]; t_pm = [P.tok("pm%d" % i) for i in range(2)]

    wload_i = [0]

    def load_weights(e, slot):
        src = wgu[e].rearrange("(k p) n -> p k n", p=128)
        for k in range(8):
            P.dma("pool", wgt[slot][:, k, :], src[:, k, :], writes=[t_wg[slot]])
        src = wd[e].rearrange("(k p) n -> p k n", p=128)
        for k in range(0, 8, 2):
            P.dma("pool", wdt[slot][:, k:k + 2, :], src[:, k:k + 2, :], writes=[t_wd[slot]])

    cg = [0]; cd = [0]; cm = [0]; cgl = [0]

    for ps_i in range(n_pass):
        tok0 = ps_i * TG
        load_weights(0, 0)
        for it in range(NT):
            s = it % NS
            r0 = tok0 + it * 128
            P.dma("sp", xs[s][:], xin[r0:r0 + 128, :], reads=[t_xin], writes=[t_xs[s]])
            P.dma("sp", pas[s][:], ma[r0:r0 + 128, :], reads=[t_ma], writes=[t_pa[s]])
            if two_mix:
                P.dma("sp", pbs[s][:], mb[r0:r0 + 128, :], reads=[t_mb], writes=[t_pb[s]])
                P.op("pool", lambda e: e.tensor_tensor(out=pas[s][:], in0=pas[s][:], in1=pbs[s][:], op=ALU.add),
                     reads=[t_pb[s], t_pa[s]], writes=[t_pa[s]])
            P.op("dve", lambda e: e.scalar_tensor_tensor(out=xs[s][:], in0=xs[s][:], scalar=ALPHA, in1=pas[s][:],
                                                         op0=ALU.mult, op1=ALU.add),
                 reads=[t_xs[s], t_pa[s]], writes=[t_xs[s]])
            layer_norm_tile(P, xs[s][:], x1s[s][:], gb[:, 0, :], gb[:, 1, :], lns[s], t_xs[s], t_x1[s], t_lns[s], t_const)
            for h in range(2):
                b = cm[0] % 2; cm[0] += 1
                for j in range(4):
                    k = h * 4 + j
                    P.op("pe", lambda e: e.transpose(out=pm[b][:, j * 128:(j + 1) * 128], in_=x1s[s][:, k * 128:(k + 1) * 128],
                                                     identity=ident[:]),
                         reads=[t_x1[s], t_const], writes=[t_pm[b]])
                P.op("act", lambda e: e.copy(out=xTf[s][:, h * 4:(h + 1) * 4, :],
                                             in_=pm[b][:].rearrange("p (j t) -> p j t", j=4)),
                     reads=[t_pm[b]], writes=[t_xTf[s]])
            P.op("pool", lambda e: e.tensor_copy(out=x1T[:, :, it * 128:(it + 1) * 128], in_=xTf[s][:]),
                 reads=[t_xTf[s]], writes=[t_x1T[it]])
            b = cm[0] % 2; cm[0] += 1
            for k in range(8):
                P.op("pe", lambda e: e.matmul(pm[b][:, 0:32], lhsT=xTf[s][:, k, :], rhs=rwt[:, k, :], start=(k == 0), stop=(k == 7)),
                     reads=[t_xTf[s], t_const], writes=[t_pm[b]])
            R = rt[s]; tR = t_rt[s]
            P.op("dve", lambda e: e.tensor_tensor(out=R["lg"][:], in0=pm[b][:, 0:32], in1=rbt[:], op=ALU.add),
                 reads=[t_pm[b], t_const], writes=[tR])
            P.op("dve", lambda e: e.max(out=R["t8"][:], in_=R["lg"][:]), reads=[tR], writes=[tR])
            P.op("dve", lambda e: e.tensor_scalar(out=R["nm"][:], in0=R["t8"][:, 0:1], scalar1=-1.0, scalar2=None, op0=ALU.mult),
                 reads=[tR], writes=[tR])
            P.op("act", lambda e: e.activation(out=R["ex"][:], in_=R["lg"][:], func=AF.Exp, bias=R["nm"][:, 0:1], scale=1.0),
                 reads=[tR], writes=[tR])
            P.op("dve", lambda e: e.tensor_scalar(out=R["mk"][:], in0=R["lg"][:], scalar1=R["t8"][:, 3:4], scalar2=None, op0=ALU.is_ge),
                 reads=[tR], writes=[tR])
            P.op("dve", lambda e: e.tensor_tensor(out=R["ex"][:], in0=R["ex"][:], in1=R["mk"][:], op=ALU.mult),
                 reads=[tR], writes=[tR])
            P.op("dve", lambda e: e.reduce_sum(out=R["sm"][:], in_=R["ex"][:], axis=mybir.AxisListType.X),
                 reads=[tR], writes=[tR])
            P.op("dve", lambda e: e.reciprocal(out=R["sm"][:], in_=R["sm"][:]), reads=[tR], writes=[tR])
            P.op("dve", lambda e: e.tensor_scalar(out=Gall[:, it, :], in0=R["ex"][:], scalar1=R["sm"][:, 0:1], scalar2=None, op0=ALU.mult),
                 reads=[tR], writes=[t_G[it]])
            b = cm[0] % 2; cm[0] += 1
            P.op("pe", lambda e: e.transpose(out=pm[b][0:32, 0:128], in_=Gall[:, it, :], identity=ident[:]),
                 reads=[t_G[it], t_const], writes=[t_pm[b]])
            P.op("act", lambda e: e.copy(out=R["GT"][:], in_=pm[b][0:32, 0:128]), reads=[t_pm[b]], writes=[tR])
            for hf in range(2):
                b = cm[0] % 2; cm[0] += 1
                P.op("pe", lambda e: e.matmul(pm[b][:, :], lhsT=R["GT"][:], rhs=bdt[:, hf * 512:(hf + 1) * 512], start=True, stop=True),
                     reads=[tR, t_const], writes=[t_pm[b]])
                P.op("dve", lambda e: e.scalar_tensor_tensor(out=acc[:, it, hf * 512:(hf + 1) * 512], in0=x1s[s][:, hf * 512:(hf + 1) * 512],
                                                             scalar=ALPHA, in1=pm[b][:, :], op0=ALU.mult, op1=ALU.add),
                     reads=[t_x1[s], t_pm[b]], writes=[t_acc[it]])

        for ex in range(n_exp):
            slot = ex % 2
            if ex + 1 < n_exp:
                load_weights(ex + 1, (ex + 1) % 2)
            W = wgt[slot]; WD = wdt[slot]
            for tb in range(TG // 512):
                tsl = slice(tb * 512, (tb + 1) * 512)
                tiles = [t_x1T[tb * 4 + i] for i in range(4)]
                for j in range(8):
                    gi = cgl[0] % NG; cgl[0] += 1
                    b = cg[0] % 3; cg[0] += 1
                    for k in range(8):
                        P.op("pe", lambda e: e.matmul(pg[b][:, :], lhsT=W[:, k, j * 128:(j + 1) * 128], rhs=x1T[:, k, tsl],
                                                      start=(k == 0), stop=(k == 7)),
                             reads=[t_wg[slot]] + tiles, writes=[t_pg[b]])
                    P.op("dve", lambda e: e.tensor_scalar(out=glt[gi][:], in0=pg[b][:, :], scalar1=bgt[:, ex * 16 + j:ex * 16 + j + 1],
                                                          scalar2=SWIGLU_LIMIT, op0=ALU.add, op1=ALU.min),
                         reads=[t_pg[b], t_const], writes=[t_gl[gi]])
                    P.op("act", lambda e: e.activation(out=sgt[gi][:], in_=glt[gi][:], func=AF.Sigmoid, scale=SWIGLU_ALPHA),
                         reads=[t_gl[gi]], writes=[t_sg[gi]])
                    P.op("pool", lambda e: e.tensor_tensor(out=sgt[gi][:], in0=sgt[gi][:], in1=glt[gi][:], op=ALU.mult),
                         reads=[t_gl[gi], t_sg[gi]], writes=[t_sg[gi]])
                    b = cg[0] % 3; cg[0] += 1
                    for k in range(8):
                        P.op("pe", lambda e: e.matmul(pg[b][:, :], lhsT=W[:, k, 1024 + j * 128:1024 + (j + 1) * 128], rhs=x1T[:, k, tsl],
                                                      start=(k == 0), stop=(k == 7)),
                             reads=[t_wg[slot]] + tiles, writes=[t_pg[b]])
                    P.op("dve", lambda e: e.tensor_scalar(out=lit[gi][:], in0=pg[b][:, :], scalar1=bgt[:, ex * 16 + 8 + j:ex * 16 + 9 + j],
                                                          scalar2=SWIGLU_LIMIT + 1.0, op0=ALU.add, op1=ALU.min),
                         reads=[t_pg[b], t_const], writes=[t_li[gi]])
                    P.op("dve", lambda e: e.scalar_tensor_tensor(out=actT[:, j, :], in0=lit[gi][:], scalar=1.0 - SWIGLU_LIMIT, in1=sgt[gi][:],
                                                                 op0=ALU.max, op1=ALU.mult),
                         reads=[t_li[gi], t_sg[gi]], writes=[t_actT])
                for tt in range(4):
                    it = tb * 4 + tt
                    for hf in range(2):
                        b = cd[0] % 3; cd[0] += 1
                        for k in range(8):
                            P.op("pe", lambda e: e.matmul(pdn[b][:, :], lhsT=actT[:, k, tt * 128:(tt + 1) * 128],
                                                          rhs=WD[:, k, hf * 512:(hf + 1) * 512], start=(k == 0), stop=(k == 7)),
                                 reads=[t_actT, t_wd[slot]], writes=[t_pd[b]])
                        P.op("dve", lambda e: e.scalar_tensor_tensor(out=acc[:, it, hf * 512:(hf + 1) * 512], in0=pdn[b][:, :],
                                                                     scalar=Gall[:, it, ex:ex + 1], in1=acc[:, it, hf * 512:(hf + 1) * 512],
                                                                     op0=ALU.mult, op1=ALU.add),
                             reads=[t_pd[b], t_G[it], t_acc[it]], writes=[t_acc[it]])

        for it in range(NT):
            s = it % NS
            r0 = tok0 + it * 128
            layer_norm_tile(P, acc[:, it, :], x1s[s][:], gb[:, 2, :], gb[:, 3, :], lns[s], t_acc[it], t_x1[s], t_lns[s], t_const)
            P.dma("sp", yout[r0:r0 + 128, :], x1s[s][:], reads=[t_x1[s]], writes=[t_yout])


def make_xT(P, x_ap, r0, ntile, xs, t_xs, xT, t_xT, pm, t_pm, ident, t_const, cm, t_xd=None):
    for tt in range(ntile):
        s = tt % len(xs)
        P.dma("sp", xs[s][:], x_ap[r0 + tt * 128:r0 + (tt + 1) * 128, :], reads=([t_xd] if t_xd is not None else []), writes=[t_xs[s]])
        for h in range(2):
            b = cm[0] % len(pm); cm[0] += 1
            for j in range(4):
                k = h * 4 + j
                P.op("pe", lambda e: e.transpose(out=pm[b][:, j * 128:(j + 1) * 128], in_=xs[s][:, k * 128:(k + 1) * 128],
                                                 identity=ident[:]),
                     reads=[t_xs[s], t_const], writes=[t_pm[b]])
            P.op("act", lambda e: e.copy(out=xT[:, h * 4:(h + 1) * 4, tt * 128:(tt + 1) * 128],
                                         in_=pm[b][:].rearrange("p (j t) -> p j t", j=4)),
                 reads=[t_pm[b]], writes=[t_xT])


def build_even(T, stop=99):
    P = Prog()
    nc = P.nc
    io = even_io(nc, "", T)
    io["t_xd"] = P.tok("xd"); io["t_outd"] = P.tok("outd")
    emit_even(P, T, io, stop)
    P.wait_all("sp", [io["t_outd"]])
    P.close()
    return nc


def even_io(nc, pfx, T, x=None, out=None):
    io = {}
    io["x"] = x if x is not None else nc.dram_tensor(pfx + "x", [T, D_MODEL], F32, kind="ExternalInput").ap()
    io["wA"] = nc.dram_tensor(pfx + "wA", [D_MODEL, 1284], F32, kind="ExternalInput").ap()
    io["wo"] = nc.dram_tensor(pfx + "wo", [512, D_MODEL], F32, kind="ExternalInput").ap()
    io["pw"] = nc.dram_tensor(pfx + "pw", [2, 128, 128], F32, kind="ExternalInput").ap()
    io["pc"] = nc.dram_tensor(pfx + "pc", [128, 30], F32, kind="ExternalInput").ap()
    io["coef0"] = nc.dram_tensor(pfx + "coef0", [128, 2 * 4 * 16], F32, kind="ExternalInput").ap()
    io["gbias"] = nc.dram_tensor(pfx + "gbias", [2, 2], F32, kind="ExternalInput").ap()
    io["mln"] = nc.dram_tensor(pfx + "mln", [1, 256], F32, kind="ExternalInput").ap()
    io["cst"] = nc.dram_tensor(pfx + "cst", [128, 128 + 128], F32, kind="ExternalInput").ap()
    io["cst2"] = nc.dram_tensor(pfx + "cst2", [2, 776], F32, kind="ExternalInput").ap()
    io["out"] = out if out is not None else nc.dram_tensor(pfx + "out", [T, D_MODEL], F32, kind="ExternalOutput").ap()
    return io


def emit_even(P, T, io, stop=99):
    TB = 512
    NB = T // TB
    NCH = TB // 64
    nc = P.nc
    x, wA, wo, pw, pc, coef0, gbias, mln, cst, cst2, out = (io[k] for k in
        ("x", "wA", "wo", "pw", "pc", "coef0", "gbias", "mln", "cst", "cst2", "out"))
    t_xd = io["t_xd"]; t_outd = io["t_outd"]

    t_const = P.tok("const")
    cs = P.sb([128, 256], F32, "cst"); ident = cs[:, 0:128]; maskT = cs[:, 128:256]
    cs2 = P.sb([2, 776], F32, "cst2"); rmask = cs2[:, 0:512]; id2 = cs2[:, 768:776]
    pct = P.sb([128, 30], F32, "pc"); c0t = P.sb([128, 128], F32, "coef0")
    gbt = P.sb([2, 2], F32, "gb"); mlt = P.sb([128, 256], F32, "mln"); eps_t = P.sb([128, 1], F32, "eps")
    wAt = P.sb([128, 8, 1284], BF16, "wA"); wot = P.sb([128, 4, D_MODEL], BF16, "wo"); pwt = P.sb([128, 2, 128], BF16, "pw")
    P.dma("sp", cs[:], cst[:, :], writes=[t_const])
    P.dma("sp", cs2[:], cst2[:, :], writes=[t_const])
    P.dma("sp", pct[:], pc[:, :], writes=[t_const])
    P.dma("sp", c0t[:], coef0[:, :], writes=[t_const])
    P.dma("sp", gbt[:], gbias[:, :], writes=[t_const])
    P.dma("sp", mlt[:], mln[0:1, :].partition_broadcast(128), writes=[t_const])
    t_w = P.tok("w")
    wv = wA.rearrange("(k p) n -> p k n", p=128)
    for k in range(8):
        P.dma("pool", wAt[:, k, :], wv[:, k, :], writes=[t_w])
    P.dma("pool", wot[:], wo.rearrange("(k p) n -> p k n", p=128), writes=[t_w])
    P.dma("pool", pwt[:], pw.rearrange("g c d -> c g d"), writes=[t_w])
    P.op("dve", lambda e: e.memset(eps_t[:], LN_EPS), writes=[t_const])
    nfb = P.sb([2, 1], F32, "nfb")
    identb = P.sb([128, 128], BF16, "identb")
    P.op("act", lambda e: e.copy(out=identb[:], in_=ident), reads=[t_const], writes=[t_const])
    P.op("dve", lambda e: e.tensor_scalar(out=nfb[:], in0=gbt[:, 1:2], scalar1=-1.0, scalar2=None, op0=ALU.mult),
         reads=[t_const], writes=[t_const])

    xs = [P.sb([128, D_MODEL], F32, "xs%d" % i) for i in range(2)]; t_xs = [P.tok("xs") for i in range(2)]
    xT = P.sb([128, 8, TB], BF16, "xT"); t_xT = P.tok("xT")
    ub = [P.sb([128, 16 + TB], F32, "ub%d" % g) for g in range(2)]; t_ub = [P.tok("ub") for g in range(2)]
    s2 = P.sb([128, 16 + TB], F32, "s2"); s4 = P.sb([128, 16 + TB], F32, "s4"); s8 = P.sb([128, 16 + TB], F32, "s8")
    s16 = P.sb([128, 16 + TB], F32, "s16"); t_s = P.tok("s")
    dacc = P.sb([128, TB], F32, "dacc"); dbf = P.sb([128, TB], BF16, "dbf"); t_d = P.tok("d")
    ycT = P.sb([128, 4, TB], BF16, "ycT"); t_yp = P.tok("yp"); t_ym = P.tok("ym")
    qkb = [P.sb([128, 3 + TB], F32, "qkb%d" % c) for c in range(4)]; t_qkb = [P.tok("qkb") for c in range(4)]
    cacc = P.sb([128, TB], F32, "cacc"); t_cacc = P.tok("cacc")
    qTe = [P.sb([128, TB], BF16, "qTe%d" % h) for h in range(2)]; qTo = [P.sb([128, TB], BF16, "qTo%d" % h) for h in range(2)]
    t_q = [P.tok("q") for h in range(2)]
    ksil = P.sb([128, TB], F32, "ksil"); t_ksil = P.tok("ksil")
    kT = [P.sb([128, TB], BF16, "kT%d" % h) for h in range(2)]; t_kT = [P.tok("kT") for h in range(2)]
    ktok = [P.sb([128, 4, 128], BF16, "ktok%d" % h) for h in range(2)]; t_ktok = [P.tok("ktok") for h in range(2)]
    vaug = P.sb([128, 4, 2, 130], BF16, "vaug"); t_v = P.tok("v")
    ogs = P.sb([128, 4, 256], F32, "ogs"); t_og = P.tok("og")
    C32 = [P.sb([128, 129], F32, "C32_%d" % h) for h in range(2)]; t_C = [P.tok("C") for h in range(2)]
    Csb = [[P.sb([128, 130], BF16, "Csb%d%d" % (h, i)) for i in range(2)] for h in range(2)]
    t_Cs = [[P.tok("Cs") for i in range(2)] for h in range(2)]
    PTm = [P.sb([128, 128], BF16, "PTm%d" % h) for h in range(2)]; t_PTm = [P.tok("PTm") for h in range(2)]
    gig = P.sb([2, TB], F32, "gig"); gsp = P.sb([2, TB], F32, "gsp"); gB = P.sb([2, TB], F32, "gB"); gu = P.sb([2, TB], F32, "gu")
    gev = P.sb([2, TB], F32, "gev"); gfl = P.sb([2, TB], F32, "gfl"); gtmp = P.sb([2, TB], F32, "gtmp")
    gmu = P.sb([2, NCH], F32, "gmu"); gg = P.sb([2, NCH], F32, "gg"); gms = P.sb([2, NCH], F32, "gms"); gMc = P.sb([2, NCH], F32, "gMc")
    gmp = P.sb([2, NCH], F32, "gmp"); gsig = P.sb([2, NCH], F32, "gsig"); mcar = P.sb([2, 1], F32, "mcar")
    t_g = P.tok("gates")
    sigb = P.sb([128, 2, NCH], F32, "sigb"); t_sigb = P.tok("sigb")
    flo = P.sb([128, 4, 2], F32, "flo"); t_flo = P.tok("flo")
    hsc = [dict(h=P.sb([128, 128], F32), dn=P.sb([128, 1], F32), st=P.sb([128, 6], F32), mv=P.sb([128, 2], F32),
                rstd=P.sb([128, 1], F32), sg=P.sb([128, 128], F32)) for i in range(2)]
    t_hsc = [P.tok("hsc") for i in range(2)]; t_hsg = [P.tok("hsg") for i in range(2)]
    yml = P.sb([128, 256], F32, "yml"); t_yml = P.tok("yml")
    osb = [P.sb([128, D_MODEL], F32, "osb%d" % i) for i in range(2)]; t_osb = [P.tok("osb") for i in range(2)]
    pp = [P.ps([128, 512], F32, "pp%d" % i) for i in range(2)]; t_pp = [P.tok("pp") for i in range(2)]
    pe_ = P.ps([128, 512], F32, "pe"); t_pe = P.tok("pe")
    pv = pe_; t_pv = t_pe
    pmisc = P.ps([128, 512], F32, "pmisc")
    pmb = P.ps([128, 1024], BF16, "pmb")
    t_pmisc = P.tok("pmisc"); t_psg = t_pmisc; t_pfl = t_pmisc; t_pkt = P.tok("pkt")
    pnum = [P.ps([128, 512], F32, "pnum%d" % h) for h in range(2)]; t_pnum = [P.tok("pnum") for h in range(2)]
    po = P.ps([128, 512], F32, "po"); t_po = P.tok("po")
    pm = [pe_, po]; t_pm = [t_pe, t_po]

    for h in range(2):
        P.op("dve", lambda e: e.memset(C32[h][:], 0.0), writes=[t_C[h]])
        P.op("pool", lambda e: e.memset(qTe[h][:], 0.0), writes=[t_q[h]])
        P.op("pool", lambda e: e.memset(qTo[h][:], 0.0), writes=[t_q[h]])
    P.op("dve", lambda e: e.memset(mcar[:], 0.0), writes=[t_g])
    P.op("pool", lambda e: e.memset(vaug[:], 1.0), writes=[t_v])
    for g in range(2):
        P.op("pool", lambda e: e.memset(ub[g][:, 0:16], 0.0), writes=[t_ub[g]])
    for c in range(4):
        P.op("pool", lambda e: e.memset(qkb[c][:, 0:3], 0.0), writes=[t_qkb[c]])

    cm = [0]; cpp = [0]; cos = [0]
    for blk in range(NB):
        r0 = blk * TB
        make_xT(P, x, r0, 4, xs, t_xs, xT, t_xT, pm, t_pm, ident, t_const, cm, t_xd)

        def proj_fm(c0, ncol):
            b = cpp[0] % 2; cpp[0] += 1
            for k in range(8):
                P.op("pe", lambda e: e.matmul(pp[b][0:ncol, :], lhsT=wAt[:, k, c0:c0 + ncol], rhs=xT[:, k, :], start=(k == 0), stop=(k == 7)),
                     reads=[t_w, t_xT], writes=[t_pp[b]])
            return b

        if stop <= 1:
            continue
        for g in range(2):
            b = proj_fm(g * 128, 128)
            P.op("act", lambda e: e.copy(out=ub[g][:, 16:16 + TB], in_=pp[b][:, :]), reads=[t_pp[b]], writes=[t_ub[g]])
            U = ub[g]; W = 16 + TB
            P.op("dve", lambda e: e.tensor_tensor(out=s2[:, 2:W], in0=U[:, 2:W], in1=U[:, 1:W - 1], op=ALU.add), reads=[t_ub[g]], writes=[t_s])
            P.op("dve", lambda e: e.tensor_tensor(out=s4[:, 4:W], in0=s2[:, 4:W], in1=s2[:, 2:W - 2], op=ALU.add), reads=[t_s], writes=[t_s])
            P.op("dve", lambda e: e.tensor_tensor(out=s8[:, 8:W], in0=s4[:, 8:W], in1=s4[:, 4:W - 4], op=ALU.add), reads=[t_s], writes=[t_s])
            P.op("dve", lambda e: e.tensor_tensor(out=s16[:, 16:W], in0=s8[:, 16:W], in1=s8[:, 8:W - 8], op=ALU.add), reads=[t_s], writes=[t_s])
            cf = pct[:, 22 + g * 4:26 + g * 4]
            lo = 16 if blk == 0 else 0
            srcs = [s2, s4, s8, s16]
            P.op("dve", lambda e: e.scalar_tensor_tensor(out=dacc[:, lo:TB], in0=s2[:, 16 + lo:W], scalar=cf[:, 0:1], in1=U[:, 16 + lo:W],
                                                         op0=ALU.mult, op1=ALU.subtract), reads=[t_s, t_ub[g], t_const], writes=[t_d])
            for wi in range(1, 4):
                P.op("dve", lambda e: e.scalar_tensor_tensor(out=dacc[:, lo:TB], in0=srcs[wi][:, 16 + lo:W], scalar=cf[:, wi:wi + 1],
                                                             in1=dacc[:, lo:TB], op0=ALU.mult, op1=ALU.add),
                     reads=[t_s, t_const, t_d], writes=[t_d])
            if blk == 0:
                c0v = c0t[:].rearrange("p (g w t) -> p g w t", g=2, w=4)
                P.op("dve", lambda e: e.tensor_tensor(out=dacc[:, 0:16], in0=s2[:, 16:32], in1=c0v[:, g, 0, :], op=ALU.mult),
                     reads=[t_s, t_const], writes=[t_d])
                P.op("dve", lambda e: e.tensor_tensor(out=dacc[:, 0:16], in0=dacc[:, 0:16], in1=U[:, 16:32], op=ALU.subtract),
                     reads=[t_d, t_ub[g]], writes=[t_d])
                for wi in range(1, 4):
                    P.op("dve", lambda e: e.tensor_tensor(out=s2[:, 0:16], in0=srcs[wi][:, 16:32], in1=c0v[:, g, wi, :], op=ALU.mult),
                         reads=[t_s, t_const], writes=[t_s])
                    P.op("dve", lambda e: e.tensor_tensor(out=dacc[:, 0:16], in0=dacc[:, 0:16], in1=s2[:, 0:16], op=ALU.add),
                         reads=[t_d, t_s], writes=[t_d])
            P.op("act", lambda e: e.copy(out=dbf[:], in_=dacc[:]), reads=[t_d], writes=[t_d])
            P.op("pool", lambda e: e.tensor_copy(out=U[:, 0:16], in_=U[:, TB:TB + 16]), reads=[t_ub[g]], writes=[t_ub[g]])
            P.op("pe", lambda e: e.matmul(po[:, :], lhsT=pwt[:, g, :], rhs=dbf[:], start=True, stop=True),
                 reads=[t_w, t_d], writes=[t_po])
            P.op("act", lambda e: e.activation(out=ycT[:, g, :], in_=po[:, :], func=AF.Identity, scale=pct[:, g:g + 1]),
                 reads=[t_po, t_const], writes=[t_yp])

        if stop <= 2:
            continue
        b = proj_fm(1280, 2)
        P.op("act", lambda e: e.activation(out=gig[:], in_=pp[b][0:2, :], func=AF.Identity, bias=gbt[:, 0:1], scale=1.0),
             reads=[t_pp[b], t_const], writes=[t_g])
        if stop <= 2.1:
            continue
        b = proj_fm(1282, 2)
        P.op("act", lambda e: e.activation(out=gsp[:], in_=pp[b][0:2, :], func=AF.Exp, bias=nfb[:, 0:1], scale=-1.0),
             reads=[t_pp[b], t_const], writes=[t_g])
        P.op("act", lambda e: e.activation(out=gsp[:], in_=gsp[:], func=AF.Ln, bias=1.0, scale=1.0), reads=[t_g], writes=[t_g])
        if stop <= 2.2:
            continue
        P.op("dve", lambda e: e.tensor_tensor_scan(out=gB[:], data0=rmask, data1=gsp[:], initial=0.0, op0=ALU.mult, op1=ALU.add),
             reads=[t_g, t_const], writes=[t_g])
        P.op("dve", lambda e: e.tensor_tensor(out=gu[:], in0=gig[:], in1=gB[:], op=ALU.add), reads=[t_g], writes=[t_g])
        if stop <= 2.3:
            continue
        gu3 = gu[:].rearrange("p (c s) -> p c s", s=64); gB3 = gB[:].rearrange("p (c s) -> p c s", s=64)
        P.op("dve", lambda e: e.tensor_reduce(out=gmu[:], in_=gu3, axis=mybir.AxisListType.X, op=ALU.max), reads=[t_g], writes=[t_g])
        P.op("dve", lambda e: e.tensor_scalar(out=gg[:], in0=gB3[:, :, 63], scalar1=-1.0, scalar2=None, op0=ALU.mult), reads=[t_g], writes=[t_g])
        if stop <= 2.4:
            continue
        P.op("dve", lambda e: e.tensor_tensor_scan(out=gms[:], data0=gmu[:], data1=gg[:], initial=mcar[:, 0:1], op0=ALU.max, op1=ALU.add),
             reads=[t_g], writes=[t_g])
        P.op("dve", lambda e: e.tensor_tensor(out=gMc[:], in0=gms[:], in1=gg[:], op=ALU.subtract), reads=[t_g], writes=[t_g])
        P.op("dve", lambda e: e.tensor_copy(out=gmp[:, 0:1], in_=mcar[:, 0:1]), reads=[t_g], writes=[t_g])
        P.op("dve", lambda e: e.tensor_copy(out=gmp[:, 1:NCH], in_=gms[:, 0:NCH - 1]), reads=[t_g], writes=[t_g])
        P.op("dve", lambda e: e.tensor_copy(out=mcar[:, 0:1], in_=gms[:, NCH - 1:NCH]), reads=[t_g], writes=[t_g])
        P.op("dve", lambda e: e.tensor_tensor(out=gsig[:], in0=gmp[:], in1=gMc[:], op=ALU.subtract), reads=[t_g], writes=[t_g])
        P.op("act", lambda e: e.activation(out=gsig[:], in_=gsig[:], func=AF.Exp), reads=[t_g], writes=[t_g])
        if stop <= 2.5:
            continue
        Mb = gMc[:].unsqueeze(2).to_broadcast([2, NCH, 64])
        P.op("dve", lambda e: e.tensor_tensor(out=gtmp[:].rearrange("p (c s) -> p c s", s=64), in0=gu3, in1=Mb, op=ALU.subtract),
             reads=[t_g], writes=[t_g])
        P.op("act", lambda e: e.activation(out=gev[:], in_=gtmp[:], func=AF.Exp), reads=[t_g], writes=[t_g])
        P.op("dve", lambda e: e.tensor_tensor(out=gtmp[:].rearrange("p (c s) -> p c s", s=64), in0=gB3, in1=Mb, op=ALU.subtract),
             reads=[t_g], writes=[t_g])
        P.op("act", lambda e: e.activation(out=gfl[:], in_=gtmp[:], func=AF.Exp), reads=[t_g], writes=[t_g])
        if stop <= 2.6:
            continue
        for h in range(2):
            P.op("pe", lambda e: e.matmul(pmisc[:, 300 + h * NCH:300 + (h + 1) * NCH], lhsT=cs2[:, 512 + h * 128:512 + (h + 1) * 128],
                                          rhs=gsig[:], start=True, stop=True), reads=[t_g, t_const], writes=[t_psg])
        P.op("act", lambda e: e.copy(out=sigb[:].rearrange("p h c -> p (h c)"), in_=pmisc[:, 300:300 + 2 * NCH]),
             reads=[t_psg], writes=[t_sigb])
        if stop <= 2.7:
            continue
        for tt in range(4):
            P.op("pe", lambda e: e.matmul(pmisc[:, 320 + 8 * tt:328 + 8 * tt], lhsT=gfl[:, tt * 128:(tt + 1) * 128], rhs=id2, start=True, stop=True),
                 reads=[t_g, t_const], writes=[t_pfl])
        if stop <= 2.8:
            continue
        P.op("act", lambda e: e.copy(out=flo[:], in_=pmisc[:, 320:352].rearrange("p (t j) -> p t j", j=8)[:, :, 0:2]), reads=[t_pfl], writes=[t_flo])

        if stop <= 3:
            continue
        for c in range(4):
            b = proj_fm(256 + c * 128, 128)
            Q = qkb[c]
            P.op("act", lambda e: e.copy(out=Q[:, 3:3 + TB], in_=pp[b][:, :]), reads=[t_pp[b]], writes=[t_qkb[c]])
            cw = pct[:, 2 + c * 4:6 + c * 4]
            P.op("dve", lambda e: e.tensor_scalar(out=cacc[:], in0=Q[:, 0:TB], scalar1=cw[:, 0:1], scalar2=pct[:, 18 + c:19 + c],
                                                  op0=ALU.mult, op1=ALU.add), reads=[t_qkb[c], t_const], writes=[t_cacc])
            for j in range(1, 4):
                P.op("dve", lambda e: e.scalar_tensor_tensor(out=cacc[:], in0=Q[:, j:j + TB], scalar=cw[:, j:j + 1], in1=cacc[:],
                                                             op0=ALU.mult, op1=ALU.add), reads=[t_qkb[c], t_const, t_cacc], writes=[t_cacc])
            P.op("pool", lambda e: e.tensor_copy(out=Q[:, 0:3], in_=Q[:, TB:TB + 3]), reads=[t_qkb[c]], writes=[t_qkb[c]])
            if c < 2:
                h = c
                ca3 = cacc[:].rearrange("p (c two s) -> p c two s", two=2, s=64)
                P.op("act", lambda e: e.activation(out=qTe[h][:].rearrange("p (c two s) -> p c two s", two=2, s=64)[:, :, 0, :],
                                                   in_=ca3[:, :, 0, :], func=AF.Silu), reads=[t_cacc], writes=[t_q[h]])
                P.op("act", lambda e: e.activation(out=qTo[h][:].rearrange("p (c two s) -> p c two s", two=2, s=64)[:, :, 1, :],
                                                   in_=ca3[:, :, 1, :], func=AF.Silu), reads=[t_cacc], writes=[t_q[h]])
            else:
                h = c - 2
                P.op("act", lambda e: e.activation(out=ksil[:], in_=cacc[:], func=AF.Silu), reads=[t_cacc], writes=[t_ksil])
                P.op("pe", lambda e: e.matmul(pe_[:, :], lhsT=cs2[:, 512 + h * 128:512 + (h + 1) * 128], rhs=gev[:], start=True, stop=True),
                     reads=[t_g, t_const], writes=[t_pe])
                P.op("dve", lambda e: e.scalar_tensor_tensor(out=kT[h][:], in0=ksil[:], scalar=128.0 ** -0.5, in1=pe_[:, :],
                                                             op0=ALU.mult, op1=ALU.mult), reads=[t_ksil, t_pe], writes=[t_kT[h]])
                for tt in range(4):
                    P.op("pe", lambda e: e.transpose(out=pmb[:, tt * 128:(tt + 1) * 128], in_=kT[h][:, tt * 128:(tt + 1) * 128],
                                                     identity=identb[:]), reads=[t_kT[h], t_const], writes=[t_pkt])
                P.op("act", lambda e: e.copy(out=ktok[h][:].rearrange("p t d -> p (t d)"), in_=pmb[:, 0:512]), reads=[t_pkt], writes=[t_ktok[h]])

        if stop <= 4:
            continue
        for tt in range(4):
            for k in range(8):
                P.op("pe", lambda e: e.matmul(pv[:, :], lhsT=xT[:, k, tt * 128:(tt + 1) * 128], rhs=wAt[:, k, 768:1280], start=(k == 0), stop=(k == 7)),
                     reads=[t_w, t_xT], writes=[t_pv])
            P.op("act", lambda e: e.copy(out=vaug[:, tt, :, 0:128], in_=pv[:, 0:256].rearrange("p (h d) -> p h d", h=2)),
                 reads=[t_pv], writes=[t_v])
            P.op("act", lambda e: e.activation(out=ogs[:, tt, :], in_=pv[:, 256:512], func=AF.Sigmoid), reads=[t_pv], writes=[t_og])

        if stop <= 5:
            continue
        pu = [pe_, po]; t_pu = [t_pe, t_po]
        for tt in range(4):
            tsl = slice(tt * 128, (tt + 1) * 128)
            H = range(2)
            for h in H:
                P.op("pe", lambda e: e.matmul(pp[h][:, 0:128], lhsT=kT[h][:, tsl], rhs=qTe[h][:, tsl], start=True, stop=False),
                     reads=[t_kT[h], t_q[h]], writes=[t_pp[h]])
                P.op("pe", lambda e: e.matmul(pp[h][:, 0:128], lhsT=kT[h][:, tsl], rhs=qTo[h][:, tsl], start=False, stop=True),
                     reads=[t_kT[h], t_q[h]], writes=[t_pp[h]])
            for h in H:
                P.op("dve", lambda e: e.tensor_tensor(out=PTm[h][:], in0=pp[h][:, 0:128], in1=maskT, op=ALU.mult),
                     reads=[t_pp[h], t_const], writes=[t_PTm[h]])
            for h in H:
                P.op("pe", lambda e: e.matmul(pnum[h][:, 0:129], lhsT=PTm[h][:], rhs=vaug[:, tt, h, 0:129], start=True, stop=False),
                     reads=[t_PTm[h], t_v], writes=[t_pnum[h]])
            for ci in range(2):
                c = tt * 2 + ci
                for h in H:
                    P.op("act", lambda e: e.activation(out=Csb[h][ci][:, 0:129], in_=C32[h][:], func=AF.Identity, scale=sigb[:, h, c:c + 1]),
                         reads=[t_C[h], t_sigb], writes=[t_Cs[h][ci]])
                for h in H:
                    qsrc = qTe[h] if ci == 0 else qTo[h]
                    P.op("pe", lambda e: e.matmul(pnum[h][:, 0:129], lhsT=qsrc[:, tsl], rhs=Csb[h][ci][:, 0:129], start=False, stop=(ci == 1)),
                         reads=[t_q[h], t_Cs[h][ci]], writes=[t_pnum[h]])
                    P.op("pe", lambda e: e.matmul(pu[h][:, 0:129], lhsT=ktok[h][ci * 64:(ci + 1) * 64, tt, :],
                                                  rhs=vaug[ci * 64:(ci + 1) * 64, tt, h, 0:129], start=True, stop=True),
                         reads=[t_ktok[h], t_v], writes=[t_pu[h]])
                for h in H:
                    P.op("dve", lambda e: e.scalar_tensor_tensor(out=C32[h][:], in0=C32[h][:], scalar=sigb[:, h, c:c + 1], in1=pu[h][:, 0:129],
                                                                 op0=ALU.mult, op1=ALU.add), reads=[t_C[h], t_sigb, t_pu[h]], writes=[t_C[h]])
            for h in H:
                S = hsc[h]; tS = t_hsc[h]
                P.op("act", lambda e: e.activation(out=S["dn"][:], in_=pnum[h][:, 128:129], func=AF.Abs), reads=[t_pnum[h]], writes=[tS])
            for h in H:
                S = hsc[h]; tS = t_hsc[h]
                P.op("dve", lambda e: e.tensor_scalar(out=S["dn"][:], in0=S["dn"][:], scalar1=flo[:, tt, h:h + 1], scalar2=None,
                                                      op0=ALU.max), reads=[tS, t_flo], writes=[tS])
            for h in H:
                S = hsc[h]; tS = t_hsc[h]
                P.op("dve", lambda e: e.reciprocal(out=S["dn"][:], in_=S["dn"][:]), reads=[tS], writes=[tS])
            for h in H:
                S = hsc[h]; tS = t_hsc[h]
                P.op("dve", lambda e: e.tensor_scalar(out=S["h"][:], in0=pnum[h][:, 0:128], scalar1=S["dn"][:, 0:1], scalar2=None, op0=ALU.mult),
                     reads=[t_pnum[h], tS], writes=[tS])
            for h in H:
                S = hsc[h]; tS = t_hsc[h]
                P.op("dve", lambda e: e.bn_stats(out=S["st"][:], in_=S["h"][:]), reads=[tS], writes=[tS])
            for h in H:
                S = hsc[h]; tS = t_hsc[h]
                P.op("dve", lambda e: e.bn_aggr(out=S["mv"][:], in_=S["st"][:]), reads=[tS], writes=[tS])
            for h in H:
                S = hsc[h]; tS = t_hsc[h]
                P.op("act", lambda e: e.activation(out=S["rstd"][:], in_=S["mv"][:, 1:2], func=AF.Sqrt, bias=eps_t[:, 0:1], scale=1.0),
                     reads=[tS, t_const], writes=[tS])
            for h in H:
                S = hsc[h]; tS = t_hsc[h]
                P.op("dve", lambda e: e.reciprocal(out=S["rstd"][:], in_=S["rstd"][:]), reads=[tS], writes=[tS])
            for h in H:
                S = hsc[h]; tS = t_hsc[h]
                P.op("dve", lambda e: e.tensor_scalar(out=S["h"][:], in0=S["h"][:], scalar1=S["mv"][:, 0:1], scalar2=S["rstd"][:, 0:1],
                                                      op0=ALU.subtract, op1=ALU.mult), reads=[tS], writes=[tS])
                P.op("pool", lambda e: e.tensor_tensor(out=S["sg"][:], in0=ogs[:, tt, h * 128:(h + 1) * 128], in1=mlt[:, h * 128:(h + 1) * 128],
                                                       op=ALU.mult), reads=[t_og, t_const], writes=[t_hsg[h]])
            for h in H:
                S = hsc[h]; tS = t_hsc[h]
                P.op("dve", lambda e: e.tensor_tensor(out=yml[:, h * 128:(h + 1) * 128], in0=S["h"][:], in1=S["sg"][:], op=ALU.mult),
                     reads=[tS, t_hsg[h]], writes=[t_yml])
            for h in range(2):
                P.op("pe", lambda e: e.transpose(out=pe_[:, 0:128], in_=yml[:, h * 128:(h + 1) * 128], identity=ident),
                     reads=[t_yml, t_const], writes=[t_pe])
                P.op("act", lambda e: e.copy(out=ycT[:, 2 + h, tsl], in_=pe_[:, 0:128]), reads=[t_pe], writes=[t_ym])

        if stop <= 6:
            continue
        for tt in range(4):
            s = cos[0] % 2; cos[0] += 1
            for hf in range(2):
                for kc in range(4):
                    P.op("pe", lambda e: e.matmul(po[:, :], lhsT=ycT[:, kc, tt * 128:(tt + 1) * 128], rhs=wot[:, kc, hf * 512:(hf + 1) * 512],
                                                  start=(kc == 0), stop=(kc == 3)), reads=[t_yp, t_ym, t_w], writes=[t_po])
                P.op("act", lambda e: e.copy(out=osb[s][:, hf * 512:(hf + 1) * 512], in_=po[:, :]), reads=[t_po], writes=[t_osb[s]])
            P.dma("sp", out[r0 + tt * 128:r0 + (tt + 1) * 128, :], osb[s][:], reads=[t_osb[s]], writes=[t_outd])


def even_inputs(x, w_in, pool_w, pool_scale, conv_w, conv_b, i_bias, f_bias, ml_norm, w_out, hp):
    f = np.float32
    h0 = 2 * hp
    u = w_in[:, 0:512][:, h0 * 128:(h0 + 2) * 128]
    q = w_in[:, 512:1024][:, h0 * 128:(h0 + 2) * 128]
    k = w_in[:, 1024:1536][:, h0 * 128:(h0 + 2) * 128]
    v = w_in[:, 1536:2048][:, h0 * 128:(h0 + 2) * 128]
    og = w_in[:, 2048:2560][:, h0 * 128:(h0 + 2) * 128]
    ig = w_in[:, 2560:2564][:, h0:h0 + 2]
    fg = w_in[:, 2564:2568][:, h0:h0 + 2]
    wA = np.ascontiguousarray(np.concatenate([u, q, k, v, og, ig, fg], axis=1), dtype=f)
    wo = np.ascontiguousarray(np.concatenate([w_out[h0 * 128:(h0 + 2) * 128], w_out[512 + h0 * 128:512 + (h0 + 2) * 128]], axis=0), dtype=f)
    pw = np.ascontiguousarray(pool_w[h0:h0 + 2], dtype=f)
    pc = np.zeros((128, 30), f)
    for g in range(2):
        pc[:, g] = pool_scale[(h0 + g) * 128:(h0 + g + 1) * 128]
    cw = np.concatenate([conv_w[:, 0:512][:, h0 * 128:(h0 + 2) * 128], conv_w[:, 512:1024][:, h0 * 128:(h0 + 2) * 128]], axis=1)
    cb = np.concatenate([conv_b[0:512][h0 * 128:(h0 + 2) * 128], conv_b[512:1024][h0 * 128:(h0 + 2) * 128]])
    for c in range(4):
        for j in range(4):
            pc[:, 2 + c * 4 + j] = cw[j, c * 128:(c + 1) * 128]
        pc[:, 18 + c] = cb[c * 128:(c + 1) * 128]
    coef0 = np.zeros((128, 2, 4, 16), f)
    for g in range(2):
        wi = h0 + g
        pc[:, 22 + g * 4 + wi] = 1.0 / (2 ** (wi + 1))
        coef0[:, g, wi, :] = 1.0 / np.minimum(np.arange(1, 17), 2 ** (wi + 1))
    gbias = np.stack([i_bias[h0:h0 + 2], f_bias[h0:h0 + 2]], axis=1).astype(f)
    mln = np.ascontiguousarray(ml_norm[h0 * 128:(h0 + 2) * 128].reshape(1, 256), dtype=f)
    cst = np.zeros((128, 256), f)
    cst[:, 0:128] = np.eye(128)
    s_i = np.arange(128)[:, None]; t_i = np.arange(128)[None, :]
    cst[:, 128:256] = ((s_i // 64 == t_i // 64) & (s_i <= t_i)).astype(f)
    cst2 = np.zeros((2, 776), f)
    cst2[:, 0:512] = (np.arange(512) % 64 != 0).astype(f)[None, :]
    cst2[0, 512:640] = 1.0
    cst2[1, 640:768] = 1.0
    cst2[:, 768:770] = np.eye(2)
    return dict(x=np.ascontiguousarray(x, dtype=f), wA=wA, wo=wo, pw=pw, pc=pc, coef0=coef0.reshape(128, 128), gbias=gbias, mln=mln,
                cst=cst, cst2=cst2)


def build_odd(T):
    P = Prog()
    nc = P.nc
    io = odd_io(nc, "", T)
    io["t_xd"] = P.tok("xd"); io["t_outd"] = P.tok("outd")
    emit_odd(P, T, io)
    P.wait_all("sp", [io["t_outd"]])
    P.close()
    return nc


def odd_io(nc, pfx, T, x=None, out=None):
    io = {}
    io["x"] = x if x is not None else nc.dram_tensor(pfx + "x", [T, D_MODEL], F32, kind="ExternalInput").ap()
    io["wA"] = nc.dram_tensor(pfx + "wA", [D_MODEL, 1552], F32, kind="ExternalInput").ap()
    io["wo"] = nc.dram_tensor(pfx + "wo", [512, D_MODEL], F32, kind="ExternalInput").ap()
    io["w2"] = nc.dram_tensor(pfx + "w2", [16, 256], F32, kind="ExternalInput").ap()
    io["pc"] = nc.dram_tensor(pfx + "pc", [128, 2], F32, kind="ExternalInput").ap()
    io["gln"] = nc.dram_tensor(pfx + "gln", [1, 512], F32, kind="ExternalInput").ap()
    io["cst"] = nc.dram_tensor(pfx + "cst", [128, 256 + 512], F32, kind="ExternalInput").ap()
    io["out"] = out if out is not None else nc.dram_tensor(pfx + "out", [T, D_MODEL], F32, kind="ExternalOutput").ap()
    return io


def emit_odd(P, T, io):
    TB = 512
    NB = T // TB
    NCH = TB // 64
    nc = P.nc
    x, wA, wo, w2, pc, gln, cst, out = (io[k] for k in ("x", "wA", "wo", "w2", "pc", "gln", "cst", "out"))
    t_xd = io["t_xd"]; t_outd = io["t_outd"]

    t_const = P.tok("const")
    cs = P.sb([128, 768], F32, "cst"); ident = cs[:, 0:128]; maskT = cs[:, 128:256]; rmask = cs[:, 256:768]
    pct = P.sb([128, 2], F32, "pc"); npct = P.sb([128, 2], F32, "npc")
    glt = P.sb([128, 512], F32, "gln"); eps_t = P.sb([128, 1], F32, "eps")
    wAt = P.sb([128, 8, 1552], BF16, "wA"); wot = P.sb([128, 4, D_MODEL], BF16, "wo"); w2t = P.sb([16, 256], BF16, "w2")
    identb = P.sb([128, 128], BF16, "identb")
    P.dma("sp", cs[:], cst[:, :], writes=[t_const])
    P.dma("sp", pct[:], pc[:, :], writes=[t_const])
    P.dma("sp", glt[:], gln[0:1, :].partition_broadcast(128), writes=[t_const])
    t_w = P.tok("w")
    wv = wA.rearrange("(k p) n -> p k n", p=128)
    for k in range(8):
        P.dma("pool", wAt[:, k, :], wv[:, k, :], writes=[t_w])
    P.dma("pool", wot[:], wo.rearrange("(k p) n -> p k n", p=128), writes=[t_w])
    P.dma("pool", w2t[:], w2[:, :], writes=[t_w])
    P.op("dve", lambda e: e.memset(eps_t[:], LN_EPS), writes=[t_const])
    P.op("dve", lambda e: e.tensor_scalar(out=npct[:], in0=pct[:], scalar1=-1.0, scalar2=None, op0=ALU.mult), reads=[t_const], writes=[t_const])
    P.op("act", lambda e: e.copy(out=identb[:], in_=ident), reads=[t_const], writes=[t_const])

    xs = [P.sb([128, D_MODEL], F32, "xs%d" % i) for i in range(2)]; t_xs = [P.tok("xs") for i in range(2)]
    xT = P.sb([128, 8, TB], BF16, "xT"); t_xT = P.tok("xT")
    glrT = P.sb([16, TB], BF16, "glrT"); t_glr = P.tok("glr")
    qf = P.sb([128, TB], F32, "qf"); kf = P.sb([128, TB], F32, "kf"); t_qk = P.tok("qkf")
    sp = P.sb([128, TB], F32, "sp"); Bp = P.sb([128, TB], F32, "Bp"); ex = P.sb([128, TB], F32, "ex"); rc = P.sb([128, TB], F32, "rc")
    t_dec = P.tok("dec")
    eBl = [P.sb([128, NCH], F32, "eBl%d" % h) for h in range(2)]; t_eBl = [P.tok("eBl") for h in range(2)]
    qTe = [P.sb([128, TB], BF16, "qTe%d" % h) for h in range(2)]; qTo = [P.sb([128, TB], BF16, "qTo%d" % h) for h in range(2)]
    t_q = [P.tok("q") for h in range(2)]
    kT = [P.sb([128, TB], BF16, "kT%d" % h) for h in range(2)]; t_kT = [P.tok("kT") for h in range(2)]
    khT = P.sb([128, TB], BF16, "khT"); t_khT = P.tok("khT")
    ktok = [P.sb([128, 4, 128], BF16, "ktok%d" % h) for h in range(2)]; t_ktok = [P.tok("ktok") for h in range(2)]
    vbf = P.sb([128, 4, 512], BF16, "vbf"); t_v = P.tok("v")
    rs = P.sb([128, 4, 512], F32, "rs"); t_r = P.tok("r")
    S32 = [P.sb([128, 256], F32, "S32_%d" % h) for h in range(2)]; t_S = [P.tok("S") for h in range(2)]
    Sb = [[P.sb([128, 256], BF16, "Sb%d%d" % (h, i)) for i in range(2)] for h in range(2)]
    t_Sb = [[P.tok("Sb") for i in range(2)] for h in range(2)]
    ATm = [P.sb([128, 128], BF16, "ATm%d" % h) for h in range(2)]; t_AT = [P.tok("AT") for h in range(2)]
    hsc = [dict(h=P.sb([128, 256], F32), st=P.sb([128, 6], F32), mv=P.sb([128, 2], F32), rstd=P.sb([128, 1], F32),
                sg=P.sb([128, 256], F32)) for i in range(2)]
    t_hsc = [P.tok("hsc") for i in range(2)]; t_hsg = [P.tok("hsg") for i in range(2)]
    yml = P.sb([128, 512], F32, "yml"); t_yml = P.tok("yml")
    ycT = P.sb([128, 4, TB], BF16, "ycT"); t_ym = P.tok("ym")
    osb = [P.sb([128, D_MODEL], F32, "osb%d" % i) for i in range(2)]; t_osb = [P.tok("osb") for i in range(2)]
    pp = [P.ps([128, 512], F32, "pp%d" % i) for i in range(2)]; t_pp = [P.tok("pp") for i in range(2)]
    pe_ = P.ps([128, 512], F32, "pe"); t_pe = P.tok("pe")
    pmb = P.ps([128, 1024], BF16, "pmb"); t_pkt = P.tok("pkt")
    pnum = [P.ps([128, 512], F32, "pnum%d" % h) for h in range(2)]; t_pnum = [P.tok("pnum") for h in range(2)]
    po = P.ps([128, 512], F32, "po"); t_po = P.tok("po")
    pm = [pe_, po]; t_pm = [t_pe, t_po]

    for h in range(2):
        P.op("dve", lambda e: e.memset(S32[h][:], 0.0), writes=[t_S[h]])
        P.op("pool", lambda e: e.memset(Sb[h][0][:], 0.0), writes=[t_Sb[h][0]])
        P.op("pool", lambda e: e.memset(qTe[h][:], 0.0), writes=[t_q[h]])
        P.op("pool", lambda e: e.memset(qTo[h][:], 0.0), writes=[t_q[h]])

    cm = [0]; cpp = [0]; cos = [0]
    for blk in range(NB):
        r0 = blk * TB
        make_xT(P, x, r0, 4, xs, t_xs, xT, t_xT, pm, t_pm, ident, t_const, cm, t_xd)

        def proj_fm(c0, ncol):
            b = cpp[0] % 2; cpp[0] += 1
            for k in range(8):
                P.op("pe", lambda e: e.matmul(pp[b][0:ncol, :], lhsT=wAt[:, k, c0:c0 + ncol], rhs=xT[:, k, :], start=(k == 0), stop=(k == 7)),
                     reads=[t_w, t_xT], writes=[t_pp[b]])
            return b

        b = proj_fm(1536, 16)
        P.op("act", lambda e: e.copy(out=glrT[:], in_=pp[b][0:16, :]), reads=[t_pp[b]], writes=[t_glr])
        for h in range(2):
            bq = proj_fm(h * 128, 128)
            P.op("act", lambda e: e.copy(out=qf[:], in_=pp[bq][:, :]), reads=[t_pp[bq]], writes=[t_qk])
            bk = proj_fm(256 + h * 128, 128)
            P.op("act", lambda e: e.copy(out=kf[:], in_=pp[bk][:, :]), reads=[t_pp[bk]], writes=[t_qk])
            b = cpp[0] % 2; cpp[0] += 1
            P.op("pe", lambda e: e.matmul(pp[b][:, :], lhsT=w2t[:, h * 128:(h + 1) * 128], rhs=glrT[:], start=True, stop=True),
                 reads=[t_w, t_glr], writes=[t_pp[b]])
            P.op("act", lambda e: e.activation(out=sp[:], in_=pp[b][:, :], func=AF.Exp, bias=npct[:, h:h + 1], scale=-1.0),
                 reads=[t_pp[b], t_const], writes=[t_dec])
            P.op("act", lambda e: e.activation(out=sp[:], in_=sp[:], func=AF.Ln, bias=1.0, scale=1.0), reads=[t_dec], writes=[t_dec])
            P.op("dve", lambda e: e.tensor_tensor_scan(out=Bp[:], data0=rmask, data1=sp[:], initial=0.0, op0=ALU.mult, op1=ALU.add),
                 reads=[t_dec, t_const], writes=[t_dec])
            Bp3 = Bp[:].rearrange("p (c s) -> p c s", s=64)
            P.op("act", lambda e: e.activation(out=ex[:], in_=Bp[:], func=AF.Exp, scale=-1.0 / 16.0), reads=[t_dec], writes=[t_dec])
            q3 = qf[:].rearrange("p (c two s) -> p c two s", two=2, s=64); e3 = ex[:].rearrange("p (c two s) -> p c two s", two=2, s=64)
            P.op("dve", lambda e: e.scalar_tensor_tensor(out=qTe[h][:].rearrange("p (c two s) -> p c two s", two=2, s=64)[:, :, 0, :],
                                                         in0=q3[:, :, 0, :], scalar=128.0 ** -0.5, in1=e3[:, :, 0, :], op0=ALU.mult, op1=ALU.mult),
                 reads=[t_qk, t_dec], writes=[t_q[h]])
            P.op("dve", lambda e: e.scalar_tensor_tensor(out=qTo[h][:].rearrange("p (c two s) -> p c two s", two=2, s=64)[:, :, 1, :],
                                                         in0=q3[:, :, 1, :], scalar=128.0 ** -0.5, in1=e3[:, :, 1, :], op0=ALU.mult, op1=ALU.mult),
                 reads=[t_qk, t_dec], writes=[t_q[h]])
            P.op("act", lambda e: e.activation(out=ex[:], in_=Bp[:], func=AF.Exp, scale=1.0 / 16.0), reads=[t_dec, t_q[h]], writes=[t_dec])
            P.op("dve", lambda e: e.tensor_tensor(out=kT[h][:], in0=kf[:], in1=ex[:], op=ALU.mult), reads=[t_qk, t_dec], writes=[t_kT[h]])
            P.op("dve", lambda e: e.tensor_tensor(out=rc[:].rearrange("p (c s) -> p c s", s=64), in0=Bp3,
                                                  in1=Bp3[:, :, 63:64].to_broadcast([128, NCH, 64]), op=ALU.subtract), reads=[t_dec], writes=[t_dec])
            P.op("act", lambda e: e.activation(out=ex[:], in_=rc[:], func=AF.Exp, scale=1.0 / 16.0), reads=[t_dec, t_kT[h]], writes=[t_dec])
            P.op("dve", lambda e: e.tensor_tensor(out=khT[:], in0=kf[:], in1=ex[:], op=ALU.mult), reads=[t_qk, t_dec], writes=[t_khT])
            P.op("act", lambda e: e.activation(out=eBl[h][:], in_=Bp3[:, :, 63], func=AF.Exp, scale=-1.0 / 16.0), reads=[t_dec], writes=[t_eBl[h]])
            for tt in range(4):
                P.op("pe", lambda e: e.transpose(out=pmb[:, tt * 128:(tt + 1) * 128], in_=khT[:, tt * 128:(tt + 1) * 128], identity=identb[:]),
                     reads=[t_khT, t_const], writes=[t_pkt])
            P.op("act", lambda e: e.copy(out=ktok[h][:].rearrange("p t d -> p (t d)"), in_=pmb[:, 0:512]), reads=[t_pkt], writes=[t_ktok[h]])

        for tt in range(4):
            for k in range(8):
                P.op("pe", lambda e: e.matmul(pe_[:, :], lhsT=xT[:, k, tt * 128:(tt + 1) * 128], rhs=wAt[:, k, 512:1024], start=(k == 0), stop=(k == 7)),
                     reads=[t_w, t_xT], writes=[t_pe])
            P.op("act", lambda e: e.copy(out=vbf[:, tt, :], in_=pe_[:, :]), reads=[t_pe], writes=[t_v])
            for k in range(8):
                P.op("pe", lambda e: e.matmul(po[:, :], lhsT=xT[:, k, tt * 128:(tt + 1) * 128], rhs=wAt[:, k, 1024:1536], start=(k == 0), stop=(k == 7)),
                     reads=[t_w, t_xT], writes=[t_po])
            P.op("act", lambda e: e.activation(out=rs[:, tt, :], in_=po[:, :], func=AF.Silu), reads=[t_po], writes=[t_r])

        pu = [pe_, po]; t_pu = [t_pe, t_po]
        for tt in range(4):
            tsl = slice(tt * 128, (tt + 1) * 128)
            H = range(2)
            for h in H:
                P.op("pe", lambda e: e.matmul(pp[h][:, 0:128], lhsT=kT[h][:, tsl], rhs=qTe[h][:, tsl], start=True, stop=False),
                     reads=[t_kT[h], t_q[h]], writes=[t_pp[h]])
                P.op("pe", lambda e: e.matmul(pp[h][:, 0:128], lhsT=kT[h][:, tsl], rhs=qTo[h][:, tsl], start=False, stop=True),
                     reads=[t_kT[h], t_q[h]], writes=[t_pp[h]])
            for h in H:
                P.op("dve", lambda e: e.tensor_tensor(out=ATm[h][:], in0=pp[h][:, 0:128], in1=maskT, op=ALU.mult),
                     reads=[t_pp[h], t_const], writes=[t_AT[h]])
            for h in H:
                P.op("pe", lambda e: e.matmul(pnum[h][:, 0:256], lhsT=ATm[h][:], rhs=vbf[:, tt, h * 256:(h + 1) * 256], start=True, stop=False),
                     reads=[t_AT[h], t_v], writes=[t_pnum[h]])
            for ci in range(2):
                c = tt * 2 + ci
                for h in H:
                    qsrc = qTe[h] if ci == 0 else qTo[h]
                    P.op("pe", lambda e: e.matmul(pnum[h][:, 0:256], lhsT=qsrc[:, tsl], rhs=Sb[h][ci][:], start=False, stop=(ci == 1)),
                         reads=[t_q[h], t_Sb[h][ci]], writes=[t_pnum[h]])
                    P.op("pe", lambda e: e.matmul(pu[h][:, 0:256], lhsT=ktok[h][ci * 64:(ci + 1) * 64, tt, :],
                                                  rhs=vbf[ci * 64:(ci + 1) * 64, tt, h * 256:(h + 1) * 256], start=True, stop=True),
                         reads=[t_ktok[h], t_v], writes=[t_pu[h]])
                for h in H:
                    P.op("dve", lambda e: e.scalar_tensor_tensor(out=S32[h][:], in0=S32[h][:], scalar=eBl[h][:, c:c + 1], in1=pu[h][:, 0:256],
                                                                 op0=ALU.mult, op1=ALU.add), reads=[t_S[h], t_eBl[h], t_pu[h]], writes=[t_S[h]])
                for h in H:
                    P.op("act", lambda e: e.copy(out=Sb[h][1 - ci][:], in_=S32[h][:]), reads=[t_S[h]], writes=[t_Sb[h][1 - ci]])
            for h in H:
                S = hsc[h]; tS = t_hsc[h]
                P.op("dve", lambda e: e.bn_stats(out=S["st"][:], in_=pnum[h][:, 0:256]), reads=[t_pnum[h]], writes=[tS])
            for h in H:
                S = hsc[h]; tS = t_hsc[h]
                P.op("dve", lambda e: e.bn_aggr(out=S["mv"][:], in_=S["st"][:]), reads=[tS], writes=[tS])
            for h in H:
                S = hsc[h]; tS = t_hsc[h]
                P.op("act", lambda e: e.activation(out=S["rstd"][:], in_=S["mv"][:, 1:2], func=AF.Sqrt, bias=eps_t[:, 0:1], scale=1.0),
                     reads=[tS, t_const], writes=[tS])
            for h in H:
                S = hsc[h]; tS = t_hsc[h]
                P.op("dve", lambda e: e.reciprocal(out=S["rstd"][:], in_=S["rstd"][:]), reads=[tS], writes=[tS])
            for h in H:
                S = hsc[h]; tS = t_hsc[h]
                P.op("dve", lambda e: e.tensor_scalar(out=S["h"][:], in0=pnum[h][:, 0:256], scalar1=S["mv"][:, 0:1], scalar2=S["rstd"][:, 0:1],
                                                      op0=ALU.subtract, op1=ALU.mult), reads=[t_pnum[h], tS], writes=[tS])
                P.op("pool", lambda e: e.tensor_tensor(out=S["sg"][:], in0=rs[:, tt, h * 256:(h + 1) * 256], in1=glt[:, h * 256:(h + 1) * 256],
                                                       op=ALU.mult), reads=[t_r, t_const], writes=[t_hsg[h]])
            for h in H:
                S = hsc[h]; tS = t_hsc[h]
                P.op("dve", lambda e: e.tensor_tensor(out=yml[:, h * 256:(h + 1) * 256], in0=S["h"][:], in1=S["sg"][:], op=ALU.mult),
                     reads=[tS, t_hsg[h]], writes=[t_yml])
            for kc in range(4):
                P.op("pe", lambda e: e.transpose(out=pe_[:, kc * 128:(kc + 1) * 128], in_=yml[:, kc * 128:(kc + 1) * 128], identity=ident),
                     reads=[t_yml, t_const], writes=[t_pe])
            P.op("act", lambda e: e.copy(out=ycT[:, :, tsl], in_=pe_[:, :].rearrange("p (k t) -> p k t", k=4)), reads=[t_pe], writes=[t_ym])

        for tt in range(4):
            s = cos[0] % 2; cos[0] += 1
            for hf in range(2):
                for kc in range(4):
                    P.op("pe", lambda e: e.matmul(po[:, :], lhsT=ycT[:, kc, tt * 128:(tt + 1) * 128], rhs=wot[:, kc, hf * 512:(hf + 1) * 512],
                                                  start=(kc == 0), stop=(kc == 3)), reads=[t_ym, t_w], writes=[t_po])
                P.op("act", lambda e: e.copy(out=osb[s][:, hf * 512:(hf + 1) * 512], in_=po[:, :]), reads=[t_po], writes=[t_osb[s]])
            P.dma("sp", out[r0 + tt * 128:r0 + (tt + 1) * 128, :], osb[s][:], reads=[t_osb[s]], writes=[t_outd])


def odd_inputs(x, w_in, gla_w2, gla_b, gla_norm, w_out, hp):
    f = np.float32
    h0 = 2 * hp
    q = w_in[:, 0:512][:, h0 * 128:(h0 + 2) * 128]
    k = w_in[:, 512:1024][:, h0 * 128:(h0 + 2) * 128]
    v = w_in[:, 1024:2048][:, h0 * 256:(h0 + 2) * 256]
    r = w_in[:, 2048:3072][:, h0 * 256:(h0 + 2) * 256]
    glr = w_in[:, 3072:3088]
    wA = np.ascontiguousarray(np.concatenate([q, k, v, r, glr], axis=1), dtype=f)
    wo = np.ascontiguousarray(w_out[h0 * 256:(h0 + 2) * 256], dtype=f)
    w2 = np.ascontiguousarray(gla_w2[:, h0 * 128:(h0 + 2) * 128], dtype=f)
    pc = np.ascontiguousarray(gla_b[h0 * 128:(h0 + 2) * 128].reshape(2, 128).T, dtype=f)
    gln = np.ascontiguousarray(gla_norm[h0 * 256:(h0 + 2) * 256].reshape(1, 512), dtype=f)
    cst = np.zeros((128, 768), f)
    cst[:, 0:128] = np.eye(128)
    s_i = np.arange(128)[:, None]; t_i = np.arange(128)[None, :]
    cst[:, 128:256] = ((s_i // 64 == t_i // 64) & (s_i <= t_i)).astype(f)
    cst[:, 256:768] = (np.arange(512) % 64 != 0).astype(f)[None, :]
    return dict(x=np.ascontiguousarray(x, dtype=f), wA=wA, wo=wo, w2=w2, pc=pc, gln=gln, cst=cst)


FUSED_CORES = BATCH
SPARSE_MOE = True


def build_fused(n_layers=DEPTH, T=SEQ, sparse=True):
    P = Prog()
    nc = P.nc
    x_ext = nc.dram_tensor("x", [T, D_MODEL], F32, kind="ExternalInput").ap()
    y_ext = nc.dram_tensor("y", [T, D_MODEL], F32, kind="ExternalOutput").ap()
    xbuf = [nc.dram_tensor("xbuf%d" % i, [T, D_MODEL], F32).ap() for i in range(2)]
    pmix = [nc.dram_tensor("pmix%d" % i, [T, D_MODEL], F32).ap() for i in range(2)]
    t_xext = P.tok("xext")
    t_y = P.tok("y"); t_y.disjoint = True
    t_xbuf = [P.tok("xbuf%d" % i) for i in range(2)]
    t_pmix = [P.tok("pmix%d" % i) for i in range(2)]
    for t in t_xbuf + t_pmix:
        t.disjoint = True
    scr = None
    if sparse:
        scr = moe_sparse_scratch(nc, T)
        for k in ("t_xbkt", "t_ybuf", "t_x1d"):
            scr[k] = P.tok(k); scr[k].disjoint = True
    for l in range(n_layers):
        xin_ap, t_xin = (x_ext, t_xext) if l == 0 else (xbuf[(l - 1) % 2], t_xbuf[(l - 1) % 2])
        yout_ap, t_yout = (y_ext, t_y) if l == n_layers - 1 else (xbuf[l % 2], t_xbuf[l % 2])
        for hp in range(2):
            P.push()
            pfx = "L%dH%d_" % (l, hp)
            if l % 2 == 0:
                io = even_io(nc, pfx, T, x=xin_ap, out=pmix[hp])
            else:
                io = odd_io(nc, pfx, T, x=xin_ap, out=pmix[hp])
            io["t_xd"] = t_xin; io["t_outd"] = t_pmix[hp]
            if l % 2 == 0:
                emit_even(P, T, io)
            else:
                emit_odd(P, T, io)
            P.pop()
        P.push()
        io = moe_io(nc, "L%d_" % l, T, xin=xin_ap, ma=pmix[0], mb=pmix[1], yout=yout_ap, sparse=sparse)
        io["t_xin"] = t_xin; io["t_ma"] = t_pmix[0]; io["t_mb"] = t_pmix[1]; io["t_yout"] = t_yout
        if sparse:
            emit_moe_sparse(P, T, io, scr, first=(l == 0))
        else:
            emit_moe(P, T // 1024, io)
        P.pop()
    P.wait_all("sp", [t_y])
    P.close()
    return nc


_NC_CACHE = {}


def fused_inputs(b, x, even_w_in, pool_w, pool_scale, conv_w, conv_b, i_bias, f_bias, ml_norm, even_w_out,
                 odd_w_in, gla_w2, gla_b, gla_norm, odd_w_out, lnps, router_w, router_b, wgu_d, bgu_l, w_down, b_down, n_layers):
    f = np.float32
    im = {"x": np.ascontiguousarray(x[b], dtype=f)}
    ident = np.eye(128, dtype=f)
    for l in range(n_layers):
        i = l // 2
        for hp in range(2):
            pfx = "L%dH%d_" % (l, hp)
            if l % 2 == 0:
                d = even_inputs(x[b], even_w_in[i], pool_w[i], pool_scale[i], conv_w[i], conv_b[i], i_bias[i], f_bias[i],
                                ml_norm[i], even_w_out[i], hp)
            else:
                d = odd_inputs(x[b], odd_w_in[i], gla_w2[i], gla_b[i], gla_norm[i], odd_w_out[i], hp)
            for k, v in d.items():
                if k != "x":
                    im[pfx + k] = v
        pfx = "L%d_" % l
        im[pfx + "lnp"] = lnps[l]
        im[pfx + "rw"] = np.ascontiguousarray(router_w[l], dtype=f)
        im[pfx + "rb"] = np.ascontiguousarray(router_b[l], dtype=f).reshape(1, N_EXPERTS)
        im[pfx + "wgu"] = wgu_d[l]
        im[pfx + "bgu"] = bgu_l[l]
        im[pfx + "wd"] = np.ascontiguousarray(w_down[l], dtype=f)
        im[pfx + "bd"] = np.ascontiguousarray(b_down[l], dtype=f)
        im[pfx + "ident"] = moe_sparse_consts() if SPARSE_MOE else ident
    return im


def kernel(x, even_w_in, pool_w, pool_scale, conv_w, conv_b, i_bias, f_bias, ml_norm, even_w_out,
           odd_w_in, gla_w2, gla_b, gla_norm, odd_w_out, ln1_g, ln1_b, ln2_g, ln2_b, router_w, router_b,
           w_gate_up, b_gate_up, w_down, b_down, _layers=DEPTH, _cores=FUSED_CORES):
    f = np.float32
    A = lambda a: np.asarray(a, dtype=f)
    x = A(x)
    wgu_d, bgu_l, lnps = [], [], []
    for l in range(_layers):
        wg = A(w_gate_up[l])
        wgu_d.append(np.ascontiguousarray(np.concatenate([wg[:, :, 0::2], wg[:, :, 1::2]], axis=-1)))
        del wg
        bg = A(b_gate_up[l])
        bd_ = np.concatenate([bg[:, 0::2], bg[:, 1::2]], axis=-1)
        bgu_l.append(np.ascontiguousarray(bd_.reshape(N_EXPERTS, 16, 128).transpose(2, 0, 1).reshape(128, N_EXPERTS * 16)))
        lnps.append(np.stack([A(ln1_g[l]), A(ln1_b[l]), A(ln2_g[l]), A(ln2_b[l])]))
    args = [A(v) for v in (even_w_in, pool_w, pool_scale, conv_w, conv_b, i_bias, f_bias, ml_norm, even_w_out,
                           odd_w_in, gla_w2, gla_b, gla_norm, odd_w_out)]
    ims = [fused_inputs(b, x, *args, lnps, A(router_w), A(router_b), wgu_d, bgu_l, A(w_down), A(b_down), _layers)
           for b in range(_cores)]
    key = ("fused", _layers)
    if key not in _NC_CACHE:
        _NC_CACHE[key] = build_fused(_layers, sparse=SPARSE_MOE)
    res = run_bass_kernel_spmd(_NC_CACHE[key], ims, core_ids=list(range(_cores)))
    out = np.stack([r["y"] for r in res.results], axis=0)
    if _cores < BATCH:
        return out
    return out.reshape(BATCH, SEQ, D_MODEL)


MOE_CAP = 768
MOE_NR = N_EXPERTS * MOE_CAP
U32 = mybir.dt.uint32


def moe_sparse_scratch(nc, T):
    return dict(xbkt=nc.dram_tensor("xbkt", [MOE_NR + 128, D_MODEL], BF16).ap(),
                ybuf=nc.dram_tensor("ybuf", [MOE_NR + 128, D_MODEL], F32).ap(),
                x1d=nc.dram_tensor("x1d", [T, D_MODEL], F32).ap())


def _idma(P, out, in_, out_off=None, in_off=None, reads=(), writes=()):
    P._deps("pool", reads, writes)
    owner = writes[0]
    key = P._dsem(owner)
    ins = P.nc.gpsimd.indirect_dma_start(out=out, out_offset=out_off, in_=in_, in_offset=in_off)
    ins.then_inc(P.sems[key], 16)
    owner.dcount += 16
    P.dtot[key] = owner.dcount
    me = (key, owner.dcount)
    for r in reads:
        r.readers.append(me)
    for w in writes:
        w.writer = me
        w.readers = []


def emit_moe_sparse(P, T, io, scr, first=False, stop_phase=9):
    NT = T // 128
    C = MOE_CAP
    CB = C // 2
    NRT = C // 128
    nc = P.nc
    xin, ma, mb, lnp, rw, rb, wgu, bgu, wd, bd, cst_in, yout = (io[k] for k in
        ("xin", "ma", "mb", "lnp", "rw", "rb", "wgu", "bgu", "wd", "bd", "ident", "yout"))
    t_xin, t_ma, t_mb, t_yout = io["t_xin"], io["t_ma"], io["t_mb"], io["t_yout"]
    xbkt, ybuf, x1d = scr["xbkt"], scr["ybuf"], scr["x1d"]
    t_xbkt, t_ybuf, t_x1d = scr["t_xbkt"], scr["t_ybuf"], scr["t_x1d"]

    t_const = P.tok("const")
    cs = P.sb([128, 128 * 3 + 32 + 1], F32, "mcst")
    ident = cs[:, 0:128]; Ltri = cs[:, 128:256]; ones = cs[:, 256:384]; ebase1 = cs[:, 384:416]; ptrash = cs[:, 416:417]
    gb = P.sb([128, 4, D_MODEL], F32, "gb")
    rwt = P.sb([128, 8, N_EXPERTS], F32, "rwt"); rbt = P.sb([128, N_EXPERTS], F32, "rbt")
    bgt = P.sb([128, N_EXPERTS * 16], F32, "bgt"); bdt = P.sb([N_EXPERTS, D_MODEL], F32, "bdt")
    eps_t = P.sb([128, 1], F32, "eps")
    Gall = P.sb([128, NT, N_EXPERTS], F32, "G"); t_G = [P.tok("G") for i in range(NT)]
    Gk = P.sb([128, NT, 4], F32, "Gk"); Sidx = P.sb([128, NT, 4], U32, "Sidx"); t_sel = [P.tok("sel") for i in range(NT)]
    base = P.sb([128, N_EXPERTS], F32, "base"); t_base = P.tok("base")
    P.dma("sp", cs[:], cst_in[:, :], writes=[t_const])
    for i in range(4):
        P.dma("sp", gb[:, i, :], lnp[i:i + 1, :].partition_broadcast(128), writes=[t_const])
    P.dma("sp", rwt[:], rw.rearrange("(k p) n -> p k n", p=128), writes=[t_const])
    P.dma("sp", rbt[:], rb[0:1, :].partition_broadcast(128), writes=[t_const])
    P.dma("sp", bgt[:], bgu[:, :], writes=[t_const])
    P.dma("sp", bdt[:], bd[:, :], writes=[t_const])
    P.op("dve", lambda e: e.memset(eps_t[:], LN_EPS), writes=[t_const])
    P.op("dve", lambda e: e.memset(base[:], 0.0), writes=[t_base])
    bgv = bgt[:].rearrange("p (e c) -> p e c", c=16)
    P.op("dve", lambda e: e.tensor_scalar(out=bgv[:, :, 8:16], in0=bgv[:, :, 8:16], scalar1=1.0, scalar2=None, op0=ALU.add),
         reads=[t_const], writes=[t_const])

    P.push()
    xs_l = [P.sb([128, D_MODEL], F32, "xs%d" % i) for i in range(2)]; t_xs_l = [P.tok("xs") for i in range(2)]
    pas_l = [P.sb([128, D_MODEL], F32, "pa%d" % i) for i in range(2)]; t_pa_l = [P.tok("pa") for i in range(2)]
    pbs_l = [P.sb([128, D_MODEL], F32, "pb%d" % i) for i in range(2)]; t_pb_l = [P.tok("pb") for i in range(2)]
    x1s = [P.sb([128, D_MODEL], F32, "x1s%d" % i) for i in range(2)]; t_x1 = [P.tok("x1s") for i in range(2)]
    x1b = [P.sb([128, D_MODEL], BF16, "x1b%d" % i) for i in range(2)]; t_x1b = [P.tok("x1b") for i in range(2)]
    xTf_l = [P.sb([128, 8, 128], F32, "xTf%d" % i) for i in range(2)]; t_xTf_l = [P.tok("xTf") for i in range(2)]
    lns_l = [dict(st=P.sb([128, 12], F32), mv=P.sb([128, 2], F32), rstd=P.sb([128, 1], F32), eps=eps_t) for i in range(2)]
    t_lns_l = [P.tok("lns") for i in range(2)]
    R_l = [dict(lg=P.sb([128, 32], F32), t8=P.sb([128, 8], F32), nm=P.sb([128, 1], F32), ex=P.sb([128, 32], F32),
                mk=P.sb([128, 32], F32), sm=P.sb([128, 1], F32), pos=P.sb([128, 32], F32), v1=P.sb([128, 32], F32),
                smat=P.sb([128, 32], F32), s8=P.sb([128, 8], F32), neg=P.sb([128, 4], F32), sf=P.sb([128, 4], F32),
                junk=P.sb([128, 32], F32)) for i in range(2)]
    tR_l = [P.tok("rt") for i in range(2)]
    pm = [P.ps([128, 512], F32, "pm%d" % i) for i in range(4)]; t_pm = [P.tok("pm") for i in range(4)]
    pr_l = [P.ps([128, 512], F32, "pr%d" % i) for i in range(2)]; t_pr_l = [P.tok("pr") for i in range(2)]
    xs = xs_l[0]; t_xs = t_xs_l[0]
    if first:
        P.op("dve", lambda e: e.memset(xs[:], 0.0), writes=[t_xs])
        P.dma("sp", ybuf[MOE_NR:MOE_NR + 128, :], xs[:], reads=[t_xs], writes=[t_ybuf])
    cm = [0]
    for it in range(NT):
        s = it % 2
        r0 = it * 128
        xs = xs_l[s]; t_xs = t_xs_l[s]; pas = pas_l[s]; t_pa = t_pa_l[s]; pbs = pbs_l[s]; t_pb = t_pb_l[s]
        xTf = xTf_l[s]; t_xTf = t_xTf_l[s]; lns = lns_l[s]; t_lns = t_lns_l[s]; R = R_l[s]; tR = tR_l[s]; pr = pr_l[s]; t_pr = t_pr_l[s]
        P.dma("sp", xs[:], xin[r0:r0 + 128, :], reads=[t_xin], writes=[t_xs])
        P.dma("sp", pas[:], ma[r0:r0 + 128, :], reads=[t_ma], writes=[t_pa])
        P.dma("sp", pbs[:], mb[r0:r0 + 128, :], reads=[t_mb], writes=[t_pb])
        P.op("dve", lambda e: e.tensor_tensor(out=pas[:], in0=pas[:], in1=pbs[:], op=ALU.add), reads=[t_pb, t_pa], writes=[t_pa])
        P.op("dve", lambda e: e.scalar_tensor_tensor(out=xs[:], in0=xs[:], scalar=ALPHA, in1=pas[:], op0=ALU.mult, op1=ALU.add),
             reads=[t_xs, t_pa], writes=[t_xs])
        layer_norm_tile(P, xs[:], x1s[s][:], gb[:, 0, :], gb[:, 1, :], lns, t_xs, t_x1[s], t_lns, t_const, gb_eng="dve")
        P.dma("sp", x1d[r0:r0 + 128, :], x1s[s][:], reads=[t_x1[s]], writes=[t_x1d])
        P.op("act", lambda e: e.copy(out=x1b[s][:], in_=x1s[s][:]), reads=[t_x1[s]], writes=[t_x1b[s]])
        for h in range(2):
            b = cm[0] % 4; cm[0] += 1
            for j in range(4):
                k = h * 4 + j
                P.op("pe", lambda e: e.transpose(out=pm[b][:, j * 128:(j + 1) * 128], in_=x1s[s][:, k * 128:(k + 1) * 128], identity=ident),
                     reads=[t_x1[s], t_const], writes=[t_pm[b]])
            P.op("act", lambda e: e.copy(out=xTf[:, h * 4:(h + 1) * 4, :], in_=pm[b][:].rearrange("p (j t) -> p j t", j=4)),
                 reads=[t_pm[b]], writes=[t_xTf])
        b = cm[0] % 4; cm[0] += 1
        for k in range(8):
            P.op("pe", lambda e: e.matmul(pm[b][:, 0:32], lhsT=xTf[:, k, :], rhs=rwt[:, k, :], start=(k == 0), stop=(k == 7)),
                 reads=[t_xTf, t_const], writes=[t_pm[b]])
        P.op("dve", lambda e: e.tensor_tensor(out=R["lg"][:], in0=pm[b][:, 0:32], in1=rbt[:], op=ALU.add), reads=[t_pm[b], t_const], writes=[tR])
        P.op("dve", lambda e: e.max(out=R["t8"][:], in_=R["lg"][:]), reads=[tR], writes=[tR])
        P.op("dve", lambda e: e.tensor_scalar(out=R["nm"][:], in0=R["t8"][:, 0:1], scalar1=-1.0, scalar2=None, op0=ALU.mult), reads=[tR], writes=[tR])
        P.op("act", lambda e: e.activation(out=R["ex"][:], in_=R["lg"][:], func=AF.Exp, bias=R["nm"][:, 0:1], scale=1.0), reads=[tR], writes=[tR])
        P.op("dve", lambda e: e.tensor_scalar(out=R["mk"][:], in0=R["lg"][:], scalar1=R["t8"][:, 3:4], scalar2=None, op0=ALU.is_ge), reads=[tR], writes=[tR])
        P.op("dve", lambda e: e.tensor_tensor(out=R["ex"][:], in0=R["ex"][:], in1=R["mk"][:], op=ALU.mult), reads=[tR], writes=[tR])
        P.op("dve", lambda e: e.reduce_sum(out=R["sm"][:], in_=R["ex"][:], axis=mybir.AxisListType.X), reads=[tR], writes=[tR])
        P.op("dve", lambda e: e.reciprocal(out=R["sm"][:], in_=R["sm"][:]), reads=[tR], writes=[tR])
        P.op("dve", lambda e: e.tensor_scalar(out=Gall[:, it, :], in0=R["ex"][:], scalar1=R["sm"][:, 0:1], scalar2=None, op0=ALU.mult),
             reads=[tR], writes=[t_G[it]])
        P.op("pe", lambda e: e.matmul(pr[:, 0:32], lhsT=Ltri, rhs=R["mk"][:], start=True, stop=True), reads=[tR, t_const], writes=[t_pr])
        P.op("pe", lambda e: e.matmul(pr[:, 32:64], lhsT=ones, rhs=R["mk"][:], start=True, stop=True), reads=[tR, t_const], writes=[t_pr])
        P.op("dve", lambda e: e.tensor_tensor(out=R["pos"][:], in0=pr[:, 0:32], in1=base[:], op=ALU.add), reads=[t_pr, t_base], writes=[tR])
        P.op("dve", lambda e: e.tensor_tensor(out=base[:], in0=pr[:, 32:64], in1=base[:], op=ALU.add), reads=[t_pr, t_base, tR], writes=[t_base])
        P.op("dve", lambda e: e.tensor_scalar(out=R["v1"][:], in0=R["pos"][:], scalar1=float(C), scalar2=None, op0=ALU.is_lt), reads=[tR], writes=[tR])
        P.op("dve", lambda e: e.tensor_tensor(out=R["v1"][:], in0=R["v1"][:], in1=R["mk"][:], op=ALU.mult), reads=[tR], writes=[tR])
        P.op("dve", lambda e: e.tensor_tensor(out=R["pos"][:], in0=R["pos"][:], in1=ebase1, op=ALU.add), reads=[tR, t_const], writes=[tR])
        P.op("dve", lambda e: e.tensor_tensor(out=R["smat"][:], in0=R["pos"][:], in1=R["v1"][:], op=ALU.mult), reads=[tR], writes=[tR])
        P.op("dve", lambda e: e.tensor_scalar(out=R["smat"][:], in0=R["smat"][:], scalar1=-1.0, scalar2=None, op0=ALU.add), reads=[tR], writes=[tR])
        P.op("dve", lambda e: e.max(out=R["s8"][:], in_=R["smat"][:]), reads=[tR], writes=[tR])
        for k in range(4):
            P.op("dve", lambda e: e.scalar_tensor_tensor(out=R["junk"][:], in0=R["smat"][:], scalar=R["s8"][:, k:k + 1], in1=Gall[:, it, :],
                                                         op0=ALU.is_equal, op1=ALU.mult, accum_out=Gk[:, it, k:k + 1]),
                 reads=[tR, t_G[it]], writes=[tR, t_sel[it]])
        P.op("dve", lambda e: e.tensor_scalar(out=R["neg"][:], in0=R["s8"][:, 0:4], scalar1=0.0, scalar2=None, op0=ALU.is_lt), reads=[tR], writes=[tR])
        P.op("dve", lambda e: e.scalar_tensor_tensor(out=R["sf"][:], in0=R["neg"][:], scalar=ptrash, in1=R["s8"][:, 0:4], op0=ALU.mult, op1=ALU.add),
             reads=[tR, t_const], writes=[tR])
        P.op("dve", lambda e: e.tensor_copy(out=Sidx[:, it, :], in_=R["sf"][:]), reads=[tR], writes=[t_sel[it]])
        for k in range(4):
            _idma(P, xbkt[:, :], x1b[s][:], out_off=bass.IndirectOffsetOnAxis(Sidx[:, it, k:k + 1], 0),
                  reads=[t_x1b[s], t_sel[it]], writes=[t_xbkt])
    P.pop()

    if stop_phase <= 1:
        return
    P.push()
    wgt = [P.sb([128, 8, 2048], BF16, "wgu%d" % i) for i in range(2)]; t_wg = [P.tok("wg") for i in range(2)]
    wdt = [P.sb([128, 8, D_MODEL], BF16, "wd%d" % i) for i in range(2)]; t_wd = [P.tok("wd") for i in range(2)]
    XT = P.sb([128, 8, C], BF16, "XT"); t_XT = P.tok("XT")
    actT = P.sb([128, 8, C], BF16, "actT"); t_actT = [P.tok("actT") for i in range(2)]
    xrow = [P.sb([128, D_MODEL], BF16, "xrow%d" % i) for i in range(2)]; t_xrow = [P.tok("xrow") for i in range(2)]
    yrow = [P.sb([128, D_MODEL], F32, "yrow%d" % i) for i in range(2)]; t_yrow = [P.tok("yrow") for i in range(2)]
    identb = P.sb([128, 128], BF16, "identb")
    P.op("act", lambda e: e.copy(out=identb[:], in_=ident), reads=[t_const], writes=[t_const])
    NG = 2
    glt = [P.sb([128, CB], F32, "gl%d" % i) for i in range(NG)]; t_gl = [P.tok("gl") for i in range(NG)]
    sgt = [P.sb([128, CB], F32, "sg%d" % i) for i in range(NG)]; t_sg = [P.tok("sg") for i in range(NG)]
    lit = [P.sb([128, CB], F32, "li%d" % i) for i in range(NG)]; t_li = [P.tok("li") for i in range(NG)]
    pg = [P.ps([128, 512], F32, "pg%d" % i) for i in range(3)]; t_pg = [P.tok("pg") for i in range(3)]
    pdn = [P.ps([128, 512], F32, "pd%d" % i) for i in range(3)]; t_pd = [P.tok("pd") for i in range(3)]
    ptb = [P.ps([128, 1024], BF16, "ptb%d" % i) for i in range(2)]; t_ptb = [P.tok("ptb") for i in range(2)]

    def load_weights(e, slot):
        src = wgu[e].rearrange("(k p) n -> p k n", p=128)
        for k in range(8):
            P.dma("pool", wgt[slot][:, k, :], src[:, k, :], writes=[t_wg[slot]])
        src = wd[e].rearrange("(k p) n -> p k n", p=128)
        for k in range(0, 8, 2):
            P.dma("pool", wdt[slot][:, k:k + 2, :], src[:, k:k + 2, :], writes=[t_wd[slot]])

    cg = [0]; cd = [0]; cgl = [0]; ct = [0]; cy = [0]
    load_weights(0, 0)
    for ex in range(N_EXPERTS):
        slot = ex % 2
        if ex + 1 < N_EXPERTS:
            load_weights(ex + 1, (ex + 1) % 2)
        W = wgt[slot]; WD = wdt[slot]
        for rt in range(NRT):
            s = ct[0] % 2; ct[0] += 1
            rr = ex * C + rt * 128
            P.dma("sp", xrow[s][:], xbkt[rr:rr + 128, :], reads=[t_xbkt], writes=[t_xrow[s]])
            for k in range(8):
                P.op("pe", lambda e: e.transpose(out=ptb[s][:, k * 128:(k + 1) * 128], in_=xrow[s][:, k * 128:(k + 1) * 128], identity=identb[:]),
                     reads=[t_xrow[s], t_const], writes=[t_ptb[s]])
            P.op("act", lambda e: e.copy(out=XT[:, :, rt * 128:(rt + 1) * 128], in_=ptb[s][:, :].rearrange("p (k t) -> p k t", k=8)),
                 reads=[t_ptb[s]], writes=[t_XT])
        for cb in range(2):
            csl = slice(cb * CB, (cb + 1) * CB)
            for j in range(8):
                gi = cgl[0] % NG; cgl[0] += 1
                b = cg[0] % 3; cg[0] += 1
                for k in range(8):
                    P.op("pe", lambda e: e.matmul(pg[b][:, 0:CB], lhsT=W[:, k, j * 128:(j + 1) * 128], rhs=XT[:, k, csl], start=(k == 0), stop=(k == 7)),
                         reads=[t_wg[slot], t_XT], writes=[t_pg[b]])
                P.op("dve", lambda e: e.tensor_scalar(out=glt[gi][:], in0=pg[b][:, 0:CB], scalar1=bgt[:, ex * 16 + j:ex * 16 + j + 1],
                                                      scalar2=SWIGLU_LIMIT, op0=ALU.add, op1=ALU.min), reads=[t_pg[b], t_const], writes=[t_gl[gi]])
                P.op("act", lambda e: e.activation(out=sgt[gi][:], in_=glt[gi][:], func=AF.Sigmoid, scale=SWIGLU_ALPHA), reads=[t_gl[gi]], writes=[t_sg[gi]])
                P.op("pool", lambda e: e.tensor_tensor(out=sgt[gi][:], in0=sgt[gi][:], in1=glt[gi][:], op=ALU.mult), reads=[t_gl[gi], t_sg[gi]], writes=[t_sg[gi]])
                b = cg[0] % 3; cg[0] += 1
                for k in range(8):
                    P.op("pe", lambda e: e.matmul(pg[b][:, 0:CB], lhsT=W[:, k, 1024 + j * 128:1024 + (j + 1) * 128], rhs=XT[:, k, csl], start=(k == 0), stop=(k == 7)),
                         reads=[t_wg[slot], t_XT], writes=[t_pg[b]])
                P.op("dve", lambda e: e.tensor_scalar(out=lit[gi][:], in0=pg[b][:, 0:CB], scalar1=bgt[:, ex * 16 + 8 + j:ex * 16 + 9 + j],
                                                      scalar2=SWIGLU_LIMIT + 1.0, op0=ALU.add, op1=ALU.min), reads=[t_pg[b], t_const], writes=[t_li[gi]])
                P.op("dve", lambda e: e.scalar_tensor_tensor(out=actT[:, j, csl], in0=lit[gi][:], scalar=1.0 - SWIGLU_LIMIT, in1=sgt[gi][:],
                                                             op0=ALU.max, op1=ALU.mult), reads=[t_li[gi], t_sg[gi]], writes=[t_actT[cb]])
        for rt in range(NRT):
            s = cy[0] % 2; cy[0] += 1
            for hf in range(2):
                b = cd[0] % 3; cd[0] += 1
                for k in range(8):
                    P.op("pe", lambda e: e.matmul(pdn[b][:, :], lhsT=actT[:, k, rt * 128:(rt + 1) * 128], rhs=WD[:, k, hf * 512:(hf + 1) * 512],
                                                  start=(k == 0), stop=(k == 7)), reads=[t_actT[0], t_actT[1], t_wd[slot]], writes=[t_pd[b]])
                P.op("act", lambda e: e.copy(out=yrow[s][:, hf * 512:(hf + 1) * 512], in_=pdn[b][:, :]), reads=[t_pd[b]], writes=[t_yrow[s]])
            rr = ex * C + rt * 128
            P.dma("sp", ybuf[rr:rr + 128, :], yrow[s][:], reads=[t_yrow[s]], writes=[t_ybuf])
    P.pop()

    if stop_phase <= 2:
        return
    P.push()
    x1s = [P.sb([128, D_MODEL], F32, "x1c%d" % i) for i in range(2)]; t_x1 = [P.tok("x1c") for i in range(2)]
    accs = [P.sb([128, D_MODEL], F32, "acc%d" % i) for i in range(2)]; t_acc = [P.tok("acc") for i in range(2)]
    yg_l = [[P.sb([128, D_MODEL], F32, "yg%d%d" % (j, i)) for i in range(4)] for j in range(2)]
    t_yg_l = [[P.tok("yg") for i in range(4)] for j in range(2)]
    outs = [P.sb([128, D_MODEL], F32, "outs%d" % i) for i in range(2)]; t_outs = [P.tok("outs") for i in range(2)]
    GT_l = [P.sb([32, 128], F32, "GT%d" % i) for i in range(2)]; t_GT_l = [P.tok("GT") for i in range(2)]
    lns_l = [dict(st=P.sb([128, 12], F32), mv=P.sb([128, 2], F32), rstd=P.sb([128, 1], F32), eps=eps_t) for i in range(2)]
    t_lns_l = [P.tok("lns") for i in range(2)]
    pm = [P.ps([128, 512], F32, "pm%d" % i) for i in range(4)]; t_pm = [P.tok("pm") for i in range(4)]
    pt_l = [P.ps([128, 512], F32, "pt%d" % i) for i in range(2)]; t_pt_l = [P.tok("pt") for i in range(2)]
    cm = [0]
    for it in range(NT):
        s = it % 2
        r0 = it * 128
        yg = yg_l[s]; t_yg = t_yg_l[s]; GT = GT_l[s]; t_GT = t_GT_l[s]; lns = lns_l[s]; t_lns = t_lns_l[s]; pt = pt_l[s]; t_pt = t_pt_l[s]
        P.dma("sp", x1s[s][:], x1d[r0:r0 + 128, :], reads=[t_x1d], writes=[t_x1[s]])
        for k in range(4):
            _idma(P, yg[k][:], ybuf[:, :], in_off=bass.IndirectOffsetOnAxis(Sidx[:, it, k:k + 1], 0), reads=[t_ybuf, t_sel[it]], writes=[t_yg[k]])
        P.op("pe", lambda e: e.transpose(out=pt[0:32, 0:128], in_=Gall[:, it, :], identity=ident), reads=[t_G[it], t_const], writes=[t_pt])
        P.op("act", lambda e: e.copy(out=GT[:], in_=pt[0:32, 0:128]), reads=[t_pt], writes=[t_GT])
        for hf in range(2):
            b = cm[0] % 4; cm[0] += 1
            P.op("pe", lambda e: e.matmul(pm[b][:, :], lhsT=GT[:], rhs=bdt[:, hf * 512:(hf + 1) * 512], start=True, stop=True),
                 reads=[t_GT, t_const], writes=[t_pm[b]])
            P.op("dve", lambda e: e.scalar_tensor_tensor(out=accs[s][:, hf * 512:(hf + 1) * 512], in0=x1s[s][:, hf * 512:(hf + 1) * 512],
                                                         scalar=ALPHA, in1=pm[b][:, :], op0=ALU.mult, op1=ALU.add),
                 reads=[t_x1[s], t_pm[b]], writes=[t_acc[s]])
        for k in range(4):
            P.op("dve", lambda e: e.scalar_tensor_tensor(out=accs[s][:], in0=yg[k][:], scalar=Gk[:, it, k:k + 1], in1=accs[s][:],
                                                         op0=ALU.mult, op1=ALU.add), reads=[t_yg[k], t_sel[it], t_acc[s]], writes=[t_acc[s]])
        layer_norm_tile(P, accs[s][:], outs[s][:], gb[:, 2, :], gb[:, 3, :], lns, t_acc[s], t_outs[s], t_lns, t_const, gb_eng="dve")
        P.dma("sp", yout[r0:r0 + 128, :], outs[s][:], reads=[t_outs[s]], writes=[t_yout])
    P.pop()


def moe_sparse_consts():
    f = np.float32
    c = np.zeros((128, 417), f)
    c[:, 0:128] = np.eye(128)
    tp = np.arange(128)[:, None]; tt = np.arange(128)[None, :]
    c[:, 128:256] = (tp < tt).astype(f)
    c[:, 256:384] = 1.0
    c[:, 384:416] = (np.arange(N_EXPERTS) * MOE_CAP + 1).astype(f)[None, :]
    c[:, 416] = MOE_NR + 1 + np.arange(128)
    return c
```

```python
import math
from contextlib import ExitStack

import numpy as np
import concourse.bass as bass
import concourse.mybir as mybir
from concourse.bass_utils import run_bass_kernel_spmd

F32 = mybir.dt.float32
BF16 = mybir.dt.bfloat16
AF = mybir.ActivationFunctionType
ALU = mybir.AluOpType

D_MODEL = 1024
BATCH = 4
SEQ = 4096
DEPTH = 4
ALPHA = (2 * DEPTH) ** 0.25
LN_EPS = 1e-5
N_EXPERTS = 32
SWIGLU_LIMIT = 7.0
SWIGLU_ALPHA = 1.702


class Tok:
    __slots__ = ("name", "writer", "readers", "dsem", "dcount", "disjoint")

    def __init__(self, name):
        self.name = name
        self.writer = None
        self.readers = []
        self.dsem = None
        self.dcount = 0
        self.disjoint = False


class Prog:
    def __init__(self):
        self.nc = bass.Bass("TRN2", target_bir_lowering=False)
        self.es = ExitStack()
        nc = self.nc
        self.eng = {"pe": nc.tensor, "dve": nc.vector, "act": nc.scalar, "pool": nc.gpsimd, "sp": nc.sync}
        self.sems = {}
        self.cnt = {}
        self.known = {k: {} for k in self.eng}
        for k in ("pe", "dve", "act", "pool"):
            self.sems[k] = self.es.enter_context(nc.semaphore("s_" + k))
            self.cnt[k] = 0
        self.nsem = 0
        self.ntens = 0
        self.scopes = []
        self.sem_pool = []
        self.dtot = {}
        self.live_toks = []

    def sb(self, shape, dtype=F32, name=None):
        self.ntens += 1
        t = self._es().enter_context(self.nc.sbuf_tensor("%s_%d" % (name or "sb", self.ntens), list(shape), dtype))
        return t

    def ps(self, shape, dtype=F32, name=None):
        self.ntens += 1
        t = self._es().enter_context(self.nc.psum_tensor("%s_%d" % (name or "ps", self.ntens), list(shape), dtype))
        return t

    def _es(self):
        return self.scopes[-1][0] if self.scopes else self.es

    def tok(self, name="t"):
        t = Tok(name)
        if self.scopes:
            self.scopes[-1][1].append(t)
        return t

    def _dsem(self, tok):
        if tok.dsem is None:
            if self.sem_pool:
                key, cnt = self.sem_pool.pop()
                tok.dcount = cnt
            else:
                self.nsem += 1
                key = "d%d" % self.nsem
                self.sems[key] = self.es.enter_context(self.nc.semaphore(key))
                self.dtot[key] = 0
            tok.dsem = key
        return tok.dsem

    def push(self):
        self.scopes.append((ExitStack(), []))

    def barrier(self):
        targets = {k: self.cnt[k] for k in ("pe", "dve", "act", "pool") if self.cnt[k] > 0}
        for k, v in self.dtot.items():
            if v > 0:
                targets[k] = v
        for e in self.eng:
            kn = self.known[e]
            for k, v in targets.items():
                if kn.get(k, 0) >= v:
                    continue
                self.eng[e].wait_ge(self.sems[k], v)
                kn[k] = v

    def pop(self):
        self.barrier()
        es, toks = self.scopes.pop()
        for t in toks:
            if t.dsem is not None:
                self.sem_pool.append((t.dsem, self.dtot[t.dsem]))
                t.dsem = None
        es.close()

    def _deps(self, e, reads, writes):
        deps = {}

        def add(d):
            if d is None:
                return
            k, v = d
            if deps.get(k, 0) < v:
                deps[k] = v

        for r in reads:
            add(r.writer)
        for w in writes:
            if not w.disjoint:
                add(w.writer)
            for rd in w.readers:
                add(rd)
        kn = self.known[e]
        for k, v in deps.items():
            if e == "pe" and k == "pe":
                continue
            if kn.get(k, 0) >= v:
                continue
            self.eng[e].wait_ge(self.sems[k], v)
            kn[k] = v

    def op(self, e, fn, reads=(), writes=()):
        self._deps(e, reads, writes)
        ins = fn(self.eng[e])
        ins.then_inc(self.sems[e], 1)
        self.cnt[e] += 1
        me = (e, self.cnt[e])
        for r in reads:
            r.readers.append(me)
        for w in writes:
            w.writer = me
            w.readers = []
        return ins

    def dma(self, q, out, in_, reads=(), writes=(), **kw):
        self._deps(q, reads, writes)
        owner = writes[0] if writes else reads[0]
        key = self._dsem(owner)
        ins = self.eng[q].dma_start(out=out, in_=in_, **kw)
        ins.then_inc(self.sems[key], 16)
        owner.dcount += 16
        self.dtot[key] = owner.dcount
        me = (key, owner.dcount)
        for r in reads:
            r.readers.append(me)
        for w in writes:
            w.writer = me
            w.readers = []
        return ins

    def wait_all(self, q, toks):
        deps = {}
        for t in toks:
            for d in [t.writer] + list(t.readers):
                if d is not None and deps.get(d[0], 0) < d[1]:
                    deps[d[0]] = d[1]
        for k, v in deps.items():
            self.eng[q].wait_ge(self.sems[k], v)

    def close(self):
        self.es.close()


def run_interleaved(gens):
    gens = list(gens)
    while gens:
        for g in list(gens):
            try:
                next(g)
            except StopIteration:
                gens.remove(g)


def layer_norm_tile(P, src, dst, g_bc, b_bc, scr, T_src, T_dst, T_scr, tgb, gb_eng="pool"):
    st, mv, rstd = scr["st"], scr["mv"], scr["rstd"]
    P.op("dve", lambda e: e.bn_stats(out=st[:, 0:6], in_=src[:, 0:512]), reads=[T_src], writes=[T_scr])
    P.op("dve", lambda e: e.bn_stats(out=st[:, 6:12], in_=src[:, 512:1024]), reads=[T_src, T_scr], writes=[T_scr])
    P.op("dve", lambda e: e.bn_aggr(out=mv[:, 0:2], in_=st[:, 0:12]), reads=[T_scr], writes=[T_scr])
    P.op("act", lambda e: e.activation(out=rstd[:, 0:1], in_=mv[:, 1:2], func=AF.Sqrt, bias=scr["eps"][:, 0:1], scale=1.0),
         reads=[T_scr, tgb], writes=[T_scr])
    P.op("dve", lambda e: e.reciprocal(out=rstd[:, 0:1], in_=rstd[:, 0:1]), reads=[T_scr], writes=[T_scr])
    P.op("dve", lambda e: e.tensor_scalar(out=dst, in0=src, scalar1=mv[:, 0:1], scalar2=rstd[:, 0:1],
                                          op0=ALU.subtract, op1=ALU.mult), reads=[T_src, T_scr], writes=[T_dst])
    P.op(gb_eng, lambda e: e.tensor_tensor(out=dst, in0=dst, in1=g_bc, op=ALU.mult), reads=[T_dst, tgb], writes=[T_dst])
    P.op(gb_eng, lambda e: e.tensor_tensor(out=dst, in0=dst, in1=b_bc, op=ALU.add), reads=[T_dst, tgb], writes=[T_dst])


def build_moe(n_pass, n_exp=N_EXPERTS, two_mix=True):
    P = Prog()
    nc = P.nc
    io = moe_io(nc, "", n_pass * 1024, n_exp)
    for k in ("t_xin", "t_ma", "t_mb", "t_yout"):
        io[k] = P.tok(k)
    emit_moe(P, n_pass, io, n_exp)
    P.wait_all("sp", [io["t_yout"]])
    P.close()
    return nc


def moe_io(nc, pfx, NTOK, n_exp=N_EXPERTS, xin=None, ma=None, mb=None, yout=None, sparse=False):
    io = {}
    io["xin"] = xin if xin is not None else nc.dram_tensor(pfx + "xin", [NTOK, D_MODEL], F32, kind="ExternalInput").ap()
    io["ma"] = ma if ma is not None else nc.dram_tensor(pfx + "ma", [NTOK, D_MODEL], F32, kind="ExternalInput").ap()
    io["mb"] = mb if mb is not None else nc.dram_tensor(pfx + "mb", [NTOK, D_MODEL], F32, kind="ExternalInput").ap()
    io["lnp"] = nc.dram_tensor(pfx + "lnp", [4, D_MODEL], F32, kind="ExternalInput").ap()
    io["rw"] = nc.dram_tensor(pfx + "rw", [D_MODEL, N_EXPERTS], F32, kind="ExternalInput").ap()
    io["rb"] = nc.dram_tensor(pfx + "rb", [1, N_EXPERTS], F32, kind="ExternalInput").ap()
    io["wgu"] = nc.dram_tensor(pfx + "wgu", [n_exp, D_MODEL, 2048], F32, kind="ExternalInput").ap()
    io["bgu"] = nc.dram_tensor(pfx + "bgu", [128, n_exp * 16], F32, kind="ExternalInput").ap()
    io["wd"] = nc.dram_tensor(pfx + "wd", [n_exp, D_MODEL, D_MODEL], F32, kind="ExternalInput").ap()
    io["bd"] = nc.dram_tensor(pfx + "bd", [N_EXPERTS, D_MODEL], F32, kind="ExternalInput").ap()
    io["ident"] = nc.dram_tensor(pfx + "ident", [128, 417 if sparse else 128], F32, kind="ExternalInput").ap()
    io["yout"] = yout if yout is not None else nc.dram_tensor(pfx + "yout", [NTOK, D_MODEL], F32, kind="ExternalOutput").ap()
    return io


def emit_moe(P, n_pass, io, n_exp=N_EXPERTS, two_mix=True):
    TG = 1024
    NT = TG // 128
    nc = P.nc
    xin, ma, mb, lnp, rw, rb, wgu, bgu, wd, bd, ident_in, yout = (io[k] for k in
        ("xin", "ma", "mb", "lnp", "rw", "rb", "wgu", "bgu", "wd", "bd", "ident", "yout"))
    t_xin, t_ma, t_mb, t_yout = io["t_xin"], io["t_ma"], io["t_mb"], io["t_yout"]

    ident = P.sb([128, 128], F32, "ident"); t_const = P.tok("const")
    gb = P.sb([128, 4, D_MODEL], F32, "gb")
    rwt = P.sb([128, 8, N_EXPERTS], F32, "rwt")
    rbt = P.sb([128, N_EXPERTS], F32, "rbt")
    bgt = P.sb([128, n_exp * 16], F32, "bgt")
    bdt = P.sb([N_EXPERTS, D_MODEL], F32, "bdt")
    eps_t = P.sb([128, 1], F32, "eps")
    P.dma("sp", ident[:], ident_in[:, :], writes=[t_const])
    for i in range(4):
        P.dma("sp", gb[:, i, :], lnp[i:i + 1, :].partition_broadcast(128), writes=[t_const])
    P.dma("sp", rwt[:], rw.rearrange("(k p) n -> p k n", p=128), writes=[t_const])
    P.dma("sp", rbt[:], rb[0:1, :].partition_broadcast(128), writes=[t_const])
    P.dma("sp", bgt[:], bgu[:, :], writes=[t_const])
    P.dma("sp", bdt[:], bd[:, :], writes=[t_const])
    P.op("dve", lambda e: e.memset(eps_t[:], LN_EPS), writes=[t_const])
    bgv = bgt[:].rearrange("p (e c) -> p e c", c=16)
    P.op("dve", lambda e: e.tensor_scalar(out=bgv[:, :, 8:16], in0=bgv[:, :, 8:16], scalar1=1.0, scalar2=None, op0=ALU.add),
         reads=[t_const], writes=[t_const])

    acc = P.sb([128, NT, D_MODEL], F32, "acc");  t_acc = [P.tok("acc%d" % i) for i in range(NT)]
    x1T = P.sb([128, 8, TG], BF16, "x1T");       t_x1T = [P.tok("x1T%d" % i) for i in range(NT)]
    Gall = P.sb([128, NT, N_EXPERTS], F32, "G"); t_G = [P.tok("G%d" % i) for i in range(NT)]
    actT = P.sb([128, 8, 512], BF16, "actT");    t_actT = P.tok("actT")
    wgt = [P.sb([128, 8, 2048], BF16, "wgu%d" % i) for i in range(2)]; t_wg = [P.tok("wg%d" % i) for i in range(2)]
    wdt = [P.sb([128, 8, D_MODEL], BF16, "wd%d" % i) for i in range(2)]; t_wd = [P.tok("wd%d" % i) for i in range(2)]
    NS = 1
    xs = [P.sb([128, D_MODEL], F32, "xs%d" % i) for i in range(NS)]; t_xs = [P.tok("xs%d" % i) for i in range(NS)]
    pas = [P.sb([128, D_MODEL], F32, "pa0")] * NS; t_pa = [P.tok("pa0")] * NS
    pbs = [P.sb([128, D_MODEL], F32, "pb0")] * NS; t_pb = [P.tok("pb0")] * NS
    x1s = [P.sb([128, D_MODEL], F32, "x1s%d" % i) for i in range(NS)]; t_x1 = [P.tok("x1s%d" % i) for i in range(NS)]
    xTf = [P.sb([128, 8, 128], F32, "xTf0")] * NS; t_xTf = [P.tok("xTf0")] * NS
    lns = [dict(st=P.sb([128, 12], F32), mv=P.sb([128, 2], F32), rstd=P.sb([128, 1], F32), eps=eps_t) for i in range(NS)]
    t_lns = [P.tok("lns%d" % i) for i in range(NS)]
    rt = [dict(lg=P.sb([128, 32], F32), t8=P.sb([128, 8], F32), nm=P.sb([128, 1], F32), ex=P.sb([128, 32], F32),
               mk=P.sb([128, 32], F32), sm=P.sb([128, 1], F32), GT=P.sb([32, 128], F32)) for i in range(NS)]
    t_rt = [P.tok("rt%d" % i) for i in range(NS)]
    NG = 2
    glt = [P.sb([128, 512], F32, "gl%d" % i) for i in range(NG)]; t_gl = [P.tok("gl%d" % i) for i in range(NG)]
    sgt = [P.sb([128, 512], F32, "sg%d" % i) for i in range(NG)]; t_sg = [P.tok("sg%d" % i) for i in range(NG)]
    lit = [P.sb([128, 512], F32, "li0")] * NG; t_li = [P.tok("li0")] * NG
    pg = [P.ps([128, 512], F32, "pg%d" % i) for i in range(3)]; t_pg = [P.tok("pg%d" % i) for i in range(3)]
    pdn = [P.ps([128, 512], F32, "pd%d" % i) for i in range(3)]; t_pd = [P.tok("pd%d" % i) for i in range(3)]
    pm = [P.ps([128, 512], F32, "pm%d" % i) for i in range(2)]; t_pm = [P.tok("pm%d" % i) for i in range(2)]

    wload_i = [0]

    def load_weights(e, slot):
        src = wgu[e].rearrange("(k p) n -> p k n", p=128)
        for k in range(8):
            P.dma("pool", wgt[slot][:, k, :], src[:, k, :], writes=[t_wg[slot]])
        src = wd[e].rearrange("(k p) n -> p k n", p=128)
        for k in range(0, 8, 2):
            P.dma("pool", wdt[slot][:, k:k + 2, :], src[:, k:k + 2, :], writes=[t_wd[slot]])

    cg = [0]; cd = [0]; cm = [0]; cgl = [0]

    for ps_i in range(n_pass):
        tok0 = ps_i * TG
        load_weights(0, 0)
        for it in range(NT):
            s = it % NS
            r0 = tok0 + it * 128
            P.dma("sp", xs[s][:], xin[r0:r0 + 128, :], reads=[t_xin], writes=[t_xs[s]])
            P.dma("sp", pas[s][:], ma[r0:r0 + 128, :], reads=[t_ma], writes=[t_pa[s]])
            if two_mix:
                P.dma("sp", pbs[s][:], mb[r0:r0 + 128, :], reads=[t_mb], writes=[t_pb[s]])
                P.op("pool", lambda e: e.tensor_tensor(out=pas[s][:], in0=pas[s][:], in1=pbs[s][:], op=ALU.add),
                     reads=[t_pb[s], t_pa[s]], writes=[t_pa[s]])
            P.op("dve", lambda e: e.scalar_tensor_tensor(out=xs[s][:], in0=xs[s][:], scalar=ALPHA, in1=pas[s][:],
                                                         op0=ALU.mult, op1=ALU.add),
                 reads=[t_xs[s], t_pa[s]], writes=[t_xs[s]])
            layer_norm_tile(P, xs[s][:], x1s[s][:], gb[:, 0, :], gb[:, 1, :], lns[s], t_xs[s], t_x1[s], t_lns[s], t_const)
            for h in range(2):
                b = cm[0] % 2; cm[0] += 1
                for j in range(4):
                    k = h * 4 + j
                    P.op("pe", lambda e: e.transpose(out=pm[b][:, j * 128:(j + 1) * 128], in_=x1s[s][:, k * 128:(k + 1) * 128],
                                                     identity=ident[:]),
                         reads=[t_x1[s], t_const], writes=[t_pm[b]])
                P.op("act", lambda e: e.copy(out=xTf[s][:, h * 4:(h + 1) * 4, :],
                                             in_=pm[b][:].rearrange("p (j t) -> p j t", j=4)),
                     reads=[t_pm[b]], writes=[t_xTf[s]])
            P.op("pool", lambda e: e.tensor_copy(out=x1T[:, :, it * 128:(it + 1) * 128], in_=xTf[s][:]),
                 reads=[t_xTf[s]], writes=[t_x1T[it]])
            b = cm[0] % 2; cm[0] += 1
            for k in range(8):
                P.op("pe", lambda e: e.matmul(pm[b][:, 0:32], lhsT=xTf[s][:, k, :], rhs=rwt[:, k, :], start=(k == 0), stop=(k == 7)),
                     reads=[t_xTf[s], t_const], writes=[t_pm[b]])
            R = rt[s]; tR = t_rt[s]
            P.op("dve", lambda e: e.tensor_tensor(out=R["lg"][:], in0=pm[b][:, 0:32], in1=rbt[:], op=ALU.add),
                 reads=[t_pm[b], t_const], writes=[tR])
            P.op("dve", lambda e: e.max(out=R["t8"][:], in_=R["lg"][:]), reads=[tR], writes=[tR])
            P.op("dve", lambda e: e.tensor_scalar(out=R["nm"][:], in0=R["t8"][:, 0:1], scalar1=-1.0, scalar2=None, op0=ALU.mult),
                 reads=[tR], writes=[tR])
            P.op("act", lambda e: e.activation(out=R["ex"][:], in_=R["lg"][:], func=AF.Exp, bias=R["nm"][:, 0:1], scale=1.0),
                 reads=[tR], writes=[tR])
            P.op("dve", lambda e: e.tensor_scalar(out=R["mk"][:], in0=R["lg"][:], scalar1=R["t8"][:, 3:4], scalar2=None, op0=ALU.is_ge),
                 reads=[tR], writes=[tR])
            P.op("dve", lambda e: e.tensor_tensor(out=R["ex"][:], in0=R["ex"][:], in1=R["mk"][:], op=ALU.mult),
                 reads=[tR], writes=[tR])
            P.op("dve", lambda e: e.reduce_sum(out=R["sm"][:], in_=R["ex"][:], axis=mybir.AxisListType.X),
                 reads=[tR], writes=[tR])
            P.op("dve", lambda e: e.reciprocal(out=R["sm"][:], in_=R["sm"][:]), reads=[tR], writes=[tR])
            P.op("dve", lambda e: e.tensor_scalar(out=Gall[:, it, :], in0=R["ex"][:], scalar1=R["sm"][:, 0:1], scalar2=None, op0=ALU.mult),
                 reads=[tR], writes=[t_G[it]])
            b = cm[0] % 2; cm[0] += 1
            P.op("pe", lambda e: e.transpose(out=pm[b][0:32, 0:128], in_=Gall[:, it, :], identity=ident[:]),
                 reads=[t_G[it], t_const], writes=[t_pm[b]])
            P.op("act", lambda e: e.copy(out=R["GT"][:], in_=pm[b][0:32, 0:128]), reads=[t_pm[b]], writes=[tR])
            for hf in range(2):
                b = cm[0] % 2; cm[0] += 1
                P.op("pe", lambda e: e.matmul(pm[b][:, :], lhsT=R["GT"][:], rhs=bdt[:, hf * 512:(hf + 1) * 512], start=True, stop=True),
                     reads=[tR, t_const], writes=[t_pm[b]])
                P.op("dve", lambda e: e.scalar_tensor_tensor(out=acc[:, it, hf * 512:(hf + 1) * 512], in0=x1s[s][:, hf * 512:(hf + 1) * 512],
                                                             scalar=ALPHA, in1=pm[b][:, :], op0=ALU.mult, op1=ALU.add),
                     reads=[t_x1[s], t_pm[b]], writes=[t_acc[it]])

        for ex in range(n_exp):
            slot = ex % 2
            if ex + 1 < n_exp:
                load_weights(ex + 1, (ex + 1) % 2)
            W = wgt[slot]; WD = wdt[slot]
            for tb in range(TG // 512):
                tsl = slice(tb * 512, (tb + 1) * 512)
                tiles = [t_x1T[tb * 4 + i] for i in range(4)]
                for j in range(8):
                    gi = cgl[0] % NG; cgl[0] += 1
                    b = cg[0] % 3; cg[0] += 1
                    for k in range(8):
                        P.op("pe", lambda e: e.matmul(pg[b][:, :], lhsT=W[:, k, j * 128:(j + 1) * 128], rhs=x1T[:, k, tsl],
                                                      start=(k == 0), stop=(k == 7)),
                             reads=[t_wg[slot]] + tiles, writes=[t_pg[b]])
                    P.op("dve", lambda e: e.tensor_scalar(out=glt[gi][:], in0=pg[b][:, :], scalar1=bgt[:, ex * 16 + j:ex * 16 + j + 1],
                                                          scalar2=SWIGLU_LIMIT, op0=ALU.add, op1=ALU.min),
                         reads=[t_pg[b], t_const], writes=[t_gl[gi]])
                    P.op("act", lambda e: e.activation(out=sgt[gi][:], in_=glt[gi][:], func=AF.Sigmoid, scale=SWIGLU_ALPHA),
                         reads=[t_gl[gi]], writes=[t_sg[gi]])
                    P.op("pool", lambda e: e.tensor_tensor(out=sgt[gi][:], in0=sgt[gi][:], in1=glt[gi][:], op=ALU.mult),
                         reads=[t_gl[gi], t_sg[gi]], writes=[t_sg[gi]])
                    b = cg[0] % 3; cg[0] += 1
                    for k in range(8):
                        P.op("pe", lambda e: e.matmul(pg[b][:, :], lhsT=W[:, k, 1024 + j * 128:1024 + (j + 1) * 128], rhs=x1T[:, k, tsl],
                                                      start=(k == 0), stop=(k == 7)),
                             reads=[t_wg[slot]] + tiles, writes=[t_pg[b]])
                    P.op("dve", lambda e: e.tensor_scalar(out=lit[gi][:], in0=pg[b][:, :], scalar1=bgt[:, ex * 16 + 8 + j:ex * 16 + 9 + j],
                                                          scalar2=SWIGLU_LIMIT + 1.0, op0=ALU.add, op1=ALU.min),
                         reads=[t_pg[b], t_const], writes=[t_li[gi]])
                    P.op("dve", lambda e: e.scalar_tensor_tensor(out=actT[:, j, :], in0=lit[gi][:], scalar=1.0 - SWIGLU_LIMIT, in1=sgt[gi][:],
                                                                 op0=ALU.max, op1=ALU.mult),
                         reads=[t_li[gi], t_sg[gi]], writes=[t_actT])
                for tt in range(4):
                    it = tb * 4 + tt
                    for hf in range(2):
                        b = cd[0] % 3; cd[0] += 1
                        for k in range(8):
                            P.op("pe", lambda e: e.matmul(pdn[b][:, :], lhsT=actT[:, k, tt * 128:(tt + 1) * 128],
                                                          rhs=WD[:, k, hf * 512:(hf + 1) * 512], start=(k == 0), stop=(k == 7)),
                                 reads=[t_actT, t_wd[slot]], writes=[t_pd[b]])
                        P.op("dve", lambda e: e.scalar_tensor_tensor(out=acc[:, it, hf * 512:(hf + 1) * 512], in0=pdn[b][:, :],
                                                                     scalar=Gall[:, it, ex:ex + 1], in1=acc[:, it, hf * 512:(hf + 1) * 512],
                                                                     op0=ALU.mult, op1=ALU.add),
                             reads=[t_pd[b], t_G[it], t_acc[it]], writes=[t_acc[it]])

        for it in range(NT):
            s = it % NS
            r0 = tok0 + it * 128
            layer_norm_tile(P, acc[:, it, :], x1s[s][:], gb[:, 2, :], gb[:, 3, :], lns[s], t_acc[it], t_x1[s], t_lns[s], t_const)
            P.dma("sp", yout[r0:r0 + 128, :], x1s[s][:], reads=[t_x1[s]], writes=[t_yout])


def make_xT(P, x_ap, r0, ntile, xs, t_xs, xT, t_xT, pm, t_pm, ident, t_const, cm, t_xd=None):
    for tt in range(ntile):
        s = tt % len(xs)
        P.dma("sp", xs[s][:], x_ap[r0 + tt * 128:r0 + (tt + 1) * 128, :], reads=([t_xd] if t_xd is not None else []), writes=[t_xs[s]])
        for h in range(2):
            b = cm[0] % len(pm); cm[0] += 1
            for j in range(4):
                k = h * 4 + j
                P.op("pe", lambda e: e.transpose(out=pm[b][:, j * 128:(j + 1) * 128], in_=xs[s][:, k * 128:(k + 1) * 128],
                                                 identity=ident[:]),
                     reads=[t_xs[s], t_const], writes=[t_pm[b]])
            P.op("act", lambda e: e.copy(out=xT[:, h * 4:(h + 1) * 4, tt * 128:(tt + 1) * 128],
                                         in_=pm[b][:].rearrange("p (j t) -> p j t", j=4)),
                 reads=[t_pm[b]], writes=[t_xT])


def build_even(T, stop=99):
    P = Prog()
    nc = P.nc
    io = even_io(nc, "", T)
    io["t_xd"] = P.tok("xd"); io["t_outd"] = P.tok("outd")
    emit_even(P, T, io, stop)
    P.wait_all("sp", [io["t_outd"]])
    P.close()
    return nc


def even_io(nc, pfx, T, x=None, out=None):
    io = {}
    io["x"] = x if x is not None else nc.dram_tensor(pfx + "x", [T, D_MODEL], F32, kind="ExternalInput").ap()
    io["wA"] = nc.dram_tensor(pfx + "wA", [D_MODEL, 1284], F32, kind="ExternalInput").ap()
    io["wo"] = nc.dram_tensor(pfx + "wo", [512, D_MODEL], F32, kind="ExternalInput").ap()
    io["pw"] = nc.dram_tensor(pfx + "pw", [2, 128, 128], F32, kind="ExternalInput").ap()
    io["pc"] = nc.dram_tensor(pfx + "pc", [128, 30], F32, kind="ExternalInput").ap()
    io["coef0"] = nc.dram_tensor(pfx + "coef0", [128, 2 * 4 * 16], F32, kind="ExternalInput").ap()
    io["gbias"] = nc.dram_tensor(pfx + "gbias", [2, 2], F32, kind="ExternalInput").ap()
    io["mln"] = nc.dram_tensor(pfx + "mln", [1, 256], F32, kind="ExternalInput").ap()
    io["cst"] = nc.dram_tensor(pfx + "cst", [128, 128 + 128], F32, kind="ExternalInput").ap()
    io["cst2"] = nc.dram_tensor(pfx + "cst2", [2, 776], F32, kind="ExternalInput").ap()
    io["out"] = out if out is not None else nc.dram_tensor(pfx + "out", [T, D_MODEL], F32, kind="ExternalOutput").ap()
    return io


def emit_even(P, T, io, stop=99):
    TB = 512
    NB = T // TB
    NCH = TB // 64
    nc = P.nc
    x, wA, wo, pw, pc, coef0, gbias, mln, cst, cst2, out = (io[k] for k in
        ("x", "wA", "wo", "pw", "pc", "coef0", "gbias", "mln", "cst", "cst2", "out"))
    t_xd = io["t_xd"]; t_outd = io["t_outd"]

    t_const = P.tok("const")
    cs = P.sb([128, 256], F32, "cst"); ident = cs[:, 0:128]; maskT = cs[:, 128:256]
    cs2 = P.sb([2, 776], F32, "cst2"); rmask = cs2[:, 0:512]; id2 = cs2[:, 768:776]
    pct = P.sb([128, 30], F32, "pc"); c0t = P.sb([128, 128], F32, "coef0")
    gbt = P.sb([2, 2], F32, "gb"); mlt = P.sb([128, 256], F32, "mln"); eps_t = P.sb([128, 1], F32, "eps")
    wAt = P.sb([128, 8, 1284], BF16, "wA"); wot = P.sb([128, 4, D_MODEL], BF16, "wo"); pwt = P.sb([128, 2, 128], BF16, "pw")
    P.dma("sp", cs[:], cst[:, :], writes=[t_const])
    P.dma("sp", cs2[:], cst2[:, :], writes=[t_const])
    P.dma("sp", pct[:], pc[:, :], writes=[t_const])
    P.dma("sp", c0t[:], coef0[:, :], writes=[t_const])
    P.dma("sp", gbt[:], gbias[:, :], writes=[t_const])
    P.dma("sp", mlt[:], mln[0:1, :].partition_broadcast(128), writes=[t_const])
    t_w = P.tok("w")
    wv = wA.rearrange("(k p) n -> p k n", p=128)
    for k in range(8):
        P.dma("pool", wAt[:, k, :], wv[:, k, :], writes=[t_w])
    P.dma("pool", wot[:], wo.rearrange("(k p) n -> p k n", p=128), writes=[t_w])
    P.dma("pool", pwt[:], pw.rearrange("g c d -> c g d"), writes=[t_w])
    P.op("dve", lambda e: e.memset(eps_t[:], LN_EPS), writes=[t_const])
    nfb = P.sb([2, 1], F32, "nfb")
    identb = P.sb([128, 128], BF16, "identb")
    P.op("act", lambda e: e.copy(out=identb[:], in_=ident), reads=[t_const], writes=[t_const])
    P.op("dve", lambda e: e.tensor_scalar(out=nfb[:], in0=gbt[:, 1:2], scalar1=-1.0, scalar2=None, op0=ALU.mult),
         reads=[t_const], writes=[t_const])

    xs = [P.sb([128, D_MODEL], F32, "xs%d" % i) for i in range(2)]; t_xs = [P.tok("xs") for i in range(2)]
    xT = P.sb([128, 8, TB], BF16, "xT"); t_xT = P.tok("xT")
    ub = [P.sb([128, 16 + TB], F32, "ub%d" % g) for g in range(2)]; t_ub = [P.tok("ub") for g in range(2)]
    s2 = P.sb([128, 16 + TB], F32, "s2"); s4 = P.sb([128, 16 + TB], F32, "s4"); s8 = P.sb([128, 16 + TB], F32, "s8")
    s16 = P.sb([128, 16 + TB], F32, "s16"); t_s = P.tok("s")
    dacc = P.sb([128, TB], F32, "dacc"); dbf = P.sb([128, TB], BF16, "dbf"); t_d = P.tok("d")
    ycT = P.sb([128, 4, TB], BF16, "ycT"); t_yp = P.tok("yp"); t_ym = P.tok("ym")
    qkb = [P.sb([128, 3 + TB], F32, "qkb%d" % c) for c in range(4)]; t_qkb = [P.tok("qkb") for c in range(4)]
    cacc_l = [P.sb([128, TB], F32, "cacc%d" % i) for i in range(2)]; t_cacc_l = [P.tok("cacc") for i in range(2)]
    qTe = [P.sb([128, TB], BF16, "qTe%d" % h) for h in range(2)]; qTo = [P.sb([128, TB], BF16, "qTo%d" % h) for h in range(2)]
    t_q = [P.tok("q") for h in range(2)]
    ksil = P.sb([128, TB], F32, "ksil"); t_ksil = P.tok("ksil")
    kT = [P.sb([128, TB], BF16, "kT%d" % h) for h in range(2)]; t_kT = [P.tok("kT") for h in range(2)]
    ktok = [P.sb([128, 4, 128], BF16, "ktok%d" % h) for h in range(2)]; t_ktok = [P.tok("ktok") for h in range(2)]
    vaug = P.sb([128, 4, 2, 130], BF16, "vaug"); t_v = P.tok("v")
    ogs = P.sb([128, 4, 256], F32, "ogs"); t_og = P.tok("og")
    C32 = [P.sb([128, 129], F32, "C32_%d" % h) for h in range(2)]; t_C = [P.tok("C") for h in range(2)]
    Csb = [[P.sb([128, 130], BF16, "Csb%d%d" % (h, i)) for i in range(2)] for h in range(2)]
    t_Cs = [[P.tok("Cs") for i in range(2)] for h in range(2)]
    PTm = [P.sb([128, 128], BF16, "PTm%d" % h) for h in range(2)]; t_PTm = [P.tok("PTm") for h in range(2)]
    gig = P.sb([2, TB], F32, "gig"); gsp = P.sb([2, TB], F32, "gsp"); gB = P.sb([2, TB], F32, "gB"); gu = P.sb([2, TB], F32, "gu")
    gev = P.sb([2, TB], F32, "gev"); gfl = P.sb([2, TB], F32, "gfl"); gtmp = P.sb([2, TB], F32, "gtmp")
    gmu = P.sb([2, NCH], F32, "gmu"); gg = P.sb([2, NCH], F32, "gg"); gms = P.sb([2, NCH], F32, "gms"); gMc = P.sb([2, NCH], F32, "gMc")
    gmp = P.sb([2, NCH], F32, "gmp"); gsig = P.sb([2, NCH], F32, "gsig"); mcar = P.sb([2, 1], F32, "mcar")
    t_g = P.tok("gates")
    sigb = P.sb([128, 2, NCH], F32, "sigb"); t_sigb = P.tok("sigb")
    flo = P.sb([128, 4, 2], F32, "flo"); t_flo = P.tok("flo")
    hsc = [dict(h=P.sb([128, 128], F32), dn=P.sb([128, 1], F32), st=P.sb([128, 6], F32), mv=P.sb([128, 2], F32),
                rstd=P.sb([128, 1], F32), sg=P.sb([128, 128], F32)) for i in range(2)]
    t_hsc = [P.tok("hsc") for i in range(2)]; t_hsg = [P.tok("hsg") for i in range(2)]
    yml = P.sb([128, 256], F32, "yml"); t_yml = P.tok("yml")
    osb = [P.sb([128, D_MODEL], F32, "osb%d" % i) for i in range(2)]; t_osb = [P.tok("osb") for i in range(2)]
    t_osbh = [[P.tok("osbh") for j in range(2)] for i in range(2)]
    pp = [P.ps([128, 512], F32, "pp%d" % i) for i in range(2)]; t_pp = [P.tok("pp") for i in range(2)]
    pe_ = P.ps([128, 512], F32, "pe"); t_pe = P.tok("pe")
    pv = pe_; t_pv = t_pe
    pmisc = P.ps([128, 512], F32, "pmisc")
    pmb = P.ps([128, 1024], BF16, "pmb")
    t_pmisc = P.tok("pmisc"); t_psg = t_pmisc; t_pfl = t_pmisc; t_pkt = P.tok("pkt")
    pnum = [P.ps([128, 512], F32, "pnum%d" % h) for h in range(2)]; t_pnum = [P.tok("pnum") for h in range(2)]
    po = P.ps([128, 512], F32, "po"); t_po = P.tok("po")
    pm = [pe_, po]; t_pm = [t_pe, t_po]

    for h in range(2):
        P.op("dve", lambda e: e.memset(C32[h][:], 0.0), writes=[t_C[h]])
        P.op("pool", lambda e: e.memset(qTe[h][:], 0.0), writes=[t_q[h]])
        P.op("pool", lambda e: e.memset(qTo[h][:], 0.0), writes=[t_q[h]])
    P.op("dve", lambda e: e.memset(mcar[:], 0.0), writes=[t_g])
    P.op("pool", lambda e: e.memset(vaug[:], 1.0), writes=[t_v])
    for g in range(2):
        P.op("pool", lambda e: e.memset(ub[g][:, 0:16], 0.0), writes=[t_ub[g]])
    for c in range(4):
        P.op("pool", lambda e: e.memset(qkb[c][:, 0:3], 0.0), writes=[t_qkb[c]])

    cm = [0]; cpp = [0]; cos = [0]
    for blk in range(NB):
        r0 = blk * TB
        make_xT(P, x, r0, 4, xs, t_xs, xT, t_xT, pm, t_pm, ident, t_const, cm, t_xd)

        def proj_fm(c0, ncol):
            b = cpp[0] % 2; cpp[0] += 1
            for k in range(8):
                P.op("pe", lambda e: e.matmul(pp[b][0:ncol, :], lhsT=wAt[:, k, c0:c0 + ncol], rhs=xT[:, k, :], start=(k == 0), stop=(k == 7)),
                     reads=[t_w, t_xT], writes=[t_pp[b]])
            return b

        if stop <= 1:
            continue
        for g in range(2):
            b = proj_fm(g * 128, 128)
            P.op("act", lambda e: e.copy(out=ub[g][:, 16:16 + TB], in_=pp[b][:, :]), reads=[t_pp[b]], writes=[t_ub[g]])
            U = ub[g]; W = 16 + TB
            P.op("dve", lambda e: e.tensor_tensor(out=s2[:, 2:W], in0=U[:, 2:W], in1=U[:, 1:W - 1], op=ALU.add), reads=[t_ub[g]], writes=[t_s])
            P.op("dve", lambda e: e.tensor_tensor(out=s4[:, 4:W], in0=s2[:, 4:W], in1=s2[:, 2:W - 2], op=ALU.add), reads=[t_s], writes=[t_s])
            P.op("dve", lambda e: e.tensor_tensor(out=s8[:, 8:W], in0=s4[:, 8:W], in1=s4[:, 4:W - 4], op=ALU.add), reads=[t_s], writes=[t_s])
            P.op("dve", lambda e: e.tensor_tensor(out=s16[:, 16:W], in0=s8[:, 16:W], in1=s8[:, 8:W - 8], op=ALU.add), reads=[t_s], writes=[t_s])
            cf = pct[:, 22 + g * 4:26 + g * 4]
            lo = 16 if blk == 0 else 0
            srcs = [s2, s4, s8, s16]
            P.op("dve", lambda e: e.scalar_tensor_tensor(out=dacc[:, lo:TB], in0=s2[:, 16 + lo:W], scalar=cf[:, 0:1], in1=U[:, 16 + lo:W],
                                                         op0=ALU.mult, op1=ALU.subtract), reads=[t_s, t_ub[g], t_const], writes=[t_d])
            for wi in range(1, 4):
                P.op("dve", lambda e: e.scalar_tensor_tensor(out=dacc[:, lo:TB], in0=srcs[wi][:, 16 + lo:W], scalar=cf[:, wi:wi + 1],
                                                             in1=dacc[:, lo:TB], op0=ALU.mult, op1=ALU.add),
                     reads=[t_s, t_const, t_d], writes=[t_d])
            if blk == 0:
                c0v = c0t[:].rearrange("p (g w t) -> p g w t", g=2, w=4)
                P.op("dve", lambda e: e.tensor_tensor(out=dacc[:, 0:16], in0=s2[:, 16:32], in1=c0v[:, g, 0, :], op=ALU.mult),
                     reads=[t_s, t_const], writes=[t_d])
                P.op("dve", lambda e: e.tensor_tensor(out=dacc[:, 0:16], in0=dacc[:, 0:16], in1=U[:, 16:32], op=ALU.subtract),
                     reads=[t_d, t_ub[g]], writes=[t_d])
                for wi in range(1, 4):
                    P.op("dve", lambda e: e.tensor_tensor(out=s2[:, 0:16], in0=srcs[wi][:, 16:32], in1=c0v[:, g, wi, :], op=ALU.mult),
                         reads=[t_s, t_const], writes=[t_s])
                    P.op("dve", lambda e: e.tensor_tensor(out=dacc[:, 0:16], in0=dacc[:, 0:16], in1=s2[:, 0:16], op=ALU.add),
                         reads=[t_d, t_s], writes=[t_d])
            P.op("act", lambda e: e.copy(out=dbf[:], in_=dacc[:]), reads=[t_d], writes=[t_d])
            P.op("pool", lambda e: e.tensor_copy(out=U[:, 0:16], in_=U[:, TB:TB + 16]), reads=[t_ub[g]], writes=[t_ub[g]])
            P.op("pe", lambda e: e.matmul(po[:, :], lhsT=pwt[:, g, :], rhs=dbf[:], start=True, stop=True),
                 reads=[t_w, t_d], writes=[t_po])
            P.op("act", lambda e: e.activation(out=ycT[:, g, :], in_=po[:, :], func=AF.Identity, scale=pct[:, g:g + 1]),
                 reads=[t_po, t_const], writes=[t_yp])

        if stop <= 2:
            continue
        b = proj_fm(1280, 2)
        P.op("act", lambda e: e.activation(out=gig[:], in_=pp[b][0:2, :], func=AF.Identity, bias=gbt[:, 0:1], scale=1.0),
             reads=[t_pp[b], t_const], writes=[t_g])
        if stop <= 2.1:
            continue
        b = proj_fm(1282, 2)
        P.op("act", lambda e: e.activation(out=gsp[:], in_=pp[b][0:2, :], func=AF.Exp, bias=nfb[:, 0:1], scale=-1.0),
             reads=[t_pp[b], t_const], writes=[t_g])
        P.op("act", lambda e: e.activation(out=gsp[:], in_=gsp[:], func=AF.Ln, bias=1.0, scale=1.0), reads=[t_g], writes=[t_g])
        if stop <= 2.2:
            continue
        P.op("dve", lambda e: e.tensor_tensor_scan(out=gB[:], data0=rmask, data1=gsp[:], initial=0.0, op0=ALU.mult, op1=ALU.add),
             reads=[t_g, t_const], writes=[t_g])
        P.op("dve", lambda e: e.tensor_tensor(out=gu[:], in0=gig[:], in1=gB[:], op=ALU.add), reads=[t_g], writes=[t_g])
        if stop <= 2.3:
            continue
        gu3 = gu[:].rearrange("p (c s) -> p c s", s=64); gB3 = gB[:].rearrange("p (c s) -> p c s", s=64)
        P.op("dve", lambda e: e.tensor_reduce(out=gmu[:], in_=gu3, axis=mybir.AxisListType.X, op=ALU.max), reads=[t_g], writes=[t_g])
        P.op("dve", lambda e: e.tensor_scalar(out=gg[:], in0=gB3[:, :, 63], scalar1=-1.0, scalar2=None, op0=ALU.mult), reads=[t_g], writes=[t_g])
        if stop <= 2.4:
            continue
        P.op("dve", lambda e: e.tensor_tensor_scan(out=gms[:], data0=gmu[:], data1=gg[:], initial=mcar[:, 0:1], op0=ALU.max, op1=ALU.add),
             reads=[t_g], writes=[t_g])
        P.op("dve", lambda e: e.tensor_tensor(out=gMc[:], in0=gms[:], in1=gg[:], op=ALU.subtract), reads=[t_g], writes=[t_g])
        P.op("dve", lambda e: e.tensor_copy(out=gmp[:, 0:1], in_=mcar[:, 0:1]), reads=[t_g], writes=[t_g])
        P.op("dve", lambda e: e.tensor_copy(out=gmp[:, 1:NCH], in_=gms[:, 0:NCH - 1]), reads=[t_g], writes=[t_g])
        P.op("dve", lambda e: e.tensor_copy(out=mcar[:, 0:1], in_=gms[:, NCH - 1:NCH]), reads=[t_g], writes=[t_g])
        P.op("dve", lambda e: e.tensor_tensor(out=gsig[:], in0=gmp[:], in1=gMc[:], op=ALU.subtract), reads=[t_g], writes=[t_g])
        P.op("act", lambda e: e.activation(out=gsig[:], in_=gsig[:], func=AF.Exp), reads=[t_g], writes=[t_g])
        if stop <= 2.5:
            continue
        Mb = gMc[:].unsqueeze(2).to_broadcast([2, NCH, 64])
        P.op("dve", lambda e: e.tensor_tensor(out=gtmp[:].rearrange("p (c s) -> p c s", s=64), in0=gu3, in1=Mb, op=ALU.subtract),
             reads=[t_g], writes=[t_g])
        P.op("act", lambda e: e.activation(out=gev[:], in_=gtmp[:], func=AF.Exp), reads=[t_g], writes=[t_g])
        P.op("dve", lambda e: e.tensor_tensor(out=gtmp[:].rearrange("p (c s) -> p c s", s=64), in0=gB3, in1=Mb, op=ALU.subtract),
             reads=[t_g], writes=[t_g])
        P.op("act", lambda e: e.activation(out=gfl[:], in_=gtmp[:], func=AF.Exp), reads=[t_g], writes=[t_g])
        if stop <= 2.6:
            continue
        for h in range(2):
            P.op("pe", lambda e: e.matmul(pmisc[:, 300 + h * NCH:300 + (h + 1) * NCH], lhsT=cs2[:, 512 + h * 128:512 + (h + 1) * 128],
                                          rhs=gsig[:], start=True, stop=True), reads=[t_g, t_const], writes=[t_psg])
        P.op("act", lambda e: e.copy(out=sigb[:].rearrange("p h c -> p (h c)"), in_=pmisc[:, 300:300 + 2 * NCH]),
             reads=[t_psg], writes=[t_sigb])
        if stop <= 2.7:
            continue
        for tt in range(4):
            P.op("pe", lambda e: e.matmul(pmisc[:, 320 + 8 * tt:328 + 8 * tt], lhsT=gfl[:, tt * 128:(tt + 1) * 128], rhs=id2, start=True, stop=True),
                 reads=[t_g, t_const], writes=[t_pfl])
        if stop <= 2.8:
            continue
        P.op("act", lambda e: e.copy(out=flo[:], in_=pmisc[:, 320:352].rearrange("p (t j) -> p t j", j=8)[:, :, 0:2]), reads=[t_pfl], writes=[t_flo])

        if stop <= 3:
            continue
        for c in range(4):
            b = proj_fm(256 + c * 128, 128)
            Q = qkb[c]; cacc = cacc_l[c % 2]; t_cacc = t_cacc_l[c % 2]
            P.op("act", lambda e: e.copy(out=Q[:, 3:3 + TB], in_=pp[b][:, :]), reads=[t_pp[b]], writes=[t_qkb[c]])
            cw = pct[:, 2 + c * 4:6 + c * 4]
            P.op("dve", lambda e: e.tensor_scalar(out=cacc[:], in0=Q[:, 0:TB], scalar1=cw[:, 0:1], scalar2=pct[:, 18 + c:19 + c],
                                                  op0=ALU.mult, op1=ALU.add), reads=[t_qkb[c], t_const], writes=[t_cacc])
            for j in range(1, 4):
                P.op("dve", lambda e: e.scalar_tensor_tensor(out=cacc[:], in0=Q[:, j:j + TB], scalar=cw[:, j:j + 1], in1=cacc[:],
                                                             op0=ALU.mult, op1=ALU.add), reads=[t_qkb[c], t_const, t_cacc], writes=[t_cacc])
            P.op("pool", lambda e: e.tensor_copy(out=Q[:, 0:3], in_=Q[:, TB:TB + 3]), reads=[t_qkb[c]], writes=[t_qkb[c]])
            if c < 2:
                h = c
                ca3 = cacc[:].rearrange("p (c two s) -> p c two s", two=2, s=64)
                P.op("act", lambda e: e.activation(out=qTe[h][:].rearrange("p (c two s) -> p c two s", two=2, s=64)[:, :, 0, :],
                                                   in_=ca3[:, :, 0, :], func=AF.Silu), reads=[t_cacc], writes=[t_q[h]])
                P.op("act", lambda e: e.activation(out=qTo[h][:].rearrange("p (c two s) -> p c two s", two=2, s=64)[:, :, 1, :],
                                                   in_=ca3[:, :, 1, :], func=AF.Silu), reads=[t_cacc], writes=[t_q[h]])
            else:
                h = c - 2
                P.op("act", lambda e: e.activation(out=ksil[:], in_=cacc[:], func=AF.Silu), reads=[t_cacc], writes=[t_ksil])
                P.op("pe", lambda e: e.matmul(pe_[:, :], lhsT=cs2[:, 512 + h * 128:512 + (h + 1) * 128], rhs=gev[:], start=True, stop=True),
                     reads=[t_g, t_const], writes=[t_pe])
                P.op("dve", lambda e: e.scalar_tensor_tensor(out=kT[h][:], in0=ksil[:], scalar=128.0 ** -0.5, in1=pe_[:, :],
                                                             op0=ALU.mult, op1=ALU.mult), reads=[t_ksil, t_pe], writes=[t_kT[h]])
                for tt in range(4):
                    P.op("pe", lambda e: e.transpose(out=pmb[:, tt * 128:(tt + 1) * 128], in_=kT[h][:, tt * 128:(tt + 1) * 128],
                                                     identity=identb[:]), reads=[t_kT[h], t_const], writes=[t_pkt])
                P.op("act", lambda e: e.copy(out=ktok[h][:].rearrange("p t d -> p (t d)"), in_=pmb[:, 0:512]), reads=[t_pkt], writes=[t_ktok[h]])

        if stop <= 4:
            continue
        for tt in range(4):
            for k in range(8):
                P.op("pe", lambda e: e.matmul(pv[:, :], lhsT=xT[:, k, tt * 128:(tt + 1) * 128], rhs=wAt[:, k, 768:1280], start=(k == 0), stop=(k == 7)),
                     reads=[t_w, t_xT], writes=[t_pv])
            P.op("act", lambda e: e.copy(out=vaug[:, tt, :, 0:128], in_=pv[:, 0:256].rearrange("p (h d) -> p h d", h=2)),
                 reads=[t_pv], writes=[t_v])
            P.op("act", lambda e: e.activation(out=ogs[:, tt, :], in_=pv[:, 256:512], func=AF.Sigmoid), reads=[t_pv], writes=[t_og])

        if stop <= 5:
            continue
        pu = [pe_, po]; t_pu = [t_pe, t_po]
        for tt in range(4):
            tsl = slice(tt * 128, (tt + 1) * 128)
            H = range(2)
            for h in H:
                P.op("pe", lambda e: e.matmul(pp[h][:, 0:128], lhsT=kT[h][:, tsl], rhs=qTe[h][:, tsl], start=True, stop=False),
                     reads=[t_kT[h], t_q[h]], writes=[t_pp[h]])
                P.op("pe", lambda e: e.matmul(pp[h][:, 0:128], lhsT=kT[h][:, tsl], rhs=qTo[h][:, tsl], start=False, stop=True),
                     reads=[t_kT[h], t_q[h]], writes=[t_pp[h]])
            for h in H:
                P.op("dve", lambda e: e.tensor_tensor(out=PTm[h][:], in0=pp[h][:, 0:128], in1=maskT, op=ALU.mult),
                     reads=[t_pp[h], t_const], writes=[t_PTm[h]])
            for h in H:
                P.op("pe", lambda e: e.matmul(pnum[h][:, 0:129], lhsT=PTm[h][:], rhs=vaug[:, tt, h, 0:129], start=True, stop=False),
                     reads=[t_PTm[h], t_v], writes=[t_pnum[h]])
            for ci in range(2):
                c = tt * 2 + ci
                for h in H:
                    P.op("act", lambda e: e.activation(out=Csb[h][ci][:, 0:129], in_=C32[h][:], func=AF.Identity, scale=sigb[:, h, c:c + 1]),
                         reads=[t_C[h], t_sigb], writes=[t_Cs[h][ci]])
                for h in H:
                    qsrc = qTe[h] if ci == 0 else qTo[h]
                    P.op("pe", lambda e: e.matmul(pnum[h][:, 0:129], lhsT=qsrc[:, tsl], rhs=Csb[h][ci][:, 0:129], start=False, stop=(ci == 1)),
                         reads=[t_q[h], t_Cs[h][ci]], writes=[t_pnum[h]])
                    P.op("pe", lambda e: e.matmul(pu[h][:, 0:129], lhsT=ktok[h][ci * 64:(ci + 1) * 64, tt, :],
                                                  rhs=vaug[ci * 64:(ci + 1) * 64, tt, h, 0:129], start=True, stop=True),
                         reads=[t_ktok[h], t_v], writes=[t_pu[h]])
                for h in H:
                    P.op("dve", lambda e: e.scalar_tensor_tensor(out=C32[h][:], in0=C32[h][:], scalar=sigb[:, h, c:c + 1], in1=pu[h][:, 0:129],
                                                                 op0=ALU.mult, op1=ALU.add), reads=[t_C[h], t_sigb, t_pu[h]], writes=[t_C[h]])
            for h in H:
                S = hsc[h]; tS = t_hsc[h]
                P.op("act", lambda e: e.activation(out=S["dn"][:], in_=pnum[h][:, 128:129], func=AF.Abs), reads=[t_pnum[h]], writes=[tS])
            for h in H:
                S = hsc[h]; tS = t_hsc[h]
                P.op("dve", lambda e: e.tensor_scalar(out=S["dn"][:], in0=S["dn"][:], scalar1=flo[:, tt, h:h + 1], scalar2=None,
                                                      op0=ALU.max), reads=[tS, t_flo], writes=[tS])
            for h in H:
                S = hsc[h]; tS = t_hsc[h]
                P.op("dve", lambda e: e.reciprocal(out=S["dn"][:], in_=S["dn"][:]), reads=[tS], writes=[tS])
            for h in H:
                S = hsc[h]; tS = t_hsc[h]
                P.op("dve", lambda e: e.tensor_scalar(out=S["h"][:], in0=pnum[h][:, 0:128], scalar1=S["dn"][:, 0:1], scalar2=None, op0=ALU.mult),
                     reads=[t_pnum[h], tS], writes=[tS])
            for h in H:
                S = hsc[h]; tS = t_hsc[h]
                P.op("dve", lambda e: e.bn_stats(out=S["st"][:], in_=S["h"][:]), reads=[tS], writes=[tS])
            for h in H:
                S = hsc[h]; tS = t_hsc[h]
                P.op("dve", lambda e: e.bn_aggr(out=S["mv"][:], in_=S["st"][:]), reads=[tS], writes=[tS])
            for h in H:
                S = hsc[h]; tS = t_hsc[h]
                P.op("act", lambda e: e.activation(out=S["rstd"][:], in_=S["mv"][:, 1:2], func=AF.Sqrt, bias=eps_t[:, 0:1], scale=1.0),
                     reads=[tS, t_const], writes=[tS])
            for h in H:
                S = hsc[h]; tS = t_hsc[h]
                P.op("dve", lambda e: e.reciprocal(out=S["rstd"][:], in_=S["rstd"][:]), reads=[tS], writes=[tS])
            for h in H:
                S = hsc[h]; tS = t_hsc[h]
                P.op("dve", lambda e: e.tensor_scalar(out=S["h"][:], in0=S["h"][:], scalar1=S["mv"][:, 0:1], scalar2=S["rstd"][:, 0:1],
                                                      op0=ALU.subtract, op1=ALU.mult), reads=[tS], writes=[tS])
                P.op("pool", lambda e: e.tensor_tensor(out=S["sg"][:], in0=ogs[:, tt, h * 128:(h + 1) * 128], in1=mlt[:, h * 128:(h + 1) * 128],
                                                       op=ALU.mult), reads=[t_og, t_const], writes=[t_hsg[h]])
            for h in H:
                S = hsc[h]; tS = t_hsc[h]
                P.op("dve", lambda e: e.tensor_tensor(out=yml[:, h * 128:(h + 1) * 128], in0=S["h"][:], in1=S["sg"][:], op=ALU.mult),
                     reads=[tS, t_hsg[h]], writes=[t_yml])
            for h in range(2):
                P.op("pe", lambda e: e.transpose(out=pe_[:, 0:128], in_=yml[:, h * 128:(h + 1) * 128], identity=ident),
                     reads=[t_yml, t_const], writes=[t_pe])
                P.op("act", lambda e: e.copy(out=ycT[:, 2 + h, tsl], in_=pe_[:, 0:128]), reads=[t_pe], writes=[t_ym])

        if stop <= 6:
            continue
        for tt in range(4):
            s = cos[0] % 2; cos[0] += 1
            for hf in range(2):
                for kc in range(4):
                    P.op("pe", lambda e: e.matmul(pp[hf][:, :], lhsT=ycT[:, kc, tt * 128:(tt + 1) * 128], rhs=wot[:, kc, hf * 512:(hf + 1) * 512],
                                                  start=(kc == 0), stop=(kc == 3)), reads=[t_yp, t_ym, t_w], writes=[t_pp[hf]])
                P.op("act" if hf == 0 else "dve", lambda e: (e.copy if hf == 0 else e.tensor_copy)(out=osb[s][:, hf * 512:(hf + 1) * 512], in_=pp[hf][:, :]),
                     reads=[t_pp[hf]], writes=[t_osbh[s][hf]])
            P.dma("sp", out[r0 + tt * 128:r0 + (tt + 1) * 128, :], osb[s][:], reads=[t_osbh[s][0], t_osbh[s][1]], writes=[t_outd])


def even_inputs(x, w_in, pool_w, pool_scale, conv_w, conv_b, i_bias, f_bias, ml_norm, w_out, hp):
    f = np.float32
    h0 = 2 * hp
    u = w_in[:, 0:512][:, h0 * 128:(h0 + 2) * 128]
    q = w_in[:, 512:1024][:, h0 * 128:(h0 + 2) * 128]
    k = w_in[:, 1024:1536][:, h0 * 128:(h0 + 2) * 128]
    v = w_in[:, 1536:2048][:, h0 * 128:(h0 + 2) * 128]
    og = w_in[:, 2048:2560][:, h0 * 128:(h0 + 2) * 128]
    ig = w_in[:, 2560:2564][:, h0:h0 + 2]
    fg = w_in[:, 2564:2568][:, h0:h0 + 2]
    wA = np.ascontiguousarray(np.concatenate([u, q, k, v, og, ig, fg], axis=1), dtype=f)
    wo = np.ascontiguousarray(np.concatenate([w_out[h0 * 128:(h0 + 2) * 128], w_out[512 + h0 * 128:512 + (h0 + 2) * 128]], axis=0), dtype=f)
    pw = np.ascontiguousarray(pool_w[h0:h0 + 2], dtype=f)
    pc = np.zeros((128, 30), f)
    for g in range(2):
        pc[:, g] = pool_scale[(h0 + g) * 128:(h0 + g + 1) * 128]
    cw = np.concatenate([conv_w[:, 0:512][:, h0 * 128:(h0 + 2) * 128], conv_w[:, 512:1024][:, h0 * 128:(h0 + 2) * 128]], axis=1)
    cb = np.concatenate([conv_b[0:512][h0 * 128:(h0 + 2) * 128], conv_b[512:1024][h0 * 128:(h0 + 2) * 128]])
    for c in range(4):
        for j in range(4):
            pc[:, 2 + c * 4 + j] = cw[j, c * 128:(c + 1) * 128]
        pc[:, 18 + c] = cb[c * 128:(c + 1) * 128]
    coef0 = np.zeros((128, 2, 4, 16), f)
    for g in range(2):
        wi = h0 + g
        pc[:, 22 + g * 4 + wi] = 1.0 / (2 ** (wi + 1))
        coef0[:, g, wi, :] = 1.0 / np.minimum(np.arange(1, 17), 2 ** (wi + 1))
    gbias = np.stack([i_bias[h0:h0 + 2], f_bias[h0:h0 + 2]], axis=1).astype(f)
    mln = np.ascontiguousarray(ml_norm[h0 * 128:(h0 + 2) * 128].reshape(1, 256), dtype=f)
    cst = np.zeros((128, 256), f)
    cst[:, 0:128] = np.eye(128)
    s_i = np.arange(128)[:, None]; t_i = np.arange(128)[None, :]
    cst[:, 128:256] = ((s_i // 64 == t_i // 64) & (s_i <= t_i)).astype(f)
    cst2 = np.zeros((2, 776), f)
    cst2[:, 0:512] = (np.arange(512) % 64 != 0).astype(f)[None, :]
    cst2[0, 512:640] = 1.0
    cst2[1, 640:768] = 1.0
    cst2[:, 768:770] = np.eye(2)
    return dict(x=np.ascontiguousarray(x, dtype=f), wA=wA, wo=wo, pw=pw, pc=pc, coef0=coef0.reshape(128, 128), gbias=gbias, mln=mln,
                cst=cst, cst2=cst2)


def build_odd(T):
    P = Prog()
    nc = P.nc
    io = odd_io(nc, "", T)
    io["t_xd"] = P.tok("xd"); io["t_outd"] = P.tok("outd")
    emit_odd(P, T, io)
    P.wait_all("sp", [io["t_outd"]])
    P.close()
    return nc


def odd_io(nc, pfx, T, x=None, out=None):
    io = {}
    io["x"] = x if x is not None else nc.dram_tensor(pfx + "x", [T, D_MODEL], F32, kind="ExternalInput").ap()
    io["wA"] = nc.dram_tensor(pfx + "wA", [D_MODEL, 1552], F32, kind="ExternalInput").ap()
    io["wo"] = nc.dram_tensor(pfx + "wo", [512, D_MODEL], F32, kind="ExternalInput").ap()
    io["w2"] = nc.dram_tensor(pfx + "w2", [16, 256], F32, kind="ExternalInput").ap()
    io["pc"] = nc.dram_tensor(pfx + "pc", [128, 2], F32, kind="ExternalInput").ap()
    io["gln"] = nc.dram_tensor(pfx + "gln", [1, 512], F32, kind="ExternalInput").ap()
    io["cst"] = nc.dram_tensor(pfx + "cst", [128, 256 + 512], F32, kind="ExternalInput").ap()
    io["out"] = out if out is not None else nc.dram_tensor(pfx + "out", [T, D_MODEL], F32, kind="ExternalOutput").ap()
    return io


def emit_odd(P, T, io):
    TB = 512
    NB = T // TB
    NCH = TB // 64
    nc = P.nc
    x, wA, wo, w2, pc, gln, cst, out = (io[k] for k in ("x", "wA", "wo", "w2", "pc", "gln", "cst", "out"))
    t_xd = io["t_xd"]; t_outd = io["t_outd"]

    t_const = P.tok("const")
    cs = P.sb([128, 768], F32, "cst"); ident = cs[:, 0:128]; maskT = cs[:, 128:256]; rmask = cs[:, 256:768]
    pct = P.sb([128, 2], F32, "pc"); npct = P.sb([128, 2], F32, "npc")
    glt = P.sb([128, 512], F32, "gln"); eps_t = P.sb([128, 1], F32, "eps")
    wAt = P.sb([128, 8, 1552], BF16, "wA"); wot = P.sb([128, 4, D_MODEL], BF16, "wo"); w2t = P.sb([16, 256], BF16, "w2")
    identb = P.sb([128, 128], BF16, "identb")
    P.dma("sp", cs[:], cst[:, :], writes=[t_const])
    P.dma("sp", pct[:], pc[:, :], writes=[t_const])
    P.dma("sp", glt[:], gln[0:1, :].partition_broadcast(128), writes=[t_const])
    t_w = P.tok("w")
    wv = wA.rearrange("(k p) n -> p k n", p=128)
    for k in range(8):
        P.dma("pool", wAt[:, k, :], wv[:, k, :], writes=[t_w])
    P.dma("pool", wot[:], wo.rearrange("(k p) n -> p k n", p=128), writes=[t_w])
    P.dma("pool", w2t[:], w2[:, :], writes=[t_w])
    P.op("dve", lambda e: e.memset(eps_t[:], LN_EPS), writes=[t_const])
    P.op("dve", lambda e: e.tensor_scalar(out=npct[:], in0=pct[:], scalar1=-1.0, scalar2=None, op0=ALU.mult), reads=[t_const], writes=[t_const])
    P.op("act", lambda e: e.copy(out=identb[:], in_=ident), reads=[t_const], writes=[t_const])

    xs = [P.sb([128, D_MODEL], F32, "xs%d" % i) for i in range(2)]; t_xs = [P.tok("xs") for i in range(2)]
    xT = P.sb([128, 8, TB], BF16, "xT"); t_xT = P.tok("xT")
    glrT = P.sb([16, TB], BF16, "glrT"); t_glr = P.tok("glr")
    qf_l = [P.sb([128, TB], F32, "qf%d" % i) for i in range(2)]; kf_l = [P.sb([128, TB], F32, "kf%d" % i) for i in range(2)]
    t_qk_l = [P.tok("qkf") for i in range(2)]
    sp_l = [P.sb([128, TB], F32, "sp%d" % i) for i in range(2)]; Bp_l = [P.sb([128, TB], F32, "Bp%d" % i) for i in range(2)]
    ex_l = [P.sb([128, TB], F32, "ex%d" % i) for i in range(2)]; rc_l = [P.sb([128, TB], F32, "rc%d" % i) for i in range(2)]
    t_dec_l = [P.tok("dec") for i in range(2)]
    eBl = [P.sb([128, NCH], F32, "eBl%d" % h) for h in range(2)]; t_eBl = [P.tok("eBl") for h in range(2)]
    qTe = [P.sb([128, TB], BF16, "qTe%d" % h) for h in range(2)]; qTo = [P.sb([128, TB], BF16, "qTo%d" % h) for h in range(2)]
    t_q = [P.tok("q") for h in range(2)]
    kT = [P.sb([128, TB], BF16, "kT%d" % h) for h in range(2)]; t_kT = [P.tok("kT") for h in range(2)]
    khT_l = [P.sb([128, TB], BF16, "khT%d" % i) for i in range(2)]; t_khT_l = [P.tok("khT") for i in range(2)]
    ktok = [P.sb([128, 4, 128], BF16, "ktok%d" % h) for h in range(2)]; t_ktok = [P.tok("ktok") for h in range(2)]
    vbf = P.sb([128, 4, 512], BF16, "vbf"); t_v = P.tok("v")
    rs = P.sb([128, 4, 512], F32, "rs"); t_r = P.tok("r")
    S32 = [P.sb([128, 256], F32, "S32_%d" % h) for h in range(2)]; t_S = [P.tok("S") for h in range(2)]
    Sb = [[P.sb([128, 256], BF16, "Sb%d%d" % (h, i)) for i in range(2)] for h in range(2)]
    t_Sb = [[P.tok("Sb") for i in range(2)] for h in range(2)]
    ATm = [P.sb([128, 128], BF16, "ATm%d" % h) for h in range(2)]; t_AT = [P.tok("AT") for h in range(2)]
    hsc = [dict(h=P.sb([128, 256], F32), st=P.sb([128, 6], F32), mv=P.sb([128, 2], F32), rstd=P.sb([128, 1], F32),
                sg=P.sb([128, 256], F32)) for i in range(2)]
    t_hsc = [P.tok("hsc") for i in range(2)]; t_hsg = [P.tok("hsg") for i in range(2)]
    yml = P.sb([128, 512], F32, "yml"); t_yml = P.tok("yml")
    ycT = P.sb([128, 4, TB], BF16, "ycT"); t_ym = P.tok("ym")
    osb = [P.sb([128, D_MODEL], F32, "osb%d" % i) for i in range(2)]; t_osb = [P.tok("osb") for i in range(2)]
    t_osbh = [[P.tok("osbh") for j in range(2)] for i in range(2)]
    pp = [P.ps([128, 512], F32, "pp%d" % i) for i in range(2)]; t_pp = [P.tok("pp") for i in range(2)]
    pe_ = P.ps([128, 512], F32, "pe"); t_pe = P.tok("pe")
    pmb = P.ps([128, 1024], BF16, "pmb"); t_pkt = P.tok("pkt")
    pnum = [P.ps([128, 512], F32, "pnum%d" % h) for h in range(2)]; t_pnum = [P.tok("pnum") for h in range(2)]
    po = P.ps([128, 512], F32, "po"); t_po = P.tok("po")
    pm = [pe_, po]; t_pm = [t_pe, t_po]

    for h in range(2):
        P.op("dve", lambda e: e.memset(S32[h][:], 0.0), writes=[t_S[h]])
        P.op("pool", lambda e: e.memset(Sb[h][0][:], 0.0), writes=[t_Sb[h][0]])
        P.op("pool", lambda e: e.memset(qTe[h][:], 0.0), writes=[t_q[h]])
        P.op("pool", lambda e: e.memset(qTo[h][:], 0.0), writes=[t_q[h]])

    cm = [0]; cpp = [0]; cos = [0]
    for blk in range(NB):
        r0 = blk * TB
        make_xT(P, x, r0, 4, xs, t_xs, xT, t_xT, pm, t_pm, ident, t_const, cm, t_xd)

        def proj_fm(c0, ncol):
            b = cpp[0] % 2; cpp[0] += 1
            for k in range(8):
                P.op("pe", lambda e: e.matmul(pp[b][0:ncol, :], lhsT=wAt[:, k, c0:c0 + ncol], rhs=xT[:, k, :], start=(k == 0), stop=(k == 7)),
                     reads=[t_w, t_xT], writes=[t_pp[b]])
            return b

        b = proj_fm(1536, 16)
        P.op("act", lambda e: e.copy(out=glrT[:], in_=pp[b][0:16, :]), reads=[t_pp[b]], writes=[t_glr])
        def head_front(h):
            bq = proj_fm(h * 128, 128)
            P.op("act", lambda e: e.copy(out=qf_l[h][:], in_=pp[bq][:, :]), reads=[t_pp[bq]], writes=[t_qk_l[h]])
            yield
            bk = proj_fm(256 + h * 128, 128)
            P.op("act", lambda e: e.copy(out=kf_l[h][:], in_=pp[bk][:, :]), reads=[t_pp[bk]], writes=[t_qk_l[h]])
            yield
            b = cpp[0] % 2; cpp[0] += 1
            P.op("pe", lambda e: e.matmul(pp[b][:, :], lhsT=w2t[:, h * 128:(h + 1) * 128], rhs=glrT[:], start=True, stop=True),
                 reads=[t_w, t_glr], writes=[t_pp[b]])
            P.op("act", lambda e: e.activation(out=sp_l[h][:], in_=pp[b][:, :], func=AF.Exp, bias=npct[:, h:h + 1], scale=-1.0),
                 reads=[t_pp[b], t_const], writes=[t_dec_l[h]])
            yield
            P.op("act", lambda e: e.activation(out=sp_l[h][:], in_=sp_l[h][:], func=AF.Ln, bias=1.0, scale=1.0), reads=[t_dec_l[h]], writes=[t_dec_l[h]])
            yield
            P.op("dve", lambda e: e.tensor_tensor_scan(out=Bp_l[h][:], data0=rmask, data1=sp_l[h][:], initial=0.0, op0=ALU.mult, op1=ALU.add),
                 reads=[t_dec_l[h], t_const], writes=[t_dec_l[h]])
            yield
            Bp3 = Bp_l[h][:].rearrange("p (c s) -> p c s", s=64)
            P.op("act", lambda e: e.activation(out=ex_l[h][:], in_=Bp_l[h][:], func=AF.Exp, scale=-1.0 / 16.0), reads=[t_dec_l[h]], writes=[t_dec_l[h]])
            yield
            q3 = qf_l[h][:].rearrange("p (c two s) -> p c two s", two=2, s=64); e3 = ex_l[h][:].rearrange("p (c two s) -> p c two s", two=2, s=64)
            P.op("dve", lambda e: e.scalar_tensor_tensor(out=qTe[h][:].rearrange("p (c two s) -> p c two s", two=2, s=64)[:, :, 0, :],
                                                         in0=q3[:, :, 0, :], scalar=128.0 ** -0.5, in1=e3[:, :, 0, :], op0=ALU.mult, op1=ALU.mult),
                 reads=[t_qk_l[h], t_dec_l[h]], writes=[t_q[h]])
            yield
            P.op("dve", lambda e: e.scalar_tensor_tensor(out=qTo[h][:].rearrange("p (c two s) -> p c two s", two=2, s=64)[:, :, 1, :],
                                                         in0=q3[:, :, 1, :], scalar=128.0 ** -0.5, in1=e3[:, :, 1, :], op0=ALU.mult, op1=ALU.mult),
                 reads=[t_qk_l[h], t_dec_l[h]], writes=[t_q[h]])
            yield
            P.op("act", lambda e: e.activation(out=ex_l[h][:], in_=Bp_l[h][:], func=AF.Exp, scale=1.0 / 16.0), reads=[t_dec_l[h], t_q[h]], writes=[t_dec_l[h]])
            yield
            P.op("dve", lambda e: e.tensor_tensor(out=kT[h][:], in0=kf_l[h][:], in1=ex_l[h][:], op=ALU.mult), reads=[t_qk_l[h], t_dec_l[h]], writes=[t_kT[h]])
            yield
            P.op("dve", lambda e: e.tensor_tensor(out=rc_l[h][:].rearrange("p (c s) -> p c s", s=64), in0=Bp3,
                                                  in1=Bp3[:, :, 63:64].to_broadcast([128, NCH, 64]), op=ALU.subtract), reads=[t_dec_l[h]], writes=[t_dec_l[h]])
            yield
            P.op("act", lambda e: e.activation(out=ex_l[h][:], in_=rc_l[h][:], func=AF.Exp, scale=1.0 / 16.0), reads=[t_dec_l[h], t_kT[h]], writes=[t_dec_l[h]])
            yield
            P.op("dve", lambda e: e.tensor_tensor(out=khT_l[h][:], in0=kf_l[h][:], in1=ex_l[h][:], op=ALU.mult), reads=[t_qk_l[h], t_dec_l[h]], writes=[t_khT_l[h]])
            yield
            P.op("act", lambda e: e.activation(out=eBl[h][:], in_=Bp3[:, :, 63], func=AF.Exp, scale=-1.0 / 16.0), reads=[t_dec_l[h]], writes=[t_eBl[h]])
            yield
            for tt in range(4):
                P.op("pe", lambda e: e.transpose(out=pmb[:, tt * 128:(tt + 1) * 128], in_=khT_l[h][:, tt * 128:(tt + 1) * 128], identity=identb[:]),
                     reads=[t_khT_l[h], t_const], writes=[t_pkt])
            P.op("act", lambda e: e.copy(out=ktok[h][:].rearrange("p t d -> p (t d)"), in_=pmb[:, 0:512]), reads=[t_pkt], writes=[t_ktok[h]])
            yield


        run_interleaved([head_front(0), head_front(1)])
        for tt in range(4):
            for k in range(8):
                P.op("pe", lambda e: e.matmul(pe_[:, :], lhsT=xT[:, k, tt * 128:(tt + 1) * 128], rhs=wAt[:, k, 512:1024], start=(k == 0), stop=(k == 7)),
                     reads=[t_w, t_xT], writes=[t_pe])
            P.op("act", lambda e: e.copy(out=vbf[:, tt, :], in_=pe_[:, :]), reads=[t_pe], writes=[t_v])
            for k in range(8):
                P.op("pe", lambda e: e.matmul(po[:, :], lhsT=xT[:, k, tt * 128:(tt + 1) * 128], rhs=wAt[:, k, 1024:1536], start=(k == 0), stop=(k == 7)),
                     reads=[t_w, t_xT], writes=[t_po])
            P.op("act", lambda e: e.activation(out=rs[:, tt, :], in_=po[:, :], func=AF.Silu), reads=[t_po], writes=[t_r])

        pu = [pe_, po]; t_pu = [t_pe, t_po]
        for tt in range(4):
            tsl = slice(tt * 128, (tt + 1) * 128)
            H = range(2)
            for h in H:
                P.op("pe", lambda e: e.matmul(pp[h][:, 0:128], lhsT=kT[h][:, tsl], rhs=qTe[h][:, tsl], start=True, stop=False),
                     reads=[t_kT[h], t_q[h]], writes=[t_pp[h]])
                P.op("pe", lambda e: e.matmul(pp[h][:, 0:128], lhsT=kT[h][:, tsl], rhs=qTo[h][:, tsl], start=False, stop=True),
                     reads=[t_kT[h], t_q[h]], writes=[t_pp[h]])
            for h in H:
                P.op("dve", lambda e: e.tensor_tensor(out=ATm[h][:], in0=pp[h][:, 0:128], in1=maskT, op=ALU.mult),
                     reads=[t_pp[h], t_const], writes=[t_AT[h]])
            for h in H:
                P.op("pe", lambda e: e.matmul(pnum[h][:, 0:256], lhsT=ATm[h][:], rhs=vbf[:, tt, h * 256:(h + 1) * 256], start=True, stop=False),
                     reads=[t_AT[h], t_v], writes=[t_pnum[h]])
            for ci in range(2):
                c = tt * 2 + ci
                for h in H:
                    qsrc = qTe[h] if ci == 0 else qTo[h]
                    P.op("pe", lambda e: e.matmul(pnum[h][:, 0:256], lhsT=qsrc[:, tsl], rhs=Sb[h][ci][:], start=False, stop=(ci == 1)),
                         reads=[t_q[h], t_Sb[h][ci]], writes=[t_pnum[h]])
                    P.op("pe", lambda e: e.matmul(pu[h][:, 0:256], lhsT=ktok[h][ci * 64:(ci + 1) * 64, tt, :],
                                                  rhs=vbf[ci * 64:(ci + 1) * 64, tt, h * 256:(h + 1) * 256], start=True, stop=True),
                         reads=[t_ktok[h], t_v], writes=[t_pu[h]])
                for h in H:
                    P.op("dve", lambda e: e.scalar_tensor_tensor(out=S32[h][:], in0=S32[h][:], scalar=eBl[h][:, c:c + 1], in1=pu[h][:, 0:256],
                                                                 op0=ALU.mult, op1=ALU.add), reads=[t_S[h], t_eBl[h], t_pu[h]], writes=[t_S[h]])
                for h in H:
                    P.op("act", lambda e: e.copy(out=Sb[h][1 - ci][:], in_=S32[h][:]), reads=[t_S[h]], writes=[t_Sb[h][1 - ci]])
            for h in H:
                S = hsc[h]; tS = t_hsc[h]
                P.op("dve", lambda e: e.bn_stats(out=S["st"][:], in_=pnum[h][:, 0:256]), reads=[t_pnum[h]], writes=[tS])
            for h in H:
                S = hsc[h]; tS = t_hsc[h]
                P.op("dve", lambda e: e.bn_aggr(out=S["mv"][:], in_=S["st"][:]), reads=[tS], writes=[tS])
            for h in H:
                S = hsc[h]; tS = t_hsc[h]
                P.op("act", lambda e: e.activation(out=S["rstd"][:], in_=S["mv"][:, 1:2], func=AF.Sqrt, bias=eps_t[:, 0:1], scale=1.0),
                     reads=[tS, t_const], writes=[tS])
            for h in H:
                S = hsc[h]; tS = t_hsc[h]
                P.op("dve", lambda e: e.reciprocal(out=S["rstd"][:], in_=S["rstd"][:]), reads=[tS], writes=[tS])
            for h in H:
                S = hsc[h]; tS = t_hsc[h]
                P.op("dve", lambda e: e.tensor_scalar(out=S["h"][:], in0=pnum[h][:, 0:256], scalar1=S["mv"][:, 0:1], scalar2=S["rstd"][:, 0:1],
                                                      op0=ALU.subtract, op1=ALU.mult), reads=[t_pnum[h], tS], writes=[tS])
                P.op("pool", lambda e: e.tensor_tensor(out=S["sg"][:], in0=rs[:, tt, h * 256:(h + 1) * 256], in1=glt[:, h * 256:(h + 1) * 256],
                                                       op=ALU.mult), reads=[t_r, t_const], writes=[t_hsg[h]])
            for h in H:
                S = hsc[h]; tS = t_hsc[h]
                P.op("dve", lambda e: e.tensor_tensor(out=yml[:, h * 256:(h + 1) * 256], in0=S["h"][:], in1=S["sg"][:], op=ALU.mult),
                     reads=[tS, t_hsg[h]], writes=[t_yml])
            for kc in range(4):
                P.op("pe", lambda e: e.transpose(out=pe_[:, kc * 128:(kc + 1) * 128], in_=yml[:, kc * 128:(kc + 1) * 128], identity=ident),
                     reads=[t_yml, t_const], writes=[t_pe])
            P.op("act", lambda e: e.copy(out=ycT[:, :, tsl], in_=pe_[:, :].rearrange("p (k t) -> p k t", k=4)), reads=[t_pe], writes=[t_ym])

        for tt in range(4):
            s = cos[0] % 2; cos[0] += 1
            for hf in range(2):
                for kc in range(4):
                    P.op("pe", lambda e: e.matmul(pp[hf][:, :], lhsT=ycT[:, kc, tt * 128:(tt + 1) * 128], rhs=wot[:, kc, hf * 512:(hf + 1) * 512],
                                                  start=(kc == 0), stop=(kc == 3)), reads=[t_ym, t_w], writes=[t_pp[hf]])
                P.op("act" if hf == 0 else "dve", lambda e: (e.copy if hf == 0 else e.tensor_copy)(out=osb[s][:, hf * 512:(hf + 1) * 512], in_=pp[hf][:, :]),
                     reads=[t_pp[hf]], writes=[t_osbh[s][hf]])
            P.dma("sp", out[r0 + tt * 128:r0 + (tt + 1) * 128, :], osb[s][:], reads=[t_osbh[s][0], t_osbh[s][1]], writes=[t_outd])


def odd_inputs(x, w_in, gla_w2, gla_b, gla_norm, w_out, hp):
    f = np.float32
    h0 = 2 * hp
    q = w_in[:, 0:512][:, h0 * 128:(h0 + 2) * 128]
    k = w_in[:, 512:1024][:, h0 * 128:(h0 + 2) * 128]
    v = w_in[:, 1024:2048][:, h0 * 256:(h0 + 2) * 256]
    r = w_in[:, 2048:3072][:, h0 * 256:(h0 + 2) * 256]
    glr = w_in[:, 3072:3088]
    wA = np.ascontiguousarray(np.concatenate([q, k, v, r, glr], axis=1), dtype=f)
    wo = np.ascontiguousarray(w_out[h0 * 256:(h0 + 2) * 256], dtype=f)
    w2 = np.ascontiguousarray(gla_w2[:, h0 * 128:(h0 + 2) * 128], dtype=f)
    pc = np.ascontiguousarray(gla_b[h0 * 128:(h0 + 2) * 128].reshape(2, 128).T, dtype=f)
    gln = np.ascontiguousarray(gla_norm[h0 * 256:(h0 + 2) * 256].reshape(1, 512), dtype=f)
    cst = np.zeros((128, 768), f)
    cst[:, 0:128] = np.eye(128)
    s_i = np.arange(128)[:, None]; t_i = np.arange(128)[None, :]
    cst[:, 128:256] = ((s_i // 64 == t_i // 64) & (s_i <= t_i)).astype(f)
    cst[:, 256:768] = (np.arange(512) % 64 != 0).astype(f)[None, :]
    return dict(x=np.ascontiguousarray(x, dtype=f), wA=wA, wo=wo, w2=w2, pc=pc, gln=gln, cst=cst)


FUSED_CORES = BATCH
SPARSE_MOE = True


def build_fused(n_layers=DEPTH, T=SEQ, sparse=True):
    P = Prog()
    nc = P.nc
    x_ext = nc.dram_tensor("x", [T, D_MODEL], F32, kind="ExternalInput").ap()
    y_ext = nc.dram_tensor("y", [T, D_MODEL], F32, kind="ExternalOutput").ap()
    xbuf = [nc.dram_tensor("xbuf%d" % i, [T, D_MODEL], F32).ap() for i in range(2)]
    pmix = [nc.dram_tensor("pmix%d" % i, [T, D_MODEL], F32).ap() for i in range(2)]
    t_xext = P.tok("xext")
    t_y = P.tok("y"); t_y.disjoint = True
    t_xbuf = [P.tok("xbuf%d" % i) for i in range(2)]
    t_pmix = [P.tok("pmix%d" % i) for i in range(2)]
    for t in t_xbuf + t_pmix:
        t.disjoint = True
    scr = None
    if sparse:
        scr = moe_sparse_scratch(nc, T)
        for k in ("t_xbkt", "t_ybuf", "t_x1d"):
            scr[k] = P.tok(k); scr[k].disjoint = True
    for l in range(n_layers):
        xin_ap, t_xin = (x_ext, t_xext) if l == 0 else (xbuf[(l - 1) % 2], t_xbuf[(l - 1) % 2])
        yout_ap, t_yout = (y_ext, t_y) if l == n_layers - 1 else (xbuf[l % 2], t_xbuf[l % 2])
        for hp in range(2):
            P.push()
            pfx = "L%dH%d_" % (l, hp)
            if l % 2 == 0:
                io = even_io(nc, pfx, T, x=xin_ap, out=pmix[hp])
            else:
                io = odd_io(nc, pfx, T, x=xin_ap, out=pmix[hp])
            io["t_xd"] = t_xin; io["t_outd"] = t_pmix[hp]
            if l % 2 == 0:
                emit_even(P, T, io)
            else:
                emit_odd(P, T, io)
            P.pop()
        P.push()
        io = moe_io(nc, "L%d_" % l, T, xin=xin_ap, ma=pmix[0], mb=pmix[1], yout=yout_ap, sparse=sparse)
        io["t_xin"] = t_xin; io["t_ma"] = t_pmix[0]; io["t_mb"] = t_pmix[1]; io["t_yout"] = t_yout
        if sparse:
            emit_moe_sparse(P, T, io, scr, first=(l == 0))
        else:
            emit_moe(P, T // 1024, io)
        P.pop()
    P.wait_all("sp", [t_y])
    P.close()
    return nc


_NC_CACHE = {}


def fused_inputs(b, x, even_w_in, pool_w, pool_scale, conv_w, conv_b, i_bias, f_bias, ml_norm, even_w_out,
                 odd_w_in, gla_w2, gla_b, gla_norm, odd_w_out, lnps, router_w, router_b, wgu_d, bgu_l, w_down, b_down, n_layers):
    f = np.float32
    im = {"x": np.ascontiguousarray(x[b], dtype=f)}
    ident = np.eye(128, dtype=f)
    for l in range(n_layers):
        i = l // 2
        for hp in range(2):
            pfx = "L%dH%d_" % (l, hp)
            if l % 2 == 0:
                d = even_inputs(x[b], even_w_in[i], pool_w[i], pool_scale[i], conv_w[i], conv_b[i], i_bias[i], f_bias[i],
                                ml_norm[i], even_w_out[i], hp)
            else:
                d = odd_inputs(x[b], odd_w_in[i], gla_w2[i], gla_b[i], gla_norm[i], odd_w_out[i], hp)
            for k, v in d.items():
                if k != "x":
                    im[pfx + k] = v
        pfx = "L%d_" % l
        im[pfx + "lnp"] = lnps[l]
        im[pfx + "rw"] = np.ascontiguousarray(router_w[l], dtype=f)
        im[pfx + "rb"] = np.ascontiguousarray(router_b[l], dtype=f).reshape(1, N_EXPERTS)
        im[pfx + "wgu"] = wgu_d[l]
        im[pfx + "bgu"] = bgu_l[l]
        im[pfx + "wd"] = np.ascontiguousarray(w_down[l], dtype=f)
        im[pfx + "bd"] = np.ascontiguousarray(b_down[l], dtype=f)
        im[pfx + "ident"] = moe_sparse_consts() if SPARSE_MOE else ident
    return im


def kernel(x, even_w_in, pool_w, pool_scale, conv_w, conv_b, i_bias, f_bias, ml_norm, even_w_out,
           odd_w_in, gla_w2, gla_b, gla_norm, odd_w_out, ln1_g, ln1_b, ln2_g, ln2_b, router_w, router_b,
           w_gate_up, b_gate_up, w_down, b_down, _layers=DEPTH, _cores=FUSED_CORES):
    f = np.float32
    A = lambda a: np.asarray(a, dtype=f)
    x = A(x)
    wgu_d, bgu_l, lnps = [], [], []
    for l in range(_layers):
        wg = A(w_gate_up[l])
        wgu_d.append(np.ascontiguousarray(np.concatenate([wg[:, :, 0::2], wg[:, :, 1::2]], axis=-1)))
        del wg
        bg = A(b_gate_up[l])
        bd_ = np.concatenate([bg[:, 0::2], bg[:, 1::2]], axis=-1)
        bgu_l.append(np.ascontiguousarray(bd_.reshape(N_EXPERTS, 16, 128).transpose(2, 0, 1).reshape(128, N_EXPERTS * 16)))
        lnps.append(np.stack([A(ln1_g[l]), A(ln1_b[l]), A(ln2_g[l]), A(ln2_b[l])]))
    args = [A(v) for v in (even_w_in, pool_w, pool_scale, conv_w, conv_b, i_bias, f_bias, ml_norm, even_w_out,
                           odd_w_in, gla_w2, gla_b, gla_norm, odd_w_out)]
    ims = [fused_inputs(b, x, *args, lnps, A(router_w), A(router_b), wgu_d, bgu_l, A(w_down), A(b_down), _layers)
           for b in range(_cores)]
    key = ("fused", _layers)
    if key not in _NC_CACHE:
        _NC_CACHE[key] = build_fused(_layers, sparse=SPARSE_MOE)
    res = run_bass_kernel_spmd(_NC_CACHE[key], ims, core_ids=list(range(_cores)))
    out = np.stack([r["y"] for r in res.results], axis=0)
    if _cores < BATCH:
        return out
    return out.reshape(BATCH, SEQ, D_MODEL)


MOE_CAP = 768
MOE_NR = N_EXPERTS * MOE_CAP
U32 = mybir.dt.uint32


def moe_sparse_scratch(nc, T):
    return dict(xbkt=nc.dram_tensor("xbkt", [MOE_NR + 128, D_MODEL], BF16).ap(),
                ybuf=nc.dram_tensor("ybuf", [MOE_NR + 128, D_MODEL], F32).ap(),
                x1d=nc.dram_tensor("x1d", [T, D_MODEL], F32).ap())


def _idma(P, out, in_, out_off=None, in_off=None, reads=(), writes=()):
    P._deps("pool", reads, writes)
    owner = writes[0]
    key = P._dsem(owner)
    ins = P.nc.gpsimd.indirect_dma_start(out=out, out_offset=out_off, in_=in_, in_offset=in_off)
    ins.then_inc(P.sems[key], 16)
    owner.dcount += 16
    P.dtot[key] = owner.dcount
    me = (key, owner.dcount)
    for r in reads:
        r.readers.append(me)
    for w in writes:
        w.writer = me
        w.readers = []


def emit_moe_sparse(P, T, io, scr, first=False, stop_phase=9):
    NT = T // 128
    C = MOE_CAP
    CB = C // 2
    NRT = C // 128
    nc = P.nc
    xin, ma, mb, lnp, rw, rb, wgu, bgu, wd, bd, cst_in, yout = (io[k] for k in
        ("xin", "ma", "mb", "lnp", "rw", "rb", "wgu", "bgu", "wd", "bd", "ident", "yout"))
    t_xin, t_ma, t_mb, t_yout = io["t_xin"], io["t_ma"], io["t_mb"], io["t_yout"]
    xbkt, ybuf, x1d = scr["xbkt"], scr["ybuf"], scr["x1d"]
    t_xbkt, t_ybuf, t_x1d = scr["t_xbkt"], scr["t_ybuf"], scr["t_x1d"]

    t_const = P.tok("const")
    cs = P.sb([128, 128 * 3 + 32 + 1], F32, "mcst")
    ident = cs[:, 0:128]; Ltri = cs[:, 128:256]; ones = cs[:, 256:384]; ebase1 = cs[:, 384:416]; ptrash = cs[:, 416:417]
    gb = P.sb([128, 4, D_MODEL], F32, "gb")
    rwt = P.sb([128, 8, N_EXPERTS], F32, "rwt"); rbt = P.sb([128, N_EXPERTS], F32, "rbt")
    bgt = P.sb([128, N_EXPERTS * 16], F32, "bgt"); bdt = P.sb([N_EXPERTS, D_MODEL], F32, "bdt")
    eps_t = P.sb([128, 1], F32, "eps")
    Gall = P.sb([128, NT, N_EXPERTS], F32, "G"); t_G = [P.tok("G") for i in range(NT)]
    Gk = P.sb([128, NT, 4], F32, "Gk"); Sidx = P.sb([128, NT, 4], U32, "Sidx"); t_sel = [P.tok("sel") for i in range(NT)]
    base = P.sb([128, N_EXPERTS], F32, "base"); t_base = P.tok("base")
    P.dma("sp", cs[:], cst_in[:, :], writes=[t_const])
    for i in range(4):
        P.dma("sp", gb[:, i, :], lnp[i:i + 1, :].partition_broadcast(128), writes=[t_const])
    P.dma("sp", rwt[:], rw.rearrange("(k p) n -> p k n", p=128), writes=[t_const])
    P.dma("sp", rbt[:], rb[0:1, :].partition_broadcast(128), writes=[t_const])
    P.dma("sp", bgt[:], bgu[:, :], writes=[t_const])
    P.dma("sp", bdt[:], bd[:, :], writes=[t_const])
    P.op("dve", lambda e: e.memset(eps_t[:], LN_EPS), writes=[t_const])
    P.op("dve", lambda e: e.memset(base[:], 0.0), writes=[t_base])
    bgv = bgt[:].rearrange("p (e c) -> p e c", c=16)
    P.op("dve", lambda e: e.tensor_scalar(out=bgv[:, :, 8:16], in0=bgv[:, :, 8:16], scalar1=1.0, scalar2=None, op0=ALU.add),
         reads=[t_const], writes=[t_const])

    P.push()
    xs_l = [P.sb([128, D_MODEL], F32, "xs%d" % i) for i in range(2)]; t_xs_l = [P.tok("xs") for i in range(2)]
    pas_l = [P.sb([128, D_MODEL], F32, "pa%d" % i) for i in range(2)]; t_pa_l = [P.tok("pa") for i in range(2)]
    pbs_l = [P.sb([128, D_MODEL], F32, "pb%d" % i) for i in range(2)]; t_pb_l = [P.tok("pb") for i in range(2)]
    x1s = [P.sb([128, D_MODEL], F32, "x1s%d" % i) for i in range(2)]; t_x1 = [P.tok("x1s") for i in range(2)]
    x1b = [P.sb([128, D_MODEL], BF16, "x1b%d" % i) for i in range(2)]; t_x1b = [P.tok("x1b") for i in range(2)]
    xTf_l = [P.sb([128, 8, 128], F32, "xTf%d" % i) for i in range(2)]; t_xTf_l = [P.tok("xTf") for i in range(2)]
    lns_l = [dict(st=P.sb([128, 12], F32), mv=P.sb([128, 2], F32), rstd=P.sb([128, 1], F32), eps=eps_t) for i in range(2)]
    t_lns_l = [P.tok("lns") for i in range(2)]
    R_l = [dict(lg=P.sb([128, 32], F32), t8=P.sb([128, 8], F32), nm=P.sb([128, 1], F32), ex=P.sb([128, 32], F32),
                mk=P.sb([128, 32], F32), sm=P.sb([128, 1], F32), pos=P.sb([128, 32], F32), v1=P.sb([128, 32], F32),
                smat=P.sb([128, 32], F32), s8=P.sb([128, 8], F32), neg=P.sb([128, 4], F32), sf=P.sb([128, 4], F32),
                junk=P.sb([128, 32], F32)) for i in range(2)]
    tR_l = [P.tok("rt") for i in range(2)]
    pm = [P.ps([128, 512], F32, "pm%d" % i) for i in range(4)]; t_pm = [P.tok("pm") for i in range(4)]
    pr_l = [P.ps([128, 512], F32, "pr%d" % i) for i in range(2)]; t_pr_l = [P.tok("pr") for i in range(2)]
    xs = xs_l[0]; t_xs = t_xs_l[0]
    if first:
        P.op("dve", lambda e: e.memset(xs[:], 0.0), writes=[t_xs])
        P.dma("sp", ybuf[MOE_NR:MOE_NR + 128, :], xs[:], reads=[t_xs], writes=[t_ybuf])
    cm = [0]
    for it in range(NT):
        s = it % 2
        r0 = it * 128
        xs = xs_l[s]; t_xs = t_xs_l[s]; pas = pas_l[s]; t_pa = t_pa_l[s]; pbs = pbs_l[s]; t_pb = t_pb_l[s]
        xTf = xTf_l[s]; t_xTf = t_xTf_l[s]; lns = lns_l[s]; t_lns = t_lns_l[s]; R = R_l[s]; tR = tR_l[s]; pr = pr_l[s]; t_pr = t_pr_l[s]
        P.dma("sp", xs[:], xin[r0:r0 + 128, :], reads=[t_xin], writes=[t_xs])
        P.dma("sp", pas[:], ma[r0:r0 + 128, :], reads=[t_ma], writes=[t_pa])
        P.dma("sp", pbs[:], mb[r0:r0 + 128, :], reads=[t_mb], writes=[t_pb])
        P.op("dve", lambda e: e.tensor_tensor(out=pas[:], in0=pas[:], in1=pbs[:], op=ALU.add), reads=[t_pb, t_pa], writes=[t_pa])
        P.op("dve", lambda e: e.scalar_tensor_tensor(out=xs[:], in0=xs[:], scalar=ALPHA, in1=pas[:], op0=ALU.mult, op1=ALU.add),
             reads=[t_xs, t_pa], writes=[t_xs])
        layer_norm_tile(P, xs[:], x1s[s][:], gb[:, 0, :], gb[:, 1, :], lns, t_xs, t_x1[s], t_lns, t_const, gb_eng="dve")
        P.dma("sp", x1d[r0:r0 + 128, :], x1s[s][:], reads=[t_x1[s]], writes=[t_x1d])
        P.op("act", lambda e: e.copy(out=x1b[s][:], in_=x1s[s][:]), reads=[t_x1[s]], writes=[t_x1b[s]])
        for h in range(2):
            b = cm[0] % 4; cm[0] += 1
            for j in range(4):
                k = h * 4 + j
                P.op("pe", lambda e: e.transpose(out=pm[b][:, j * 128:(j + 1) * 128], in_=x1s[s][:, k * 128:(k + 1) * 128], identity=ident),
                     reads=[t_x1[s], t_const], writes=[t_pm[b]])
            P.op("act", lambda e: e.copy(out=xTf[:, h * 4:(h + 1) * 4, :], in_=pm[b][:].rearrange("p (j t) -> p j t", j=4)),
                 reads=[t_pm[b]], writes=[t_xTf])
        b = cm[0] % 4; cm[0] += 1
        for k in range(8):
            P.op("pe", lambda e: e.matmul(pm[b][:, 0:32], lhsT=xTf[:, k, :], rhs=rwt[:, k, :], start=(k == 0), stop=(k == 7)),
                 reads=[t_xTf, t_const], writes=[t_pm[b]])
        P.op("dve", lambda e: e.tensor_tensor(out=R["lg"][:], in0=pm[b][:, 0:32], in1=rbt[:], op=ALU.add), reads=[t_pm[b], t_const], writes=[tR])
        P.op("dve", lambda e: e.max(out=R["t8"][:], in_=R["lg"][:]), reads=[tR], writes=[tR])
        P.op("dve", lambda e: e.tensor_scalar(out=R["nm"][:], in0=R["t8"][:, 0:1], scalar1=-1.0, scalar2=None, op0=ALU.mult), reads=[tR], writes=[tR])
        P.op("act", lambda e: e.activation(out=R["ex"][:], in_=R["lg"][:], func=AF.Exp, bias=R["nm"][:, 0:1], scale=1.0), reads=[tR], writes=[tR])
        P.op("dve", lambda e: e.tensor_scalar(out=R["mk"][:], in0=R["lg"][:], scalar1=R["t8"][:, 3:4], scalar2=None, op0=ALU.is_ge), reads=[tR], writes=[tR])
        P.op("dve", lambda e: e.tensor_tensor(out=R["ex"][:], in0=R["ex"][:], in1=R["mk"][:], op=ALU.mult), reads=[tR], writes=[tR])
        P.op("dve", lambda e: e.reduce_sum(out=R["sm"][:], in_=R["ex"][:], axis=mybir.AxisListType.X), reads=[tR], writes=[tR])
        P.op("dve", lambda e: e.reciprocal(out=R["sm"][:], in_=R["sm"][:]), reads=[tR], writes=[tR])
        P.op("dve", lambda e: e.tensor_scalar(out=Gall[:, it, :], in0=R["ex"][:], scalar1=R["sm"][:, 0:1], scalar2=None, op0=ALU.mult),
             reads=[tR], writes=[t_G[it]])
        P.op("pe", lambda e: e.matmul(pr[:, 0:32], lhsT=Ltri, rhs=R["mk"][:], start=True, stop=True), reads=[tR, t_const], writes=[t_pr])
        P.op("pe", lambda e: e.matmul(pr[:, 32:64], lhsT=ones, rhs=R["mk"][:], start=True, stop=True), reads=[tR, t_const], writes=[t_pr])
        P.op("dve", lambda e: e.tensor_tensor(out=R["pos"][:], in0=pr[:, 0:32], in1=base[:], op=ALU.add), reads=[t_pr, t_base], writes=[tR])
        P.op("dve", lambda e: e.tensor_tensor(out=base[:], in0=pr[:, 32:64], in1=base[:], op=ALU.add), reads=[t_pr, t_base, tR], writes=[t_base])
        P.op("dve", lambda e: e.tensor_scalar(out=R["v1"][:], in0=R["pos"][:], scalar1=float(C), scalar2=None, op0=ALU.is_lt), reads=[tR], writes=[tR])
        P.op("dve", lambda e: e.tensor_tensor(out=R["v1"][:], in0=R["v1"][:], in1=R["mk"][:], op=ALU.mult), reads=[tR], writes=[tR])
        P.op("dve", lambda e: e.tensor_tensor(out=R["pos"][:], in0=R["pos"][:], in1=ebase1, op=ALU.add), reads=[tR, t_const], writes=[tR])
        P.op("dve", lambda e: e.tensor_tensor(out=R["smat"][:], in0=R["pos"][:], in1=R["v1"][:], op=ALU.mult), reads=[tR], writes=[tR])
        P.op("dve", lambda e: e.tensor_scalar(out=R["smat"][:], in0=R["smat"][:], scalar1=-1.0, scalar2=None, op0=ALU.add), reads=[tR], writes=[tR])
        P.op("dve", lambda e: e.max(out=R["s8"][:], in_=R["smat"][:]), reads=[tR], writes=[tR])
        for k in range(4):
            P.op("dve", lambda e: e.scalar_tensor_tensor(out=R["junk"][:], in0=R["smat"][:], scalar=R["s8"][:, k:k + 1], in1=Gall[:, it, :],
                                                         op0=ALU.is_equal, op1=ALU.mult, accum_out=Gk[:, it, k:k + 1]),
                 reads=[tR, t_G[it]], writes=[tR, t_sel[it]])
        P.op("dve", lambda e: e.tensor_scalar(out=R["neg"][:], in0=R["s8"][:, 0:4], scalar1=0.0, scalar2=None, op0=ALU.is_lt), reads=[tR], writes=[tR])
        P.op("dve", lambda e: e.scalar_tensor_tensor(out=R["sf"][:], in0=R["neg"][:], scalar=ptrash, in1=R["s8"][:, 0:4], op0=ALU.mult, op1=ALU.add),
             reads=[tR, t_const], writes=[tR])
        P.op("dve", lambda e: e.tensor_copy(out=Sidx[:, it, :], in_=R["sf"][:]), reads=[tR], writes=[t_sel[it]])
        for k in range(4):
            _idma(P, xbkt[:, :], x1b[s][:], out_off=bass.IndirectOffsetOnAxis(Sidx[:, it, k:k + 1], 0),
                  reads=[t_x1b[s], t_sel[it]], writes=[t_xbkt])
    P.pop()

    if stop_phase <= 1:
        return
    P.push()
    wgt = [P.sb([128, 8, 2048], BF16, "wgu%d" % i) for i in range(2)]; t_wg = [P.tok("wg") for i in range(2)]
    wdt = [P.sb([128, 8, D_MODEL], BF16, "wd%d" % i) for i in range(2)]; t_wd = [P.tok("wd") for i in range(2)]
    XT = P.sb([128, 8, C], BF16, "XT"); t_XT = P.tok("XT")
    actT = P.sb([128, 8, C], BF16, "actT"); t_actT = [P.tok("actT") for i in range(2)]
    xrow = [P.sb([128, D_MODEL], BF16, "xrow%d" % i) for i in range(2)]; t_xrow = [P.tok("xrow") for i in range(2)]
    yrow = [P.sb([128, D_MODEL], F32, "yrow%d" % i) for i in range(2)]; t_yrow = [P.tok("yrow") for i in range(2)]
    identb = P.sb([128, 128], BF16, "identb")
    P.op("act", lambda e: e.copy(out=identb[:], in_=ident), reads=[t_const], writes=[t_const])
    NG = 2
    glt = [P.sb([128, CB], F32, "gl%d" % i) for i in range(NG)]; t_gl = [P.tok("gl") for i in range(NG)]
    sgt = [P.sb([128, CB], F32, "sg%d" % i) for i in range(NG)]; t_sg = [P.tok("sg") for i in range(NG)]
    lit = [P.sb([128, CB], F32, "li%d" % i) for i in range(NG)]; t_li = [P.tok("li") for i in range(NG)]
    pg = [P.ps([128, 512], F32, "pg%d" % i) for i in range(3)]; t_pg = [P.tok("pg") for i in range(3)]
    pdn = [P.ps([128, 512], F32, "pd%d" % i) for i in range(3)]; t_pd = [P.tok("pd") for i in range(3)]
    ptb = [P.ps([128, 1024], BF16, "ptb%d" % i) for i in range(2)]; t_ptb = [P.tok("ptb") for i in range(2)]

    def load_weights(e, slot):
        src = wgu[e].rearrange("(k p) n -> p k n", p=128)
        for k in range(8):
            P.dma("pool", wgt[slot][:, k, :], src[:, k, :], writes=[t_wg[slot]])
        src = wd[e].rearrange("(k p) n -> p k n", p=128)
        for k in range(0, 8, 2):
            P.dma("pool", wdt[slot][:, k:k + 2, :], src[:, k:k + 2, :], writes=[t_wd[slot]])

    cg = [0]; cd = [0]; cgl = [0]; ct = [0]; cy = [0]
    load_weights(0, 0)
    for ex in range(N_EXPERTS):
        slot = ex % 2
        if ex + 1 < N_EXPERTS:
            load_weights(ex + 1, (ex + 1) % 2)
        W = wgt[slot]; WD = wdt[slot]
        for rt in range(NRT):
            s = ct[0] % 2; ct[0] += 1
            rr = ex * C + rt * 128
            P.dma("sp", xrow[s][:], xbkt[rr:rr + 128, :], reads=[t_xbkt], writes=[t_xrow[s]])
            for k in range(8):
                P.op("pe", lambda e: e.transpose(out=ptb[s][:, k * 128:(k + 1) * 128], in_=xrow[s][:, k * 128:(k + 1) * 128], identity=identb[:]),
                     reads=[t_xrow[s], t_const], writes=[t_ptb[s]])
            P.op("act", lambda e: e.copy(out=XT[:, :, rt * 128:(rt + 1) * 128], in_=ptb[s][:, :].rearrange("p (k t) -> p k t", k=8)),
                 reads=[t_ptb[s]], writes=[t_XT])
        for cb in range(2):
            csl = slice(cb * CB, (cb + 1) * CB)
            for j in range(8):
                gi = cgl[0] % NG; cgl[0] += 1
                b = cg[0] % 3; cg[0] += 1
                for k in range(8):
                    P.op("pe", lambda e: e.matmul(pg[b][:, 0:CB], lhsT=W[:, k, j * 128:(j + 1) * 128], rhs=XT[:, k, csl], start=(k == 0), stop=(k == 7)),
                         reads=[t_wg[slot], t_XT], writes=[t_pg[b]])
                P.op("dve", lambda e: e.tensor_scalar(out=glt[gi][:], in0=pg[b][:, 0:CB], scalar1=bgt[:, ex * 16 + j:ex * 16 + j + 1],
                                                      scalar2=SWIGLU_LIMIT, op0=ALU.add, op1=ALU.min), reads=[t_pg[b], t_const], writes=[t_gl[gi]])
                P.op("act", lambda e: e.activation(out=sgt[gi][:], in_=glt[gi][:], func=AF.Sigmoid, scale=SWIGLU_ALPHA), reads=[t_gl[gi]], writes=[t_sg[gi]])
                P.op("pool", lambda e: e.tensor_tensor(out=sgt[gi][:], in0=sgt[gi][:], in1=glt[gi][:], op=ALU.mult), reads=[t_gl[gi], t_sg[gi]], writes=[t_sg[gi]])
                b = cg[0] % 3; cg[0] += 1
                for k in range(8):
                    P.op("pe", lambda e: e.matmul(pg[b][:, 0:CB], lhsT=W[:, k, 1024 + j * 128:1024 + (j + 1) * 128], rhs=XT[:, k, csl], start=(k == 0), stop=(k == 7)),
                         reads=[t_wg[slot], t_XT], writes=[t_pg[b]])
                P.op("dve", lambda e: e.tensor_scalar(out=lit[gi][:], in0=pg[b][:, 0:CB], scalar1=bgt[:, ex * 16 + 8 + j:ex * 16 + 9 + j],
                                                      scalar2=SWIGLU_LIMIT + 1.0, op0=ALU.add, op1=ALU.min), reads=[t_pg[b], t_const], writes=[t_li[gi]])
                P.op("dve", lambda e: e.scalar_tensor_tensor(out=actT[:, j, csl], in0=lit[gi][:], scalar=1.0 - SWIGLU_LIMIT, in1=sgt[gi][:],
                                                             op0=ALU.max, op1=ALU.mult), reads=[t_li[gi], t_sg[gi]], writes=[t_actT[cb]])
        for rt in range(NRT):
            s = cy[0] % 2; cy[0] += 1
            for hf in range(2):
                b = cd[0] % 3; cd[0] += 1
                for k in range(8):
                    P.op("pe", lambda e: e.matmul(pdn[b][:, :], lhsT=actT[:, k, rt * 128:(rt + 1) * 128], rhs=WD[:, k, hf * 512:(hf + 1) * 512],
                                                  start=(k == 0), stop=(k == 7)), reads=[t_actT[0], t_actT[1], t_wd[slot]], writes=[t_pd[b]])
                P.op("act", lambda e: e.copy(out=yrow[s][:, hf * 512:(hf + 1) * 512], in_=pdn[b][:, :]), reads=[t_pd[b]], writes=[t_yrow[s]])
            rr = ex * C + rt * 128
            P.dma("sp", ybuf[rr:rr + 128, :], yrow[s][:], reads=[t_yrow[s]], writes=[t_ybuf])
    P.pop()

    if stop_phase <= 2:
        return
    P.push()
    x1s = [P.sb([128, D_MODEL], F32, "x1c%d" % i) for i in range(2)]; t_x1 = [P.tok("x1c") for i in range(2)]
    accs = [P.sb([128, D_MODEL], F32, "acc%d" % i) for i in range(2)]; t_acc = [P.tok("acc") for i in range(2)]
    yg_l = [[P.sb([128, D_MODEL], F32, "yg%d%d" % (j, i)) for i in range(4)] for j in range(2)]
    t_yg_l = [[P.tok("yg") for i in range(4)] for j in range(2)]
    outs = [P.sb([128, D_MODEL], F32, "outs%d" % i) for i in range(2)]; t_outs = [P.tok("outs") for i in range(2)]
    GT_l = [P.sb([32, 128], F32, "GT%d" % i) for i in range(2)]; t_GT_l = [P.tok("GT") for i in range(2)]
    lns_l = [dict(st=P.sb([128, 12], F32), mv=P.sb([128, 2], F32), rstd=P.sb([128, 1], F32), eps=eps_t) for i in range(2)]
    t_lns_l = [P.tok("lns") for i in range(2)]
    pm = [P.ps([128, 512], F32, "pm%d" % i) for i in range(4)]; t_pm = [P.tok("pm") for i in range(4)]
    pt_l = [P.ps([128, 512], F32, "pt%d" % i) for i in range(2)]; t_pt_l = [P.tok("pt") for i in range(2)]
    cm = [0]
    for it in range(NT):
        s = it % 2
        r0 = it * 128
        yg = yg_l[s]; t_yg = t_yg_l[s]; GT = GT_l[s]; t_GT = t_GT_l[s]; lns = lns_l[s]; t_lns = t_lns_l[s]; pt = pt_l[s]; t_pt = t_pt_l[s]
        P.dma("sp", x1s[s][:], x1d[r0:r0 + 128, :], reads=[t_x1d], writes=[t_x1[s]])
        for k in range(4):
            _idma(P, yg[k][:], ybuf[:, :], in_off=bass.IndirectOffsetOnAxis(Sidx[:, it, k:k + 1], 0), reads=[t_ybuf, t_sel[it]], writes=[t_yg[k]])
        P.op("pe", lambda e: e.transpose(out=pt[0:32, 0:128], in_=Gall[:, it, :], identity=ident), reads=[t_G[it], t_const], writes=[t_pt])
        P.op("act", lambda e: e.copy(out=GT[:], in_=pt[0:32, 0:128]), reads=[t_pt], writes=[t_GT])
        for hf in range(2):
            b = cm[0] % 4; cm[0] += 1
            P.op("pe", lambda e: e.matmul(pm[b][:, :], lhsT=GT[:], rhs=bdt[:, hf * 512:(hf + 1) * 512], start=True, stop=True),
                 reads=[t_GT, t_const], writes=[t_pm[b]])
            P.op("dve", lambda e: e.scalar_tensor_tensor(out=accs[s][:, hf * 512:(hf + 1) * 512], in0=x1s[s][:, hf * 512:(hf + 1) * 512],
                                                         scalar=ALPHA, in1=pm[b][:, :], op0=ALU.mult, op1=ALU.add),
                 reads=[t_x1[s], t_pm[b]], writes=[t_acc[s]])
        for k in range(4):
            P.op("dve", lambda e: e.scalar_tensor_tensor(out=accs[s][:], in0=yg[k][:], scalar=Gk[:, it, k:k + 1], in1=accs[s][:],
                                                         op0=ALU.mult, op1=ALU.add), reads=[t_yg[k], t_sel[it], t_acc[s]], writes=[t_acc[s]])
        layer_norm_tile(P, accs[s][:], outs[s][:], gb[:, 2, :], gb[:, 3, :], lns, t_acc[s], t_outs[s], t_lns, t_const, gb_eng="dve")
        P.dma("sp", yout[r0:r0 + 128, :], outs[s][:], reads=[t_outs[s]], writes=[t_yout])
    P.pop()


def moe_sparse_consts():
    f = np.float32
    c = np.zeros((128, 417), f)
    c[:, 0:128] = np.eye(128)
    tp = np.arange(128)[:, None]; tt = np.arange(128)[None, :]
    c[:, 128:256] = (tp < tt).astype(f)
    c[:, 256:384] = 1.0
    c[:, 384:416] = (np.arange(N_EXPERTS) * MOE_CAP + 1).astype(f)[None, :]
    c[:, 416] = MOE_NR + 1 + np.arange(128)
    return c
```

```python
import math
from contextlib import ExitStack

import numpy as np
import concourse.bass as bass
import concourse.mybir as mybir
from concourse.bass_utils import run_bass_kernel_spmd

F32 = mybir.dt.float32
BF16 = mybir.dt.bfloat16
AF = mybir.ActivationFunctionType
ALU = mybir.AluOpType

D_MODEL = 1024
BATCH = 4
SEQ = 4096
DEPTH = 4
ALPHA = (2 * DEPTH) ** 0.25
LN_EPS = 1e-5
N_EXPERTS = 32
SWIGLU_LIMIT = 7.0
SWIGLU_ALPHA = 1.702


class Tok:
    __slots__ = ("name", "writer", "readers", "dsem", "dcount", "disjoint")

    def __init__(self, name):
        self.name = name
        self.writer = None
        self.readers = []
        self.dsem = None
        self.dcount = 0
        self.disjoint = False


class Prog:
    def __init__(self):
        self.nc = bass.Bass("TRN2", target_bir_lowering=False)
        self.es = ExitStack()
        nc = self.nc
        self.eng = {"pe": nc.tensor, "dve": nc.vector, "act": nc.scalar, "pool": nc.gpsimd, "sp": nc.sync}
        self.sems = {}
        self.cnt = {}
        self.known = {k: {} for k in self.eng}
        for k in ("pe", "dve", "act", "pool"):
            self.sems[k] = self.es.enter_context(nc.semaphore("s_" + k))
            self.cnt[k] = 0
        self.nsem = 0
        self.ntens = 0
        self.scopes = []
        self.sem_pool = []
        self.dtot = {}
        self.live_toks = []

    def sb(self, shape, dtype=F32, name=None):
        self.ntens += 1
        t = self._es().enter_context(self.nc.sbuf_tensor("%s_%d" % (name or "sb", self.ntens), list(shape), dtype))
        return t

    def ps(self, shape, dtype=F32, name=None):
        self.ntens += 1
        t = self._es().enter_context(self.nc.psum_tensor("%s_%d" % (name or "ps", self.ntens), list(shape), dtype))
        return t

    def _es(self):
        return self.scopes[-1][0] if self.scopes else self.es

    def tok(self, name="t"):
        t = Tok(name)
        if self.scopes:
            self.scopes[-1][1].append(t)
        return t

    def _dsem(self, tok):
        if tok.dsem is None:
            if self.sem_pool:
                key, cnt = self.sem_pool.pop()
                tok.dcount = cnt
            else:
                self.nsem += 1
                key = "d%d" % self.nsem
                self.sems[key] = self.es.enter_context(self.nc.semaphore(key))
                self.dtot[key] = 0
            tok.dsem = key
        return tok.dsem

    def push(self):
        self.scopes.append((ExitStack(), []))

    def barrier(self):
        targets = {k: self.cnt[k] for k in ("pe", "dve", "act", "pool") if self.cnt[k] > 0}
        for k, v in self.dtot.items():
            if v > 0:
                targets[k] = v
        for e in self.eng:
            kn = self.known[e]
            for k, v in targets.items():
                if kn.get(k, 0) >= v:
                    continue
                self.eng[e].wait_ge(self.sems[k], v)
                kn[k] = v

    def pop(self):
        self.barrier()
        es, toks = self.scopes.pop()
        for t in toks:
            if t.dsem is not None:
                self.sem_pool.append((t.dsem, self.dtot[t.dsem]))
                t.dsem = None
        es.close()

    def _deps(self, e, reads, writes):
        deps = {}

        def add(d):
            if d is None:
                return
            k, v = d
            if deps.get(k, 0) < v:
                deps[k] = v

        for r in reads:
            add(r.writer)
        for w in writes:
            if not w.disjoint:
                add(w.writer)
            for rd in w.readers:
                add(rd)
        kn = self.known[e]
        for k, v in deps.items():
            if e == "pe" and k == "pe":
                continue
            if kn.get(k, 0) >= v:
                continue
            self.eng[e].wait_ge(self.sems[k], v)
            kn[k] = v

    def op(self, e, fn, reads=(), writes=()):
        self._deps(e, reads, writes)
        ins = fn(self.eng[e])
        ins.then_inc(self.sems[e], 1)
        self.cnt[e] += 1
        me = (e, self.cnt[e])
        for r in reads:
            r.readers.append(me)
        for w in writes:
            w.writer = me
            w.readers = []
        return ins

    def dma(self, q, out, in_, reads=(), writes=(), **kw):
        self._deps(q, reads, writes)
        owner = writes[0] if writes else reads[0]
        key = self._dsem(owner)
        ins = self.eng[q].dma_start(out=out, in_=in_, **kw)
        ins.then_inc(self.sems[key], 16)
        owner.dcount += 16
        self.dtot[key] = owner.dcount
        me = (key, owner.dcount)
        for r in reads:
            r.readers.append(me)
        for w in writes:
            w.writer = me
            w.readers = []
        return ins

    def wait_all(self, q, toks):
        deps = {}
        for t in toks:
            for d in [t.writer] + list(t.readers):
                if d is not None and deps.get(d[0], 0) < d[1]:
                    deps[d[0]] = d[1]
        for k, v in deps.items():
            self.eng[q].wait_ge(self.sems[k], v)

    def close(self):
        self.es.close()


def run_interleaved(gens):
    gens = list(gens)
    while gens:
        for g in list(gens):
            try:
                next(g)
            except StopIteration:
                gens.remove(g)


def layer_norm_tile(P, src, dst, g_bc, b_bc, scr, T_src, T_dst, T_scr, tgb, gb_eng="pool"):
    st, mv, rstd = scr["st"], scr["mv"], scr["rstd"]
    P.op("dve", lambda e: e.bn_stats(out=st[:, 0:6], in_=src[:, 0:512]), reads=[T_src], writes=[T_scr])
    P.op("dve", lambda e: e.bn_stats(out=st[:, 6:12], in_=src[:, 512:1024]), reads=[T_src, T_scr], writes=[T_scr])
    P.op("dve", lambda e: e.bn_aggr(out=mv[:, 0:2], in_=st[:, 0:12]), reads=[T_scr], writes=[T_scr])
    P.op("act", lambda e: e.activation(out=rstd[:, 0:1], in_=mv[:, 1:2], func=AF.Sqrt, bias=scr["eps"][:, 0:1], scale=1.0),
         reads=[T_scr, tgb], writes=[T_scr])
    P.op("dve", lambda e: e.reciprocal(out=rstd[:, 0:1], in_=rstd[:, 0:1]), reads=[T_scr], writes=[T_scr])
    P.op("dve", lambda e: e.tensor_scalar(out=dst, in0=src, scalar1=mv[:, 0:1], scalar2=rstd[:, 0:1],
                                          op0=ALU.subtract, op1=ALU.mult), reads=[T_src, T_scr], writes=[T_dst])
    P.op(gb_eng, lambda e: e.tensor_tensor(out=dst, in0=dst, in1=g_bc, op=ALU.mult), reads=[T_dst, tgb], writes=[T_dst])
    P.op(gb_eng, lambda e: e.tensor_tensor(out=dst, in0=dst, in1=b_bc, op=ALU.add), reads=[T_dst, tgb], writes=[T_dst])


def build_moe(n_pass, n_exp=N_EXPERTS, two_mix=True):
    P = Prog()
    nc = P.nc
    io = moe_io(nc, "", n_pass * 1024, n_exp)
    for k in ("t_xin", "t_ma", "t_mb", "t_yout"):
        io[k] = P.tok(k)
    emit_moe(P, n_pass, io, n_exp)
    P.wait_all("sp", [io["t_yout"]])
    P.close()
    return nc


def moe_io(nc, pfx, NTOK, n_exp=N_EXPERTS, xin=None, ma=None, mb=None, yout=None, sparse=False):
    io = {}
    io["xin"] = xin if xin is not None else nc.dram_tensor(pfx + "xin", [NTOK, D_MODEL], F32, kind="ExternalInput").ap()
    io["ma"] = ma if ma is not None else nc.dram_tensor(pfx + "ma", [NTOK, D_MODEL], F32, kind="ExternalInput").ap()
    io["mb"] = mb if mb is not None else nc.dram_tensor(pfx + "mb", [NTOK, D_MODEL], F32, kind="ExternalInput").ap()
    io["lnp"] = nc.dram_tensor(pfx + "lnp", [4, D_MODEL], F32, kind="ExternalInput").ap()
    io["rw"] = nc.dram_tensor(pfx + "rw", [D_MODEL, N_EXPERTS], F32, kind="ExternalInput").ap()
    io["rb"] = nc.dram_tensor(pfx + "rb", [1, N_EXPERTS], F32, kind="ExternalInput").ap()
    io["wgu"] = nc.dram_tensor(pfx + "wgu", [n_exp, D_MODEL, 2048], F32, kind="ExternalInput").ap()
    io["bgu"] = nc.dram_tensor(pfx + "bgu", [128, n_exp * 16], F32, kind="ExternalInput").ap()
    io["wd"] = nc.dram_tensor(pfx + "wd", [n_exp, D_MODEL, D_MODEL], F32, kind="ExternalInput").ap()
    io["bd"] = nc.dram_tensor(pfx + "bd", [N_EXPERTS, D_MODEL], F32, kind="ExternalInput").ap()
    io["ident"] = nc.dram_tensor(pfx + "ident", [128, 417 if sparse else 128], F32, kind="ExternalInput").ap()
    io["yout"] = yout if yout is not None else nc.dram_tensor(pfx + "yout", [NTOK, D_MODEL], F32, kind="ExternalOutput").ap()
    return io


def emit_moe(P, n_pass, io, n_exp=N_EXPERTS, two_mix=True):
    TG = 1024
    NT = TG // 128
    nc = P.nc
    xin, ma, mb, lnp, rw, rb, wgu, bgu, wd, bd, ident_in, yout = (io[k] for k in
        ("xin", "ma", "mb", "lnp", "rw", "rb", "wgu", "bgu", "wd", "bd", "ident", "yout"))
    t_xin, t_ma, t_mb, t_yout = io["t_xin"], io["t_ma"], io["t_mb"], io["t_yout"]

    ident = P.sb([128, 128], F32, "ident"); t_const = P.tok("const")
    gb = P.sb([128, 4, D_MODEL], F32, "gb")
    rwt = P.sb([128, 8, N_EXPERTS], F32, "rwt")
    rbt = P.sb([128, N_EXPERTS], F32, "rbt")
    bgt = P.sb([128, n_exp * 16], F32, "bgt")
    bdt = P.sb([N_EXPERTS, D_MODEL], F32, "bdt")
    eps_t = P.sb([128, 1], F32, "eps")
    P.dma("sp", ident[:], ident_in[:, :], writes=[t_const])
    for i in range(4):
        P.dma("sp", gb[:, i, :], lnp[i:i + 1, :].partition_broadcast(128), writes=[t_const])
    P.dma("sp", rwt[:], rw.rearrange("(k p) n -> p k n", p=128), writes=[t_const])
    P.dma("sp", rbt[:], rb[0:1, :].partition_broadcast(128), writes=[t_const])
    P.dma("sp", bgt[:], bgu[:, :], writes=[t_const])
    P.dma("sp", bdt[:], bd[:, :], writes=[t_const])
    P.op("dve", lambda e: e.memset(eps_t[:], LN_EPS), writes=[t_const])
    bgv = bgt[:].rearrange("p (e c) -> p e c", c=16)
    P.op("dve", lambda e: e.tensor_scalar(out=bgv[:, :, 8:16], in0=bgv[:, :, 8:16], scalar1=1.0, scalar2=None, op0=ALU.add),
         reads=[t_const], writes=[t_const])

    acc = P.sb([128, NT, D_MODEL], F32, "acc");  t_acc = [P.tok("acc%d" % i) for i in range(NT)]
    x1T = P.sb([128, 8, TG], BF16, "x1T");       t_x1T = [P.tok("x1T%d" % i) for i in range(NT)]
    Gall = P.sb([128, NT, N_EXPERTS], F32, "G"); t_G = [P.tok("G%d" % i) for i in range(NT)]
    actT = P.sb([128, 8, 512], BF16, "actT");    t_actT = P.tok("actT")
    wgt = [P.sb([128, 8, 2048], BF16, "wgu%d" % i) for i in range(2)]; t_wg = [P.tok("wg%d" % i) for i in range(2)]
    wdt = [P.sb([128, 8, D_MODEL], BF16, "wd%d" % i) for i in range(2)]; t_wd = [P.tok("wd%d" % i) for i in range(2)]
    NS = 1
    xs = [P.sb([128, D_MODEL], F32, "xs%d" % i) for i in range(NS)]; t_xs = [P.tok("xs%d" % i) for i in range(NS)]
    pas = [P.sb([128, D_MODEL], F32, "pa0")] * NS; t_pa = [P.tok("pa0")] * NS
    pbs = [P.sb([128, D_MODEL], F32, "pb0")] * NS; t_pb = [P.tok("pb0")] * NS
    x1s = [P.sb([128, D_MODEL], F32, "x1s%d" % i) for i in range(NS)]; t_x1 = [P.tok("x1s%d" % i) for i in range(NS)]
    xTf = [P.sb([128, 8, 128], F32, "xTf0")] * NS; t_xTf = [P.tok("xTf0")] * NS
    lns = [dict(st=P.sb([128, 12], F32), mv=P.sb([128, 2], F32), rstd=P.sb([128, 1], F32), eps=eps_t) for i in range(NS)]
    t_lns = [P.tok("lns%d" % i) for i in range(NS)]
    rt = [dict(lg=P.sb([128, 32], F32), t8=P.sb([128, 8], F32), nm=P.sb([128, 1], F32), ex=P.sb([128, 32], F32),
               mk=P.sb([128, 32], F32), sm=P.sb([128, 1], F32), GT=P.sb([32, 128], F32)) for i in range(NS)]
    t_rt = [P.tok("rt%d" % i) for i in range(NS)]
    NG = 2
    glt = [P.sb([128, 512], F32, "gl%d" % i) for i in range(NG)]; t_gl = [P.tok("gl%d" % i) for i in range(NG)]
    sgt = [P.sb([128, 512], F32, "sg%d" % i) for i in range(NG)]; t_sg = [P.tok("sg%d" % i) for i in range(NG)]
    lit = [P.sb([128, 512], F32, "li0")] * NG; t_li = [P.tok("li0")] * NG
    pg = [P.ps([128, 512], F32, "pg%d" % i) for i in range(3)]; t_pg = [P.tok("pg%d" % i) for i in range(3)]
    pdn = [P.ps([128, 512], F32, "pd%d" % i) for i in range(3)]; t_pd = [P.tok("pd%d" % i) for i in range(3)]
    pm = [P.ps([128, 512], F32, "pm%d" % i) for i in range(2)]; t_pm = [P.tok("pm%d" % i) for i in range(2)]

    wload_i = [0]

    def load_weights(e, slot):
        src = wgu[e].rearrange("(k p) n -> p k n", p=128)
        for k in range(8):
            P.dma("pool", wgt[slot][:, k, :], src[:, k, :], writes=[t_wg[slot]])
        src = wd[e].rearrange("(k p) n -> p k n", p=128)
        for k in range(0, 8, 2):
            P.dma("pool", wdt[slot][:, k:k + 2, :], src[:, k:k + 2, :], writes=[t_wd[slot]])

    cg = [0]; cd = [0]; cm = [0]; cgl = [0]

    for ps_i in range(n_pass):
        tok0 = ps_i * TG
        load_weights(0, 0)
        for it in range(NT):
            s = it % NS
            r0 = tok0 + it * 128
            P.dma("sp", xs[s][:], xin[r0:r0 + 128, :], reads=[t_xin], writes=[t_xs[s]])
            P.dma("sp", pas[s][:], ma[r0:r0 + 128, :], reads=[t_ma], writes=[t_pa[s]])
            if two_mix:
                P.dma("sp", pbs[s][:], mb[r0:r0 + 128, :], reads=[t_mb], writes=[t_pb[s]])
                P.op("pool", lambda e: e.tensor_tensor(out=pas[s][:], in0=pas[s][:], in1=pbs[s][:], op=ALU.add),
                     reads=[t_pb[s], t_pa[s]], writes=[t_pa[s]])
            P.op("dve", lambda e: e.scalar_tensor_tensor(out=xs[s][:], in0=xs[s][:], scalar=ALPHA, in1=pas[s][:],
                                                         op0=ALU.mult, op1=ALU.add),
                 reads=[t_xs[s], t_pa[s]], writes=[t_xs[s]])
            layer_norm_tile(P, xs[s][:], x1s[s][:], gb[:, 0, :], gb[:, 1, :], lns[s], t_xs[s], t_x1[s], t_lns[s], t_const)
            for h in range(2):
                b = cm[0] % 2; cm[0] += 1
                for j in range(4):
                    k = h * 4 + j
                    P.op("pe", lambda e: e.transpose(out=pm[b][:, j * 128:(j + 1) * 128], in_=x1s[s][:, k * 128:(k + 1) * 128],
                                                     identity=ident[:]),
                         reads=[t_x1[s], t_const], writes=[t_pm[b]])
                P.op("act", lambda e: e.copy(out=xTf[s][:, h * 4:(h + 1) * 4, :],
                                             in_=pm[b][:].rearrange("p (j t) -> p j t", j=4)),
                     reads=[t_pm[b]], writes=[t_xTf[s]])
            P.op("pool", lambda e: e.tensor_copy(out=x1T[:, :, it * 128:(it + 1) * 128], in_=xTf[s][:]),
                 reads=[t_xTf[s]], writes=[t_x1T[it]])
            b = cm[0] % 2; cm[0] += 1
            for k in range(8):
                P.op("pe", lambda e: e.matmul(pm[b][:, 0:32], lhsT=xTf[s][:, k, :], rhs=rwt[:, k, :], start=(k == 0), stop=(k == 7)),
                     reads=[t_xTf[s], t_const], writes=[t_pm[b]])
            R = rt[s]; tR = t_rt[s]
            P.op("dve", lambda e: e.tensor_tensor(out=R["lg"][:], in0=pm[b][:, 0:32], in1=rbt[:], op=ALU.add),
                 reads=[t_pm[b], t_const], writes=[tR])
            P.op("dve", lambda e: e.max(out=R["t8"][:], in_=R["lg"][:]), reads=[tR], writes=[tR])
            P.op("dve", lambda e: e.tensor_scalar(out=R["nm"][:], in0=R["t8"][:, 0:1], scalar1=-1.0, scalar2=None, op0=ALU.mult),
                 reads=[tR], writes=[tR])
            P.op("act", lambda e: e.activation(out=R["ex"][:], in_=R["lg"][:], func=AF.Exp, bias=R["nm"][:, 0:1], scale=1.0),
                 reads=[tR], writes=[tR])
            P.op("dve", lambda e: e.tensor_scalar(out=R["mk"][:], in0=R["lg"][:], scalar1=R["t8"][:, 3:4], scalar2=None, op0=ALU.is_ge),
                 reads=[tR], writes=[tR])
            P.op("dve", lambda e: e.tensor_tensor(out=R["ex"][:], in0=R["ex"][:], in1=R["mk"][:], op=ALU.mult),
                 reads=[tR], writes=[tR])
            P.op("dve", lambda e: e.reduce_sum(out=R["sm"][:], in_=R["ex"][:], axis=mybir.AxisListType.X),
                 reads=[tR], writes=[tR])
            P.op("dve", lambda e: e.reciprocal(out=R["sm"][:], in_=R["sm"][:]), reads=[tR], writes=[tR])
            P.op("dve", lambda e: e.tensor_scalar(out=Gall[:, it, :], in0=R["ex"][:], scalar1=R["sm"][:, 0:1], scalar2=None, op0=ALU.mult),
                 reads=[tR], writes=[t_G[it]])
            b = cm[0] % 2; cm[0] += 1
            P.op("pe", lambda e: e.transpose(out=pm[b][0:32, 0:128], in_=Gall[:, it, :], identity=ident[:]),
                 reads=[t_G[it], t_const], writes=[t_pm[b]])
            P.op("act", lambda e: e.copy(out=R["GT"][:], in_=pm[b][0:32, 0:128]), reads=[t_pm[b]], writes=[tR])
            for hf in range(2):
                b = cm[0] % 2; cm[0] += 1
                P.op("pe", lambda e: e.matmul(pm[b][:, :], lhsT=R["GT"][:], rhs=bdt[:, hf * 512:(hf + 1) * 512], start=True, stop=True),
                     reads=[tR, t_const], writes=[t_pm[b]])
                P.op("dve", lambda e: e.scalar_tensor_tensor(out=acc[:, it, hf * 512:(hf + 1) * 512], in0=x1s[s][:, hf * 512:(hf + 1) * 512],
                                                             scalar=ALPHA, in1=pm[b][:, :], op0=ALU.mult, op1=ALU.add),
                     reads=[t_x1[s], t_pm[b]], writes=[t_acc[it]])

        for ex in range(n_exp):
            slot = ex % 2
            if ex + 1 < n_exp:
                load_weights(ex + 1, (ex + 1) % 2)
            W = wgt[slot]; WD = wdt[slot]
            for tb in range(TG // 512):
                tsl = slice(tb * 512, (tb + 1) * 512)
                tiles = [t_x1T[tb * 4 + i] for i in range(4)]
                for j in range(8):
                    gi = cgl[0] % NG; cgl[0] += 1
                    b = cg[0] % 3; cg[0] += 1
                    for k in range(8):
                        P.op("pe", lambda e: e.matmul(pg[b][:, :], lhsT=W[:, k, j * 128:(j + 1) * 128], rhs=x1T[:, k, tsl],
                                                      start=(k == 0), stop=(k == 7)),
                             reads=[t_wg[slot]] + tiles, writes=[t_pg[b]])
                    P.op("dve", lambda e: e.tensor_scalar(out=glt[gi][:], in0=pg[b][:, :], scalar1=bgt[:, ex * 16 + j:ex * 16 + j + 1],
                                                          scalar2=SWIGLU_LIMIT, op0=ALU.add, op1=ALU.min),
                         reads=[t_pg[b], t_const], writes=[t_gl[gi]])
                    P.op("act", lambda e: e.activation(out=sgt[gi][:], in_=glt[gi][:], func=AF.Sigmoid, scale=SWIGLU_ALPHA),
                         reads=[t_gl[gi]], writes=[t_sg[gi]])
                    P.op("pool", lambda e: e.tensor_tensor(out=sgt[gi][:], in0=sgt[gi][:], in1=glt[gi][:], op=ALU.mult),
                         reads=[t_gl[gi], t_sg[gi]], writes=[t_sg[gi]])
                    b = cg[0] % 3; cg[0] += 1
                    for k in range(8):
                        P.op("pe", lambda e: e.matmul(pg[b][:, :], lhsT=W[:, k, 1024 + j * 128:1024 + (j + 1) * 128], rhs=x1T[:, k, tsl],
                                                      start=(k == 0), stop=(k == 7)),
                             reads=[t_wg[slot]] + tiles, writes=[t_pg[b]])
                    P.op("dve", lambda e: e.tensor_scalar(out=lit[gi][:], in0=pg[b][:, :], scalar1=bgt[:, ex * 16 + 8 + j:ex * 16 + 9 + j],
                                                          scalar2=SWIGLU_LIMIT + 1.0, op0=ALU.add, op1=ALU.min),
                         reads=[t_pg[b], t_const], writes=[t_li[gi]])
                    P.op("dve", lambda e: e.scalar_tensor_tensor(out=actT[:, j, :], in0=lit[gi][:], scalar=1.0 - SWIGLU_LIMIT, in1=sgt[gi][:],
                                                                 op0=ALU.max, op1=ALU.mult),
                         reads=[t_li[gi], t_sg[gi]], writes=[t_actT])
                for tt in range(4):
                    it = tb * 4 + tt
                    for hf in range(2):
                        b = cd[0] % 3; cd[0] += 1
                        for k in range(8):
                            P.op("pe", lambda e: e.matmul(pdn[b][:, :], lhsT=actT[:, k, tt * 128:(tt + 1) * 128],
                                                          rhs=WD[:, k, hf * 512:(hf + 1) * 512], start=(k == 0), stop=(k == 7)),
                                 reads=[t_actT, t_wd[slot]], writes=[t_pd[b]])
                        P.op("dve", lambda e: e.scalar_tensor_tensor(out=acc[:, it, hf * 512:(hf + 1) * 512], in0=pdn[b][:, :],
                                                                     scalar=Gall[:, it, ex:ex + 1], in1=acc[:, it, hf * 512:(hf + 1) * 512],
                                                                     op0=ALU.mult, op1=ALU.add),
                             reads=[t_pd[b], t_G[it], t_acc[it]], writes=[t_acc[it]])

        for it in range(NT):
            s = it % NS
            r0 = tok0 + it * 128
            layer_norm_tile(P, acc[:, it, :], x1s[s][:], gb[:, 2, :], gb[:, 3, :], lns[s], t_acc[it], t_x1[s], t_lns[s], t_const)
            P.dma("sp", yout[r0:r0 + 128, :], x1s[s][:], reads=[t_x1[s]], writes=[t_yout])


def make_xT(P, x_ap, r0, ntile, xs, t_xs, xT, t_xT, pm, t_pm, ident, t_const, cm, t_xd=None):
    for tt in range(ntile):
        s = tt % len(xs)
        P.dma("sp", xs[s][:], x_ap[r0 + tt * 128:r0 + (tt + 1) * 128, :], reads=([t_xd] if t_xd is not None else []), writes=[t_xs[s]])
        for h in range(2):
            b = cm[0] % len(pm); cm[0] += 1
            for j in range(4):
                k = h * 4 + j
                P.op("pe", lambda e: e.transpose(out=pm[b][:, j * 128:(j + 1) * 128], in_=xs[s][:, k * 128:(k + 1) * 128],
                                                 identity=ident[:]),
                     reads=[t_xs[s], t_const], writes=[t_pm[b]])
            P.op("act", lambda e: e.copy(out=xT[:, h * 4:(h + 1) * 4, tt * 128:(tt + 1) * 128],
                                         in_=pm[b][:].rearrange("p (j t) -> p j t", j=4)),
                 reads=[t_pm[b]], writes=[t_xT])


def build_even(T, stop=99):
    P = Prog()
    nc = P.nc
    io = even_io(nc, "", T)
    io["t_xd"] = P.tok("xd"); io["t_outd"] = P.tok("outd")
    emit_even(P, T, io, stop)
    P.wait_all("sp", [io["t_outd"]])
    P.close()
    return nc


def even_io(nc, pfx, T, x=None, out=None):
    io = {}
    io["x"] = x if x is not None else nc.dram_tensor(pfx + "x", [T, D_MODEL], F32, kind="ExternalInput").ap()
    io["wA"] = nc.dram_tensor(pfx + "wA", [D_MODEL, 1284], F32, kind="ExternalInput").ap()
    io["wo"] = nc.dram_tensor(pfx + "wo", [512, D_MODEL], F32, kind="ExternalInput").ap()
    io["pw"] = nc.dram_tensor(pfx + "pw", [2, 128, 128], F32, kind="ExternalInput").ap()
    io["pc"] = nc.dram_tensor(pfx + "pc", [128, 30], F32, kind="ExternalInput").ap()
    io["coef0"] = nc.dram_tensor(pfx + "coef0", [128, 2 * 4 * 16], F32, kind="ExternalInput").ap()
    io["gbias"] = nc.dram_tensor(pfx + "gbias", [2, 2], F32, kind="ExternalInput").ap()
    io["mln"] = nc.dram_tensor(pfx + "mln", [1, 256], F32, kind="ExternalInput").ap()
    io["cst"] = nc.dram_tensor(pfx + "cst", [128, 128 + 128], F32, kind="ExternalInput").ap()
    io["cst2"] = nc.dram_tensor(pfx + "cst2", [2, 776], F32, kind="ExternalInput").ap()
    io["out"] = out if out is not None else nc.dram_tensor(pfx + "out", [T, D_MODEL], F32, kind="ExternalOutput").ap()
    return io


def emit_even(P, T, io, stop=99):
    TB = 512
    NB = T // TB
    NCH = TB // 64
    nc = P.nc
    x, wA, wo, pw, pc, coef0, gbias, mln, cst, cst2, out = (io[k] for k in
        ("x", "wA", "wo", "pw", "pc", "coef0", "gbias", "mln", "cst", "cst2", "out"))
    t_xd = io["t_xd"]; t_outd = io["t_outd"]

    t_const = P.tok("const")
    cs = P.sb([128, 256], F32, "cst"); ident = cs[:, 0:128]; maskT = cs[:, 128:256]
    cs2 = P.sb([2, 776], F32, "cst2"); rmask = cs2[:, 0:512]; id2 = cs2[:, 768:776]
    pct = P.sb([128, 30], F32, "pc"); c0t = P.sb([128, 128], F32, "coef0")
    gbt = P.sb([2, 2], F32, "gb"); mlt = P.sb([128, 256], F32, "mln"); eps_t = P.sb([128, 1], F32, "eps")
    wAt = P.sb([128, 8, 1284], BF16, "wA"); wot = P.sb([128, 4, D_MODEL], BF16, "wo"); pwt = P.sb([128, 2, 128], BF16, "pw")
    P.dma("sp", cs[:], cst[:, :], writes=[t_const])
    P.dma("sp", cs2[:], cst2[:, :], writes=[t_const])
    P.dma("sp", pct[:], pc[:, :], writes=[t_const])
    P.dma("sp", c0t[:], coef0[:, :], writes=[t_const])
    P.dma("sp", gbt[:], gbias[:, :], writes=[t_const])
    P.dma("sp", mlt[:], mln[0:1, :].partition_broadcast(128), writes=[t_const])
    t_w = P.tok("w")
    wv = wA.rearrange("(k p) n -> p k n", p=128)
    for k in range(8):
        P.dma("pool", wAt[:, k, :], wv[:, k, :], writes=[t_w])
    P.dma("pool", wot[:], wo.rearrange("(k p) n -> p k n", p=128), writes=[t_w])
    P.dma("pool", pwt[:], pw.rearrange("g c d -> c g d"), writes=[t_w])
    P.op("dve", lambda e: e.memset(eps_t[:], LN_EPS), writes=[t_const])
    nfb = P.sb([2, 1], F32, "nfb")
    identb = P.sb([128, 128], BF16, "identb")
    P.op("act", lambda e: e.copy(out=identb[:], in_=ident), reads=[t_const], writes=[t_const])
    P.op("dve", lambda e: e.tensor_scalar(out=nfb[:], in0=gbt[:, 1:2], scalar1=-1.0, scalar2=None, op0=ALU.mult),
         reads=[t_const], writes=[t_const])

    xs = [P.sb([128, D_MODEL], F32, "xs%d" % i) for i in range(2)]; t_xs = [P.tok("xs") for i in range(2)]
    xT = P.sb([128, 8, TB], BF16, "xT"); t_xT = P.tok("xT")
    ub = [P.sb([128, 16 + TB], F32, "ub%d" % g) for g in range(2)]; t_ub = [P.tok("ub") for g in range(2)]
    s2_l = [P.sb([128, 16 + TB], F32, "s2_%d" % i) for i in range(2)]; s4_l = [P.sb([128, 16 + TB], F32, "s4_%d" % i) for i in range(2)]
    s8_l = [P.sb([128, 16 + TB], F32, "s8_%d" % i) for i in range(2)]; s16_l = [P.sb([128, 16 + TB], F32, "s16_%d" % i) for i in range(2)]
    t_s_l = [P.tok("s") for i in range(2)]
    dacc_l = [P.sb([128, TB], F32, "dacc%d" % i) for i in range(2)]; dbf_l = [P.sb([128, TB], BF16, "dbf%d" % i) for i in range(2)]
    t_d_l = [P.tok("d") for i in range(2)]
    ycT = P.sb([128, 4, TB], BF16, "ycT"); t_yp = P.tok("yp"); t_ym = P.tok("ym")
    qkb = [P.sb([128, 3 + TB], F32, "qkb%d" % c) for c in range(4)]; t_qkb = [P.tok("qkb") for c in range(4)]
    cacc_l = [P.sb([128, TB], F32, "cacc%d" % i) for i in range(2)]; t_cacc_l = [P.tok("cacc") for i in range(2)]
    qTe = [P.sb([128, TB], BF16, "qTe%d" % h) for h in range(2)]; qTo = [P.sb([128, TB], BF16, "qTo%d" % h) for h in range(2)]
    t_q = [P.tok("q") for h in range(2)]
    ksil_l = [P.sb([128, TB], F32, "ksil%d" % i) for i in range(2)]; t_ksil_l = [P.tok("ksil") for i in range(2)]
    kT = [P.sb([128, TB], BF16, "kT%d" % h) for h in range(2)]; t_kT = [P.tok("kT") for h in range(2)]
    ktok = [P.sb([128, 4, 128], BF16, "ktok%d" % h) for h in range(2)]; t_ktok = [P.tok("ktok") for h in range(2)]
    vaug = P.sb([128, 4, 2, 130], BF16, "vaug"); t_v = P.tok("v")
    ogs = P.sb([128, 4, 256], F32, "ogs"); t_og = P.tok("og")
    C32 = [P.sb([128, 129], F32, "C32_%d" % h) for h in range(2)]; t_C = [P.tok("C") for h in range(2)]
    Csb = [[P.sb([128, 130], BF16, "Csb%d%d" % (h, i)) for i in range(2)] for h in range(2)]
    t_Cs = [[P.tok("Cs") for i in range(2)] for h in range(2)]
    PTm = [P.sb([128, 128], BF16, "PTm%d" % h) for h in range(2)]; t_PTm = [P.tok("PTm") for h in range(2)]
    gig = P.sb([2, TB], F32, "gig"); gsp = P.sb([2, TB], F32, "gsp"); gB = P.sb([2, TB], F32, "gB"); gu = P.sb([2, TB], F32, "gu")
    gev = P.sb([2, TB], F32, "gev"); gfl = P.sb([2, TB], F32, "gfl"); gtmp = P.sb([2, TB], F32, "gtmp")
    gmu = P.sb([2, NCH], F32, "gmu"); gg = P.sb([2, NCH], F32, "gg"); gms = P.sb([2, NCH], F32, "gms"); gMc = P.sb([2, NCH], F32, "gMc")
    gmp = P.sb([2, NCH], F32, "gmp"); gsig = P.sb([2, NCH], F32, "gsig"); mcar = P.sb([2, 1], F32, "mcar")
    t_g = P.tok("gates")
    sigb = P.sb([128, 2, NCH], F32, "sigb"); t_sigb = P.tok("sigb")
    flo = P.sb([128, 4, 2], F32, "flo"); t_flo = P.tok("flo")
    hsc = [dict(h=P.sb([128, 128], F32), dn=P.sb([128, 1], F32), st=P.sb([128, 6], F32), mv=P.sb([128, 2], F32),
                rstd=P.sb([128, 1], F32), sg=P.sb([128, 128], F32)) for i in range(2)]
    t_hsc = [P.tok("hsc") for i in range(2)]; t_hsg = [P.tok("hsg") for i in range(2)]
    yml = P.sb([128, 256], F32, "yml"); t_yml = P.tok("yml")
    osb = [P.sb([128, D_MODEL], F32, "osb%d" % i) for i in range(2)]; t_osb = [P.tok("osb") for i in range(2)]
    t_osbh = [[P.tok("osbh") for j in range(2)] for i in range(2)]
    pp = [P.ps([128, 512], F32, "pp%d" % i) for i in range(2)]; t_pp = [P.tok("pp") for i in range(2)]
    pe_ = P.ps([128, 512], F32, "pe"); t_pe = P.tok("pe")
    pv = pe_; t_pv = t_pe
    pmisc = P.ps([128, 512], F32, "pmisc")
    pmb = P.ps([128, 1024], BF16, "pmb")
    t_pmisc = P.tok("pmisc"); t_psg = t_pmisc; t_pfl = t_pmisc; t_pkt = P.tok("pkt")
    pnum = [P.ps([128, 512], F32, "pnum%d" % h) for h in range(2)]; t_pnum = [P.tok("pnum") for h in range(2)]
    po = P.ps([128, 512], F32, "po"); t_po = P.tok("po")
    pm = [pe_, po]; t_pm = [t_pe, t_po]

    for h in range(2):
        P.op("dve", lambda e: e.memset(C32[h][:], 0.0), writes=[t_C[h]])
        P.op("pool", lambda e: e.memset(qTe[h][:], 0.0), writes=[t_q[h]])
        P.op("pool", lambda e: e.memset(qTo[h][:], 0.0), writes=[t_q[h]])
    P.op("dve", lambda e: e.memset(mcar[:], 0.0), writes=[t_g])
    P.op("pool", lambda e: e.memset(vaug[:], 1.0), writes=[t_v])
    for g in range(2):
        P.op("pool", lambda e: e.memset(ub[g][:, 0:16], 0.0), writes=[t_ub[g]])
    for c in range(4):
        P.op("pool", lambda e: e.memset(qkb[c][:, 0:3], 0.0), writes=[t_qkb[c]])

    cm = [0]; cpp = [0]; cos = [0]
    for blk in range(NB):
        r0 = blk * TB
        make_xT(P, x, r0, 4, xs, t_xs, xT, t_xT, pm, t_pm, ident, t_const, cm, t_xd)

        def proj_fm(c0, ncol):
            b = cpp[0] % 2; cpp[0] += 1
            for k in range(8):
                P.op("pe", lambda e: e.matmul(pp[b][0:ncol, :], lhsT=wAt[:, k, c0:c0 + ncol], rhs=xT[:, k, :], start=(k == 0), stop=(k == 7)),
                     reads=[t_w, t_xT], writes=[t_pp[b]])
            return b

        if stop <= 1:
            continue
        def pool_group(g):
            b = proj_fm(g * 128, 128)
            P.op("act", lambda e: e.copy(out=ub[g][:, 16:16 + TB], in_=pp[b][:, :]), reads=[t_pp[b]], writes=[t_ub[g]])
            yield
            U = ub[g]; W = 16 + TB
            P.op("dve", lambda e: e.tensor_tensor(out=s2_l[g][:, 2:W], in0=U[:, 2:W], in1=U[:, 1:W - 1], op=ALU.add), reads=[t_ub[g]], writes=[t_s_l[g]])
            yield
            P.op("dve", lambda e: e.tensor_tensor(out=s4_l[g][:, 4:W], in0=s2_l[g][:, 4:W], in1=s2_l[g][:, 2:W - 2], op=ALU.add), reads=[t_s_l[g]], writes=[t_s_l[g]])
            yield
            P.op("dve", lambda e: e.tensor_tensor(out=s8_l[g][:, 8:W], in0=s4_l[g][:, 8:W], in1=s4_l[g][:, 4:W - 4], op=ALU.add), reads=[t_s_l[g]], writes=[t_s_l[g]])
            yield
            P.op("dve", lambda e: e.tensor_tensor(out=s16_l[g][:, 16:W], in0=s8_l[g][:, 16:W], in1=s8_l[g][:, 8:W - 8], op=ALU.add), reads=[t_s_l[g]], writes=[t_s_l[g]])
            yield
            cf = pct[:, 22 + g * 4:26 + g * 4]
            lo = 16 if blk == 0 else 0
            srcs = [s2_l[g], s4_l[g], s8_l[g], s16_l[g]]
            P.op("dve", lambda e: e.scalar_tensor_tensor(out=dacc_l[g][:, lo:TB], in0=s2_l[g][:, 16 + lo:W], scalar=cf[:, 0:1], in1=U[:, 16 + lo:W],
                                                         op0=ALU.mult, op1=ALU.subtract), reads=[t_s_l[g], t_ub[g], t_const], writes=[t_d_l[g]])
            yield
            for wi in range(1, 4):
                P.op("dve", lambda e: e.scalar_tensor_tensor(out=dacc_l[g][:, lo:TB], in0=srcs[wi][:, 16 + lo:W], scalar=cf[:, wi:wi + 1],
                                                             in1=dacc_l[g][:, lo:TB], op0=ALU.mult, op1=ALU.add),
                     reads=[t_s_l[g], t_const, t_d_l[g]], writes=[t_d_l[g]])
                yield
            if blk == 0:
                c0v = c0t[:].rearrange("p (g w t) -> p g w t", g=2, w=4)
                P.op("dve", lambda e: e.tensor_tensor(out=dacc_l[g][:, 0:16], in0=s2_l[g][:, 16:32], in1=c0v[:, g, 0, :], op=ALU.mult),
                     reads=[t_s_l[g], t_const], writes=[t_d_l[g]])
                yield
                P.op("dve", lambda e: e.tensor_tensor(out=dacc_l[g][:, 0:16], in0=dacc_l[g][:, 0:16], in1=U[:, 16:32], op=ALU.subtract),
                     reads=[t_d_l[g], t_ub[g]], writes=[t_d_l[g]])
                yield
                for wi in range(1, 4):
                    P.op("dve", lambda e: e.tensor_tensor(out=s2_l[g][:, 0:16], in0=srcs[wi][:, 16:32], in1=c0v[:, g, wi, :], op=ALU.mult),
                         reads=[t_s_l[g], t_const], writes=[t_s_l[g]])
                    yield
                    P.op("dve", lambda e: e.tensor_tensor(out=dacc_l[g][:, 0:16], in0=dacc_l[g][:, 0:16], in1=s2_l[g][:, 0:16], op=ALU.add),
                         reads=[t_d_l[g], t_s_l[g]], writes=[t_d_l[g]])
                    yield
            P.op("act", lambda e: e.copy(out=dbf_l[g][:], in_=dacc_l[g][:]), reads=[t_d_l[g]], writes=[t_d_l[g]])
            yield
            P.op("pool", lambda e: e.tensor_copy(out=U[:, 0:16], in_=U[:, TB:TB + 16]), reads=[t_ub[g]], writes=[t_ub[g]])
            yield
            P.op("pe", lambda e: e.matmul(po[:, :], lhsT=pwt[:, g, :], rhs=dbf_l[g][:], start=True, stop=True),
                 reads=[t_w, t_d_l[g]], writes=[t_po])
            P.op("act", lambda e: e.activation(out=ycT[:, g, :], in_=po[:, :], func=AF.Identity, scale=pct[:, g:g + 1]),
                 reads=[t_po, t_const], writes=[t_yp])
            yield


        run_interleaved([pool_group(0), pool_group(1)])
        if stop <= 2:
            continue
        b = proj_fm(1280, 2)
        P.op("act", lambda e: e.activation(out=gig[:], in_=pp[b][0:2, :], func=AF.Identity, bias=gbt[:, 0:1], scale=1.0),
             reads=[t_pp[b], t_const], writes=[t_g])
        if stop <= 2.1:
            continue
        b = proj_fm(1282, 2)
        P.op("act", lambda e: e.activation(out=gsp[:], in_=pp[b][0:2, :], func=AF.Exp, bias=nfb[:, 0:1], scale=-1.0),
             reads=[t_pp[b], t_const], writes=[t_g])
        P.op("act", lambda e: e.activation(out=gsp[:], in_=gsp[:], func=AF.Ln, bias=1.0, scale=1.0), reads=[t_g], writes=[t_g])
        if stop <= 2.2:
            continue
        P.op("dve", lambda e: e.tensor_tensor_scan(out=gB[:], data0=rmask, data1=gsp[:], initial=0.0, op0=ALU.mult, op1=ALU.add),
             reads=[t_g, t_const], writes=[t_g])
        P.op("dve", lambda e: e.tensor_tensor(out=gu[:], in0=gig[:], in1=gB[:], op=ALU.add), reads=[t_g], writes=[t_g])
        if stop <= 2.3:
            continue
        gu3 = gu[:].rearrange("p (c s) -> p c s", s=64); gB3 = gB[:].rearrange("p (c s) -> p c s", s=64)
        P.op("dve", lambda e: e.tensor_reduce(out=gmu[:], in_=gu3, axis=mybir.AxisListType.X, op=ALU.max), reads=[t_g], writes=[t_g])
        P.op("dve", lambda e: e.tensor_scalar(out=gg[:], in0=gB3[:, :, 63], scalar1=-1.0, scalar2=None, op0=ALU.mult), reads=[t_g], writes=[t_g])
        if stop <= 2.4:
            continue
        P.op("dve", lambda e: e.tensor_tensor_scan(out=gms[:], data0=gmu[:], data1=gg[:], initial=mcar[:, 0:1], op0=ALU.max, op1=ALU.add),
             reads=[t_g], writes=[t_g])
        P.op("dve", lambda e: e.tensor_tensor(out=gMc[:], in0=gms[:], in1=gg[:], op=ALU.subtract), reads=[t_g], writes=[t_g])
        P.op("dve", lambda e: e.tensor_copy(out=gmp[:, 0:1], in_=mcar[:, 0:1]), reads=[t_g], writes=[t_g])
        P.op("dve", lambda e: e.tensor_copy(out=gmp[:, 1:NCH], in_=gms[:, 0:NCH - 1]), reads=[t_g], writes=[t_g])
        P.op("dve", lambda e: e.tensor_copy(out=mcar[:, 0:1], in_=gms[:, NCH - 1:NCH]), reads=[t_g], writes=[t_g])
        P.op("dve", lambda e: e.tensor_tensor(out=gsig[:], in0=gmp[:], in1=gMc[:], op=ALU.subtract), reads=[t_g], writes=[t_g])
        P.op("act", lambda e: e.activation(out=gsig[:], in_=gsig[:], func=AF.Exp), reads=[t_g], writes=[t_g])
        if stop <= 2.5:
            continue
        Mb = gMc[:].unsqueeze(2).to_broadcast([2, NCH, 64])
        P.op("dve", lambda e: e.tensor_tensor(out=gtmp[:].rearrange("p (c s) -> p c s", s=64), in0=gu3, in1=Mb, op=ALU.subtract),
             reads=[t_g], writes=[t_g])
        P.op("act", lambda e: e.activation(out=gev[:], in_=gtmp[:], func=AF.Exp), reads=[t_g], writes=[t_g])
        P.op("dve", lambda e: e.tensor_tensor(out=gtmp[:].rearrange("p (c s) -> p c s", s=64), in0=gB3, in1=Mb, op=ALU.subtract),
             reads=[t_g], writes=[t_g])
        P.op("act", lambda e: e.activation(out=gfl[:], in_=gtmp[:], func=AF.Exp), reads=[t_g], writes=[t_g])
        if stop <= 2.6:
            continue
        for h in range(2):
            P.op("pe", lambda e: e.matmul(pmisc[:, 300 + h * NCH:300 + (h + 1) * NCH], lhsT=cs2[:, 512 + h * 128:512 + (h + 1) * 128],
                                          rhs=gsig[:], start=True, stop=True), reads=[t_g, t_const], writes=[t_psg])
        P.op("act", lambda e: e.copy(out=sigb[:].rearrange("p h c -> p (h c)"), in_=pmisc[:, 300:300 + 2 * NCH]),
             reads=[t_psg], writes=[t_sigb])
        if stop <= 2.7:
            continue
        for tt in range(4):
            P.op("pe", lambda e: e.matmul(pmisc[:, 320 + 8 * tt:328 + 8 * tt], lhsT=gfl[:, tt * 128:(tt + 1) * 128], rhs=id2, start=True, stop=True),
                 reads=[t_g, t_const], writes=[t_pfl])
        if stop <= 2.8:
            continue
        P.op("act", lambda e: e.copy(out=flo[:], in_=pmisc[:, 320:352].rearrange("p (t j) -> p t j", j=8)[:, :, 0:2]), reads=[t_pfl], writes=[t_flo])

        if stop <= 3:
            continue
        def qk_chunk(c):
            b = proj_fm(256 + c * 128, 128)
            Q = qkb[c]; cacc = cacc_l[c % 2]; t_cacc = t_cacc_l[c % 2]
            P.op("act", lambda e: e.copy(out=Q[:, 3:3 + TB], in_=pp[b][:, :]), reads=[t_pp[b]], writes=[t_qkb[c]])
            yield
            cw = pct[:, 2 + c * 4:6 + c * 4]
            P.op("dve", lambda e: e.tensor_scalar(out=cacc[:], in0=Q[:, 0:TB], scalar1=cw[:, 0:1], scalar2=pct[:, 18 + c:19 + c],
                                                  op0=ALU.mult, op1=ALU.add), reads=[t_qkb[c], t_const], writes=[t_cacc])
            yield
            for j in range(1, 4):
                P.op("dve", lambda e: e.scalar_tensor_tensor(out=cacc[:], in0=Q[:, j:j + TB], scalar=cw[:, j:j + 1], in1=cacc[:],
                                                             op0=ALU.mult, op1=ALU.add), reads=[t_qkb[c], t_const, t_cacc], writes=[t_cacc])
                yield
            P.op("pool", lambda e: e.tensor_copy(out=Q[:, 0:3], in_=Q[:, TB:TB + 3]), reads=[t_qkb[c]], writes=[t_qkb[c]])
            yield
            if c < 2:
                h = c
                ca3 = cacc[:].rearrange("p (c two s) -> p c two s", two=2, s=64)
                P.op("act", lambda e: e.activation(out=qTe[h][:].rearrange("p (c two s) -> p c two s", two=2, s=64)[:, :, 0, :],
                                                   in_=ca3[:, :, 0, :], func=AF.Silu), reads=[t_cacc], writes=[t_q[h]])
                yield
                P.op("act", lambda e: e.activation(out=qTo[h][:].rearrange("p (c two s) -> p c two s", two=2, s=64)[:, :, 1, :],
                                                   in_=ca3[:, :, 1, :], func=AF.Silu), reads=[t_cacc], writes=[t_q[h]])
                yield
            else:
                h = c - 2
                P.op("act", lambda e: e.activation(out=ksil_l[h][:], in_=cacc[:], func=AF.Silu), reads=[t_cacc], writes=[t_ksil_l[h]])
                yield
                P.op("pe", lambda e: e.matmul(pe_[:, :], lhsT=cs2[:, 512 + h * 128:512 + (h + 1) * 128], rhs=gev[:], start=True, stop=True),
                     reads=[t_g, t_const], writes=[t_pe])
                P.op("dve", lambda e: e.scalar_tensor_tensor(out=kT[h][:], in0=ksil_l[h][:], scalar=128.0 ** -0.5, in1=pe_[:, :],
                                                             op0=ALU.mult, op1=ALU.mult), reads=[t_ksil_l[h], t_pe], writes=[t_kT[h]])
                yield
                for tt in range(4):
                    P.op("pe", lambda e: e.transpose(out=pmb[:, tt * 128:(tt + 1) * 128], in_=kT[h][:, tt * 128:(tt + 1) * 128],
                                                     identity=identb[:]), reads=[t_kT[h], t_const], writes=[t_pkt])
                P.op("act", lambda e: e.copy(out=ktok[h][:].rearrange("p t d -> p (t d)"), in_=pmb[:, 0:512]), reads=[t_pkt], writes=[t_ktok[h]])
                yield


        run_interleaved([qk_chunk(0), qk_chunk(1)])
        run_interleaved([qk_chunk(2), qk_chunk(3)])
        if stop <= 4:
            continue
        for tt in range(4):
            for k in range(8):
                P.op("pe", lambda e: e.matmul(pv[:, :], lhsT=xT[:, k, tt * 128:(tt + 1) * 128], rhs=wAt[:, k, 768:1280], start=(k == 0), stop=(k == 7)),
                     reads=[t_w, t_xT], writes=[t_pv])
            P.op("act", lambda e: e.copy(out=vaug[:, tt, :, 0:128], in_=pv[:, 0:256].rearrange("p (h d) -> p h d", h=2)),
                 reads=[t_pv], writes=[t_v])
            P.op("act", lambda e: e.activation(out=ogs[:, tt, :], in_=pv[:, 256:512], func=AF.Sigmoid), reads=[t_pv], writes=[t_og])

        if stop <= 5:
            continue
        pu = [pe_, po]; t_pu = [t_pe, t_po]
        for tt in range(4):
            tsl = slice(tt * 128, (tt + 1) * 128)
            H = range(2)
            for h in H:
                P.op("pe", lambda e: e.matmul(pp[h][:, 0:128], lhsT=kT[h][:, tsl], rhs=qTe[h][:, tsl], start=True, stop=False),
                     reads=[t_kT[h], t_q[h]], writes=[t_pp[h]])
                P.op("pe", lambda e: e.matmul(pp[h][:, 0:128], lhsT=kT[h][:, tsl], rhs=qTo[h][:, tsl], start=False, stop=True),
                     reads=[t_kT[h], t_q[h]], writes=[t_pp[h]])
            for h in H:
                P.op("dve", lambda e: e.tensor_tensor(out=PTm[h][:], in0=pp[h][:, 0:128], in1=maskT, op=ALU.mult),
                     reads=[t_pp[h], t_const], writes=[t_PTm[h]])
            for h in H:
                P.op("pe", lambda e: e.matmul(pnum[h][:, 0:129], lhsT=PTm[h][:], rhs=vaug[:, tt, h, 0:129], start=True, stop=False),
                     reads=[t_PTm[h], t_v], writes=[t_pnum[h]])
            for ci in range(2):
                c = tt * 2 + ci
                for h in H:
                    P.op("act", lambda e: e.activation(out=Csb[h][ci][:, 0:129], in_=C32[h][:], func=AF.Identity, scale=sigb[:, h, c:c + 1]),
                         reads=[t_C[h], t_sigb], writes=[t_Cs[h][ci]])
                for h in H:
                    qsrc = qTe[h] if ci == 0 else qTo[h]
                    P.op("pe", lambda e: e.matmul(pnum[h][:, 0:129], lhsT=qsrc[:, tsl], rhs=Csb[h][ci][:, 0:129], start=False, stop=(ci == 1)),
                         reads=[t_q[h], t_Cs[h][ci]], writes=[t_pnum[h]])
                    P.op("pe", lambda e: e.matmul(pu[h][:, 0:129], lhsT=ktok[h][ci * 64:(ci + 1) * 64, tt, :],
                                                  rhs=vaug[ci * 64:(ci + 1) * 64, tt, h, 0:129], start=True, stop=True),
                         reads=[t_ktok[h], t_v], writes=[t_pu[h]])
                for h in H:
                    P.op("dve", lambda e: e.scalar_tensor_tensor(out=C32[h][:], in0=C32[h][:], scalar=sigb[:, h, c:c + 1], in1=pu[h][:, 0:129],
                                                                 op0=ALU.mult, op1=ALU.add), reads=[t_C[h], t_sigb, t_pu[h]], writes=[t_C[h]])
            for h in H:
                S = hsc[h]; tS = t_hsc[h]
                P.op("act", lambda e: e.activation(out=S["dn"][:], in_=pnum[h][:, 128:129], func=AF.Abs), reads=[t_pnum[h]], writes=[tS])
            for h in H:
                S = hsc[h]; tS = t_hsc[h]
                P.op("dve", lambda e: e.tensor_scalar(out=S["dn"][:], in0=S["dn"][:], scalar1=flo[:, tt, h:h + 1], scalar2=None,
                                                      op0=ALU.max), reads=[tS, t_flo], writes=[tS])
            for h in H:
                S = hsc[h]; tS = t_hsc[h]
                P.op("dve", lambda e: e.reciprocal(out=S["dn"][:], in_=S["dn"][:]), reads=[tS], writes=[tS])
            for h in H:
                S = hsc[h]; tS = t_hsc[h]
                P.op("dve", lambda e: e.tensor_scalar(out=S["h"][:], in0=pnum[h][:, 0:128], scalar1=S["dn"][:, 0:1], scalar2=None, op0=ALU.mult),
                     reads=[t_pnum[h], tS], writes=[tS])
            for h in H:
                S = hsc[h]; tS = t_hsc[h]
                P.op("dve", lambda e: e.bn_stats(out=S["st"][:], in_=S["h"][:]), reads=[tS], writes=[tS])
            for h in H:
                S = hsc[h]; tS = t_hsc[h]
                P.op("dve", lambda e: e.bn_aggr(out=S["mv"][:], in_=S["st"][:]), reads=[tS], writes=[tS])
            for h in H:
                S = hsc[h]; tS = t_hsc[h]
                P.op("act", lambda e: e.activation(out=S["rstd"][:], in_=S["mv"][:, 1:2], func=AF.Sqrt, bias=eps_t[:, 0:1], scale=1.0),
                     reads=[tS, t_const], writes=[tS])
            for h in H:
                S = hsc[h]; tS = t_hsc[h]
                P.op("dve", lambda e: e.reciprocal(out=S["rstd"][:], in_=S["rstd"][:]), reads=[tS], writes=[tS])
            for h in H:
                S = hsc[h]; tS = t_hsc[h]
                P.op("dve", lambda e: e.tensor_scalar(out=S["h"][:], in0=S["h"][:], scalar1=S["mv"][:, 0:1], scalar2=S["rstd"][:, 0:1],
                                                      op0=ALU.subtract, op1=ALU.mult), reads=[tS], writes=[tS])
                P.op("pool", lambda e: e.tensor_tensor(out=S["sg"][:], in0=ogs[:, tt, h * 128:(h + 1) * 128], in1=mlt[:, h * 128:(h + 1) * 128],
                                                       op=ALU.mult), reads=[t_og, t_const], writes=[t_hsg[h]])
            for h in H:
                S = hsc[h]; tS = t_hsc[h]
                P.op("dve", lambda e: e.tensor_tensor(out=yml[:, h * 128:(h + 1) * 128], in0=S["h"][:], in1=S["sg"][:], op=ALU.mult),
                     reads=[tS, t_hsg[h]], writes=[t_yml])
            for h in range(2):
                P.op("pe", lambda e: e.transpose(out=pe_[:, 0:128], in_=yml[:, h * 128:(h + 1) * 128], identity=ident),
                     reads=[t_yml, t_const], writes=[t_pe])
                P.op("act", lambda e: e.copy(out=ycT[:, 2 + h, tsl], in_=pe_[:, 0:128]), reads=[t_pe], writes=[t_ym])

        if stop <= 6:
            continue
        for tt in range(4):
            s = cos[0] % 2; cos[0] += 1
            for hf in range(2):
                for kc in range(4):
                    P.op("pe", lambda e: e.matmul(pp[hf][:, :], lhsT=ycT[:, kc, tt * 128:(tt + 1) * 128], rhs=wot[:, kc, hf * 512:(hf + 1) * 512],
                                                  start=(kc == 0), stop=(kc == 3)), reads=[t_yp, t_ym, t_w], writes=[t_pp[hf]])
                P.op("act" if hf == 0 else "dve", lambda e: (e.copy if hf == 0 else e.tensor_copy)(out=osb[s][:, hf * 512:(hf + 1) * 512], in_=pp[hf][:, :]),
                     reads=[t_pp[hf]], writes=[t_osbh[s][hf]])
            P.dma("sp", out[r0 + tt * 128:r0 + (tt + 1) * 128, :], osb[s][:], reads=[t_osbh[s][0], t_osbh[s][1]], writes=[t_outd])


def even_inputs(x, w_in, pool_w, pool_scale, conv_w, conv_b, i_bias, f_bias, ml_norm, w_out, hp):
    f = np.float32
    h0 = 2 * hp
    u = w_in[:, 0:512][:, h0 * 128:(h0 + 2) * 128]
    q = w_in[:, 512:1024][:, h0 * 128:(h0 + 2) * 128]
    k = w_in[:, 1024:1536][:, h0 * 128:(h0 + 2) * 128]
    v = w_in[:, 1536:2048][:, h0 * 128:(h0 + 2) * 128]
    og = w_in[:, 2048:2560][:, h0 * 128:(h0 + 2) * 128]
    ig = w_in[:, 2560:2564][:, h0:h0 + 2]
    fg = w_in[:, 2564:2568][:, h0:h0 + 2]
    wA = np.ascontiguousarray(np.concatenate([u, q, k, v, og, ig, fg], axis=1), dtype=f)
    wo = np.ascontiguousarray(np.concatenate([w_out[h0 * 128:(h0 + 2) * 128], w_out[512 + h0 * 128:512 + (h0 + 2) * 128]], axis=0), dtype=f)
    pw = np.ascontiguousarray(pool_w[h0:h0 + 2], dtype=f)
    pc = np.zeros((128, 30), f)
    for g in range(2):
        pc[:, g] = pool_scale[(h0 + g) * 128:(h0 + g + 1) * 128]
    cw = np.concatenate([conv_w[:, 0:512][:, h0 * 128:(h0 + 2) * 128], conv_w[:, 512:1024][:, h0 * 128:(h0 + 2) * 128]], axis=1)
    cb = np.concatenate([conv_b[0:512][h0 * 128:(h0 + 2) * 128], conv_b[512:1024][h0 * 128:(h0 + 2) * 128]])
    for c in range(4):
        for j in range(4):
            pc[:, 2 + c * 4 + j] = cw[j, c * 128:(c + 1) * 128]
        pc[:, 18 + c] = cb[c * 128:(c + 1) * 128]
    coef0 = np.zeros((128, 2, 4, 16), f)
    for g in range(2):
        wi = h0 + g
        pc[:, 22 + g * 4 + wi] = 1.0 / (2 ** (wi + 1))
        coef0[:, g, wi, :] = 1.0 / np.minimum(np.arange(1, 17), 2 ** (wi + 1))
    gbias = np.stack([i_bias[h0:h0 + 2], f_bias[h0:h0 + 2]], axis=1).astype(f)
    mln = np.ascontiguousarray(ml_norm[h0 * 128:(h0 + 2) * 128].reshape(1, 256), dtype=f)
    cst = np.zeros((128, 256), f)
    cst[:, 0:128] = np.eye(128)
    s_i = np.arange(128)[:, None]; t_i = np.arange(128)[None, :]
    cst[:, 128:256] = ((s_i // 64 == t_i // 64) & (s_i <= t_i)).astype(f)
    cst2 = np.zeros((2, 776), f)
    cst2[:, 0:512] = (np.arange(512) % 64 != 0).astype(f)[None, :]
    cst2[0, 512:640] = 1.0
    cst2[1, 640:768] = 1.0
    cst2[:, 768:770] = np.eye(2)
    return dict(x=np.ascontiguousarray(x, dtype=f), wA=wA, wo=wo, pw=pw, pc=pc, coef0=coef0.reshape(128, 128), gbias=gbias, mln=mln,
                cst=cst, cst2=cst2)


def build_odd(T):
    P = Prog()
    nc = P.nc
    io = odd_io(nc, "", T)
    io["t_xd"] = P.tok("xd"); io["t_outd"] = P.tok("outd")
    emit_odd(P, T, io)
    P.wait_all("sp", [io["t_outd"]])
    P.close()
    return nc


def odd_io(nc, pfx, T, x=None, out=None):
    io = {}
    io["x"] = x if x is not None else nc.dram_tensor(pfx + "x", [T, D_MODEL], F32, kind="ExternalInput").ap()
    io["wA"] = nc.dram_tensor(pfx + "wA", [D_MODEL, 1552], F32, kind="ExternalInput").ap()
    io["wo"] = nc.dram_tensor(pfx + "wo", [512, D_MODEL], F32, kind="ExternalInput").ap()
    io["w2"] = nc.dram_tensor(pfx + "w2", [16, 256], F32, kind="ExternalInput").ap()
    io["pc"] = nc.dram_tensor(pfx + "pc", [128, 2], F32, kind="ExternalInput").ap()
    io["gln"] = nc.dram_tensor(pfx + "gln", [1, 512], F32, kind="ExternalInput").ap()
    io["cst"] = nc.dram_tensor(pfx + "cst", [128, 256 + 512], F32, kind="ExternalInput").ap()
    io["out"] = out if out is not None else nc.dram_tensor(pfx + "out", [T, D_MODEL], F32, kind="ExternalOutput").ap()
    return io


def emit_odd(P, T, io):
    TB = 512
    NB = T // TB
    NCH = TB // 64
    nc = P.nc
    x, wA, wo, w2, pc, gln, cst, out = (io[k] for k in ("x", "wA", "wo", "w2", "pc", "gln", "cst", "out"))
    t_xd = io["t_xd"]; t_outd = io["t_outd"]

    t_const = P.tok("const")
    cs = P.sb([128, 768], F32, "cst"); ident = cs[:, 0:128]; maskT = cs[:, 128:256]; rmask = cs[:, 256:768]
    pct = P.sb([128, 2], F32, "pc"); npct = P.sb([128, 2], F32, "npc")
    glt = P.sb([128, 512], F32, "gln"); eps_t = P.sb([128, 1], F32, "eps")
    wAt = P.sb([128, 8, 1552], BF16, "wA"); wot = P.sb([128, 4, D_MODEL], BF16, "wo"); w2t = P.sb([16, 256], BF16, "w2")
    identb = P.sb([128, 128], BF16, "identb")
    P.dma("sp", cs[:], cst[:, :], writes=[t_const])
    P.dma("sp", pct[:], pc[:, :], writes=[t_const])
    P.dma("sp", glt[:], gln[0:1, :].partition_broadcast(128), writes=[t_const])
    t_w = P.tok("w")
    wv = wA.rearrange("(k p) n -> p k n", p=128)
    for k in range(8):
        P.dma("pool", wAt[:, k, :], wv[:, k, :], writes=[t_w])
    P.dma("pool", wot[:], wo.rearrange("(k p) n -> p k n", p=128), writes=[t_w])
    P.dma("pool", w2t[:], w2[:, :], writes=[t_w])
    P.op("dve", lambda e: e.memset(eps_t[:], LN_EPS), writes=[t_const])
    P.op("dve", lambda e: e.tensor_scalar(out=npct[:], in0=pct[:], scalar1=-1.0, scalar2=None, op0=ALU.mult), reads=[t_const], writes=[t_const])
    P.op("act", lambda e: e.copy(out=identb[:], in_=ident), reads=[t_const], writes=[t_const])

    xs = [P.sb([128, D_MODEL], F32, "xs%d" % i) for i in range(2)]; t_xs = [P.tok("xs") for i in range(2)]
    xT = P.sb([128, 8, TB], BF16, "xT"); t_xT = P.tok("xT")
    glrT = P.sb([16, TB], BF16, "glrT"); t_glr = P.tok("glr")
    qf_l = [P.sb([128, TB], F32, "qf%d" % i) for i in range(2)]; kf_l = [P.sb([128, TB], F32, "kf%d" % i) for i in range(2)]
    t_qk_l = [P.tok("qkf") for i in range(2)]
    sp_l = [P.sb([128, TB], F32, "sp%d" % i) for i in range(2)]; Bp_l = [P.sb([128, TB], F32, "Bp%d" % i) for i in range(2)]
    ex_l = [P.sb([128, TB], F32, "ex%d" % i) for i in range(2)]; rc_l = [P.sb([128, TB], F32, "rc%d" % i) for i in range(2)]
    t_dec_l = [P.tok("dec") for i in range(2)]
    eBl = [P.sb([128, NCH], F32, "eBl%d" % h) for h in range(2)]; t_eBl = [P.tok("eBl") for h in range(2)]
    qTe = [P.sb([128, TB], BF16, "qTe%d" % h) for h in range(2)]; qTo = [P.sb([128, TB], BF16, "qTo%d" % h) for h in range(2)]
    t_q = [P.tok("q") for h in range(2)]
    kT = [P.sb([128, TB], BF16, "kT%d" % h) for h in range(2)]; t_kT = [P.tok("kT") for h in range(2)]
    khT_l = [P.sb([128, TB], BF16, "khT%d" % i) for i in range(2)]; t_khT_l = [P.tok("khT") for i in range(2)]
    ktok = [P.sb([128, 4, 128], BF16, "ktok%d" % h) for h in range(2)]; t_ktok = [P.tok("ktok") for h in range(2)]
    vbf = P.sb([128, 4, 512], BF16, "vbf"); t_v = P.tok("v")
    rs = P.sb([128, 4, 512], F32, "rs"); t_r = P.tok("r")
    S32 = [P.sb([128, 256], F32, "S32_%d" % h) for h in range(2)]; t_S = [P.tok("S") for h in range(2)]
    Sb = [[P.sb([128, 256], BF16, "Sb%d%d" % (h, i)) for i in range(2)] for h in range(2)]
    t_Sb = [[P.tok("Sb") for i in range(2)] for h in range(2)]
    ATm = [P.sb([128, 128], BF16, "ATm%d" % h) for h in range(2)]; t_AT = [P.tok("AT") for h in range(2)]
    hsc = [dict(h=P.sb([128, 256], F32), st=P.sb([128, 6], F32), mv=P.sb([128, 2], F32), rstd=P.sb([128, 1], F32),
                sg=P.sb([128, 256], F32)) for i in range(2)]
    t_hsc = [P.tok("hsc") for i in range(2)]; t_hsg = [P.tok("hsg") for i in range(2)]
    yml = P.sb([128, 512], F32, "yml"); t_yml = P.tok("yml")
    ycT = P.sb([128, 4, TB], BF16, "ycT"); t_ym = P.tok("ym")
    osb = [P.sb([128, D_MODEL], F32, "osb%d" % i) for i in range(2)]; t_osb = [P.tok("osb") for i in range(2)]
    t_osbh = [[P.tok("osbh") for j in range(2)] for i in range(2)]
    pp = [P.ps([128, 512], F32, "pp%d" % i) for i in range(2)]; t_pp = [P.tok("pp") for i in range(2)]
    pe_ = P.ps([128, 512], F32, "pe"); t_pe = P.tok("pe")
    pmb = P.ps([128, 1024], BF16, "pmb"); t_pkt = P.tok("pkt")
    pnum = [P.ps([128, 512], F32, "pnum%d" % h) for h in range(2)]; t_pnum = [P.tok("pnum") for h in range(2)]
    po = P.ps([128, 512], F32, "po"); t_po = P.tok("po")
    pm = [pe_, po]; t_pm = [t_pe, t_po]

    for h in range(2):
        P.op("dve", lambda e: e.memset(S32[h][:], 0.0), writes=[t_S[h]])
        P.op("pool", lambda e: e.memset(Sb[h][0][:], 0.0), writes=[t_Sb[h][0]])
        P.op("pool", lambda e: e.memset(qTe[h][:], 0.0), writes=[t_q[h]])
        P.op("pool", lambda e: e.memset(qTo[h][:], 0.0), writes=[t_q[h]])

    cm = [0]; cpp = [0]; cos = [0]
    for blk in range(NB):
        r0 = blk * TB
        make_xT(P, x, r0, 4, xs, t_xs, xT, t_xT, pm, t_pm, ident, t_const, cm, t_xd)

        def proj_fm(c0, ncol):
            b = cpp[0] % 2; cpp[0] += 1
            for k in range(8):
                P.op("pe", lambda e: e.matmul(pp[b][0:ncol, :], lhsT=wAt[:, k, c0:c0 + ncol], rhs=xT[:, k, :], start=(k == 0), stop=(k == 7)),
                     reads=[t_w, t_xT], writes=[t_pp[b]])
            return b

        b = proj_fm(1536, 16)
        P.op("act", lambda e: e.copy(out=glrT[:], in_=pp[b][0:16, :]), reads=[t_pp[b]], writes=[t_glr])
        def head_front(h):
            bq = proj_fm(h * 128, 128)
            P.op("act", lambda e: e.copy(out=qf_l[h][:], in_=pp[bq][:, :]), reads=[t_pp[bq]], writes=[t_qk_l[h]])
            yield
            bk = proj_fm(256 + h * 128, 128)
            P.op("act", lambda e: e.copy(out=kf_l[h][:], in_=pp[bk][:, :]), reads=[t_pp[bk]], writes=[t_qk_l[h]])
            yield
            b = cpp[0] % 2; cpp[0] += 1
            P.op("pe", lambda e: e.matmul(pp[b][:, :], lhsT=w2t[:, h * 128:(h + 1) * 128], rhs=glrT[:], start=True, stop=True),
                 reads=[t_w, t_glr], writes=[t_pp[b]])
            P.op("act", lambda e: e.activation(out=sp_l[h][:], in_=pp[b][:, :], func=AF.Exp, bias=npct[:, h:h + 1], scale=-1.0),
                 reads=[t_pp[b], t_const], writes=[t_dec_l[h]])
            yield
            P.op("act", lambda e: e.activation(out=sp_l[h][:], in_=sp_l[h][:], func=AF.Ln, bias=1.0, scale=1.0), reads=[t_dec_l[h]], writes=[t_dec_l[h]])
            yield
            P.op("dve", lambda e: e.tensor_tensor_scan(out=Bp_l[h][:], data0=rmask, data1=sp_l[h][:], initial=0.0, op0=ALU.mult, op1=ALU.add),
                 reads=[t_dec_l[h], t_const], writes=[t_dec_l[h]])
            yield
            Bp3 = Bp_l[h][:].rearrange("p (c s) -> p c s", s=64)
            P.op("act", lambda e: e.activation(out=ex_l[h][:], in_=Bp_l[h][:], func=AF.Exp, scale=-1.0 / 16.0), reads=[t_dec_l[h]], writes=[t_dec_l[h]])
            yield
            q3 = qf_l[h][:].rearrange("p (c two s) -> p c two s", two=2, s=64); e3 = ex_l[h][:].rearrange("p (c two s) -> p c two s", two=2, s=64)
            P.op("dve", lambda e: e.scalar_tensor_tensor(out=qTe[h][:].rearrange("p (c two s) -> p c two s", two=2, s=64)[:, :, 0, :],
                                                         in0=q3[:, :, 0, :], scalar=128.0 ** -0.5, in1=e3[:, :, 0, :], op0=ALU.mult, op1=ALU.mult),
                 reads=[t_qk_l[h], t_dec_l[h]], writes=[t_q[h]])
            yield
            P.op("dve", lambda e: e.scalar_tensor_tensor(out=qTo[h][:].rearrange("p (c two s) -> p c two s", two=2, s=64)[:, :, 1, :],
                                                         in0=q3[:, :, 1, :], scalar=128.0 ** -0.5, in1=e3[:, :, 1, :], op0=ALU.mult, op1=ALU.mult),
                 reads=[t_qk_l[h], t_dec_l[h]], writes=[t_q[h]])
            yield
            P.op("act", lambda e: e.activation(out=ex_l[h][:], in_=Bp_l[h][:], func=AF.Exp, scale=1.0 / 16.0), reads=[t_dec_l[h], t_q[h]], writes=[t_dec_l[h]])
            yield
            P.op("dve", lambda e: e.tensor_tensor(out=kT[h][:], in0=kf_l[h][:], in1=ex_l[h][:], op=ALU.mult), reads=[t_qk_l[h], t_dec_l[h]], writes=[t_kT[h]])
            yield
            P.op("dve", lambda e: e.tensor_tensor(out=rc_l[h][:].rearrange("p (c s) -> p c s", s=64), in0=Bp3,
                                                  in1=Bp3[:, :, 63:64].to_broadcast([128, NCH, 64]), op=ALU.subtract), reads=[t_dec_l[h]], writes=[t_dec_l[h]])
            yield
            P.op("act", lambda e: e.activation(out=ex_l[h][:], in_=rc_l[h][:], func=AF.Exp, scale=1.0 / 16.0), reads=[t_dec_l[h], t_kT[h]], writes=[t_dec_l[h]])
            yield
            P.op("dve", lambda e: e.tensor_tensor(out=khT_l[h][:], in0=kf_l[h][:], in1=ex_l[h][:], op=ALU.mult), reads=[t_qk_l[h], t_dec_l[h]], writes=[t_khT_l[h]])
            yield
            P.op("act", lambda e: e.activation(out=eBl[h][:], in_=Bp3[:, :, 63], func=AF.Exp, scale=-1.0 / 16.0), reads=[t_dec_l[h]], writes=[t_eBl[h]])
            yield
            for tt in range(4):
                P.op("pe", lambda e: e.transpose(out=pmb[:, tt * 128:(tt + 1) * 128], in_=khT_l[h][:, tt * 128:(tt + 1) * 128], identity=identb[:]),
                     reads=[t_khT_l[h], t_const], writes=[t_pkt])
            P.op("act", lambda e: e.copy(out=ktok[h][:].rearrange("p t d -> p (t d)"), in_=pmb[:, 0:512]), reads=[t_pkt], writes=[t_ktok[h]])
            yield


        run_interleaved([head_front(0), head_front(1)])
        for tt in range(4):
            for k in range(8):
                P.op("pe", lambda e: e.matmul(pe_[:, :], lhsT=xT[:, k, tt * 128:(tt + 1) * 128], rhs=wAt[:, k, 512:1024], start=(k == 0), stop=(k == 7)),
                     reads=[t_w, t_xT], writes=[t_pe])
            P.op("act", lambda e: e.copy(out=vbf[:, tt, :], in_=pe_[:, :]), reads=[t_pe], writes=[t_v])
            for k in range(8):
                P.op("pe", lambda e: e.matmul(po[:, :], lhsT=xT[:, k, tt * 128:(tt + 1) * 128], rhs=wAt[:, k, 1024:1536], start=(k == 0), stop=(k == 7)),
                     reads=[t_w, t_xT], writes=[t_po])
            P.op("act", lambda e: e.activation(out=rs[:, tt, :], in_=po[:, :], func=AF.Silu), reads=[t_po], writes=[t_r])

        pu = [pe_, po]; t_pu = [t_pe, t_po]
        for tt in range(4):
            tsl = slice(tt * 128, (tt + 1) * 128)
            H = range(2)
            for h in H:
                P.op("pe", lambda e: e.matmul(pp[h][:, 0:128], lhsT=kT[h][:, tsl], rhs=qTe[h][:, tsl], start=True, stop=False),
                     reads=[t_kT[h], t_q[h]], writes=[t_pp[h]])
                P.op("pe", lambda e: e.matmul(pp[h][:, 0:128], lhsT=kT[h][:, tsl], rhs=qTo[h][:, tsl], start=False, stop=True),
                     reads=[t_kT[h], t_q[h]], writes=[t_pp[h]])
            for h in H:
                P.op("dve", lambda e: e.tensor_tensor(out=ATm[h][:], in0=pp[h][:, 0:128], in1=maskT, op=ALU.mult),
                     reads=[t_pp[h], t_const], writes=[t_AT[h]])
            for h in H:
                P.op("pe", lambda e: e.matmul(pnum[h][:, 0:256], lhsT=ATm[h][:], rhs=vbf[:, tt, h * 256:(h + 1) * 256], start=True, stop=False),
                     reads=[t_AT[h], t_v], writes=[t_pnum[h]])
            for ci in range(2):
                c = tt * 2 + ci
                for h in H:
                    qsrc = qTe[h] if ci == 0 else qTo[h]
                    P.op("pe", lambda e: e.matmul(pnum[h][:, 0:256], lhsT=qsrc[:, tsl], rhs=Sb[h][ci][:], start=False, stop=(ci == 1)),
                         reads=[t_q[h], t_Sb[h][ci]], writes=[t_pnum[h]])
                    P.op("pe", lambda e: e.matmul(pu[h][:, 0:256], lhsT=ktok[h][ci * 64:(ci + 1) * 64, tt, :],
                                                  rhs=vbf[ci * 64:(ci + 1) * 64, tt, h * 256:(h + 1) * 256], start=True, stop=True),
                         reads=[t_ktok[h], t_v], writes=[t_pu[h]])
                for h in H:
                    P.op("dve", lambda e: e.scalar_tensor_tensor(out=S32[h][:], in0=S32[h][:], scalar=eBl[h][:, c:c + 1], in1=pu[h][:, 0:256],
                                                                 op0=ALU.mult, op1=ALU.add), reads=[t_S[h], t_eBl[h], t_pu[h]], writes=[t_S[h]])
                for h in H:
                    P.op("act", lambda e: e.copy(out=Sb[h][1 - ci][:], in_=S32[h][:]), reads=[t_S[h]], writes=[t_Sb[h][1 - ci]])
            for h in H:
                S = hsc[h]; tS = t_hsc[h]
                P.op("dve", lambda e: e.bn_stats(out=S["st"][:], in_=pnum[h][:, 0:256]), reads=[t_pnum[h]], writes=[tS])
            for h in H:
                S = hsc[h]; tS = t_hsc[h]
                P.op("dve", lambda e: e.bn_aggr(out=S["mv"][:], in_=S["st"][:]), reads=[tS], writes=[tS])
            for h in H:
                S = hsc[h]; tS = t_hsc[h]
                P.op("act", lambda e: e.activation(out=S["rstd"][:], in_=S["mv"][:, 1:2], func=AF.Sqrt, bias=eps_t[:, 0:1], scale=1.0),
                     reads=[tS, t_const], writes=[tS])
            for h in H:
                S = hsc[h]; tS = t_hsc[h]
                P.op("dve", lambda e: e.reciprocal(out=S["rstd"][:], in_=S["rstd"][:]), reads=[tS], writes=[tS])
            for h in H:
                S = hsc[h]; tS = t_hsc[h]
                P.op("dve", lambda e: e.tensor_scalar(out=S["h"][:], in0=pnum[h][:, 0:256], scalar1=S["mv"][:, 0:1], scalar2=S["rstd"][:, 0:1],
                                                      op0=ALU.subtract, op1=ALU.mult), reads=[t_pnum[h], tS], writes=[tS])
                P.op("pool", lambda e: e.tensor_tensor(out=S["sg"][:], in0=rs[:, tt, h * 256:(h + 1) * 256], in1=glt[:, h * 256:(h + 1) * 256],
                                                       op=ALU.mult), reads=[t_r, t_const], writes=[t_hsg[h]])
            for h in H:
                S = hsc[h]; tS = t_hsc[h]
                P.op("dve", lambda e: e.tensor_tensor(out=yml[:, h * 256:(h + 1) * 256], in0=S["h"][:], in1=S["sg"][:], op=ALU.mult),
                     reads=[tS, t_hsg[h]], writes=[t_yml])
            for kc in range(4):
                P.op("pe", lambda e: e.transpose(out=pe_[:, kc * 128:(kc + 1) * 128], in_=yml[:, kc * 128:(kc + 1) * 128], identity=ident),
                     reads=[t_yml, t_const], writes=[t_pe])
            P.op("act", lambda e: e.copy(out=ycT[:, :, tsl], in_=pe_[:, :].rearrange("p (k t) -> p k t", k=4)), reads=[t_pe], writes=[t_ym])

        for tt in range(4):
            s = cos[0] % 2; cos[0] += 1
            for hf in range(2):
                for kc in range(4):
                    P.op("pe", lambda e: e.matmul(pp[hf][:, :], lhsT=ycT[:, kc, tt * 128:(tt + 1) * 128], rhs=wot[:, kc, hf * 512:(hf + 1) * 512],
                                                  start=(kc == 0), stop=(kc == 3)), reads=[t_ym, t_w], writes=[t_pp[hf]])
                P.op("act" if hf == 0 else "dve", lambda e: (e.copy if hf == 0 else e.tensor_copy)(out=osb[s][:, hf * 512:(hf + 1) * 512], in_=pp[hf][:, :]),
                     reads=[t_pp[hf]], writes=[t_osbh[s][hf]])
            P.dma("sp", out[r0 + tt * 128:r0 + (tt + 1) * 128, :], osb[s][:], reads=[t_osbh[s][0], t_osbh[s][1]], writes=[t_outd])


def odd_inputs(x, w_in, gla_w2, gla_b, gla_norm, w_out, hp):
    f = np.float32
    h0 = 2 * hp
    q = w_in[:, 0:512][:, h0 * 128:(h0 + 2) * 128]
    k = w_in[:, 512:1024][:, h0 * 128:(h0 + 2) * 128]
    v = w_in[:, 1024:2048][:, h0 * 256:(h0 + 2) * 256]
    r = w_in[:, 2048:3072][:, h0 * 256:(h0 + 2) * 256]
    glr = w_in[:, 3072:3088]
    wA = np.ascontiguousarray(np.concatenate([q, k, v, r, glr], axis=1), dtype=f)
    wo = np.ascontiguousarray(w_out[h0 * 256:(h0 + 2) * 256], dtype=f)
    w2 = np.ascontiguousarray(gla_w2[:, h0 * 128:(h0 + 2) * 128], dtype=f)
    pc = np.ascontiguousarray(gla_b[h0 * 128:(h0 + 2) * 128].reshape(2, 128).T, dtype=f)
    gln = np.ascontiguousarray(gla_norm[h0 * 256:(h0 + 2) * 256].reshape(1, 512), dtype=f)
    cst = np.zeros((128, 768), f)
    cst[:, 0:128] = np.eye(128)
    s_i = np.arange(128)[:, None]; t_i = np.arange(128)[None, :]
    cst[:, 128:256] = ((s_i // 64 == t_i // 64) & (s_i <= t_i)).astype(f)
    cst[:, 256:768] = (np.arange(512) % 64 != 0).astype(f)[None, :]
    return dict(x=np.ascontiguousarray(x, dtype=f), wA=wA, wo=wo, w2=w2, pc=pc, gln=gln, cst=cst)


FUSED_CORES = BATCH
SPARSE_MOE = True


def build_fused(n_layers=DEPTH, T=SEQ, sparse=True):
    P = Prog()
    nc = P.nc
    x_ext = nc.dram_tensor("x", [T, D_MODEL], F32, kind="ExternalInput").ap()
    y_ext = nc.dram_tensor("y", [T, D_MODEL], F32, kind="ExternalOutput").ap()
    xbuf = [nc.dram_tensor("xbuf%d" % i, [T, D_MODEL], F32).ap() for i in range(2)]
    pmix = [nc.dram_tensor("pmix%d" % i, [T, D_MODEL], F32).ap() for i in range(2)]
    t_xext = P.tok("xext")
    t_y = P.tok("y"); t_y.disjoint = True
    t_xbuf = [P.tok("xbuf%d" % i) for i in range(2)]
    t_pmix = [P.tok("pmix%d" % i) for i in range(2)]
    for t in t_xbuf + t_pmix:
        t.disjoint = True
    scr = None
    if sparse:
        scr = moe_sparse_scratch(nc, T)
        for k in ("t_xbkt", "t_ybuf", "t_x1d"):
            scr[k] = P.tok(k); scr[k].disjoint = True
    for l in range(n_layers):
        xin_ap, t_xin = (x_ext, t_xext) if l == 0 else (xbuf[(l - 1) % 2], t_xbuf[(l - 1) % 2])
        yout_ap, t_yout = (y_ext, t_y) if l == n_layers - 1 else (xbuf[l % 2], t_xbuf[l % 2])
        for hp in range(2):
            P.push()
            pfx = "L%dH%d_" % (l, hp)
            if l % 2 == 0:
                io = even_io(nc, pfx, T, x=xin_ap, out=pmix[hp])
            else:
                io = odd_io(nc, pfx, T, x=xin_ap, out=pmix[hp])
            io["t_xd"] = t_xin; io["t_outd"] = t_pmix[hp]
            if l % 2 == 0:
                emit_even(P, T, io)
            else:
                emit_odd(P, T, io)
            P.pop()
        P.push()
        io = moe_io(nc, "L%d_" % l, T, xin=xin_ap, ma=pmix[0], mb=pmix[1], yout=yout_ap, sparse=sparse)
        io["t_xin"] = t_xin; io["t_ma"] = t_pmix[0]; io["t_mb"] = t_pmix[1]; io["t_yout"] = t_yout
        if sparse:
            emit_moe_sparse(P, T, io, scr, first=(l == 0))
        else:
            emit_moe(P, T // 1024, io)
        P.pop()
    P.wait_all("sp", [t_y])
    P.close()
    return nc


_NC_CACHE = {}


def fused_inputs(b, x, even_w_in, pool_w, pool_scale, conv_w, conv_b, i_bias, f_bias, ml_norm, even_w_out,
                 odd_w_in, gla_w2, gla_b, gla_norm, odd_w_out, lnps, router_w, router_b, wgu_d, bgu_l, w_down, b_down, n_layers):
    f = np.float32
    im = {"x": np.ascontiguousarray(x[b], dtype=f)}
    ident = np.eye(128, dtype=f)
    for l in range(n_layers):
        i = l // 2
        for hp in range(2):
            pfx = "L%dH%d_" % (l, hp)
            if l % 2 == 0:
                d = even_inputs(x[b], even_w_in[i], pool_w[i], pool_scale[i], conv_w[i], conv_b[i], i_bias[i], f_bias[i],
                                ml_norm[i], even_w_out[i], hp)
            else:
                d = odd_inputs(x[b], odd_w_in[i], gla_w2[i], gla_b[i], gla_norm[i], odd_w_out[i], hp)
            for k, v in d.items():
                if k != "x":
                    im[pfx + k] = v
        pfx = "L%d_" % l
        im[pfx + "lnp"] = lnps[l]
        im[pfx + "rw"] = np.ascontiguousarray(router_w[l], dtype=f)
        im[pfx + "rb"] = np.ascontiguousarray(router_b[l], dtype=f).reshape(1, N_EXPERTS)
        im[pfx + "wgu"] = wgu_d[l]
        im[pfx + "bgu"] = bgu_l[l]
        im[pfx + "wd"] = np.ascontiguousarray(w_down[l], dtype=f)
        im[pfx + "bd"] = np.ascontiguousarray(b_down[l], dtype=f)
        im[pfx + "ident"] = moe_sparse_consts() if SPARSE_MOE else ident
    return im


def kernel(x, even_w_in, pool_w, pool_scale, conv_w, conv_b, i_bias, f_bias, ml_norm, even_w_out,
           odd_w_in, gla_w2, gla_b, gla_norm, odd_w_out, ln1_g, ln1_b, ln2_g, ln2_b, router_w, router_b,
           w_gate_up, b_gate_up, w_down, b_down, _layers=DEPTH, _cores=FUSED_CORES):
    f = np.float32
    A = lambda a: np.asarray(a, dtype=f)
    x = A(x)
    wgu_d, bgu_l, lnps = [], [], []
    for l in range(_layers):
        wg = A(w_gate_up[l])
        wgu_d.append(np.ascontiguousarray(np.concatenate([wg[:, :, 0::2], wg[:, :, 1::2]], axis=-1)))
        del wg
        bg = A(b_gate_up[l])
        bd_ = np.concatenate([bg[:, 0::2], bg[:, 1::2]], axis=-1)
        bgu_l.append(np.ascontiguousarray(bd_.reshape(N_EXPERTS, 16, 128).transpose(2, 0, 1).reshape(128, N_EXPERTS * 16)))
        lnps.append(np.stack([A(ln1_g[l]), A(ln1_b[l]), A(ln2_g[l]), A(ln2_b[l])]))
    args = [A(v) for v in (even_w_in, pool_w, pool_scale, conv_w, conv_b, i_bias, f_bias, ml_norm, even_w_out,
                           odd_w_in, gla_w2, gla_b, gla_norm, odd_w_out)]
    ims = [fused_inputs(b, x, *args, lnps, A(router_w), A(router_b), wgu_d, bgu_l, A(w_down), A(b_down), _layers)
           for b in range(_cores)]
    key = ("fused", _layers)
    if key not in _NC_CACHE:
        _NC_CACHE[key] = build_fused(_layers, sparse=SPARSE_MOE)
    res = run_bass_kernel_spmd(_NC_CACHE[key], ims, core_ids=list(range(_cores)))
    out = np.stack([r["y"] for r in res.results], axis=0)
    if _cores < BATCH:
        return out
    return out.reshape(BATCH, SEQ, D_MODEL)


MOE_CAP = 768
MOE_NR = N_EXPERTS * MOE_CAP
U32 = mybir.dt.uint32


def moe_sparse_scratch(nc, T):
    return dict(xbkt=nc.dram_tensor("xbkt", [MOE_NR + 128, D_MODEL], BF16).ap(),
                ybuf=nc.dram_tensor("ybuf", [MOE_NR + 128, D_MODEL], F32).ap(),
                x1d=nc.dram_tensor("x1d", [T, D_MODEL], F32).ap())


def _idma(P, out, in_, out_off=None, in_off=None, reads=(), writes=()):
    P._deps("pool", reads, writes)
    owner = writes[0]
    key = P._dsem(owner)
    ins = P.nc.gpsimd.indirect_dma_start(out=out, out_offset=out_off, in_=in_, in_offset=in_off)
    ins.then_inc(P.sems[key], 16)
    owner.dcount += 16
    P.dtot[key] = owner.dcount
    me = (key, owner.dcount)
    for r in reads:
        r.readers.append(me)
    for w in writes:
        w.writer = me
        w.readers = []


def emit_moe_sparse(P, T, io, scr, first=False, stop_phase=9):
    NT = T // 128
    C = MOE_CAP
    CB = C // 2
    NRT = C // 128
    nc = P.nc
    xin, ma, mb, lnp, rw, rb, wgu, bgu, wd, bd, cst_in, yout = (io[k] for k in
        ("xin", "ma", "mb", "lnp", "rw", "rb", "wgu", "bgu", "wd", "bd", "ident", "yout"))
    t_xin, t_ma, t_mb, t_yout = io["t_xin"], io["t_ma"], io["t_mb"], io["t_yout"]
    xbkt, ybuf, x1d = scr["xbkt"], scr["ybuf"], scr["x1d"]
    t_xbkt, t_ybuf, t_x1d = scr["t_xbkt"], scr["t_ybuf"], scr["t_x1d"]

    t_const = P.tok("const")
    cs = P.sb([128, 128 * 3 + 32 + 1], F32, "mcst")
    ident = cs[:, 0:128]; Ltri = cs[:, 128:256]; ones = cs[:, 256:384]; ebase1 = cs[:, 384:416]; ptrash = cs[:, 416:417]
    gb = P.sb([128, 4, D_MODEL], F32, "gb")
    rwt = P.sb([128, 8, N_EXPERTS], F32, "rwt"); rbt = P.sb([128, N_EXPERTS], F32, "rbt")
    bgt = P.sb([128, N_EXPERTS * 16], F32, "bgt"); bdt = P.sb([N_EXPERTS, D_MODEL], F32, "bdt")
    eps_t = P.sb([128, 1], F32, "eps")
    Gall = P.sb([128, NT, N_EXPERTS], F32, "G"); t_G = [P.tok("G") for i in range(NT)]
    Gk = P.sb([128, NT, 4], F32, "Gk"); Sidx = P.sb([128, NT, 4], U32, "Sidx"); t_sel = [P.tok("sel") for i in range(NT)]
    base = P.sb([128, N_EXPERTS], F32, "base"); t_base = P.tok("base")
    P.dma("sp", cs[:], cst_in[:, :], writes=[t_const])
    for i in range(4):
        P.dma("sp", gb[:, i, :], lnp[i:i + 1, :].partition_broadcast(128), writes=[t_const])
    P.dma("sp", rwt[:], rw.rearrange("(k p) n -> p k n", p=128), writes=[t_const])
    P.dma("sp", rbt[:], rb[0:1, :].partition_broadcast(128), writes=[t_const])
    P.dma("sp", bgt[:], bgu[:, :], writes=[t_const])
    P.dma("sp", bdt[:], bd[:, :], writes=[t_const])
    P.op("dve", lambda e: e.memset(eps_t[:], LN_EPS), writes=[t_const])
    P.op("dve", lambda e: e.memset(base[:], 0.0), writes=[t_base])
    bgv = bgt[:].rearrange("p (e c) -> p e c", c=16)
    P.op("dve", lambda e: e.tensor_scalar(out=bgv[:, :, 8:16], in0=bgv[:, :, 8:16], scalar1=1.0, scalar2=None, op0=ALU.add),
         reads=[t_const], writes=[t_const])

    P.push()
    xs_l = [P.sb([128, D_MODEL], F32, "xs%d" % i) for i in range(2)]; t_xs_l = [P.tok("xs") for i in range(2)]
    pas_l = [P.sb([128, D_MODEL], F32, "pa%d" % i) for i in range(2)]; t_pa_l = [P.tok("pa") for i in range(2)]
    pbs_l = [P.sb([128, D_MODEL], F32, "pb%d" % i) for i in range(2)]; t_pb_l = [P.tok("pb") for i in range(2)]
    x1s = [P.sb([128, D_MODEL], F32, "x1s%d" % i) for i in range(2)]; t_x1 = [P.tok("x1s") for i in range(2)]
    x1b = [P.sb([128, D_MODEL], BF16, "x1b%d" % i) for i in range(2)]; t_x1b = [P.tok("x1b") for i in range(2)]
    xTf_l = [P.sb([128, 8, 128], F32, "xTf%d" % i) for i in range(2)]; t_xTf_l = [P.tok("xTf") for i in range(2)]
    lns_l = [dict(st=P.sb([128, 12], F32), mv=P.sb([128, 2], F32), rstd=P.sb([128, 1], F32), eps=eps_t) for i in range(2)]
    t_lns_l = [P.tok("lns") for i in range(2)]
    R_l = [dict(lg=P.sb([128, 32], F32), t8=P.sb([128, 8], F32), nm=P.sb([128, 1], F32), ex=P.sb([128, 32], F32),
                mk=P.sb([128, 32], F32), sm=P.sb([128, 1], F32), pos=P.sb([128, 32], F32), v1=P.sb([128, 32], F32),
                smat=P.sb([128, 32], F32), s8=P.sb([128, 8], F32), neg=P.sb([128, 4], F32), sf=P.sb([128, 4], F32),
                junk=P.sb([128, 32], F32)) for i in range(2)]
    tR_l = [P.tok("rt") for i in range(2)]
    pm = [P.ps([128, 512], F32, "pm%d" % i) for i in range(4)]; t_pm = [P.tok("pm") for i in range(4)]
    pr_l = [P.ps([128, 512], F32, "pr%d" % i) for i in range(2)]; t_pr_l = [P.tok("pr") for i in range(2)]
    xs = xs_l[0]; t_xs = t_xs_l[0]
    if first:
        P.op("dve", lambda e: e.memset(xs[:], 0.0), writes=[t_xs])
        P.dma("sp", ybuf[MOE_NR:MOE_NR + 128, :], xs[:], reads=[t_xs], writes=[t_ybuf])
    cm = [0]
    for it in range(NT):
        s = it % 2
        r0 = it * 128
        xs = xs_l[s]; t_xs = t_xs_l[s]; pas = pas_l[s]; t_pa = t_pa_l[s]; pbs = pbs_l[s]; t_pb = t_pb_l[s]
        xTf = xTf_l[s]; t_xTf = t_xTf_l[s]; lns = lns_l[s]; t_lns = t_lns_l[s]; R = R_l[s]; tR = tR_l[s]; pr = pr_l[s]; t_pr = t_pr_l[s]
        P.dma("sp", xs[:], xin[r0:r0 + 128, :], reads=[t_xin], writes=[t_xs])
        P.dma("sp", pas[:], ma[r0:r0 + 128, :], reads=[t_ma], writes=[t_pa])
        P.dma("sp", pbs[:], mb[r0:r0 + 128, :], reads=[t_mb], writes=[t_pb])
        P.op("dve", lambda e: e.tensor_tensor(out=pas[:], in0=pas[:], in1=pbs[:], op=ALU.add), reads=[t_pb, t_pa], writes=[t_pa])
        P.op("dve", lambda e: e.scalar_tensor_tensor(out=xs[:], in0=xs[:], scalar=ALPHA, in1=pas[:], op0=ALU.mult, op1=ALU.add),
             reads=[t_xs, t_pa], writes=[t_xs])
        layer_norm_tile(P, xs[:], x1s[s][:], gb[:, 0, :], gb[:, 1, :], lns, t_xs, t_x1[s], t_lns, t_const, gb_eng="dve")
        P.dma("sp", x1d[r0:r0 + 128, :], x1s[s][:], reads=[t_x1[s]], writes=[t_x1d])
        P.op("act", lambda e: e.copy(out=x1b[s][:], in_=x1s[s][:]), reads=[t_x1[s]], writes=[t_x1b[s]])
        for h in range(2):
            b = cm[0] % 4; cm[0] += 1
            for j in range(4):
                k = h * 4 + j
                P.op("pe", lambda e: e.transpose(out=pm[b][:, j * 128:(j + 1) * 128], in_=x1s[s][:, k * 128:(k + 1) * 128], identity=ident),
                     reads=[t_x1[s], t_const], writes=[t_pm[b]])
            P.op("act", lambda e: e.copy(out=xTf[:, h * 4:(h + 1) * 4, :], in_=pm[b][:].rearrange("p (j t) -> p j t", j=4)),
                 reads=[t_pm[b]], writes=[t_xTf])
        b = cm[0] % 4; cm[0] += 1
        for k in range(8):
            P.op("pe", lambda e: e.matmul(pm[b][:, 0:32], lhsT=xTf[:, k, :], rhs=rwt[:, k, :], start=(k == 0), stop=(k == 7)),
                 reads=[t_xTf, t_const], writes=[t_pm[b]])
        P.op("dve", lambda e: e.tensor_tensor(out=R["lg"][:], in0=pm[b][:, 0:32], in1=rbt[:], op=ALU.add), reads=[t_pm[b], t_const], writes=[tR])
        P.op("dve", lambda e: e.max(out=R["t8"][:], in_=R["lg"][:]), reads=[tR], writes=[tR])
        P.op("dve", lambda e: e.tensor_scalar(out=R["nm"][:], in0=R["t8"][:, 0:1], scalar1=-1.0, scalar2=None, op0=ALU.mult), reads=[tR], writes=[tR])
        P.op("act", lambda e: e.activation(out=R["ex"][:], in_=R["lg"][:], func=AF.Exp, bias=R["nm"][:, 0:1], scale=1.0), reads=[tR], writes=[tR])
        P.op("dve", lambda e: e.tensor_scalar(out=R["mk"][:], in0=R["lg"][:], scalar1=R["t8"][:, 3:4], scalar2=None, op0=ALU.is_ge), reads=[tR], writes=[tR])
        P.op("dve", lambda e: e.tensor_tensor(out=R["ex"][:], in0=R["ex"][:], in1=R["mk"][:], op=ALU.mult), reads=[tR], writes=[tR])
        P.op("dve", lambda e: e.reduce_sum(out=R["sm"][:], in_=R["ex"][:], axis=mybir.AxisListType.X), reads=[tR], writes=[tR])
        P.op("dve", lambda e: e.reciprocal(out=R["sm"][:], in_=R["sm"][:]), reads=[tR], writes=[tR])
        P.op("dve", lambda e: e.tensor_scalar(out=Gall[:, it, :], in0=R["ex"][:], scalar1=R["sm"][:, 0:1], scalar2=None, op0=ALU.mult),
             reads=[tR], writes=[t_G[it]])
        P.op("pe", lambda e: e.matmul(pr[:, 0:32], lhsT=Ltri, rhs=R["mk"][:], start=True, stop=True), reads=[tR, t_const], writes=[t_pr])
        P.op("pe", lambda e: e.matmul(pr[:, 32:64], lhsT=ones, rhs=R["mk"][:], start=True, stop=True), reads=[tR, t_const], writes=[t_pr])
        P.op("dve", lambda e: e.tensor_tensor(out=R["pos"][:], in0=pr[:, 0:32], in1=base[:], op=ALU.add), reads=[t_pr, t_base], writes=[tR])
        P.op("dve", lambda e: e.tensor_tensor(out=base[:], in0=pr[:, 32:64], in1=base[:], op=ALU.add), reads=[t_pr, t_base, tR], writes=[t_base])
        P.op("dve", lambda e: e.tensor_scalar(out=R["v1"][:], in0=R["pos"][:], scalar1=float(C), scalar2=None, op0=ALU.is_lt), reads=[tR], writes=[tR])
        P.op("dve", lambda e: e.tensor_tensor(out=R["v1"][:], in0=R["v1"][:], in1=R["mk"][:], op=ALU.mult), reads=[tR], writes=[tR])
        P.op("dve", lambda e: e.tensor_tensor(out=R["pos"][:], in0=R["pos"][:], in1=ebase1, op=ALU.add), reads=[tR, t_const], writes=[tR])
        P.op("dve", lambda e: e.tensor_tensor(out=R["smat"][:], in0=R["pos"][:], in1=R["v1"][:], op=ALU.mult), reads=[tR], writes=[tR])
        P.op("dve", lambda e: e.tensor_scalar(out=R["smat"][:], in0=R["smat"][:], scalar1=-1.0, scalar2=None, op0=ALU.add), reads=[tR], writes=[tR])
        P.op("dve", lambda e: e.max(out=R["s8"][:], in_=R["smat"][:]), reads=[tR], writes=[tR])
        for k in range(4):
            P.op("dve", lambda e: e.scalar_tensor_tensor(out=R["junk"][:], in0=R["smat"][:], scalar=R["s8"][:, k:k + 1], in1=Gall[:, it, :],
                                                         op0=ALU.is_equal, op1=ALU.mult, accum_out=Gk[:, it, k:k + 1]),
                 reads=[tR, t_G[it]], writes=[tR, t_sel[it]])
        P.op("dve", lambda e: e.tensor_scalar(out=R["neg"][:], in0=R["s8"][:, 0:4], scalar1=0.0, scalar2=None, op0=ALU.is_lt), reads=[tR], writes=[tR])
        P.op("dve", lambda e: e.scalar_tensor_tensor(out=R["sf"][:], in0=R["neg"][:], scalar=ptrash, in1=R["s8"][:, 0:4], op0=ALU.mult, op1=ALU.add),
             reads=[tR, t_const], writes=[tR])
        P.op("dve", lambda e: e.tensor_copy(out=Sidx[:, it, :], in_=R["sf"][:]), reads=[tR], writes=[t_sel[it]])
        for k in range(4):
            _idma(P, xbkt[:, :], x1b[s][:], out_off=bass.IndirectOffsetOnAxis(Sidx[:, it, k:k + 1], 0),
                  reads=[t_x1b[s], t_sel[it]], writes=[t_xbkt])
    P.pop()

    if stop_phase <= 1:
        return
    P.push()
    wgt = [P.sb([128, 8, 2048], BF16, "wgu%d" % i) for i in range(2)]; t_wg = [P.tok("wg") for i in range(2)]
    wdt = [P.sb([128, 8, D_MODEL], BF16, "wd%d" % i) for i in range(2)]; t_wd = [P.tok("wd") for i in range(2)]
    XT = P.sb([128, 8, C], BF16, "XT"); t_XT = P.tok("XT")
    actT = P.sb([128, 8, C], BF16, "actT"); t_actT = [P.tok("actT") for i in range(2)]
    xrow = [P.sb([128, D_MODEL], BF16, "xrow%d" % i) for i in range(2)]; t_xrow = [P.tok("xrow") for i in range(2)]
    yrow = [P.sb([128, D_MODEL], F32, "yrow%d" % i) for i in range(2)]; t_yrow = [P.tok("yrow") for i in range(2)]
    identb = P.sb([128, 128], BF16, "identb")
    P.op("act", lambda e: e.copy(out=identb[:], in_=ident), reads=[t_const], writes=[t_const])
    NG = 2
    glt = [P.sb([128, CB], F32, "gl%d" % i) for i in range(NG)]; t_gl = [P.tok("gl") for i in range(NG)]
    sgt = [P.sb([128, CB], F32, "sg%d" % i) for i in range(NG)]; t_sg = [P.tok("sg") for i in range(NG)]
    lit = [P.sb([128, CB], F32, "li%d" % i) for i in range(NG)]; t_li = [P.tok("li") for i in range(NG)]
    pg = [P.ps([128, 512], F32, "pg%d" % i) for i in range(3)]; t_pg = [P.tok("pg") for i in range(3)]
    pdn = [P.ps([128, 512], F32, "pd%d" % i) for i in range(3)]; t_pd = [P.tok("pd") for i in range(3)]
    ptb = [P.ps([128, 1024], BF16, "ptb%d" % i) for i in range(2)]; t_ptb = [P.tok("ptb") for i in range(2)]

    def load_weights(e, slot):
        src = wgu[e].rearrange("(k p) n -> p k n", p=128)
        for k in range(8):
            P.dma("pool", wgt[slot][:, k, :], src[:, k, :], writes=[t_wg[slot]])
        src = wd[e].rearrange("(k p) n -> p k n", p=128)
        for k in range(0, 8, 2):
            P.dma("pool", wdt[slot][:, k:k + 2, :], src[:, k:k + 2, :], writes=[t_wd[slot]])

    cg = [0]; cd = [0]; cgl = [0]; ct = [0]; cy = [0]
    load_weights(0, 0)
    for ex in range(N_EXPERTS):
        slot = ex % 2
        if ex + 1 < N_EXPERTS:
            load_weights(ex + 1, (ex + 1) % 2)
        W = wgt[slot]; WD = wdt[slot]
        for rt in range(NRT):
            s = ct[0] % 2; ct[0] += 1
            rr = ex * C + rt * 128
            P.dma("sp", xrow[s][:], xbkt[rr:rr + 128, :], reads=[t_xbkt], writes=[t_xrow[s]])
            for k in range(8):
                P.op("pe", lambda e: e.transpose(out=ptb[s][:, k * 128:(k + 1) * 128], in_=xrow[s][:, k * 128:(k + 1) * 128], identity=identb[:]),
                     reads=[t_xrow[s], t_const], writes=[t_ptb[s]])
            P.op("act", lambda e: e.copy(out=XT[:, :, rt * 128:(rt + 1) * 128], in_=ptb[s][:, :].rearrange("p (k t) -> p k t", k=8)),
                 reads=[t_ptb[s]], writes=[t_XT])
        for cb in range(2):
            csl = slice(cb * CB, (cb + 1) * CB)
            for j in range(8):
                gi = cgl[0] % NG; cgl[0] += 1
                b = cg[0] % 3; cg[0] += 1
                for k in range(8):
                    P.op("pe", lambda e: e.matmul(pg[b][:, 0:CB], lhsT=W[:, k, j * 128:(j + 1) * 128], rhs=XT[:, k, csl], start=(k == 0), stop=(k == 7)),
                         reads=[t_wg[slot], t_XT], writes=[t_pg[b]])
                P.op("dve", lambda e: e.tensor_scalar(out=glt[gi][:], in0=pg[b][:, 0:CB], scalar1=bgt[:, ex * 16 + j:ex * 16 + j + 1],
                                                      scalar2=SWIGLU_LIMIT, op0=ALU.add, op1=ALU.min), reads=[t_pg[b], t_const], writes=[t_gl[gi]])
                P.op("act", lambda e: e.activation(out=sgt[gi][:], in_=glt[gi][:], func=AF.Sigmoid, scale=SWIGLU_ALPHA), reads=[t_gl[gi]], writes=[t_sg[gi]])
                P.op("pool", lambda e: e.tensor_tensor(out=sgt[gi][:], in0=sgt[gi][:], in1=glt[gi][:], op=ALU.mult), reads=[t_gl[gi], t_sg[gi]], writes=[t_sg[gi]])
                b = cg[0] % 3; cg[0] += 1
                for k in range(8):
                    P.op("pe", lambda e: e.matmul(pg[b][:, 0:CB], lhsT=W[:, k, 1024 + j * 128:1024 + (j + 1) * 128], rhs=XT[:, k, csl], start=(k == 0), stop=(k == 7)),
                         reads=[t_wg[slot], t_XT], writes=[t_pg[b]])
                P.op("dve", lambda e: e.tensor_scalar(out=lit[gi][:], in0=pg[b][:, 0:CB], scalar1=bgt[:, ex * 16 + 8 + j:ex * 16 + 9 + j],
                                                      scalar2=SWIGLU_LIMIT + 1.0, op0=ALU.add, op1=ALU.min), reads=[t_pg[b], t_const], writes=[t_li[gi]])
                P.op("dve", lambda e: e.scalar_tensor_tensor(out=actT[:, j, csl], in0=lit[gi][:], scalar=1.0 - SWIGLU_LIMIT, in1=sgt[gi][:],
                                                             op0=ALU.max, op1=ALU.mult), reads=[t_li[gi], t_sg[gi]], writes=[t_actT[cb]])
        for rt in range(NRT):
            s = cy[0] % 2; cy[0] += 1
            for hf in range(2):
                b = cd[0] % 3; cd[0] += 1
                for k in range(8):
                    P.op("pe", lambda e: e.matmul(pdn[b][:, :], lhsT=actT[:, k, rt * 128:(rt + 1) * 128], rhs=WD[:, k, hf * 512:(hf + 1) * 512],
                                                  start=(k == 0), stop=(k == 7)), reads=[t_actT[0], t_actT[1], t_wd[slot]], writes=[t_pd[b]])
                P.op("act", lambda e: e.copy(out=yrow[s][:, hf * 512:(hf + 1) * 512], in_=pdn[b][:, :]), reads=[t_pd[b]], writes=[t_yrow[s]])
            rr = ex * C + rt * 128
            P.dma("sp", ybuf[rr:rr + 128, :], yrow[s][:], reads=[t_yrow[s]], writes=[t_ybuf])
    P.pop()

    if stop_phase <= 2:
        return
    P.push()
    x1s = [P.sb([128, D_MODEL], F32, "x1c%d" % i) for i in range(2)]; t_x1 = [P.tok("x1c") for i in range(2)]
    accs = [P.sb([128, D_MODEL], F32, "acc%d" % i) for i in range(2)]; t_acc = [P.tok("acc") for i in range(2)]
    yg_l = [[P.sb([128, D_MODEL], F32, "yg%d%d" % (j, i)) for i in range(4)] for j in range(2)]
    t_yg_l = [[P.tok("yg") for i in range(4)] for j in range(2)]
    outs = [P.sb([128, D_MODEL], F32, "outs%d" % i) for i in range(2)]; t_outs = [P.tok("outs") for i in range(2)]
    GT_l = [P.sb([32, 128], F32, "GT%d" % i) for i in range(2)]; t_GT_l = [P.tok("GT") for i in range(2)]
    lns_l = [dict(st=P.sb([128, 12], F32), mv=P.sb([128, 2], F32), rstd=P.sb([128, 1], F32), eps=eps_t) for i in range(2)]
    t_lns_l = [P.tok("lns") for i in range(2)]
    pm = [P.ps([128, 512], F32, "pm%d" % i) for i in range(4)]; t_pm = [P.tok("pm") for i in range(4)]
    pt_l = [P.ps([128, 512], F32, "pt%d" % i) for i in range(2)]; t_pt_l = [P.tok("pt") for i in range(2)]
    cm = [0]
    for it in range(NT):
        s = it % 2
        r0 = it * 128
        yg = yg_l[s]; t_yg = t_yg_l[s]; GT = GT_l[s]; t_GT = t_GT_l[s]; lns = lns_l[s]; t_lns = t_lns_l[s]; pt = pt_l[s]; t_pt = t_pt_l[s]
        P.dma("sp", x1s[s][:], x1d[r0:r0 + 128, :], reads=[t_x1d], writes=[t_x1[s]])
        for k in range(4):
            _idma(P, yg[k][:], ybuf[:, :], in_off=bass.IndirectOffsetOnAxis(Sidx[:, it, k:k + 1], 0), reads=[t_ybuf, t_sel[it]], writes=[t_yg[k]])
        P.op("pe", lambda e: e.transpose(out=pt[0:32, 0:128], in_=Gall[:, it, :], identity=ident), reads=[t_G[it], t_const], writes=[t_pt])
        P.op("act", lambda e: e.copy(out=GT[:], in_=pt[0:32, 0:128]), reads=[t_pt], writes=[t_GT])
        for hf in range(2):
            b = cm[0] % 4; cm[0] += 1
            P.op("pe", lambda e: e.matmul(pm[b][:, :], lhsT=GT[:], rhs=bdt[:, hf * 512:(hf + 1) * 512], start=True, stop=True),
                 reads=[t_GT, t_const], writes=[t_pm[b]])
            P.op("dve", lambda e: e.scalar_tensor_tensor(out=accs[s][:, hf * 512:(hf + 1) * 512], in0=x1s[s][:, hf * 512:(hf + 1) * 512],
                                                         scalar=ALPHA, in1=pm[b][:, :], op0=ALU.mult, op1=ALU.add),
                 reads=[t_x1[s], t_pm[b]], writes=[t_acc[s]])
        for k in range(4):
            P.op("dve", lambda e: e.scalar_tensor_tensor(out=accs[s][:], in0=yg[k][:], scalar=Gk[:, it, k:k + 1], in1=accs[s][:],
                                                         op0=ALU.mult, op1=ALU.add), reads=[t_yg[k], t_sel[it], t_acc[s]], writes=[t_acc[s]])
        layer_norm_tile(P, accs[s][:], outs[s][:], gb[:, 2, :], gb[:, 3, :], lns, t_acc[s], t_outs[s], t_lns, t_const, gb_eng="dve")
        P.dma("sp", yout[r0:r0 + 128, :], outs[s][:], reads=[t_outs[s]], writes=[t_yout])
    P.pop()


def moe_sparse_consts():
    f = np.float32
    c = np.zeros((128, 417), f)
    c[:, 0:128] = np.eye(128)
    tp = np.arange(128)[:, None]; tt = np.arange(128)[None, :]
    c[:, 128:256] = (tp < tt).astype(f)
    c[:, 256:384] = 1.0
    c[:, 384:416] = (np.arange(N_EXPERTS) * MOE_CAP + 1).astype(f)[None, :]
    c[:, 416] = MOE_NR + 1 + np.arange(128)
    return c
```

```python
import math
from contextlib import ExitStack

import numpy as np
import concourse.bass as bass
import concourse.mybir as mybir
from concourse.bass_utils import run_bass_kernel_spmd

F32 = mybir.dt.float32
BF16 = mybir.dt.bfloat16
AF = mybir.ActivationFunctionType
ALU = mybir.AluOpType

D_MODEL = 1024
BATCH = 4
SEQ = 4096
DEPTH = 4
ALPHA = (2 * DEPTH) ** 0.25
LN_EPS = 1e-5
N_EXPERTS = 32
SWIGLU_LIMIT = 7.0
SWIGLU_ALPHA = 1.702


class Tok:
    __slots__ = ("name", "writer", "readers", "dsem", "dcount", "disjoint")

    def __init__(self, name):
        self.name = name
        self.writer = None
        self.readers = []
        self.dsem = None
        self.dcount = 0
        self.disjoint = False


class Prog:
    def __init__(self):
        self.nc = bass.Bass("TRN2", target_bir_lowering=False)
        self.es = ExitStack()
        nc = self.nc
        self.eng = {"pe": nc.tensor, "dve": nc.vector, "act": nc.scalar, "pool": nc.gpsimd, "sp": nc.sync}
        self.sems = {}
        self.cnt = {}
        self.known = {k: {} for k in self.eng}
        for k in ("pe", "dve", "act", "pool"):
            self.sems[k] = self.es.enter_context(nc.semaphore("s_" + k))
            self.cnt[k] = 0
        self.nsem = 0
        self.ntens = 0
        self.scopes = []
        self.sem_pool = []
        self.dtot = {}
        self.live_toks = []

    def sb(self, shape, dtype=F32, name=None):
        self.ntens += 1
        t = self._es().enter_context(self.nc.sbuf_tensor("%s_%d" % (name or "sb", self.ntens), list(shape), dtype))
        return t

    def ps(self, shape, dtype=F32, name=None):
        self.ntens += 1
        t = self._es().enter_context(self.nc.psum_tensor("%s_%d" % (name or "ps", self.ntens), list(shape), dtype))
        return t

    def _es(self):
        return self.scopes[-1][0] if self.scopes else self.es

    def tok(self, name="t"):
        t = Tok(name)
        if self.scopes:
            self.scopes[-1][1].append(t)
        return t

    def _dsem(self, tok):
        if tok.dsem is None:
            if self.sem_pool:
                key, cnt = self.sem_pool.pop()
                tok.dcount = cnt
            else:
                self.nsem += 1
                key = "d%d" % self.nsem
                self.sems[key] = self.es.enter_context(self.nc.semaphore(key))
                self.dtot[key] = 0
            tok.dsem = key
        return tok.dsem

    def push(self):
        self.scopes.append((ExitStack(), []))

    def barrier(self):
        targets = {k: self.cnt[k] for k in ("pe", "dve", "act", "pool") if self.cnt[k] > 0}
        for k, v in self.dtot.items():
            if v > 0:
                targets[k] = v
        for e in self.eng:
            kn = self.known[e]
            for k, v in targets.items():
                if kn.get(k, 0) >= v:
                    continue
                self.eng[e].wait_ge(self.sems[k], v)
                kn[k] = v

    def pop(self):
        self.barrier()
        es, toks = self.scopes.pop()
        for t in toks:
            if t.dsem is not None:
                self.sem_pool.append((t.dsem, self.dtot[t.dsem]))
                t.dsem = None
        es.close()

    def _deps(self, e, reads, writes):
        deps = {}

        def add(d):
            if d is None:
                return
            k, v = d
            if deps.get(k, 0) < v:
                deps[k] = v

        for r in reads:
            add(r.writer)
        for w in writes:
            if not w.disjoint:
                add(w.writer)
            for rd in w.readers:
                add(rd)
        kn = self.known[e]
        for k, v in deps.items():
            if e == "pe" and k == "pe":
                continue
            if kn.get(k, 0) >= v:
                continue
            self.eng[e].wait_ge(self.sems[k], v)
            kn[k] = v

    def op(self, e, fn, reads=(), writes=()):
        self._deps(e, reads, writes)
        ins = fn(self.eng[e])
        ins.then_inc(self.sems[e], 1)
        self.cnt[e] += 1
        me = (e, self.cnt[e])
        for r in reads:
            r.readers.append(me)
        for w in writes:
            w.writer = me
            w.readers = []
        return ins

    def dma(self, q, out, in_, reads=(), writes=(), **kw):
        self._deps(q, reads, writes)
        owner = writes[0] if writes else reads[0]
        key = self._dsem(owner)
        ins = self.eng[q].dma_start(out=out, in_=in_, **kw)
        ins.then_inc(self.sems[key], 16)
        owner.dcount += 16
        self.dtot[key] = owner.dcount
        me = (key, owner.dcount)
        for r in reads:
            r.readers.append(me)
        for w in writes:
            w.writer = me
            w.readers = []
        return ins

    def wait_all(self, q, toks):
        deps = {}
        for t in toks:
            for d in [t.writer] + list(t.readers):
                if d is not None and deps.get(d[0], 0) < d[1]:
                    deps[d[0]] = d[1]
        for k, v in deps.items():
            self.eng[q].wait_ge(self.sems[k], v)

    def close(self):
        self.es.close()


def run_interleaved(gens):
    gens = list(gens)
    while gens:
        for g in list(gens):
            try:
                next(g)
            except StopIteration:
                gens.remove(g)


def layer_norm_tile(P, src, dst, g_bc, b_bc, scr, T_src, T_dst, T_scr, tgb, gb_eng="pool"):
    st, mv, rstd = scr["st"], scr["mv"], scr["rstd"]
    P.op("dve", lambda e: e.bn_stats(out=st[:, 0:6], in_=src[:, 0:512]), reads=[T_src], writes=[T_scr])
    P.op("dve", lambda e: e.bn_stats(out=st[:, 6:12], in_=src[:, 512:1024]), reads=[T_src, T_scr], writes=[T_scr])
    P.op("dve", lambda e: e.bn_aggr(out=mv[:, 0:2], in_=st[:, 0:12]), reads=[T_scr], writes=[T_scr])
    P.op("act", lambda e: e.activation(out=rstd[:, 0:1], in_=mv[:, 1:2], func=AF.Sqrt, bias=scr["eps"][:, 0:1], scale=1.0),
         reads=[T_scr, tgb], writes=[T_scr])
    P.op("dve", lambda e: e.reciprocal(out=rstd[:, 0:1], in_=rstd[:, 0:1]), reads=[T_scr], writes=[T_scr])
    P.op("dve", lambda e: e.tensor_scalar(out=dst, in0=src, scalar1=mv[:, 0:1], scalar2=rstd[:, 0:1],
                                          op0=ALU.subtract, op1=ALU.mult), reads=[T_src, T_scr], writes=[T_dst])
    P.op(gb_eng, lambda e: e.tensor_tensor(out=dst, in0=dst, in1=g_bc, op=ALU.mult), reads=[T_dst, tgb], writes=[T_dst])
    P.op(gb_eng, lambda e: e.tensor_tensor(out=dst, in0=dst, in1=b_bc, op=ALU.add), reads=[T_dst, tgb], writes=[T_dst])


def build_moe(n_pass, n_exp=N_EXPERTS, two_mix=True):
    P = Prog()
    nc = P.nc
    io = moe_io(nc, "", n_pass * 1024, n_exp)
    for k in ("t_xin", "t_ma", "t_mb", "t_yout"):
        io[k] = P.tok(k)
    emit_moe(P, n_pass, io, n_exp)
    P.wait_all("sp", [io["t_yout"]])
    P.close()
    return nc


def moe_io(nc, pfx, NTOK, n_exp=N_EXPERTS, xin=None, ma=None, mb=None, yout=None, sparse=False):
    io = {}
    io["xin"] = xin if xin is not None else nc.dram_tensor(pfx + "xin", [NTOK, D_MODEL], F32, kind="ExternalInput").ap()
    io["ma"] = ma if ma is not None else nc.dram_tensor(pfx + "ma", [NTOK, D_MODEL], F32, kind="ExternalInput").ap()
    io["mb"] = mb if mb is not None else nc.dram_tensor(pfx + "mb", [NTOK, D_MODEL], F32, kind="ExternalInput").ap()
    io["lnp"] = nc.dram_tensor(pfx + "lnp", [4, D_MODEL], F32, kind="ExternalInput").ap()
    io["rw"] = nc.dram_tensor(pfx + "rw", [D_MODEL, N_EXPERTS], F32, kind="ExternalInput").ap()
    io["rb"] = nc.dram_tensor(pfx + "rb", [1, N_EXPERTS], F32, kind="ExternalInput").ap()
    io["wgu"] = nc.dram_tensor(pfx + "wgu", [n_exp, D_MODEL, 2048], F32, kind="ExternalInput").ap()
    io["bgu"] = nc.dram_tensor(pfx + "bgu", [128, n_exp * 16], F32, kind="ExternalInput").ap()
    io["wd"] = nc.dram_tensor(pfx + "wd", [n_exp, D_MODEL, D_MODEL], F32, kind="ExternalInput").ap()
    io["bd"] = nc.dram_tensor(pfx + "bd", [N_EXPERTS, D_MODEL], F32, kind="ExternalInput").ap()
    io["ident"] = nc.dram_tensor(pfx + "ident", [128, 417 if sparse else 128], F32, kind="ExternalInput").ap()
    io["yout"] = yout if yout is not None else nc.dram_tensor(pfx + "yout", [NTOK, D_MODEL], F32, kind="ExternalOutput").ap()
    return io


def emit_moe(P, n_pass, io, n_exp=N_EXPERTS, two_mix=True):
    TG = 1024
    NT = TG // 128
    nc = P.nc
    xin, ma, mb, lnp, rw, rb, wgu, bgu, wd, bd, ident_in, yout = (io[k] for k in
        ("xin", "ma", "mb", "lnp", "rw", "rb", "wgu", "bgu", "wd", "bd", "ident", "yout"))
    t_xin, t_ma, t_mb, t_yout = io["t_xin"], io["t_ma"], io["t_mb"], io["t_yout"]

    ident = P.sb([128, 128], F32, "ident"); t_const = P.tok("const")
    gb = P.sb([128, 4, D_MODEL], F32, "gb")
    rwt = P.sb([128, 8, N_EXPERTS], F32, "rwt")
    rbt = P.sb([128, N_EXPERTS], F32, "rbt")
    bgt = P.sb([128, n_exp * 16], F32, "bgt")
    bdt = P.sb([N_EXPERTS, D_MODEL], F32, "bdt")
    eps_t = P.sb([128, 1], F32, "eps")
    P.dma("sp", ident[:], ident_in[:, :], writes=[t_const])
    for i in range(4):
        P.dma("sp", gb[:, i, :], lnp[i:i + 1, :].partition_broadcast(128), writes=[t_const])
    P.dma("sp", rwt[:], rw.rearrange("(k p) n -> p k n", p=128), writes=[t_const])
    P.dma("sp", rbt[:], rb[0:1, :].partition_broadcast(128), writes=[t_const])
    P.dma("sp", bgt[:], bgu[:, :], writes=[t_const])
    P.dma("sp", bdt[:], bd[:, :], writes=[t_const])
    P.op("dve", lambda e: e.memset(eps_t[:], LN_EPS), writes=[t_const])
    bgv = bgt[:].rearrange("p (e c) -> p e c", c=16)
    P.op("dve", lambda e: e.tensor_scalar(out=bgv[:, :, 8:16], in0=bgv[:, :, 8:16], scalar1=1.0, scalar2=None, op0=ALU.add),
         reads=[t_const], writes=[t_const])

    acc = P.sb([128, NT, D_MODEL], F32, "acc");  t_acc = [P.tok("acc%d" % i) for i in range(NT)]
    x1T = P.sb([128, 8, TG], BF16, "x1T");       t_x1T = [P.tok("x1T%d" % i) for i in range(NT)]
    Gall = P.sb([128, NT, N_EXPERTS], F32, "G"); t_G = [P.tok("G%d" % i) for i in range(NT)]
    actT = P.sb([128, 8, 512], BF16, "actT");    t_actT = P.tok("actT")
    wgt = [P.sb([128, 8, 2048], BF16, "wgu%d" % i) for i in range(2)]; t_wg = [P.tok("wg%d" % i) for i in range(2)]
    wdt = [P.sb([128, 8, D_MODEL], BF16, "wd%d" % i) for i in range(2)]; t_wd = [P.tok("wd%d" % i) for i in range(2)]
    NS = 1
    xs = [P.sb([128, D_MODEL], F32, "xs%d" % i) for i in range(NS)]; t_xs = [P.tok("xs%d" % i) for i in range(NS)]
    pas = [P.sb([128, D_MODEL], F32, "pa0")] * NS; t_pa = [P.tok("pa0")] * NS
    pbs = [P.sb([128, D_MODEL], F32, "pb0")] * NS; t_pb = [P.tok("pb0")] * NS
    x1s = [P.sb([128, D_MODEL], F32, "x1s%d" % i) for i in range(NS)]; t_x1 = [P.tok("x1s%d" % i) for i in range(NS)]
    xTf = [P.sb([128, 8, 128], F32, "xTf0")] * NS; t_xTf = [P.tok("xTf0")] * NS
    lns = [dict(st=P.sb([128, 12], F32), mv=P.sb([128, 2], F32), rstd=P.sb([128, 1], F32), eps=eps_t) for i in range(NS)]
    t_lns = [P.tok("lns%d" % i) for i in range(NS)]
    rt = [dict(lg=P.sb([128, 32], F32), t8=P.sb([128, 8], F32), nm=P.sb([128, 1], F32), ex=P.sb([128, 32], F32),
               mk=P.sb([128, 32], F32), sm=P.sb([128, 1], F32), GT=P.sb([32, 128], F32)) for i in range(NS)]
    t_rt = [P.tok("rt%d" % i) for i in range(NS)]
    NG = 2
    glt = [P.sb([128, 512], F32, "gl%d" % i) for i in range(NG)]; t_gl = [P.tok("gl%d" % i) for i in range(NG)]
    sgt = [P.sb([128, 512], F32, "sg%d" % i) for i in range(NG)]; t_sg = [P.tok("sg%d" % i) for i in range(NG)]
    lit = [P.sb([128, 512], F32, "li0")] * NG; t_li = [P.tok("li0")] * NG
    pg = [P.ps([128, 512], F32, "pg%d" % i) for i in range(3)]; t_pg = [P.tok("pg%d" % i) for i in range(3)]
    pdn = [P.ps([128, 512], F32, "pd%d" % i) for i in range(3)]; t_pd = [P.tok("pd%d" % i) for i in range(3)]
    pm = [P.ps([128, 512], F32, "pm%d" % i) for i in range(2)]; t_pm = [P.tok("pm%d" % i) for i in range(2)]

    wload_i = [0]

    def load_weights(e, slot):
        src = wgu[e].rearrange("(k p) n -> p k n", p=128)
        for k in range(8):
            P.dma("pool", wgt[slot][:, k, :], src[:, k, :], writes=[t_wg[slot]])
        src = wd[e].rearrange("(k p) n -> p k n", p=128)
        for k in range(0, 8, 2):
            P.dma("pool", wdt[slot][:, k:k + 2, :], src[:, k:k + 2, :], writes=[t_wd[slot]])

    cg = [0]; cd = [0]; cm = [0]; cgl = [0]

    for ps_i in range(n_pass):
        tok0 = ps_i * TG
        load_weights(0, 0)
        for it in range(NT):
            s = it % NS
            r0 = tok0 + it * 128
            P.dma("sp", xs[s][:], xin[r0:r0 + 128, :], reads=[t_xin], writes=[t_xs[s]])
            P.dma("sp", pas[s][:], ma[r0:r0 + 128, :], reads=[t_ma], writes=[t_pa[s]])
            if two_mix:
                P.dma("sp", pbs[s][:], mb[r0:r0 + 128, :], reads=[t_mb], writes=[t_pb[s]])
                P.op("pool", lambda e: e.tensor_tensor(out=pas[s][:], in0=pas[s][:], in1=pbs[s][:], op=ALU.add),
                     reads=[t_pb[s], t_pa[s]], writes=[t_pa[s]])
            P.op("dve", lambda e: e.scalar_tensor_tensor(out=xs[s][:], in0=xs[s][:], scalar=ALPHA, in1=pas[s][:],
                                                         op0=ALU.mult, op1=ALU.add),
                 reads=[t_xs[s], t_pa[s]], writes=[t_xs[s]])
            layer_norm_tile(P, xs[s][:], x1s[s][:], gb[:, 0, :], gb[:, 1, :], lns[s], t_xs[s], t_x1[s], t_lns[s], t_const)
            for h in range(2):
                b = cm[0] % 2; cm[0] += 1
                for j in range(4):
                    k = h * 4 + j
                    P.op("pe", lambda e: e.transpose(out=pm[b][:, j * 128:(j + 1) * 128], in_=x1s[s][:, k * 128:(k + 1) * 128],
                                                     identity=ident[:]),
                         reads=[t_x1[s], t_const], writes=[t_pm[b]])
                P.op("act", lambda e: e.copy(out=xTf[s][:, h * 4:(h + 1) * 4, :],
                                             in_=pm[b][:].rearrange("p (j t) -> p j t", j=4)),
                     reads=[t_pm[b]], writes=[t_xTf[s]])
            P.op("pool", lambda e: e.tensor_copy(out=x1T[:, :, it * 128:(it + 1) * 128], in_=xTf[s][:]),
                 reads=[t_xTf[s]], writes=[t_x1T[it]])
            b = cm[0] % 2; cm[0] += 1
            for k in range(8):
                P.op("pe", lambda e: e.matmul(pm[b][:, 0:32], lhsT=xTf[s][:, k, :], rhs=rwt[:, k, :], start=(k == 0), stop=(k == 7)),
                     reads=[t_xTf[s], t_const], writes=[t_pm[b]])
            R = rt[s]; tR = t_rt[s]
            P.op("dve", lambda e: e.tensor_tensor(out=R["lg"][:], in0=pm[b][:, 0:32], in1=rbt[:], op=ALU.add),
                 reads=[t_pm[b], t_const], writes=[tR])
            P.op("dve", lambda e: e.max(out=R["t8"][:], in_=R["lg"][:]), reads=[tR], writes=[tR])
            P.op("dve", lambda e: e.tensor_scalar(out=R["nm"][:], in0=R["t8"][:, 0:1], scalar1=-1.0, scalar2=None, op0=ALU.mult),
                 reads=[tR], writes=[tR])
            P.op("act", lambda e: e.activation(out=R["ex"][:], in_=R["lg"][:], func=AF.Exp, bias=R["nm"][:, 0:1], scale=1.0),
                 reads=[tR], writes=[tR])
            P.op("dve", lambda e: e.tensor_scalar(out=R["mk"][:], in0=R["lg"][:], scalar1=R["t8"][:, 3:4], scalar2=None, op0=ALU.is_ge),
                 reads=[tR], writes=[tR])
            P.op("dve", lambda e: e.tensor_tensor(out=R["ex"][:], in0=R["ex"][:], in1=R["mk"][:], op=ALU.mult),
                 reads=[tR], writes=[tR])
            P.op("dve", lambda e: e.reduce_sum(out=R["sm"][:], in_=R["ex"][:], axis=mybir.AxisListType.X),
                 reads=[tR], writes=[tR])
            P.op("dve", lambda e: e.reciprocal(out=R["sm"][:], in_=R["sm"][:]), reads=[tR], writes=[tR])
            P.op("dve", lambda e: e.tensor_scalar(out=Gall[:, it, :], in0=R["ex"][:], scalar1=R["sm"][:, 0:1], scalar2=None, op0=ALU.mult),
                 reads=[tR], writes=[t_G[it]])
            b = cm[0] % 2; cm[0] += 1
            P.op("pe", lambda e: e.transpose(out=pm[b][0:32, 0:128], in_=Gall[:, it, :], identity=ident[:]),
                 reads=[t_G[it], t_const], writes=[t_pm[b]])
            P.op("act", lambda e: e.copy(out=R["GT"][:], in_=pm[b][0:32, 0:128]), reads=[t_pm[b]], writes=[tR])
            for hf in range(2):
                b = cm[0] % 2; cm[0] += 1
                P.op("pe", lambda e: e.matmul(pm[b][:, :], lhsT=R["GT"][:], rhs=bdt[:, hf * 512:(hf + 1) * 512], start=True, stop=True),
                     reads=[tR, t_const], writes=[t_pm[b]])
                P.op("dve", lambda e: e.scalar_tensor_tensor(out=acc[:, it, hf * 512:(hf + 1) * 512], in0=x1s[s][:, hf * 512:(hf + 1) * 512],
                                                             scalar=ALPHA, in1=pm[b][:, :], op0=ALU.mult, op1=ALU.add),
                     reads=[t_x1[s], t_pm[b]], writes=[t_acc[it]])

        for ex in range(n_exp):
            slot = ex % 2
            if ex + 1 < n_exp:
                load_weights(ex + 1, (ex + 1) % 2)
            W = wgt[slot]; WD = wdt[slot]
            for tb in range(TG // 512):
                tsl = slice(tb * 512, (tb + 1) * 512)
                tiles = [t_x1T[tb * 4 + i] for i in range(4)]
                for j in range(8):
                    gi = cgl[0] % NG; cgl[0] += 1
                    b = cg[0] % 3; cg[0] += 1
                    for k in range(8):
                        P.op("pe", lambda e: e.matmul(pg[b][:, :], lhsT=W[:, k, j * 128:(j + 1) * 128], rhs=x1T[:, k, tsl],
                                                      start=(k == 0), stop=(k == 7)),
                             reads=[t_wg[slot]] + tiles, writes=[t_pg[b]])
                    P.op("dve", lambda e: e.tensor_scalar(out=glt[gi][:], in0=pg[b][:, :], scalar1=bgt[:, ex * 16 + j:ex * 16 + j + 1],
                                                          scalar2=SWIGLU_LIMIT, op0=ALU.add, op1=ALU.min),
                         reads=[t_pg[b], t_const], writes=[t_gl[gi]])
                    P.op("act", lambda e: e.activation(out=sgt[gi][:], in_=glt[gi][:], func=AF.Sigmoid, scale=SWIGLU_ALPHA),
                         reads=[t_gl[gi]], writes=[t_sg[gi]])
                    P.op("pool", lambda e: e.tensor_tensor(out=sgt[gi][:], in0=sgt[gi][:], in1=glt[gi][:], op=ALU.mult),
                         reads=[t_gl[gi], t_sg[gi]], writes=[t_sg[gi]])
                    b = cg[0] % 3; cg[0] += 1
                    for k in range(8):
                        P.op("pe", lambda e: e.matmul(pg[b][:, :], lhsT=W[:, k, 1024 + j * 128:1024 + (j + 1) * 128], rhs=x1T[:, k, tsl],
                                                      start=(k == 0), stop=(k == 7)),
                             reads=[t_wg[slot]] + tiles, writes=[t_pg[b]])
                    P.op("dve", lambda e: e.tensor_scalar(out=lit[gi][:], in0=pg[b][:, :], scalar1=bgt[:, ex * 16 + 8 + j:ex * 16 + 9 + j],
                                                          scalar2=SWIGLU_LIMIT + 1.0, op0=ALU.add, op1=ALU.min),
                         reads=[t_pg[b], t_const], writes=[t_li[gi]])
                    P.op("dve", lambda e: e.scalar_tensor_tensor(out=actT[:, j, :], in0=lit[gi][:], scalar=1.0 - SWIGLU_LIMIT, in1=sgt[gi][:],
                                                                 op0=ALU.max, op1=ALU.mult),
                         reads=[t_li[gi], t_sg[gi]], writes=[t_actT])
                for tt in range(4):
                    it = tb * 4 + tt
                    for hf in range(2):
                        b = cd[0] % 3; cd[0] += 1
                        for k in range(8):
                            P.op("pe", lambda e: e.matmul(pdn[b][:, :], lhsT=actT[:, k, tt * 128:(tt + 1) * 128],
                                                          rhs=WD[:, k, hf * 512:(hf + 1) * 512], start=(k == 0), stop=(k == 7)),
                                 reads=[t_actT, t_wd[slot]], writes=[t_pd[b]])
                        P.op("dve", lambda e: e.scalar_tensor_tensor(out=acc[:, it, hf * 512:(hf + 1) * 512], in0=pdn[b][:, :],
                                                                     scalar=Gall[:, it, ex:ex + 1], in1=acc[:, it, hf * 512:(hf + 1) * 512],
                                                                     op0=ALU.mult, op1=ALU.add),
                             reads=[t_pd[b], t_G[it], t_acc[it]], writes=[t_acc[it]])

        for it in range(NT):
            s = it % NS
            r0 = tok0 + it * 128
            layer_norm_tile(P, acc[:, it, :], x1s[s][:], gb[:, 2, :], gb[:, 3, :], lns[s], t_acc[it], t_x1[s], t_lns[s], t_const)
            P.dma("sp", yout[r0:r0 + 128, :], x1s[s][:], reads=[t_x1[s]], writes=[t_yout])


def make_xT(P, x_ap, r0, ntile, xs, t_xs, xT, t_xT, pm, t_pm, ident, t_const, cm, t_xd=None):
    for tt in range(ntile):
        s = tt % len(xs)
        P.dma("sp", xs[s][:], x_ap[r0 + tt * 128:r0 + (tt + 1) * 128, :], reads=([t_xd] if t_xd is not None else []), writes=[t_xs[s]])
        for h in range(2):
            b = cm[0] % len(pm); cm[0] += 1
            for j in range(4):
                k = h * 4 + j
                P.op("pe", lambda e: e.transpose(out=pm[b][:, j * 128:(j + 1) * 128], in_=xs[s][:, k * 128:(k + 1) * 128],
                                                 identity=ident[:]),
                     reads=[t_xs[s], t_const], writes=[t_pm[b]])
            P.op("act", lambda e: e.copy(out=xT[:, h * 4:(h + 1) * 4, tt * 128:(tt + 1) * 128],
                                         in_=pm[b][:].rearrange("p (j t) -> p j t", j=4)),
                 reads=[t_pm[b]], writes=[t_xT])


def build_even(T, stop=99):
    P = Prog()
    nc = P.nc
    io = even_io(nc, "", T)
    io["t_xd"] = P.tok("xd"); io["t_outd"] = P.tok("outd")
    emit_even(P, T, io, stop)
    P.wait_all("sp", [io["t_outd"]])
    P.close()
    return nc


def even_io(nc, pfx, T, x=None, out=None):
    io = {}
    io["x"] = x if x is not None else nc.dram_tensor(pfx + "x", [T, D_MODEL], F32, kind="ExternalInput").ap()
    io["wA"] = nc.dram_tensor(pfx + "wA", [D_MODEL, 1284], F32, kind="ExternalInput").ap()
    io["wo"] = nc.dram_tensor(pfx + "wo", [512, D_MODEL], F32, kind="ExternalInput").ap()
    io["pw"] = nc.dram_tensor(pfx + "pw", [2, 128, 128], F32, kind="ExternalInput").ap()
    io["pc"] = nc.dram_tensor(pfx + "pc", [128, 30], F32, kind="ExternalInput").ap()
    io["coef0"] = nc.dram_tensor(pfx + "coef0", [128, 2 * 4 * 16], F32, kind="ExternalInput").ap()
    io["gbias"] = nc.dram_tensor(pfx + "gbias", [2, 2], F32, kind="ExternalInput").ap()
    io["mln"] = nc.dram_tensor(pfx + "mln", [1, 256], F32, kind="ExternalInput").ap()
    io["cst"] = nc.dram_tensor(pfx + "cst", [128, 128 + 128], F32, kind="ExternalInput").ap()
    io["cst2"] = nc.dram_tensor(pfx + "cst2", [2, 776], F32, kind="ExternalInput").ap()
    io["out"] = out if out is not None else nc.dram_tensor(pfx + "out", [T, D_MODEL], F32, kind="ExternalOutput").ap()
    return io


def emit_even(P, T, io, stop=99):
    TB = 512
    NB = T // TB
    NCH = TB // 64
    nc = P.nc
    x, wA, wo, pw, pc, coef0, gbias, mln, cst, cst2, out = (io[k] for k in
        ("x", "wA", "wo", "pw", "pc", "coef0", "gbias", "mln", "cst", "cst2", "out"))
    t_xd = io["t_xd"]; t_outd = io["t_outd"]

    t_const = P.tok("const")
    cs = P.sb([128, 256], F32, "cst"); ident = cs[:, 0:128]; maskT = cs[:, 128:256]
    cs2 = P.sb([2, 776], F32, "cst2"); rmask = cs2[:, 0:512]; id2 = cs2[:, 768:776]
    pct = P.sb([128, 30], F32, "pc"); c0t = P.sb([128, 128], F32, "coef0")
    gbt = P.sb([2, 2], F32, "gb"); mlt = P.sb([128, 256], F32, "mln"); eps_t = P.sb([128, 1], F32, "eps")
    wAt = P.sb([128, 8, 1284], BF16, "wA"); wot = P.sb([128, 4, D_MODEL], BF16, "wo"); pwt = P.sb([128, 2, 128], BF16, "pw")
    P.dma("sp", cs[:], cst[:, :], writes=[t_const])
    P.dma("sp", cs2[:], cst2[:, :], writes=[t_const])
    P.dma("sp", pct[:], pc[:, :], writes=[t_const])
    P.dma("sp", c0t[:], coef0[:, :], writes=[t_const])
    P.dma("sp", gbt[:], gbias[:, :], writes=[t_const])
    P.dma("sp", mlt[:], mln[0:1, :].partition_broadcast(128), writes=[t_const])
    t_w = P.tok("w")
    wv = wA.rearrange("(k p) n -> p k n", p=128)
    for k in range(8):
        P.dma("pool", wAt[:, k, :], wv[:, k, :], writes=[t_w])
    P.dma("pool", wot[:], wo.rearrange("(k p) n -> p k n", p=128), writes=[t_w])
    P.dma("pool", pwt[:], pw.rearrange("g c d -> c g d"), writes=[t_w])
    P.op("dve", lambda e: e.memset(eps_t[:], LN_EPS), writes=[t_const])
    nfb = P.sb([2, 1], F32, "nfb")
    identb = P.sb([128, 128], BF16, "identb")
    P.op("act", lambda e: e.copy(out=identb[:], in_=ident), reads=[t_const], writes=[t_const])
    P.op("dve", lambda e: e.tensor_scalar(out=nfb[:], in0=gbt[:, 1:2], scalar1=-1.0, scalar2=None, op0=ALU.mult),
         reads=[t_const], writes=[t_const])

    xs = [P.sb([128, D_MODEL], F32, "xs%d" % i) for i in range(2)]; t_xs = [P.tok("xs") for i in range(2)]
    xT = P.sb([128, 8, TB], BF16, "xT"); t_xT = P.tok("xT")
    ub = [P.sb([128, 16 + TB], F32, "ub%d" % g) for g in range(2)]; t_ub = [P.tok("ub") for g in range(2)]
    s2_l = [P.sb([128, 16 + TB], F32, "s2_%d" % i) for i in range(2)]; s4_l = [P.sb([128, 16 + TB], F32, "s4_%d" % i) for i in range(2)]
    s8_l = [P.sb([128, 16 + TB], F32, "s8_%d" % i) for i in range(2)]; s16_l = [P.sb([128, 16 + TB], F32, "s16_%d" % i) for i in range(2)]
    t_s_l = [P.tok("s") for i in range(2)]
    dacc_l = [P.sb([128, TB], F32, "dacc%d" % i) for i in range(2)]; dbf_l = [P.sb([128, TB], BF16, "dbf%d" % i) for i in range(2)]
    t_d_l = [P.tok("d") for i in range(2)]
    ycT = P.sb([128, 4, TB], BF16, "ycT"); t_yp = P.tok("yp"); t_ym = P.tok("ym")
    qkb = [P.sb([128, 3 + TB], F32, "qkb%d" % c) for c in range(4)]; t_qkb = [P.tok("qkb") for c in range(4)]
    cacc_l = [P.sb([128, TB], F32, "cacc%d" % i) for i in range(2)]; t_cacc_l = [P.tok("cacc") for i in range(2)]
    qTe = [P.sb([128, TB], BF16, "qTe%d" % h) for h in range(2)]; qTo = [P.sb([128, TB], BF16, "qTo%d" % h) for h in range(2)]
    t_q = [P.tok("q") for h in range(2)]
    ksil_l = [P.sb([128, TB], F32, "ksil%d" % i) for i in range(2)]; t_ksil_l = [P.tok("ksil") for i in range(2)]
    kT = [P.sb([128, TB], BF16, "kT%d" % h) for h in range(2)]; t_kT = [P.tok("kT") for h in range(2)]
    ktok = [P.sb([128, 4, 128], BF16, "ktok%d" % h) for h in range(2)]; t_ktok = [P.tok("ktok") for h in range(2)]
    vaug = P.sb([128, 4, 2, 130], BF16, "vaug"); t_v = P.tok("v")
    ogs = P.sb([128, 4, 256], F32, "ogs"); t_og = P.tok("og")
    C32 = [P.sb([128, 129], F32, "C32_%d" % h) for h in range(2)]; t_C = [P.tok("C") for h in range(2)]
    Csb = [[P.sb([128, 130], BF16, "Csb%d%d" % (h, i)) for i in range(2)] for h in range(2)]
    t_Cs = [[P.tok("Cs") for i in range(2)] for h in range(2)]
    PTm = [P.sb([128, 128], BF16, "PTm%d" % h) for h in range(2)]; t_PTm = [P.tok("PTm") for h in range(2)]
    gig = P.sb([2, TB], F32, "gig"); gsp = P.sb([2, TB], F32, "gsp"); gB = P.sb([2, TB], F32, "gB"); gu = P.sb([2, TB], F32, "gu")
    gev = P.sb([2, TB], F32, "gev"); gfl = P.sb([2, TB], F32, "gfl"); gtmp = P.sb([2, TB], F32, "gtmp")
    gmu = P.sb([2, NCH], F32, "gmu"); gg = P.sb([2, NCH], F32, "gg"); gms = P.sb([2, NCH], F32, "gms"); gMc = P.sb([2, NCH], F32, "gMc")
    gmp = P.sb([2, NCH], F32, "gmp"); gsig = P.sb([2, NCH], F32, "gsig"); mcar = P.sb([2, 1], F32, "mcar")
    t_g = P.tok("gates")
    sigb = P.sb([128, 2, NCH], F32, "sigb"); t_sigb = P.tok("sigb")
    flo = P.sb([128, 4, 2], F32, "flo"); t_flo = P.tok("flo")
    hsc = [dict(h=P.sb([128, 128], F32), dn=P.sb([128, 1], F32), st=P.sb([128, 6], F32), mv=P.sb([128, 2], F32),
                rstd=P.sb([128, 1], F32), sg=P.sb([128, 128], F32)) for i in range(2)]
    t_hsc = [P.tok("hsc") for i in range(2)]; t_hsg = [P.tok("hsg") for i in range(2)]
    yml = P.sb([128, 256], F32, "yml"); t_yml = P.tok("yml")
    osb = [P.sb([128, D_MODEL], F32, "osb%d" % i) for i in range(2)]; t_osb = [P.tok("osb") for i in range(2)]
    t_osbh = [[P.tok("osbh") for j in range(2)] for i in range(2)]
    pp = [P.ps([128, 512], F32, "pp%d" % i) for i in range(2)]; t_pp = [P.tok("pp") for i in range(2)]
    pe_ = P.ps([128, 512], F32, "pe"); t_pe = P.tok("pe")
    pv = pe_; t_pv = t_pe
    pmisc = P.ps([128, 512], F32, "pmisc")
    pmb = P.ps([128, 1024], BF16, "pmb")
    t_pmisc = P.tok("pmisc"); t_psg = t_pmisc; t_pfl = t_pmisc; t_pkt = P.tok("pkt")
    pnum = [P.ps([128, 512], F32, "pnum%d" % h) for h in range(2)]; t_pnum = [P.tok("pnum") for h in range(2)]
    po = P.ps([128, 512], F32, "po"); t_po = P.tok("po")
    pm = [pe_, po]; t_pm = [t_pe, t_po]

    for h in range(2):
        P.op("dve", lambda e: e.memset(C32[h][:], 0.0), writes=[t_C[h]])
        P.op("pool", lambda e: e.memset(qTe[h][:], 0.0), writes=[t_q[h]])
        P.op("pool", lambda e: e.memset(qTo[h][:], 0.0), writes=[t_q[h]])
    P.op("dve", lambda e: e.memset(mcar[:], 0.0), writes=[t_g])
    P.op("pool", lambda e: e.memset(vaug[:], 1.0), writes=[t_v])
    for g in range(2):
        P.op("pool", lambda e: e.memset(ub[g][:, 0:16], 0.0), writes=[t_ub[g]])
    for c in range(4):
        P.op("pool", lambda e: e.memset(qkb[c][:, 0:3], 0.0), writes=[t_qkb[c]])

    cm = [0]; cpp = [0]; cos = [0]
    for blk in range(NB):
        r0 = blk * TB
        make_xT(P, x, r0, 4, xs, t_xs, xT, t_xT, pm, t_pm, ident, t_const, cm, t_xd)

        def proj_fm(c0, ncol):
            b = cpp[0] % 2; cpp[0] += 1
            for k in range(8):
                P.op("pe", lambda e: e.matmul(pp[b][0:ncol, :], lhsT=wAt[:, k, c0:c0 + ncol], rhs=xT[:, k, :], start=(k == 0), stop=(k == 7)),
                     reads=[t_w, t_xT], writes=[t_pp[b]])
            return b

        if stop <= 1:
            continue
        def pool_group(g):
            b = proj_fm(g * 128, 128)
            P.op("act", lambda e: e.copy(out=ub[g][:, 16:16 + TB], in_=pp[b][:, :]), reads=[t_pp[b]], writes=[t_ub[g]])
            yield
            U = ub[g]; W = 16 + TB
            P.op("dve", lambda e: e.tensor_tensor(out=s2_l[g][:, 2:W], in0=U[:, 2:W], in1=U[:, 1:W - 1], op=ALU.add), reads=[t_ub[g]], writes=[t_s_l[g]])
            yield
            P.op("dve", lambda e: e.tensor_tensor(out=s4_l[g][:, 4:W], in0=s2_l[g][:, 4:W], in1=s2_l[g][:, 2:W - 2], op=ALU.add), reads=[t_s_l[g]], writes=[t_s_l[g]])
            yield
            P.op("dve", lambda e: e.tensor_tensor(out=s8_l[g][:, 8:W], in0=s4_l[g][:, 8:W], in1=s4_l[g][:, 4:W - 4], op=ALU.add), reads=[t_s_l[g]], writes=[t_s_l[g]])
            yield
            P.op("dve", lambda e: e.tensor_tensor(out=s16_l[g][:, 16:W], in0=s8_l[g][:, 16:W], in1=s8_l[g][:, 8:W - 8], op=ALU.add), reads=[t_s_l[g]], writes=[t_s_l[g]])
            yield
            cf = pct[:, 22 + g * 4:26 + g * 4]
            lo = 16 if blk == 0 else 0
            srcs = [s2_l[g], s4_l[g], s8_l[g], s16_l[g]]
            P.op("dve", lambda e: e.scalar_tensor_tensor(out=dacc_l[g][:, lo:TB], in0=s2_l[g][:, 16 + lo:W], scalar=cf[:, 0:1], in1=U[:, 16 + lo:W],
                                                         op0=ALU.mult, op1=ALU.subtract), reads=[t_s_l[g], t_ub[g], t_const], writes=[t_d_l[g]])
            yield
            for wi in range(1, 4):
                P.op("dve", lambda e: e.scalar_tensor_tensor(out=dacc_l[g][:, lo:TB], in0=srcs[wi][:, 16 + lo:W], scalar=cf[:, wi:wi + 1],
                                                             in1=dacc_l[g][:, lo:TB], op0=ALU.mult, op1=ALU.add),
                     reads=[t_s_l[g], t_const, t_d_l[g]], writes=[t_d_l[g]])
                yield
            if blk == 0:
                c0v = c0t[:].rearrange("p (g w t) -> p g w t", g=2, w=4)
                P.op("dve", lambda e: e.tensor_tensor(out=dacc_l[g][:, 0:16], in0=s2_l[g][:, 16:32], in1=c0v[:, g, 0, :], op=ALU.mult),
                     reads=[t_s_l[g], t_const], writes=[t_d_l[g]])
                yield
                P.op("dve", lambda e: e.tensor_tensor(out=dacc_l[g][:, 0:16], in0=dacc_l[g][:, 0:16], in1=U[:, 16:32], op=ALU.subtract),
                     reads=[t_d_l[g], t_ub[g]], writes=[t_d_l[g]])
                yield
                for wi in range(1, 4):
                    P.op("dve", lambda e: e.tensor_tensor(out=s2_l[g][:, 0:16], in0=srcs[wi][:, 16:32], in1=c0v[:, g, wi, :], op=ALU.mult),
                         reads=[t_s_l[g], t_const], writes=[t_s_l[g]])
                    yield
                    P.op("dve", lambda e: e.tensor_tensor(out=dacc_l[g][:, 0:16], in0=dacc_l[g][:, 0:16], in1=s2_l[g][:, 0:16], op=ALU.add),
                         reads=[t_d_l[g], t_s_l[g]], writes=[t_d_l[g]])
                    yield
            P.op("act", lambda e: e.copy(out=dbf_l[g][:], in_=dacc_l[g][:]), reads=[t_d_l[g]], writes=[t_d_l[g]])
            yield
            P.op("pool", lambda e: e.tensor_copy(out=U[:, 0:16], in_=U[:, TB:TB + 16]), reads=[t_ub[g]], writes=[t_ub[g]])
            yield
            P.op("pe", lambda e: e.matmul(po[:, :], lhsT=pwt[:, g, :], rhs=dbf_l[g][:], start=True, stop=True),
                 reads=[t_w, t_d_l[g]], writes=[t_po])
            P.op("act", lambda e: e.activation(out=ycT[:, g, :], in_=po[:, :], func=AF.Identity, scale=pct[:, g:g + 1]),
                 reads=[t_po, t_const], writes=[t_yp])
            yield


        def gates_chain():
            b = proj_fm(1280, 2)
            P.op("act", lambda e: e.activation(out=gig[:], in_=pp[b][0:2, :], func=AF.Identity, bias=gbt[:, 0:1], scale=1.0),
                 reads=[t_pp[b], t_const], writes=[t_g])
            yield
            b = proj_fm(1282, 2)
            P.op("act", lambda e: e.activation(out=gsp[:], in_=pp[b][0:2, :], func=AF.Exp, bias=nfb[:, 0:1], scale=-1.0),
                 reads=[t_pp[b], t_const], writes=[t_g])
            yield
            P.op("act", lambda e: e.activation(out=gsp[:], in_=gsp[:], func=AF.Ln, bias=1.0, scale=1.0), reads=[t_g], writes=[t_g])
            yield
            P.op("dve", lambda e: e.tensor_tensor_scan(out=gB[:], data0=rmask, data1=gsp[:], initial=0.0, op0=ALU.mult, op1=ALU.add),
                 reads=[t_g, t_const], writes=[t_g])
            yield
            P.op("dve", lambda e: e.tensor_tensor(out=gu[:], in0=gig[:], in1=gB[:], op=ALU.add), reads=[t_g], writes=[t_g])
            yield
            gu3 = gu[:].rearrange("p (c s) -> p c s", s=64); gB3 = gB[:].rearrange("p (c s) -> p c s", s=64)
            P.op("dve", lambda e: e.tensor_reduce(out=gmu[:], in_=gu3, axis=mybir.AxisListType.X, op=ALU.max), reads=[t_g], writes=[t_g])
            yield
            P.op("dve", lambda e: e.tensor_scalar(out=gg[:], in0=gB3[:, :, 63], scalar1=-1.0, scalar2=None, op0=ALU.mult), reads=[t_g], writes=[t_g])
            yield
            P.op("dve", lambda e: e.tensor_tensor_scan(out=gms[:], data0=gmu[:], data1=gg[:], initial=mcar[:, 0:1], op0=ALU.max, op1=ALU.add),
                 reads=[t_g], writes=[t_g])
            yield
            P.op("dve", lambda e: e.tensor_tensor(out=gMc[:], in0=gms[:], in1=gg[:], op=ALU.subtract), reads=[t_g], writes=[t_g])
            yield
            P.op("dve", lambda e: e.tensor_copy(out=gmp[:, 0:1], in_=mcar[:, 0:1]), reads=[t_g], writes=[t_g])
            yield
            P.op("dve", lambda e: e.tensor_copy(out=gmp[:, 1:NCH], in_=gms[:, 0:NCH - 1]), reads=[t_g], writes=[t_g])
            yield
            P.op("dve", lambda e: e.tensor_copy(out=mcar[:, 0:1], in_=gms[:, NCH - 1:NCH]), reads=[t_g], writes=[t_g])
            yield
            P.op("dve", lambda e: e.tensor_tensor(out=gsig[:], in0=gmp[:], in1=gMc[:], op=ALU.subtract), reads=[t_g], writes=[t_g])
            yield
            P.op("act", lambda e: e.activation(out=gsig[:], in_=gsig[:], func=AF.Exp), reads=[t_g], writes=[t_g])
            yield
            Mb = gMc[:].unsqueeze(2).to_broadcast([2, NCH, 64])
            P.op("dve", lambda e: e.tensor_tensor(out=gtmp[:].rearrange("p (c s) -> p c s", s=64), in0=gu3, in1=Mb, op=ALU.subtract),
                 reads=[t_g], writes=[t_g])
            yield
            P.op("act", lambda e: e.activation(out=gev[:], in_=gtmp[:], func=AF.Exp), reads=[t_g], writes=[t_g])
            yield
            P.op("dve", lambda e: e.tensor_tensor(out=gtmp[:].rearrange("p (c s) -> p c s", s=64), in0=gB3, in1=Mb, op=ALU.subtract),
                 reads=[t_g], writes=[t_g])
            yield
            P.op("act", lambda e: e.activation(out=gfl[:], in_=gtmp[:], func=AF.Exp), reads=[t_g], writes=[t_g])
            yield
            for h in range(2):
                P.op("pe", lambda e: e.matmul(pmisc[:, 300 + h * NCH:300 + (h + 1) * NCH], lhsT=cs2[:, 512 + h * 128:512 + (h + 1) * 128],
                                              rhs=gsig[:], start=True, stop=True), reads=[t_g, t_const], writes=[t_psg])
            P.op("act", lambda e: e.copy(out=sigb[:].rearrange("p h c -> p (h c)"), in_=pmisc[:, 300:300 + 2 * NCH]),
                 reads=[t_psg], writes=[t_sigb])
            yield
            for tt in range(4):
                P.op("pe", lambda e: e.matmul(pmisc[:, 320 + 8 * tt:328 + 8 * tt], lhsT=gfl[:, tt * 128:(tt + 1) * 128], rhs=id2, start=True, stop=True),
                     reads=[t_g, t_const], writes=[t_pfl])
            P.op("act", lambda e: e.copy(out=flo[:], in_=pmisc[:, 320:352].rearrange("p (t j) -> p t j", j=8)[:, :, 0:2]), reads=[t_pfl], writes=[t_flo])
            yield


        run_interleaved([pool_group(0), pool_group(1), gates_chain()])
        if stop <= 3:
            continue
        def qk_chunk(c):
            b = proj_fm(256 + c * 128, 128)
            Q = qkb[c]; cacc = cacc_l[c % 2]; t_cacc = t_cacc_l[c % 2]
            P.op("act", lambda e: e.copy(out=Q[:, 3:3 + TB], in_=pp[b][:, :]), reads=[t_pp[b]], writes=[t_qkb[c]])
            yield
            cw = pct[:, 2 + c * 4:6 + c * 4]
            P.op("dve", lambda e: e.tensor_scalar(out=cacc[:], in0=Q[:, 0:TB], scalar1=cw[:, 0:1], scalar2=pct[:, 18 + c:19 + c],
                                                  op0=ALU.mult, op1=ALU.add), reads=[t_qkb[c], t_const], writes=[t_cacc])
            yield
            for j in range(1, 4):
                P.op("dve", lambda e: e.scalar_tensor_tensor(out=cacc[:], in0=Q[:, j:j + TB], scalar=cw[:, j:j + 1], in1=cacc[:],
                                                             op0=ALU.mult, op1=ALU.add), reads=[t_qkb[c], t_const, t_cacc], writes=[t_cacc])
                yield
            P.op("pool", lambda e: e.tensor_copy(out=Q[:, 0:3], in_=Q[:, TB:TB + 3]), reads=[t_qkb[c]], writes=[t_qkb[c]])
            yield
            if c < 2:
                h = c
                ca3 = cacc[:].rearrange("p (c two s) -> p c two s", two=2, s=64)
                P.op("act", lambda e: e.activation(out=qTe[h][:].rearrange("p (c two s) -> p c two s", two=2, s=64)[:, :, 0, :],
                                                   in_=ca3[:, :, 0, :], func=AF.Silu), reads=[t_cacc], writes=[t_q[h]])
                yield
                P.op("act", lambda e: e.activation(out=qTo[h][:].rearrange("p (c two s) -> p c two s", two=2, s=64)[:, :, 1, :],
                                                   in_=ca3[:, :, 1, :], func=AF.Silu), reads=[t_cacc], writes=[t_q[h]])
                yield
            else:
                h = c - 2
                P.op("act", lambda e: e.activation(out=ksil_l[h][:], in_=cacc[:], func=AF.Silu), reads=[t_cacc], writes=[t_ksil_l[h]])
                yield
                P.op("pe", lambda e: e.matmul(pe_[:, :], lhsT=cs2[:, 512 + h * 128:512 + (h + 1) * 128], rhs=gev[:], start=True, stop=True),
                     reads=[t_g, t_const], writes=[t_pe])
                P.op("dve", lambda e: e.scalar_tensor_tensor(out=kT[h][:], in0=ksil_l[h][:], scalar=128.0 ** -0.5, in1=pe_[:, :],
                                                             op0=ALU.mult, op1=ALU.mult), reads=[t_ksil_l[h], t_pe], writes=[t_kT[h]])
                yield
                for tt in range(4):
                    P.op("pe", lambda e: e.transpose(out=pmb[:, tt * 128:(tt + 1) * 128], in_=kT[h][:, tt * 128:(tt + 1) * 128],
                                                     identity=identb[:]), reads=[t_kT[h], t_const], writes=[t_pkt])
                P.op("act", lambda e: e.copy(out=ktok[h][:].rearrange("p t d -> p (t d)"), in_=pmb[:, 0:512]), reads=[t_pkt], writes=[t_ktok[h]])
                yield


        run_interleaved([qk_chunk(0), qk_chunk(1)])
        run_interleaved([qk_chunk(2), qk_chunk(3)])
        if stop <= 4:
            continue
        for tt in range(4):
            for k in range(8):
                P.op("pe", lambda e: e.matmul(pv[:, :], lhsT=xT[:, k, tt * 128:(tt + 1) * 128], rhs=wAt[:, k, 768:1280], start=(k == 0), stop=(k == 7)),
                     reads=[t_w, t_xT], writes=[t_pv])
            P.op("act", lambda e: e.copy(out=vaug[:, tt, :, 0:128], in_=pv[:, 0:256].rearrange("p (h d) -> p h d", h=2)),
                 reads=[t_pv], writes=[t_v])
            P.op("act", lambda e: e.activation(out=ogs[:, tt, :], in_=pv[:, 256:512], func=AF.Sigmoid), reads=[t_pv], writes=[t_og])

        if stop <= 5:
            continue
        pu = [pe_, po]; t_pu = [t_pe, t_po]
        for tt in range(4):
            tsl = slice(tt * 128, (tt + 1) * 128)
            H = range(2)
            for h in H:
                P.op("pe", lambda e: e.matmul(pp[h][:, 0:128], lhsT=kT[h][:, tsl], rhs=qTe[h][:, tsl], start=True, stop=False),
                     reads=[t_kT[h], t_q[h]], writes=[t_pp[h]])
                P.op("pe", lambda e: e.matmul(pp[h][:, 0:128], lhsT=kT[h][:, tsl], rhs=qTo[h][:, tsl], start=False, stop=True),
                     reads=[t_kT[h], t_q[h]], writes=[t_pp[h]])
            for h in H:
                P.op("dve", lambda e: e.tensor_tensor(out=PTm[h][:], in0=pp[h][:, 0:128], in1=maskT, op=ALU.mult),
                     reads=[t_pp[h], t_const], writes=[t_PTm[h]])
            for h in H:
                P.op("pe", lambda e: e.matmul(pnum[h][:, 0:129], lhsT=PTm[h][:], rhs=vaug[:, tt, h, 0:129], start=True, stop=False),
                     reads=[t_PTm[h], t_v], writes=[t_pnum[h]])
            for ci in range(2):
                c = tt * 2 + ci
                for h in H:
                    P.op("act", lambda e: e.activation(out=Csb[h][ci][:, 0:129], in_=C32[h][:], func=AF.Identity, scale=sigb[:, h, c:c + 1]),
                         reads=[t_C[h], t_sigb], writes=[t_Cs[h][ci]])
                for h in H:
                    qsrc = qTe[h] if ci == 0 else qTo[h]
                    P.op("pe", lambda e: e.matmul(pnum[h][:, 0:129], lhsT=qsrc[:, tsl], rhs=Csb[h][ci][:, 0:129], start=False, stop=(ci == 1)),
                         reads=[t_q[h], t_Cs[h][ci]], writes=[t_pnum[h]])
                    P.op("pe", lambda e: e.matmul(pu[h][:, 0:129], lhsT=ktok[h][ci * 64:(ci + 1) * 64, tt, :],
                                                  rhs=vaug[ci * 64:(ci + 1) * 64, tt, h, 0:129], start=True, stop=True),
                         reads=[t_ktok[h], t_v], writes=[t_pu[h]])
                for h in H:
                    P.op("dve", lambda e: e.scalar_tensor_tensor(out=C32[h][:], in0=C32[h][:], scalar=sigb[:, h, c:c + 1], in1=pu[h][:, 0:129],
                                                                 op0=ALU.mult, op1=ALU.add), reads=[t_C[h], t_sigb, t_pu[h]], writes=[t_C[h]])
            for h in H:
                S = hsc[h]; tS = t_hsc[h]
                P.op("act", lambda e: e.activation(out=S["dn"][:], in_=pnum[h][:, 128:129], func=AF.Abs), reads=[t_pnum[h]], writes=[tS])
            for h in H:
                S = hsc[h]; tS = t_hsc[h]
                P.op("dve", lambda e: e.tensor_scalar(out=S["dn"][:], in0=S["dn"][:], scalar1=flo[:, tt, h:h + 1], scalar2=None,
                                                      op0=ALU.max), reads=[tS, t_flo], writes=[tS])
            for h in H:
                S = hsc[h]; tS = t_hsc[h]
                P.op("dve", lambda e: e.reciprocal(out=S["dn"][:], in_=S["dn"][:]), reads=[tS], writes=[tS])
            for h in H:
                S = hsc[h]; tS = t_hsc[h]
                P.op("dve", lambda e: e.tensor_scalar(out=S["h"][:], in0=pnum[h][:, 0:128], scalar1=S["dn"][:, 0:1], scalar2=None, op0=ALU.mult),
                     reads=[t_pnum[h], tS], writes=[tS])
            for h in H:
                S = hsc[h]; tS = t_hsc[h]
                P.op("dve", lambda e: e.bn_stats(out=S["st"][:], in_=S["h"][:]), reads=[tS], writes=[tS])
            for h in H:
                S = hsc[h]; tS = t_hsc[h]
                P.op("dve", lambda e: e.bn_aggr(out=S["mv"][:], in_=S["st"][:]), reads=[tS], writes=[tS])
            for h in H:
                S = hsc[h]; tS = t_hsc[h]
                P.op("act", lambda e: e.activation(out=S["rstd"][:], in_=S["mv"][:, 1:2], func=AF.Sqrt, bias=eps_t[:, 0:1], scale=1.0),
                     reads=[tS, t_const], writes=[tS])
            for h in H:
                S = hsc[h]; tS = t_hsc[h]
                P.op("dve", lambda e: e.reciprocal(out=S["rstd"][:], in_=S["rstd"][:]), reads=[tS], writes=[tS])
            for h in H:
                S = hsc[h]; tS = t_hsc[h]
                P.op("dve", lambda e: e.tensor_scalar(out=S["h"][:], in0=S["h"][:], scalar1=S["mv"][:, 0:1], scalar2=S["rstd"][:, 0:1],
                                                      op0=ALU.subtract, op1=ALU.mult), reads=[tS], writes=[tS])
                P.op("pool", lambda e: e.tensor_tensor(out=S["sg"][:], in0=ogs[:, tt, h * 128:(h + 1) * 128], in1=mlt[:, h * 128:(h + 1) * 128],
                                                       op=ALU.mult), reads=[t_og, t_const], writes=[t_hsg[h]])
            for h in H:
                S = hsc[h]; tS = t_hsc[h]
                P.op("dve", lambda e: e.tensor_tensor(out=yml[:, h * 128:(h + 1) * 128], in0=S["h"][:], in1=S["sg"][:], op=ALU.mult),
                     reads=[tS, t_hsg[h]], writes=[t_yml])
            for h in range(2):
                P.op("pe", lambda e: e.transpose(out=pe_[:, 0:128], in_=yml[:, h * 128:(h + 1) * 128], identity=ident),
                     reads=[t_yml, t_const], writes=[t_pe])
                P.op("act", lambda e: e.copy(out=ycT[:, 2 + h, tsl], in_=pe_[:, 0:128]), reads=[t_pe], writes=[t_ym])

        if stop <= 6:
            continue
        for tt in range(4):
            s = cos[0] % 2; cos[0] += 1
            for hf in range(2):
                for kc in range(4):
                    P.op("pe", lambda e: e.matmul(pp[hf][:, :], lhsT=ycT[:, kc, tt * 128:(tt + 1) * 128], rhs=wot[:, kc, hf * 512:(hf + 1) * 512],
                                                  start=(kc == 0), stop=(kc == 3)), reads=[t_yp, t_ym, t_w], writes=[t_pp[hf]])
                P.op("act" if hf == 0 else "dve", lambda e: (e.copy if hf == 0 else e.tensor_copy)(out=osb[s][:, hf * 512:(hf + 1) * 512], in_=pp[hf][:, :]),
                     reads=[t_pp[hf]], writes=[t_osbh[s][hf]])
            P.dma("sp", out[r0 + tt * 128:r0 + (tt + 1) * 128, :], osb[s][:], reads=[t_osbh[s][0], t_osbh[s][1]], writes=[t_outd])


def even_inputs(x, w_in, pool_w, pool_scale, conv_w, conv_b, i_bias, f_bias, ml_norm, w_out, hp):
    f = np.float32
    h0 = 2 * hp
    u = w_in[:, 0:512][:, h0 * 128:(h0 + 2) * 128]
    q = w_in[:, 512:1024][:, h0 * 128:(h0 + 2) * 128]
    k = w_in[:, 1024:1536][:, h0 * 128:(h0 + 2) * 128]
    v = w_in[:, 1536:2048][:, h0 * 128:(h0 + 2) * 128]
    og = w_in[:, 2048:2560][:, h0 * 128:(h0 + 2) * 128]
    ig = w_in[:, 2560:2564][:, h0:h0 + 2]
    fg = w_in[:, 2564:2568][:, h0:h0 + 2]
    wA = np.ascontiguousarray(np.concatenate([u, q, k, v, og, ig, fg], axis=1), dtype=f)
    wo = np.ascontiguousarray(np.concatenate([w_out[h0 * 128:(h0 + 2) * 128], w_out[512 + h0 * 128:512 + (h0 + 2) * 128]], axis=0), dtype=f)
    pw = np.ascontiguousarray(pool_w[h0:h0 + 2], dtype=f)
    pc = np.zeros((128, 30), f)
    for g in range(2):
        pc[:, g] = pool_scale[(h0 + g) * 128:(h0 + g + 1) * 128]
    cw = np.concatenate([conv_w[:, 0:512][:, h0 * 128:(h0 + 2) * 128], conv_w[:, 512:1024][:, h0 * 128:(h0 + 2) * 128]], axis=1)
    cb = np.concatenate([conv_b[0:512][h0 * 128:(h0 + 2) * 128], conv_b[512:1024][h0 * 128:(h0 + 2) * 128]])
    for c in range(4):
        for j in range(4):
            pc[:, 2 + c * 4 + j] = cw[j, c * 128:(c + 1) * 128]
        pc[:, 18 + c] = cb[c * 128:(c + 1) * 128]
    coef0 = np.zeros((128, 2, 4, 16), f)
    for g in range(2):
        wi = h0 + g
        pc[:, 22 + g * 4 + wi] = 1.0 / (2 ** (wi + 1))
        coef0[:, g, wi, :] = 1.0 / np.minimum(np.arange(1, 17), 2 ** (wi + 1))
    gbias = np.stack([i_bias[h0:h0 + 2], f_bias[h0:h0 + 2]], axis=1).astype(f)
    mln = np.ascontiguousarray(ml_norm[h0 * 128:(h0 + 2) * 128].reshape(1, 256), dtype=f)
    cst = np.zeros((128, 256), f)
    cst[:, 0:128] = np.eye(128)
    s_i = np.arange(128)[:, None]; t_i = np.arange(128)[None, :]
    cst[:, 128:256] = ((s_i // 64 == t_i // 64) & (s_i <= t_i)).astype(f)
    cst2 = np.zeros((2, 776), f)
    cst2[:, 0:512] = (np.arange(512) % 64 != 0).astype(f)[None, :]
    cst2[0, 512:640] = 1.0
    cst2[1, 640:768] = 1.0
    cst2[:, 768:770] = np.eye(2)
    return dict(x=np.ascontiguousarray(x, dtype=f), wA=wA, wo=wo, pw=pw, pc=pc, coef0=coef0.reshape(128, 128), gbias=gbias, mln=mln,
                cst=cst, cst2=cst2)


def build_odd(T):
    P = Prog()
    nc = P.nc
    io = odd_io(nc, "", T)
    io["t_xd"] = P.tok("xd"); io["t_outd"] = P.tok("outd")
    emit_odd(P, T, io)
    P.wait_all("sp", [io["t_outd"]])
    P.close()
    return nc


def odd_io(nc, pfx, T, x=None, out=None):
    io = {}
    io["x"] = x if x is not None else nc.dram_tensor(pfx + "x", [T, D_MODEL], F32, kind="ExternalInput").ap()
    io["wA"] = nc.dram_tensor(pfx + "wA", [D_MODEL, 1552], F32, kind="ExternalInput").ap()
    io["wo"] = nc.dram_tensor(pfx + "wo", [512, D_MODEL], F32, kind="ExternalInput").ap()
    io["w2"] = nc.dram_tensor(pfx + "w2", [16, 256], F32, kind="ExternalInput").ap()
    io["pc"] = nc.dram_tensor(pfx + "pc", [128, 2], F32, kind="ExternalInput").ap()
    io["gln"] = nc.dram_tensor(pfx + "gln", [1, 512], F32, kind="ExternalInput").ap()
    io["cst"] = nc.dram_tensor(pfx + "cst", [128, 256 + 512], F32, kind="ExternalInput").ap()
    io["out"] = out if out is not None else nc.dram_tensor(pfx + "out", [T, D_MODEL], F32, kind="ExternalOutput").ap()
    return io


def emit_odd(P, T, io):
    TB = 512
    NB = T // TB
    NCH = TB // 64
    nc = P.nc
    x, wA, wo, w2, pc, gln, cst, out = (io[k] for k in ("x", "wA", "wo", "w2", "pc", "gln", "cst", "out"))
    t_xd = io["t_xd"]; t_outd = io["t_outd"]

    t_const = P.tok("const")
    cs = P.sb([128, 768], F32, "cst"); ident = cs[:, 0:128]; maskT = cs[:, 128:256]; rmask = cs[:, 256:768]
    pct = P.sb([128, 2], F32, "pc"); npct = P.sb([128, 2], F32, "npc")
    glt = P.sb([128, 512], F32, "gln"); eps_t = P.sb([128, 1], F32, "eps")
    wAt = P.sb([128, 8, 1552], BF16, "wA"); wot = P.sb([128, 4, D_MODEL], BF16, "wo"); w2t = P.sb([16, 256], BF16, "w2")
    identb = P.sb([128, 128], BF16, "identb")
    P.dma("sp", cs[:], cst[:, :], writes=[t_const])
    P.dma("sp", pct[:], pc[:, :], writes=[t_const])
    P.dma("sp", glt[:], gln[0:1, :].partition_broadcast(128), writes=[t_const])
    t_w = P.tok("w")
    wv = wA.rearrange("(k p) n -> p k n", p=128)
    for k in range(8):
        P.dma("pool", wAt[:, k, :], wv[:, k, :], writes=[t_w])
    P.dma("pool", wot[:], wo.rearrange("(k p) n -> p k n", p=128), writes=[t_w])
    P.dma("pool", w2t[:], w2[:, :], writes=[t_w])
    P.op("dve", lambda e: e.memset(eps_t[:], LN_EPS), writes=[t_const])
    P.op("dve", lambda e: e.tensor_scalar(out=npct[:], in0=pct[:], scalar1=-1.0, scalar2=None, op0=ALU.mult), reads=[t_const], writes=[t_const])
    P.op("act", lambda e: e.copy(out=identb[:], in_=ident), reads=[t_const], writes=[t_const])

    xs = [P.sb([128, D_MODEL], F32, "xs%d" % i) for i in range(2)]; t_xs = [P.tok("xs") for i in range(2)]
    xT = P.sb([128, 8, TB], BF16, "xT"); t_xT = P.tok("xT")
    glrT = P.sb([16, TB], BF16, "glrT"); t_glr = P.tok("glr")
    qf_l = [P.sb([128, TB], F32, "qf%d" % i) for i in range(2)]; kf_l = [P.sb([128, TB], F32, "kf%d" % i) for i in range(2)]
    t_qk_l = [P.tok("qkf") for i in range(2)]
    sp_l = [P.sb([128, TB], F32, "sp%d" % i) for i in range(2)]; Bp_l = [P.sb([128, TB], F32, "Bp%d" % i) for i in range(2)]
    ex_l = [P.sb([128, TB], F32, "ex%d" % i) for i in range(2)]; rc_l = [P.sb([128, TB], F32, "rc%d" % i) for i in range(2)]
    t_dec_l = [P.tok("dec") for i in range(2)]
    eBl = [P.sb([128, NCH], F32, "eBl%d" % h) for h in range(2)]; t_eBl = [P.tok("eBl") for h in range(2)]
    qTe = [P.sb([128, TB], BF16, "qTe%d" % h) for h in range(2)]; qTo = [P.sb([128, TB], BF16, "qTo%d" % h) for h in range(2)]
    t_q = [P.tok("q") for h in range(2)]
    kT = [P.sb([128, TB], BF16, "kT%d" % h) for h in range(2)]; t_kT = [P.tok("kT") for h in range(2)]
    khT_l = [P.sb([128, TB], BF16, "khT%d" % i) for i in range(2)]; t_khT_l = [P.tok("khT") for i in range(2)]
    ktok = [P.sb([128, 4, 128], BF16, "ktok%d" % h) for h in range(2)]; t_ktok = [P.tok("ktok") for h in range(2)]
    vbf = P.sb([128, 4, 512], BF16, "vbf"); t_v = P.tok("v")
    rs = P.sb([128, 4, 512], F32, "rs"); t_r = P.tok("r")
    S32 = [P.sb([128, 256], F32, "S32_%d" % h) for h in range(2)]; t_S = [P.tok("S") for h in range(2)]
    Sb = [[P.sb([128, 256], BF16, "Sb%d%d" % (h, i)) for i in range(2)] for h in range(2)]
    t_Sb = [[P.tok("Sb") for i in range(2)] for h in range(2)]
    ATm = [P.sb([128, 128], BF16, "ATm%d" % h) for h in range(2)]; t_AT = [P.tok("AT") for h in range(2)]
    hsc = [dict(h=P.sb([128, 256], F32), st=P.sb([128, 6], F32), mv=P.sb([128, 2], F32), rstd=P.sb([128, 1], F32),
                sg=P.sb([128, 256], F32)) for i in range(2)]
    t_hsc = [P.tok("hsc") for i in range(2)]; t_hsg = [P.tok("hsg") for i in range(2)]
    yml = P.sb([128, 512], F32, "yml"); t_yml = P.tok("yml")
    ycT = P.sb([128, 4, TB], BF16, "ycT"); t_ym = P.tok("ym")
    osb = [P.sb([128, D_MODEL], F32, "osb%d" % i) for i in range(2)]; t_osb = [P.tok("osb") for i in range(2)]
    t_osbh = [[P.tok("osbh") for j in range(2)] for i in range(2)]
    pp = [P.ps([128, 512], F32, "pp%d" % i) for i in range(2)]; t_pp = [P.tok("pp") for i in range(2)]
    pe_ = P.ps([128, 512], F32, "pe"); t_pe = P.tok("pe")
    pmb = P.ps([128, 1024], BF16, "pmb"); t_pkt = P.tok("pkt")
    pnum = [P.ps([128, 512], F32, "pnum%d" % h) for h in range(2)]; t_pnum = [P.tok("pnum") for h in range(2)]
    po = P.ps([128, 512], F32, "po"); t_po = P.tok("po")
    pm = [pe_, po]; t_pm = [t_pe, t_po]

    for h in range(2):
        P.op("dve", lambda e: e.memset(S32[h][:], 0.0), writes=[t_S[h]])
        P.op("pool", lambda e: e.memset(Sb[h][0][:], 0.0), writes=[t_Sb[h][0]])
        P.op("pool", lambda e: e.memset(qTe[h][:], 0.0), writes=[t_q[h]])
        P.op("pool", lambda e: e.memset(qTo[h][:], 0.0), writes=[t_q[h]])

    cm = [0]; cpp = [0]; cos = [0]
    for blk in range(NB):
        r0 = blk * TB
        make_xT(P, x, r0, 4, xs, t_xs, xT, t_xT, pm, t_pm, ident, t_const, cm, t_xd)

        def proj_fm(c0, ncol):
            b = cpp[0] % 2; cpp[0] += 1
            for k in range(8):
                P.op("pe", lambda e: e.matmul(pp[b][0:ncol, :], lhsT=wAt[:, k, c0:c0 + ncol], rhs=xT[:, k, :], start=(k == 0), stop=(k == 7)),
                     reads=[t_w, t_xT], writes=[t_pp[b]])
            return b

        b = proj_fm(1536, 16)
        P.op("act", lambda e: e.copy(out=glrT[:], in_=pp[b][0:16, :]), reads=[t_pp[b]], writes=[t_glr])
        def head_front(h):
            bq = proj_fm(h * 128, 128)
            P.op("act", lambda e: e.copy(out=qf_l[h][:], in_=pp[bq][:, :]), reads=[t_pp[bq]], writes=[t_qk_l[h]])
            yield
            bk = proj_fm(256 + h * 128, 128)
            P.op("act", lambda e: e.copy(out=kf_l[h][:], in_=pp[bk][:, :]), reads=[t_pp[bk]], writes=[t_qk_l[h]])
            yield
            b = cpp[0] % 2; cpp[0] += 1
            P.op("pe", lambda e: e.matmul(pp[b][:, :], lhsT=w2t[:, h * 128:(h + 1) * 128], rhs=glrT[:], start=True, stop=True),
                 reads=[t_w, t_glr], writes=[t_pp[b]])
            P.op("act", lambda e: e.activation(out=sp_l[h][:], in_=pp[b][:, :], func=AF.Exp, bias=npct[:, h:h + 1], scale=-1.0),
                 reads=[t_pp[b], t_const], writes=[t_dec_l[h]])
            yield
            P.op("act", lambda e: e.activation(out=sp_l[h][:], in_=sp_l[h][:], func=AF.Ln, bias=1.0, scale=1.0), reads=[t_dec_l[h]], writes=[t_dec_l[h]])
            yield
            P.op("dve", lambda e: e.tensor_tensor_scan(out=Bp_l[h][:], data0=rmask, data1=sp_l[h][:], initial=0.0, op0=ALU.mult, op1=ALU.add),
                 reads=[t_dec_l[h], t_const], writes=[t_dec_l[h]])
            yield
            Bp3 = Bp_l[h][:].rearrange("p (c s) -> p c s", s=64)
            P.op("act", lambda e: e.activation(out=ex_l[h][:], in_=Bp_l[h][:], func=AF.Exp, scale=-1.0 / 16.0), reads=[t_dec_l[h]], writes=[t_dec_l[h]])
            yield
            q3 = qf_l[h][:].rearrange("p (c two s) -> p c two s", two=2, s=64); e3 = ex_l[h][:].rearrange("p (c two s) -> p c two s", two=2, s=64)
            P.op("dve", lambda e: e.scalar_tensor_tensor(out=qTe[h][:].rearrange("p (c two s) -> p c two s", two=2, s=64)[:, :, 0, :],
                                                         in0=q3[:, :, 0, :], scalar=128.0 ** -0.5, in1=e3[:, :, 0, :], op0=ALU.mult, op1=ALU.mult),
                 reads=[t_qk_l[h], t_dec_l[h]], writes=[t_q[h]])
            yield
            P.op("dve", lambda e: e.scalar_tensor_tensor(out=qTo[h][:].rearrange("p (c two s) -> p c two s", two=2, s=64)[:, :, 1, :],
                                                         in0=q3[:, :, 1, :], scalar=128.0 ** -0.5, in1=e3[:, :, 1, :], op0=ALU.mult, op1=ALU.mult),
                 reads=[t_qk_l[h], t_dec_l[h]], writes=[t_q[h]])
            yield
            P.op("act", lambda e: e.activation(out=ex_l[h][:], in_=Bp_l[h][:], func=AF.Exp, scale=1.0 / 16.0), reads=[t_dec_l[h], t_q[h]], writes=[t_dec_l[h]])
            yield
            P.op("dve", lambda e: e.tensor_tensor(out=kT[h][:], in0=kf_l[h][:], in1=ex_l[h][:], op=ALU.mult), reads=[t_qk_l[h], t_dec_l[h]], writes=[t_kT[h]])
            yield
            P.op("dve", lambda e: e.tensor_tensor(out=rc_l[h][:].rearrange("p (c s) -> p c s", s=64), in0=Bp3,
                                                  in1=Bp3[:, :, 63:64].to_broadcast([128, NCH, 64]), op=ALU.subtract), reads=[t_dec_l[h]], writes=[t_dec_l[h]])
            yield
            P.op("act", lambda e: e.activation(out=ex_l[h][:], in_=rc_l[h][:], func=AF.Exp, scale=1.0 / 16.0), reads=[t_dec_l[h], t_kT[h]], writes=[t_dec_l[h]])
            yield
            P.op("dve", lambda e: e.tensor_tensor(out=khT_l[h][:], in0=kf_l[h][:], in1=ex_l[h][:], op=ALU.mult), reads=[t_qk_l[h], t_dec_l[h]], writes=[t_khT_l[h]])
            yield
            P.op("act", lambda e: e.activation(out=eBl[h][:], in_=Bp3[:, :, 63], func=AF.Exp, scale=-1.0 / 16.0), reads=[t_dec_l[h]], writes=[t_eBl[h]])
            yield
            for tt in range(4):
                P.op("pe", lambda e: e.transpose(out=pmb[:, tt * 128:(tt + 1) * 128], in_=khT_l[h][:, tt * 128:(tt + 1) * 128], identity=identb[:]),
                     reads=[t_khT_l[h], t_const], writes=[t_pkt])
            P.op("act", lambda e: e.copy(out=ktok[h][:].rearrange("p t d -> p (t d)"), in_=pmb[:, 0:512]), reads=[t_pkt], writes=[t_ktok[h]])
            yield


        run_interleaved([head_front(0), head_front(1)])
        for tt in range(4):
            for k in range(8):
                P.op("pe", lambda e: e.matmul(pe_[:, :], lhsT=xT[:, k, tt * 128:(tt + 1) * 128], rhs=wAt[:, k, 512:1024], start=(k == 0), stop=(k == 7)),
                     reads=[t_w, t_xT], writes=[t_pe])
            P.op("act", lambda e: e.copy(out=vbf[:, tt, :], in_=pe_[:, :]), reads=[t_pe], writes=[t_v])
            for k in range(8):
                P.op("pe", lambda e: e.matmul(po[:, :], lhsT=xT[:, k, tt * 128:(tt + 1) * 128], rhs=wAt[:, k, 1024:1536], start=(k == 0), stop=(k == 7)),
                     reads=[t_w, t_xT], writes=[t_po])
            P.op("act", lambda e: e.activation(out=rs[:, tt, :], in_=po[:, :], func=AF.Silu), reads=[t_po], writes=[t_r])

        pu = [pe_, po]; t_pu = [t_pe, t_po]
        for tt in range(4):
            tsl = slice(tt * 128, (tt + 1) * 128)
            H = range(2)
            for h in H:
                P.op("pe", lambda e: e.matmul(pp[h][:, 0:128], lhsT=kT[h][:, tsl], rhs=qTe[h][:, tsl], start=True, stop=False),
                     reads=[t_kT[h], t_q[h]], writes=[t_pp[h]])
                P.op("pe", lambda e: e.matmul(pp[h][:, 0:128], lhsT=kT[h][:, tsl], rhs=qTo[h][:, tsl], start=False, stop=True),
                     reads=[t_kT[h], t_q[h]], writes=[t_pp[h]])
            for h in H:
                P.op("dve", lambda e: e.tensor_tensor(out=ATm[h][:], in0=pp[h][:, 0:128], in1=maskT, op=ALU.mult),
                     reads=[t_pp[h], t_const], writes=[t_AT[h]])
            for h in H:
                P.op("pe", lambda e: e.matmul(pnum[h][:, 0:256], lhsT=ATm[h][:], rhs=vbf[:, tt, h * 256:(h + 1) * 256], start=True, stop=False),
                     reads=[t_AT[h], t_v], writes=[t_pnum[h]])
            for ci in range(2):
                c = tt * 2 + ci
                for h in H:
                    qsrc = qTe[h] if ci == 0 else qTo[h]
                    P.op("pe", lambda e: e.matmul(pnum[h][:, 0:256], lhsT=qsrc[:, tsl], rhs=Sb[h][ci][:], start=False, stop=(ci == 1)),
                         reads=[t_q[h], t_Sb[h][ci]], writes=[t_pnum[h]])
                    P.op("pe", lambda e: e.matmul(pu[h][:, 0:256], lhsT=ktok[h][ci * 64:(ci + 1) * 64, tt, :],
                                                  rhs=vbf[ci * 64:(ci + 1) * 64, tt, h * 256:(h + 1) * 256], start=True, stop=True),
                         reads=[t_ktok[h], t_v], writes=[t_pu[h]])
                for h in H:
                    P.op("dve", lambda e: e.scalar_tensor_tensor(out=S32[h][:], in0=S32[h][:], scalar=eBl[h][:, c:c + 1], in1=pu[h][:, 0:256],
                                                                 op0=ALU.mult, op1=ALU.add), reads=[t_S[h], t_eBl[h], t_pu[h]], writes=[t_S[h]])
                for h in H:
                    P.op("act", lambda e: e.copy(out=Sb[h][1 - ci][:], in_=S32[h][:]), reads=[t_S[h]], writes=[t_Sb[h][1 - ci]])
            for h in H:
                S = hsc[h]; tS = t_hsc[h]
                P.op("dve", lambda e: e.bn_stats(out=S["st"][:], in_=pnum[h][:, 0:256]), reads=[t_pnum[h]], writes=[tS])
            for h in H:
                S = hsc[h]; tS = t_hsc[h]
                P.op("dve", lambda e: e.bn_aggr(out=S["mv"][:], in_=S["st"][:]), reads=[tS], writes=[tS])
            for h in H:
                S = hsc[h]; tS = t_hsc[h]
                P.op("act", lambda e: e.activation(out=S["rstd"][:], in_=S["mv"][:, 1:2], func=AF.Sqrt, bias=eps_t[:, 0:1], scale=1.0),
                     reads=[tS, t_const], writes=[tS])
            for h in H:
                S = hsc[h]; tS = t_hsc[h]
                P.op("dve", lambda e: e.reciprocal(out=S["rstd"][:], in_=S["rstd"][:]), reads=[tS], writes=[tS])
            for h in H:
                S = hsc[h]; tS = t_hsc[h]
                P.op("dve", lambda e: e.tensor_scalar(out=S["h"][:], in0=pnum[h][:, 0:256], scalar1=S["mv"][:, 0:1], scalar2=S["rstd"][:, 0:1],
                                                      op0=ALU.subtract, op1=ALU.mult), reads=[t_pnum[h], tS], writes=[tS])
                P.op("pool", lambda e: e.tensor_tensor(out=S["sg"][:], in0=rs[:, tt, h * 256:(h + 1) * 256], in1=glt[:, h * 256:(h + 1) * 256],
                                                       op=ALU.mult), reads=[t_r, t_const], writes=[t_hsg[h]])
            for h in H:
                S = hsc[h]; tS = t_hsc[h]
                P.op("dve", lambda e: e.tensor_tensor(out=yml[:, h * 256:(h + 1) * 256], in0=S["h"][:], in1=S["sg"][:], op=ALU.mult),
                     reads=[tS, t_hsg[h]], writes=[t_yml])
            for kc in range(4):
                P.op("pe", lambda e: e.transpose(out=pe_[:, kc * 128:(kc + 1) * 128], in_=yml[:, kc * 128:(kc + 1) * 128], identity=ident),
                     reads=[t_yml, t_const], writes=[t_pe])
            P.op("act", lambda e: e.copy(out=ycT[:, :, tsl], in_=pe_[:, :].rearrange("p (k t) -> p k t", k=4)), reads=[t_pe], writes=[t_ym])

        for tt in range(4):
            s = cos[0] % 2; cos[0] += 1
            for hf in range(2):
                for kc in range(4):
                    P.op("pe", lambda e: e.matmul(pp[hf][:, :], lhsT=ycT[:, kc, tt * 128:(tt + 1) * 128], rhs=wot[:, kc, hf * 512:(hf + 1) * 512],
                                                  start=(kc == 0), stop=(kc == 3)), reads=[t_ym, t_w], writes=[t_pp[hf]])
                P.op("act" if hf == 0 else "dve", lambda e: (e.copy if hf == 0 else e.tensor_copy)(out=osb[s][:, hf * 512:(hf + 1) * 512], in_=pp[hf][:, :]),
                     reads=[t_pp[hf]], writes=[t_osbh[s][hf]])
            P.dma("sp", out[r0 + tt * 128:r0 + (tt + 1) * 128, :], osb[s][:], reads=[t_osbh[s][0], t_osbh[s][1]], writes=[t_outd])


def odd_inputs(x, w_in, gla_w2, gla_b, gla_norm, w_out, hp):
    f = np.float32
    h0 = 2 * hp
    q = w_in[:, 0:512][:, h0 * 128:(h0 + 2) * 128]
    k = w_in[:, 512:1024][:, h0 * 128:(h0 + 2) * 128]
    v = w_in[:, 1024:2048][:, h0 * 256:(h0 + 2) * 256]
    r = w_in[:, 2048:3072][:, h0 * 256:(h0 + 2) * 256]
    glr = w_in[:, 3072:3088]
    wA = np.ascontiguousarray(np.concatenate([q, k, v, r, glr], axis=1), dtype=f)
    wo = np.ascontiguousarray(w_out[h0 * 256:(h0 + 2) * 256], dtype=f)
    w2 = np.ascontiguousarray(gla_w2[:, h0 * 128:(h0 + 2) * 128], dtype=f)
    pc = np.ascontiguousarray(gla_b[h0 * 128:(h0 + 2) * 128].reshape(2, 128).T, dtype=f)
    gln = np.ascontiguousarray(gla_norm[h0 * 256:(h0 + 2) * 256].reshape(1, 512), dtype=f)
    cst = np.zeros((128, 768), f)
    cst[:, 0:128] = np.eye(128)
    s_i = np.arange(128)[:, None]; t_i = np.arange(128)[None, :]
    cst[:, 128:256] = ((s_i // 64 == t_i // 64) & (s_i <= t_i)).astype(f)
    cst[:, 256:768] = (np.arange(512) % 64 != 0).astype(f)[None, :]
    return dict(x=np.ascontiguousarray(x, dtype=f), wA=wA, wo=wo, w2=w2, pc=pc, gln=gln, cst=cst)


FUSED_CORES = BATCH
SPARSE_MOE = True


def build_fused(n_layers=DEPTH, T=SEQ, sparse=True):
    P = Prog()
    nc = P.nc
    x_ext = nc.dram_tensor("x", [T, D_MODEL], F32, kind="ExternalInput").ap()
    y_ext = nc.dram_tensor("y", [T, D_MODEL], F32, kind="ExternalOutput").ap()
    xbuf = [nc.dram_tensor("xbuf%d" % i, [T, D_MODEL], F32).ap() for i in range(2)]
    pmix = [nc.dram_tensor("pmix%d" % i, [T, D_MODEL], F32).ap() for i in range(2)]
    t_xext = P.tok("xext")
    t_y = P.tok("y"); t_y.disjoint = True
    t_xbuf = [P.tok("xbuf%d" % i) for i in range(2)]
    t_pmix = [P.tok("pmix%d" % i) for i in range(2)]
    for t in t_xbuf + t_pmix:
        t.disjoint = True
    scr = None
    if sparse:
        scr = moe_sparse_scratch(nc, T)
        for k in ("t_xbkt", "t_ybuf", "t_x1d"):
            scr[k] = P.tok(k); scr[k].disjoint = True
    for l in range(n_layers):
        xin_ap, t_xin = (x_ext, t_xext) if l == 0 else (xbuf[(l - 1) % 2], t_xbuf[(l - 1) % 2])
        yout_ap, t_yout = (y_ext, t_y) if l == n_layers - 1 else (xbuf[l % 2], t_xbuf[l % 2])
        for hp in range(2):
            P.push()
            pfx = "L%dH%d_" % (l, hp)
            if l % 2 == 0:
                io = even_io(nc, pfx, T, x=xin_ap, out=pmix[hp])
            else:
                io = odd_io(nc, pfx, T, x=xin_ap, out=pmix[hp])
            io["t_xd"] = t_xin; io["t_outd"] = t_pmix[hp]
            if l % 2 == 0:
                emit_even(P, T, io)
            else:
                emit_odd(P, T, io)
            P.pop()
        P.push()
        io = moe_io(nc, "L%d_" % l, T, xin=xin_ap, ma=pmix[0], mb=pmix[1], yout=yout_ap, sparse=sparse)
        io["t_xin"] = t_xin; io["t_ma"] = t_pmix[0]; io["t_mb"] = t_pmix[1]; io["t_yout"] = t_yout
        if sparse:
            emit_moe_sparse(P, T, io, scr, first=(l == 0))
        else:
            emit_moe(P, T // 1024, io)
        P.pop()
    P.wait_all("sp", [t_y])
    P.close()
    return nc


_NC_CACHE = {}


def fused_inputs(b, x, even_w_in, pool_w, pool_scale, conv_w, conv_b, i_bias, f_bias, ml_norm, even_w_out,
                 odd_w_in, gla_w2, gla_b, gla_norm, odd_w_out, lnps, router_w, router_b, wgu_d, bgu_l, w_down, b_down, n_layers):
    f = np.float32
    im = {"x": np.ascontiguousarray(x[b], dtype=f)}
    ident = np.eye(128, dtype=f)
    for l in range(n_layers):
        i = l // 2
        for hp in range(2):
            pfx = "L%dH%d_" % (l, hp)
            if l % 2 == 0:
                d = even_inputs(x[b], even_w_in[i], pool_w[i], pool_scale[i], conv_w[i], conv_b[i], i_bias[i], f_bias[i],
                                ml_norm[i], even_w_out[i], hp)
            else:
                d = odd_inputs(x[b], odd_w_in[i], gla_w2[i], gla_b[i], gla_norm[i], odd_w_out[i], hp)
            for k, v in d.items():
                if k != "x":
                    im[pfx + k] = v
        pfx = "L%d_" % l
        im[pfx + "lnp"] = lnps[l]
        im[pfx + "rw"] = np.ascontiguousarray(router_w[l], dtype=f)
        im[pfx + "rb"] = np.ascontiguousarray(router_b[l], dtype=f).reshape(1, N_EXPERTS)
        im[pfx + "wgu"] = wgu_d[l]
        im[pfx + "bgu"] = bgu_l[l]
        im[pfx + "wd"] = np.ascontiguousarray(w_down[l], dtype=f)
        im[pfx + "bd"] = np.ascontiguousarray(b_down[l], dtype=f)
        im[pfx + "ident"] = moe_sparse_consts() if SPARSE_MOE else ident
    return im


def kernel(x, even_w_in, pool_w, pool_scale, conv_w, conv_b, i_bias, f_bias, ml_norm, even_w_out,
           odd_w_in, gla_w2, gla_b, gla_norm, odd_w_out, ln1_g, ln1_b, ln2_g, ln2_b, router_w, router_b,
           w_gate_up, b_gate_up, w_down, b_down, _layers=DEPTH, _cores=FUSED_CORES):
    f = np.float32
    A = lambda a: np.asarray(a, dtype=f)
    x = A(x)
    wgu_d, bgu_l, lnps = [], [], []
    for l in range(_layers):
        wg = A(w_gate_up[l])
        wgu_d.append(np.ascontiguousarray(np.concatenate([wg[:, :, 0::2], wg[:, :, 1::2]], axis=-1)))
        del wg
        bg = A(b_gate_up[l])
        bd_ = np.concatenate([bg[:, 0::2], bg[:, 1::2]], axis=-1)
        bgu_l.append(np.ascontiguousarray(bd_.reshape(N_EXPERTS, 16, 128).transpose(2, 0, 1).reshape(128, N_EXPERTS * 16)))
        lnps.append(np.stack([A(ln1_g[l]), A(ln1_b[l]), A(ln2_g[l]), A(ln2_b[l])]))
    args = [A(v) for v in (even_w_in, pool_w, pool_scale, conv_w, conv_b, i_bias, f_bias, ml_norm, even_w_out,
                           odd_w_in, gla_w2, gla_b, gla_norm, odd_w_out)]
    ims = [fused_inputs(b, x, *args, lnps, A(router_w), A(router_b), wgu_d, bgu_l, A(w_down), A(b_down), _layers)
           for b in range(_cores)]
    key = ("fused", _layers)
    if key not in _NC_CACHE:
        _NC_CACHE[key] = build_fused(_layers, sparse=SPARSE_MOE)
    res = run_bass_kernel_spmd(_NC_CACHE[key], ims, core_ids=list(range(_cores)))
    out = np.stack([r["y"] for r in res.results], axis=0)
    if _cores < BATCH:
        return out
    return out.reshape(BATCH, SEQ, D_MODEL)


MOE_CAP = 768
MOE_NR = N_EXPERTS * MOE_CAP
U32 = mybir.dt.uint32


def moe_sparse_scratch(nc, T):
    return dict(xbkt=nc.dram_tensor("xbkt", [MOE_NR + 128, D_MODEL], BF16).ap(),
                ybuf=nc.dram_tensor("ybuf", [MOE_NR + 128, D_MODEL], F32).ap(),
                x1d=nc.dram_tensor("x1d", [T, D_MODEL], F32).ap())


def _idma(P, out, in_, out_off=None, in_off=None, reads=(), writes=()):
    P._deps("pool", reads, writes)
    owner = writes[0]
    key = P._dsem(owner)
    ins = P.nc.gpsimd.indirect_dma_start(out=out, out_offset=out_off, in_=in_, in_offset=in_off)
    ins.then_inc(P.sems[key], 16)
    owner.dcount += 16
    P.dtot[key] = owner.dcount
    me = (key, owner.dcount)
    for r in reads:
        r.readers.append(me)
    for w in writes:
        w.writer = me
        w.readers = []


def emit_moe_sparse(P, T, io, scr, first=False, stop_phase=9):
    NT = T // 128
    C = MOE_CAP
    CB = C // 2
    NRT = C // 128
    nc = P.nc
    xin, ma, mb, lnp, rw, rb, wgu, bgu, wd, bd, cst_in, yout = (io[k] for k in
        ("xin", "ma", "mb", "lnp", "rw", "rb", "wgu", "bgu", "wd", "bd", "ident", "yout"))
    t_xin, t_ma, t_mb, t_yout = io["t_xin"], io["t_ma"], io["t_mb"], io["t_yout"]
    xbkt, ybuf, x1d = scr["xbkt"], scr["ybuf"], scr["x1d"]
    t_xbkt, t_ybuf, t_x1d = scr["t_xbkt"], scr["t_ybuf"], scr["t_x1d"]

    t_const = P.tok("const")
    cs = P.sb([128, 128 * 3 + 32 + 1], F32, "mcst")
    ident = cs[:, 0:128]; Ltri = cs[:, 128:256]; ones = cs[:, 256:384]; ebase1 = cs[:, 384:416]; ptrash = cs[:, 416:417]
    gb = P.sb([128, 4, D_MODEL], F32, "gb")
    rwt = P.sb([128, 8, N_EXPERTS], F32, "rwt"); rbt = P.sb([128, N_EXPERTS], F32, "rbt")
    bgt = P.sb([128, N_EXPERTS * 16], F32, "bgt"); bdt = P.sb([N_EXPERTS, D_MODEL], F32, "bdt")
    eps_t = P.sb([128, 1], F32, "eps")
    Gall = P.sb([128, NT, N_EXPERTS], F32, "G"); t_G = [P.tok("G") for i in range(NT)]
    Gk = P.sb([128, NT, 4], F32, "Gk"); Sidx = P.sb([128, NT, 4], U32, "Sidx"); t_sel = [P.tok("sel") for i in range(NT)]
    base = P.sb([128, N_EXPERTS], F32, "base"); t_base = P.tok("base")
    P.dma("sp", cs[:], cst_in[:, :], writes=[t_const])
    for i in range(4):
        P.dma("sp", gb[:, i, :], lnp[i:i + 1, :].partition_broadcast(128), writes=[t_const])
    P.dma("sp", rwt[:], rw.rearrange("(k p) n -> p k n", p=128), writes=[t_const])
    P.dma("sp", rbt[:], rb[0:1, :].partition_broadcast(128), writes=[t_const])
    P.dma("sp", bgt[:], bgu[:, :], writes=[t_const])
    P.dma("sp", bdt[:], bd[:, :], writes=[t_const])
    P.op("dve", lambda e: e.memset(eps_t[:], LN_EPS), writes=[t_const])
    P.op("dve", lambda e: e.memset(base[:], 0.0), writes=[t_base])
    bgv = bgt[:].rearrange("p (e c) -> p e c", c=16)
    P.op("dve", lambda e: e.tensor_scalar(out=bgv[:, :, 8:16], in0=bgv[:, :, 8:16], scalar1=1.0, scalar2=None, op0=ALU.add),
         reads=[t_const], writes=[t_const])

    P.push()
    xs_l = [P.sb([128, D_MODEL], F32, "xs%d" % i) for i in range(2)]; t_xs_l = [P.tok("xs") for i in range(2)]
    pas_l = [P.sb([128, D_MODEL], F32, "pa%d" % i) for i in range(2)]; t_pa_l = [P.tok("pa") for i in range(2)]
    pbs_l = [P.sb([128, D_MODEL], F32, "pb%d" % i) for i in range(2)]; t_pb_l = [P.tok("pb") for i in range(2)]
    x1s = [P.sb([128, D_MODEL], F32, "x1s%d" % i) for i in range(2)]; t_x1 = [P.tok("x1s") for i in range(2)]
    x1b = [P.sb([128, D_MODEL], BF16, "x1b%d" % i) for i in range(2)]; t_x1b = [P.tok("x1b") for i in range(2)]
    xTf_l = [P.sb([128, 8, 128], F32, "xTf%d" % i) for i in range(2)]; t_xTf_l = [P.tok("xTf") for i in range(2)]
    lns_l = [dict(st=P.sb([128, 12], F32), mv=P.sb([128, 2], F32), rstd=P.sb([128, 1], F32), eps=eps_t) for i in range(2)]
    t_lns_l = [P.tok("lns") for i in range(2)]
    R_l = [dict(lg=P.sb([128, 32], F32), t8=P.sb([128, 8], F32), nm=P.sb([128, 1], F32), ex=P.sb([128, 32], F32),
                mk=P.sb([128, 32], F32), sm=P.sb([128, 1], F32), pos=P.sb([128, 32], F32), v1=P.sb([128, 32], F32),
                smat=P.sb([128, 32], F32), s8=P.sb([128, 8], F32), neg=P.sb([128, 4], F32), sf=P.sb([128, 4], F32),
                junk=P.sb([128, 32], F32)) for i in range(2)]
    tR_l = [P.tok("rt") for i in range(2)]
    pm = [P.ps([128, 512], F32, "pm%d" % i) for i in range(4)]; t_pm = [P.tok("pm") for i in range(4)]
    pr_l = [P.ps([128, 512], F32, "pr%d" % i) for i in range(2)]; t_pr_l = [P.tok("pr") for i in range(2)]
    xs = xs_l[0]; t_xs = t_xs_l[0]
    if first:
        P.op("dve", lambda e: e.memset(xs[:], 0.0), writes=[t_xs])
        P.dma("sp", ybuf[MOE_NR:MOE_NR + 128, :], xs[:], reads=[t_xs], writes=[t_ybuf])
    cm = [0]
    for it in range(NT):
        s = it % 2
        r0 = it * 128
        xs = xs_l[s]; t_xs = t_xs_l[s]; pas = pas_l[s]; t_pa = t_pa_l[s]; pbs = pbs_l[s]; t_pb = t_pb_l[s]
        xTf = xTf_l[s]; t_xTf = t_xTf_l[s]; lns = lns_l[s]; t_lns = t_lns_l[s]; R = R_l[s]; tR = tR_l[s]; pr = pr_l[s]; t_pr = t_pr_l[s]
        P.dma("sp", xs[:], xin[r0:r0 + 128, :], reads=[t_xin], writes=[t_xs])
        P.dma("sp", pas[:], ma[r0:r0 + 128, :], reads=[t_ma], writes=[t_pa])
        P.dma("sp", pbs[:], mb[r0:r0 + 128, :], reads=[t_mb], writes=[t_pb])
        P.op("dve", lambda e: e.tensor_tensor(out=pas[:], in0=pas[:], in1=pbs[:], op=ALU.add), reads=[t_pb, t_pa], writes=[t_pa])
        P.op("dve", lambda e: e.scalar_tensor_tensor(out=xs[:], in0=xs[:], scalar=ALPHA, in1=pas[:], op0=ALU.mult, op1=ALU.add),
             reads=[t_xs, t_pa], writes=[t_xs])
        layer_norm_tile(P, xs[:], x1s[s][:], gb[:, 0, :], gb[:, 1, :], lns, t_xs, t_x1[s], t_lns, t_const, gb_eng="dve")
        P.dma("sp", x1d[r0:r0 + 128, :], x1s[s][:], reads=[t_x1[s]], writes=[t_x1d])
        P.op("act", lambda e: e.copy(out=x1b[s][:], in_=x1s[s][:]), reads=[t_x1[s]], writes=[t_x1b[s]])
        for h in range(2):
            b = cm[0] % 4; cm[0] += 1
            for j in range(4):
                k = h * 4 + j
                P.op("pe", lambda e: e.transpose(out=pm[b][:, j * 128:(j + 1) * 128], in_=x1s[s][:, k * 128:(k + 1) * 128], identity=ident),
                     reads=[t_x1[s], t_const], writes=[t_pm[b]])
            P.op("act", lambda e: e.copy(out=xTf[:, h * 4:(h + 1) * 4, :], in_=pm[b][:].rearrange("p (j t) -> p j t", j=4)),
                 reads=[t_pm[b]], writes=[t_xTf])
        b = cm[0] % 4; cm[0] += 1
        for k in range(8):
            P.op("pe", lambda e: e.matmul(pm[b][:, 0:32], lhsT=xTf[:, k, :], rhs=rwt[:, k, :], start=(k == 0), stop=(k == 7)),
                 reads=[t_xTf, t_const], writes=[t_pm[b]])
        P.op("dve", lambda e: e.tensor_tensor(out=R["lg"][:], in0=pm[b][:, 0:32], in1=rbt[:], op=ALU.add), reads=[t_pm[b], t_const], writes=[tR])
        P.op("dve", lambda e: e.max(out=R["t8"][:], in_=R["lg"][:]), reads=[tR], writes=[tR])
        P.op("dve", lambda e: e.tensor_scalar(out=R["nm"][:], in0=R["t8"][:, 0:1], scalar1=-1.0, scalar2=None, op0=ALU.mult), reads=[tR], writes=[tR])
        P.op("act", lambda e: e.activation(out=R["ex"][:], in_=R["lg"][:], func=AF.Exp, bias=R["nm"][:, 0:1], scale=1.0), reads=[tR], writes=[tR])
        P.op("dve", lambda e: e.tensor_scalar(out=R["mk"][:], in0=R["lg"][:], scalar1=R["t8"][:, 3:4], scalar2=None, op0=ALU.is_ge), reads=[tR], writes=[tR])
        P.op("dve", lambda e: e.tensor_tensor(out=R["ex"][:], in0=R["ex"][:], in1=R["mk"][:], op=ALU.mult), reads=[tR], writes=[tR])
        P.op("dve", lambda e: e.reduce_sum(out=R["sm"][:], in_=R["ex"][:], axis=mybir.AxisListType.X), reads=[tR], writes=[tR])
        P.op("dve", lambda e: e.reciprocal(out=R["sm"][:], in_=R["sm"][:]), reads=[tR], writes=[tR])
        P.op("dve", lambda e: e.tensor_scalar(out=Gall[:, it, :], in0=R["ex"][:], scalar1=R["sm"][:, 0:1], scalar2=None, op0=ALU.mult),
             reads=[tR], writes=[t_G[it]])
        P.op("pe", lambda e: e.matmul(pr[:, 0:32], lhsT=Ltri, rhs=R["mk"][:], start=True, stop=True), reads=[tR, t_const], writes=[t_pr])
        P.op("pe", lambda e: e.matmul(pr[:, 32:64], lhsT=ones, rhs=R["mk"][:], start=True, stop=True), reads=[tR, t_const], writes=[t_pr])
        P.op("dve", lambda e: e.tensor_tensor(out=R["pos"][:], in0=pr[:, 0:32], in1=base[:], op=ALU.add), reads=[t_pr, t_base], writes=[tR])
        P.op("dve", lambda e: e.tensor_tensor(out=base[:], in0=pr[:, 32:64], in1=base[:], op=ALU.add), reads=[t_pr, t_base, tR], writes=[t_base])
        P.op("dve", lambda e: e.tensor_scalar(out=R["v1"][:], in0=R["pos"][:], scalar1=float(C), scalar2=None, op0=ALU.is_lt), reads=[tR], writes=[tR])
        P.op("dve", lambda e: e.tensor_tensor(out=R["v1"][:], in0=R["v1"][:], in1=R["mk"][:], op=ALU.mult), reads=[tR], writes=[tR])
        P.op("dve", lambda e: e.tensor_tensor(out=R["pos"][:], in0=R["pos"][:], in1=ebase1, op=ALU.add), reads=[tR, t_const], writes=[tR])
        P.op("dve", lambda e: e.tensor_tensor(out=R["smat"][:], in0=R["pos"][:], in1=R["v1"][:], op=ALU.mult), reads=[tR], writes=[tR])
        P.op("dve", lambda e: e.tensor_scalar(out=R["smat"][:], in0=R["smat"][:], scalar1=-1.0, scalar2=None, op0=ALU.add), reads=[tR], writes=[tR])
        P.op("dve", lambda e: e.max(out=R["s8"][:], in_=R["smat"][:]), reads=[tR], writes=[tR])
        for k in range(4):
            P.op("dve", lambda e: e.scalar_tensor_tensor(out=R["junk"][:], in0=R["smat"][:], scalar=R["s8"][:, k:k + 1], in1=Gall[:, it, :],
                                                         op0=ALU.is_equal, op1=ALU.mult, accum_out=Gk[:, it, k:k + 1]),
                 reads=[tR, t_G[it]], writes=[tR, t_sel[it]])
        P.op("dve", lambda e: e.tensor_scalar(out=R["neg"][:], in0=R["s8"][:, 0:4], scalar1=0.0, scalar2=None, op0=ALU.is_lt), reads=[tR], writes=[tR])
        P.op("dve", lambda e: e.scalar_tensor_tensor(out=R["sf"][:], in0=R["neg"][:], scalar=ptrash, in1=R["s8"][:, 0:4], op0=ALU.mult, op1=ALU.add),
             reads=[tR, t_const], writes=[tR])
        P.op("dve", lambda e: e.tensor_copy(out=Sidx[:, it, :], in_=R["sf"][:]), reads=[tR], writes=[t_sel[it]])
        for k in range(4):
            _idma(P, xbkt[:, :], x1b[s][:], out_off=bass.IndirectOffsetOnAxis(Sidx[:, it, k:k + 1], 0),
                  reads=[t_x1b[s], t_sel[it]], writes=[t_xbkt])
    P.pop()

    if stop_phase <= 1:
        return
    P.push()
    wgt = [P.sb([128, 8, 2048], BF16, "wgu%d" % i) for i in range(2)]; t_wg = [P.tok("wg") for i in range(2)]
    wdt = [P.sb([128, 8, D_MODEL], BF16, "wd%d" % i) for i in range(2)]; t_wd = [P.tok("wd") for i in range(2)]
    XT = P.sb([128, 8, C], BF16, "XT"); t_XT = P.tok("XT")
    actT = P.sb([128, 8, C], BF16, "actT"); t_actT = [P.tok("actT") for i in range(2)]
    xrow = [P.sb([128, D_MODEL], BF16, "xrow%d" % i) for i in range(2)]; t_xrow = [P.tok("xrow") for i in range(2)]
    yrow = [P.sb([128, D_MODEL], F32, "yrow%d" % i) for i in range(2)]; t_yrow = [P.tok("yrow") for i in range(2)]
    identb = P.sb([128, 128], BF16, "identb")
    P.op("act", lambda e: e.copy(out=identb[:], in_=ident), reads=[t_const], writes=[t_const])
    NG = 2
    glt = [P.sb([128, CB], F32, "gl%d" % i) for i in range(NG)]; t_gl = [P.tok("gl") for i in range(NG)]
    sgt = [P.sb([128, CB], F32, "sg%d" % i) for i in range(NG)]; t_sg = [P.tok("sg") for i in range(NG)]
    lit = [P.sb([128, CB], F32, "li%d" % i) for i in range(NG)]; t_li = [P.tok("li") for i in range(NG)]
    pg = [P.ps([128, 512], F32, "pg%d" % i) for i in range(3)]; t_pg = [P.tok("pg") for i in range(3)]
    pdn = [P.ps([128, 512], F32, "pd%d" % i) for i in range(3)]; t_pd = [P.tok("pd") for i in range(3)]
    ptb = [P.ps([128, 1024], BF16, "ptb%d" % i) for i in range(2)]; t_ptb = [P.tok("ptb") for i in range(2)]

    def load_weights(e, slot):
        src = wgu[e].rearrange("(k p) n -> p k n", p=128)
        for k in range(8):
            P.dma("pool", wgt[slot][:, k, :], src[:, k, :], writes=[t_wg[slot]])
        src = wd[e].rearrange("(k p) n -> p k n", p=128)
        for k in range(0, 8, 2):
            P.dma("pool", wdt[slot][:, k:k + 2, :], src[:, k:k + 2, :], writes=[t_wd[slot]])

    cg = [0]; cd = [0]; cgl = [0]; ct = [0]; cy = [0]
    load_weights(0, 0)
    for ex in range(N_EXPERTS):
        slot = ex % 2
        if ex + 1 < N_EXPERTS:
            load_weights(ex + 1, (ex + 1) % 2)
        W = wgt[slot]; WD = wdt[slot]
        for rt in range(NRT):
            s = ct[0] % 2; ct[0] += 1
            rr = ex * C + rt * 128
            P.dma("sp", xrow[s][:], xbkt[rr:rr + 128, :], reads=[t_xbkt], writes=[t_xrow[s]])
            for k in range(8):
                P.op("pe", lambda e: e.transpose(out=ptb[s][:, k * 128:(k + 1) * 128], in_=xrow[s][:, k * 128:(k + 1) * 128], identity=identb[:]),
                     reads=[t_xrow[s], t_const], writes=[t_ptb[s]])
            P.op("act", lambda e: e.copy(out=XT[:, :, rt * 128:(rt + 1) * 128], in_=ptb[s][:, :].rearrange("p (k t) -> p k t", k=8)),
                 reads=[t_ptb[s]], writes=[t_XT])
        for cb in range(2):
            csl = slice(cb * CB, (cb + 1) * CB)
            for j in range(8):
                gi = cgl[0] % NG; cgl[0] += 1
                b = cg[0] % 3; cg[0] += 1
                for k in range(8):
                    P.op("pe", lambda e: e.matmul(pg[b][:, 0:CB], lhsT=W[:, k, j * 128:(j + 1) * 128], rhs=XT[:, k, csl], start=(k == 0), stop=(k == 7)),
                         reads=[t_wg[slot], t_XT], writes=[t_pg[b]])
                P.op("dve", lambda e: e.tensor_scalar(out=glt[gi][:], in0=pg[b][:, 0:CB], scalar1=bgt[:, ex * 16 + j:ex * 16 + j + 1],
                                                      scalar2=SWIGLU_LIMIT, op0=ALU.add, op1=ALU.min), reads=[t_pg[b], t_const], writes=[t_gl[gi]])
                P.op("act", lambda e: e.activation(out=sgt[gi][:], in_=glt[gi][:], func=AF.Sigmoid, scale=SWIGLU_ALPHA), reads=[t_gl[gi]], writes=[t_sg[gi]])
                P.op("pool", lambda e: e.tensor_tensor(out=sgt[gi][:], in0=sgt[gi][:], in1=glt[gi][:], op=ALU.mult), reads=[t_gl[gi], t_sg[gi]], writes=[t_sg[gi]])
                b = cg[0] % 3; cg[0] += 1
                for k in range(8):
                    P.op("pe", lambda e: e.matmul(pg[b][:, 0:CB], lhsT=W[:, k, 1024 + j * 128:1024 + (j + 1) * 128], rhs=XT[:, k, csl], start=(k == 0), stop=(k == 7)),
                         reads=[t_wg[slot], t_XT], writes=[t_pg[b]])
                P.op("dve", lambda e: e.tensor_scalar(out=lit[gi][:], in0=pg[b][:, 0:CB], scalar1=bgt[:, ex * 16 + 8 + j:ex * 16 + 9 + j],
                                                      scalar2=SWIGLU_LIMIT + 1.0, op0=ALU.add, op1=ALU.min), reads=[t_pg[b], t_const], writes=[t_li[gi]])
                P.op("dve", lambda e: e.scalar_tensor_tensor(out=actT[:, j, csl], in0=lit[gi][:], scalar=1.0 - SWIGLU_LIMIT, in1=sgt[gi][:],
                                                             op0=ALU.max, op1=ALU.mult), reads=[t_li[gi], t_sg[gi]], writes=[t_actT[cb]])
        for rt in range(NRT):
            s = cy[0] % 2; cy[0] += 1
            for hf in range(2):
                b = cd[0] % 3; cd[0] += 1
                for k in range(8):
                    P.op("pe", lambda e: e.matmul(pdn[b][:, :], lhsT=actT[:, k, rt * 128:(rt + 1) * 128], rhs=WD[:, k, hf * 512:(hf + 1) * 512],
                                                  start=(k == 0), stop=(k == 7)), reads=[t_actT[0], t_actT[1], t_wd[slot]], writes=[t_pd[b]])
                P.op("act", lambda e: e.copy(out=yrow[s][:, hf * 512:(hf + 1) * 512], in_=pdn[b][:, :]), reads=[t_pd[b]], writes=[t_yrow[s]])
            rr = ex * C + rt * 128
            P.dma("sp", ybuf[rr:rr + 128, :], yrow[s][:], reads=[t_yrow[s]], writes=[t_ybuf])
    P.pop()

    if stop_phase <= 2:
        return
    P.push()
    x1s = [P.sb([128, D_MODEL], F32, "x1c%d" % i) for i in range(2)]; t_x1 = [P.tok("x1c") for i in range(2)]
    accs = [P.sb([128, D_MODEL], F32, "acc%d" % i) for i in range(2)]; t_acc = [P.tok("acc") for i in range(2)]
    yg_l = [[P.sb([128, D_MODEL], F32, "yg%d%d" % (j, i)) for i in range(4)] for j in range(2)]
    t_yg_l = [[P.tok("yg") for i in range(4)] for j in range(2)]
    outs = [P.sb([128, D_MODEL], F32, "outs%d" % i) for i in range(2)]; t_outs = [P.tok("outs") for i in range(2)]
    GT_l = [P.sb([32, 128], F32, "GT%d" % i) for i in range(2)]; t_GT_l = [P.tok("GT") for i in range(2)]
    lns_l = [dict(st=P.sb([128, 12], F32), mv=P.sb([128, 2], F32), rstd=P.sb([128, 1], F32), eps=eps_t) for i in range(2)]
    t_lns_l = [P.tok("lns") for i in range(2)]
    pm = [P.ps([128, 512], F32, "pm%d" % i) for i in range(4)]; t_pm = [P.tok("pm") for i in range(4)]
    pt_l = [P.ps([128, 512], F32, "pt%d" % i) for i in range(2)]; t_pt_l = [P.tok("pt") for i in range(2)]
    cm = [0]
    for it in range(NT):
        s = it % 2
        r0 = it * 128
        yg = yg_l[s]; t_yg = t_yg_l[s]; GT = GT_l[s]; t_GT = t_GT_l[s]; lns = lns_l[s]; t_lns = t_lns_l[s]; pt = pt_l[s]; t_pt = t_pt_l[s]
        P.dma("sp", x1s[s][:], x1d[r0:r0 + 128, :], reads=[t_x1d], writes=[t_x1[s]])
        for k in range(4):
            _idma(P, yg[k][:], ybuf[:, :], in_off=bass.IndirectOffsetOnAxis(Sidx[:, it, k:k + 1], 0), reads=[t_ybuf, t_sel[it]], writes=[t_yg[k]])
        P.op("pe", lambda e: e.transpose(out=pt[0:32, 0:128], in_=Gall[:, it, :], identity=ident), reads=[t_G[it], t_const], writes=[t_pt])
        P.op("act", lambda e: e.copy(out=GT[:], in_=pt[0:32, 0:128]), reads=[t_pt], writes=[t_GT])
        for hf in range(2):
            b = cm[0] % 4; cm[0] += 1
            P.op("pe", lambda e: e.matmul(pm[b][:, :], lhsT=GT[:], rhs=bdt[:, hf * 512:(hf + 1) * 512], start=True, stop=True),
                 reads=[t_GT, t_const], writes=[t_pm[b]])
            P.op("dve", lambda e: e.scalar_tensor_tensor(out=accs[s][:, hf * 512:(hf + 1) * 512], in0=x1s[s][:, hf * 512:(hf + 1) * 512],
                                                         scalar=ALPHA, in1=pm[b][:, :], op0=ALU.mult, op1=ALU.add),
                 reads=[t_x1[s], t_pm[b]], writes=[t_acc[s]])
        for k in range(4):
            P.op("dve", lambda e: e.scalar_tensor_tensor(out=accs[s][:], in0=yg[k][:], scalar=Gk[:, it, k:k + 1], in1=accs[s][:],
                                                         op0=ALU.mult, op1=ALU.add), reads=[t_yg[k], t_sel[it], t_acc[s]], writes=[t_acc[s]])
        layer_norm_tile(P, accs[s][:], outs[s][:], gb[:, 2, :], gb[:, 3, :], lns, t_acc[s], t_outs[s], t_lns, t_const, gb_eng="dve")
        P.dma("sp", yout[r0:r0 + 128, :], outs[s][:], reads=[t_outs[s]], writes=[t_yout])
    P.pop()


def moe_sparse_consts():
    f = np.float32
    c = np.zeros((128, 417), f)
    c[:, 0:128] = np.eye(128)
    tp = np.arange(128)[:, None]; tt = np.arange(128)[None, :]
    c[:, 128:256] = (tp < tt).astype(f)
    c[:, 256:384] = 1.0
    c[:, 384:416] = (np.arange(N_EXPERTS) * MOE_CAP + 1).astype(f)[None, :]
    c[:, 416] = MOE_NR + 1 + np.arange(128)
    return c
```
